# Optimizing a Trainium2 kernel written in Bass

```python
import math
import jax, jax.numpy as jnp
from jax import lax
import numpy as np

D_MODEL = 2048
BATCH = 4
SEQ = 2048
DEPTH = 2

CTX_LEN = 256
GRID_W = 64
N_MIXERS = 4
GROUP_W = D_MODEL // N_MIXERS

CHUNK = 128
SGU_HEADS = 4
SGU_HEAD_DIM = GROUP_W // SGU_HEADS

MLA_HEADS = 4
QK_NOPE = 128
QK_ROPE = 64
V_DIM = GROUP_W // MLA_HEADS
Q_LORA = 384
KV_LORA = 256
ROPE_THETA = 10000.0
Q_BLOCK = 128

S5_CH = 16
S5_GROUPS = GROUP_W // S5_CH
S5_STATE = 64
S5_MAX_RE = -1e-4
S5_DT_MIN = 1e-3
S5_DT_MAX = 1e-1

CONV_WIDTH = 3

N_EXPERTS = 16
EXPERT_FF = D_MODEL // 2
CAPACITY_FACTOR = 2

EPS = 1e-6

IN_SGU = 2 * GROUP_W
IN_MLA = Q_LORA + KV_LORA + QK_ROPE
IN_S5 = GROUP_W
IN_CONV = 3 * GROUP_W
IN_COLS = IN_SGU + IN_MLA + IN_S5 + IN_CONV
IN_SPLITS = (IN_SGU, IN_SGU + IN_MLA, IN_SGU + IN_MLA + IN_S5)

kernel_name = "hybrid_parallel_dit_block"


def rmsnorm(x, g):
    xf = x.astype(jnp.float32)
    y = xf * lax.rsqrt(jnp.mean(xf * xf, axis=-1, keepdims=True) + EPS)
    return (y * g.astype(jnp.float32)).astype(x.dtype)


def sgu_mix(p, norm_g, w_s, b_s):
    bsz, n, _ = p.shape
    u, v = jnp.split(jax.nn.gelu(p), 2, axis=-1)
    v = rmsnorm(v, norm_g).reshape(bsz, n // CHUNK, CHUNK, SGU_HEADS, SGU_HEAD_DIM)
    mixed = jnp.einsum("hpq,bkqhc->bkphc", w_s, v) + jnp.swapaxes(b_s, 0, 1)[None, None, :, :, None]
    return u * mixed.reshape(bsz, n, GROUP_W)


def rotate_pairs(xh, ang):
    x1, x2 = jnp.split(xh, 2, axis=-1)
    cos = jnp.cos(ang).astype(xh.dtype)
    sin = jnp.sin(ang).astype(xh.dtype)
    return jnp.concatenate([x1 * cos - x2 * sin, x2 * cos + x1 * sin], axis=-1)


def rope_2d(x, ang_row, ang_col):
    xr, xc = jnp.split(x, 2, axis=-1)
    return jnp.concatenate([rotate_pairs(xr, ang_row), rotate_pairs(xc, ang_col)], axis=-1)


def mla_queries(p_q, q_norm_g, w_uq):
    bsz, n, _ = p_q.shape
    q = (rmsnorm(p_q, q_norm_g) @ w_uq).reshape(bsz, n, MLA_HEADS, QK_NOPE + QK_ROPE)
    return q[..., :QK_NOPE], q[..., QK_NOPE:]


def mla_keys_values(p_kv, kv_norm_g, w_ukv):
    bsz, n, _ = p_kv.shape
    kv = (rmsnorm(p_kv, kv_norm_g) @ w_ukv).reshape(bsz, n, MLA_HEADS, QK_NOPE + V_DIM)
    return kv[..., :QK_NOPE], kv[..., QK_NOPE:]


def attend(qn, qr, kn, kr, v):
    s = jnp.einsum("bqhd,bkhd->bhqk", qn, kn) + jnp.einsum("bqhr,bkr->bhqk", qr, kr)
    pr = jax.nn.softmax(s.astype(jnp.float32) * (QK_NOPE + QK_ROPE) ** -0.5, axis=-1)
    return jnp.einsum("bhqk,bkhd->bqhd", pr.astype(v.dtype), v)


def blocked_attention(qn, qr, kn, kr, v):
    bsz, n = qn.shape[0], qn.shape[1]
    nblk = n // Q_BLOCK

    def to_blocks(t):
        return jnp.moveaxis(t.reshape(bsz, nblk, Q_BLOCK, *t.shape[2:]), 1, 0)

    out = lax.map(lambda qb: attend(qb[0], qb[1], kn, kr, v), (to_blocks(qn), to_blocks(qr)))
    return jnp.moveaxis(out, 0, 1).reshape(bsz, n, MLA_HEADS * V_DIM)


def s5_discretise(a_re, a_im, log_dt, b_re, b_im, c_re, c_im):
    a = lax.complex(jnp.minimum(a_re.astype(jnp.float32), S5_MAX_RE), a_im.astype(jnp.float32))
    dt = jnp.exp(log_dt.astype(jnp.float32))[..., None]
    abar = jnp.exp(a * dt)
    bbar = ((abar - 1.0) / a)[..., None] * lax.complex(b_re.astype(jnp.float32), b_im.astype(jnp.float32))
    cmat = lax.complex(c_re.astype(jnp.float32), c_im.astype(jnp.float32))
    return abar, bbar, cmat


def ssm_combine(e1, e2):
    a1, b1 = e1
    a2, b2 = e2
    return a1 * a2, a2 * b1 + b2


def linear_scan(abar, bu, s0, reverse):
    if s0 is not None:
        edge = -1 if reverse else 0
        bu = bu.at[:, edge].add(abar * s0)
    a = jnp.broadcast_to(abar, bu.shape)
    _, s = lax.associative_scan(ssm_combine, (a, bu), reverse=reverse, axis=1)
    return s


def s5_states(u, abar, bbar, s0_f, s0_b):
    bsz, n, _ = u.shape
    uf = u.astype(jnp.float32).reshape(bsz, n, S5_GROUPS, S5_CH)
    bu = jnp.einsum("blgc,zgnc->zblgn", uf, bbar)
    s_f = linear_scan(abar[0], bu[0], s0_f, reverse=False)
    s_b = linear_scan(abar[1], bu[1], s0_b, reverse=True)
    return s_f, s_b


def s5_readout(u, s_f, s_b, cmat, d_skip, w_glu, b_glu):
    bsz, n, _ = u.shape
    uf = u.astype(jnp.float32).reshape(bsz, n, S5_GROUPS, S5_CH)
    y = jnp.real(jnp.einsum("blgn,gcn->blgc", s_f, cmat[0]) + jnp.einsum("blgn,gcn->blgc", s_b, cmat[1]))
    y = y + d_skip.astype(jnp.float32).reshape(S5_GROUPS, S5_CH) * uf
    g = jax.nn.gelu(y.reshape(bsz, n, GROUP_W).astype(u.dtype))
    return g * jax.nn.sigmoid(g @ w_glu + b_glu)


def conv_mix(p, conv_w):
    b_gate, c_gate, h = jnp.split(p, 3, axis=-1)
    z = c_gate * h
    y = lax.conv_general_dilated(
        z, conv_w[:, None, :], window_strides=(1,),
        padding=((CONV_WIDTH // 2, CONV_WIDTH // 2),),
        dimension_numbers=("NWC", "WIO", "NWC"), feature_group_count=GROUP_W)
    return b_gate * y


def token_mixing(h_l, h_c, ang_row, ang_col, with_ctx_out, w_in, w_out,
                 sgu_norm_g, sgu_w, sgu_b, q_norm_g, w_uq, kv_norm_g, w_ukv,
                 a_re, a_im, log_dt, b_re, b_im, c_re, c_im, d_skip, w_glu, b_glu, conv_w):
    p_l = jnp.split(h_l @ w_in, IN_SPLITS, axis=-1)
    p_c = jnp.split(h_c @ w_in, IN_SPLITS, axis=-1)

    kn_c, v_c = mla_keys_values(p_c[1][..., Q_LORA:Q_LORA + KV_LORA], kv_norm_g, w_ukv)
    kr_c = p_c[1][..., Q_LORA + KV_LORA:]
    qn_l, qr_l = mla_queries(p_l[1][..., :Q_LORA], q_norm_g, w_uq)
    kn_l, v_l = mla_keys_values(p_l[1][..., Q_LORA:Q_LORA + KV_LORA], kv_norm_g, w_ukv)
    qr_l = rope_2d(qr_l, ang_row[:, None, :], ang_col[:, None, :])
    kr_l = rope_2d(p_l[1][..., Q_LORA + KV_LORA:], ang_row, ang_col)
    attn_l = blocked_attention(qn_l, qr_l,
                               jnp.concatenate([kn_c, kn_l], axis=1),
                               jnp.concatenate([kr_c, kr_l], axis=1),
                               jnp.concatenate([v_c, v_l], axis=1))

    abar, bbar, cmat = s5_discretise(a_re, a_im, log_dt, b_re, b_im, c_re, c_im)
    sf_c, sb_c = s5_states(p_c[2], abar, bbar, None, None)
    sf_l, sb_l = s5_states(p_l[2], abar, bbar, sf_c[:, -1], sb_c[:, 0])
    ssm_l = s5_readout(p_l[2], sf_l, sb_l, cmat, d_skip, w_glu, b_glu)

    out_l = jnp.concatenate([sgu_mix(p_l[0], sgu_norm_g, sgu_w, sgu_b), attn_l, ssm_l,
                             conv_mix(p_l[3], conv_w)], axis=-1) @ w_out
    if not with_ctx_out:
        return out_l, None

    bsz, n_c = h_c.shape[0], h_c.shape[1]
    qn_c, qr_c = mla_queries(p_c[1][..., :Q_LORA], q_norm_g, w_uq)
    attn_c = attend(qn_c, qr_c, kn_c, kr_c, v_c).reshape(bsz, n_c, GROUP_W)
    ssm_c = s5_readout(p_c[2], sf_c, sb_c, cmat, d_skip, w_glu, b_glu)
    out_c = jnp.concatenate([sgu_mix(p_c[0], sgu_norm_g, sgu_w, sgu_b), attn_c, ssm_c,
                             conv_mix(p_c[3], conv_w)], axis=-1) @ w_out
    return out_l, out_c


def ec_moe(h, w_router, w_gate, w_up, w_down):
    bsz, n, _ = h.shape
    cap = max(1, CAPACITY_FACTOR * n // N_EXPERTS)
    aff = jax.nn.softmax((h @ w_router).astype(jnp.float32), axis=-1)
    top_aff, top_idx = lax.top_k(jnp.swapaxes(aff, 1, 2), cap)
    bidx = jnp.arange(bsz)[:, None, None]
    xs = h[bidx, top_idx]
    hid = jax.nn.silu(jnp.einsum("becd,edf->becf", xs, w_gate)) * jnp.einsum("becd,edf->becf", xs, w_up)
    ys = jnp.einsum("becf,efd->becd", hid, w_down) * top_aff[..., None].astype(h.dtype)
    return jnp.zeros_like(h).at[bidx, top_idx].add(ys)


def setup_inputs(seed: int = 0) -> dict:
    key = jax.random.key(seed)
    ks = iter(jax.random.split(key, 48))
    f32 = jnp.float32

    def nrm(shape, scale):
        return scale * jax.random.normal(next(ks), shape, f32)

    L, G, N = DEPTH, S5_GROUPS, S5_STATE
    return {
        "x": nrm((BATCH, SEQ, D_MODEL), 1.0),
        "c": nrm((BATCH, D_MODEL), 1.0),
        "ctx": nrm((BATCH, CTX_LEN, D_MODEL), 1.0),
        "c_ctx": nrm((D_MODEL,), 1.0),
        "norm1_g": 1.0 + nrm((L, D_MODEL), 0.1),
        "norm2_g": 1.0 + nrm((L, D_MODEL), 0.1),
        "w_ada": nrm((L, D_MODEL, 6 * D_MODEL), 0.5 * D_MODEL ** -0.5),
        "b_ada": nrm((L, 6 * D_MODEL), 0.02),
        "w_in": nrm((L, D_MODEL, IN_COLS), D_MODEL ** -0.5),
        "w_out": nrm((L, N_MIXERS * GROUP_W, D_MODEL), (N_MIXERS * GROUP_W) ** -0.5),
        "sgu_norm_g": 1.0 + nrm((L, GROUP_W), 0.1),
        "sgu_w": nrm((L, SGU_HEADS, CHUNK, CHUNK), CHUNK ** -0.5),
        "sgu_b": 1.0 + nrm((L, SGU_HEADS, CHUNK), 0.1),
        "mla_q_norm_g": 1.0 + nrm((L, Q_LORA), 0.1),
        "mla_w_uq": nrm((L, Q_LORA, MLA_HEADS * (QK_NOPE + QK_ROPE)), Q_LORA ** -0.5),
        "mla_kv_norm_g": 1.0 + nrm((L, KV_LORA), 0.1),
        "mla_w_ukv": nrm((L, KV_LORA, MLA_HEADS * (QK_NOPE + V_DIM)), KV_LORA ** -0.5),
        "s5_a_re": -0.5 + nrm((L, 2, G, N), 0.01),
        "s5_a_im": math.pi * jnp.arange(N, dtype=f32) + nrm((L, 2, G, N), 0.01),
        "s5_log_dt": jax.random.uniform(next(ks), (L, 2, G), f32, math.log(S5_DT_MIN), math.log(S5_DT_MAX)),
        "s5_b_re": nrm((L, 2, G, N, S5_CH), (2 * S5_CH) ** -0.5),
        "s5_b_im": nrm((L, 2, G, N, S5_CH), (2 * S5_CH) ** -0.5),
        "s5_c_re": nrm((L, 2, G, S5_CH, N), (2 * N) ** -0.5),
        "s5_c_im": nrm((L, 2, G, S5_CH, N), (2 * N) ** -0.5),
        "s5_d": nrm((L, GROUP_W), 1.0),
        "s5_w_glu": nrm((L, GROUP_W, GROUP_W), GROUP_W ** -0.5),
        "s5_b_glu": nrm((L, GROUP_W), 0.02),
        "conv_w": nrm((L, CONV_WIDTH, GROUP_W), CONV_WIDTH ** -0.5),
        "moe_w_router": nrm((L, D_MODEL, N_EXPERTS), D_MODEL ** -0.5),
        "moe_w_gate": nrm((L, N_EXPERTS, D_MODEL, EXPERT_FF), D_MODEL ** -0.5),
        "moe_w_up": nrm((L, N_EXPERTS, D_MODEL, EXPERT_FF), D_MODEL ** -0.5),
        "moe_w_down": nrm((L, N_EXPERTS, EXPERT_FF, D_MODEL), EXPERT_FF ** -0.5),
        "final_norm_g": 1.0 + nrm((D_MODEL,), 0.1),
    }


def reference(x, c, ctx, c_ctx, norm1_g, norm2_g, w_ada, b_ada, w_in, w_out,
              sgu_norm_g, sgu_w, sgu_b, mla_q_norm_g, mla_w_uq, mla_kv_norm_g, mla_w_ukv,
              s5_a_re, s5_a_im, s5_log_dt, s5_b_re, s5_b_im, s5_c_re, s5_c_im, s5_d,
              s5_w_glu, s5_b_glu, conv_w, moe_w_router, moe_w_gate, moe_w_up, moe_w_down,
              final_norm_g):
    n = x.shape[1]
    rows = n // GRID_W
    row_id = jnp.repeat(jnp.arange(rows, dtype=jnp.float32), GRID_W)
    col_id = jnp.tile(jnp.arange(GRID_W, dtype=jnp.float32), rows)
    n_freq = QK_ROPE // 4
    inv_freq = ROPE_THETA ** (-jnp.arange(n_freq, dtype=jnp.float32) / n_freq)
    ang_row = row_id[:, None] * inv_freq
    ang_col = col_id[:, None] * inv_freq

    silu_c = jax.nn.silu(c)
    silu_cc = jax.nn.silu(c_ctx)
    x_l, x_c = x, ctx
    for i in range(DEPTH):
        last = i == DEPTH - 1
        mod_l = [m[:, None, :] for m in jnp.split(silu_c @ w_ada[i] + b_ada[i], 6, axis=-1)]
        mod_c = jnp.split(silu_cc @ w_ada[i] + b_ada[i], 6, axis=-1)

        h_l = rmsnorm(x_l, norm1_g[i]) * (1.0 + mod_l[1]) + mod_l[0]
        h_c = rmsnorm(x_c, norm1_g[i]) * (1.0 + mod_c[1]) + mod_c[0]
        o_l, o_c = token_mixing(h_l, h_c, ang_row, ang_col, not last, w_in[i], w_out[i],
                                sgu_norm_g[i], sgu_w[i], sgu_b[i],
                                mla_q_norm_g[i], mla_w_uq[i], mla_kv_norm_g[i], mla_w_ukv[i],
                                s5_a_re[i], s5_a_im[i], s5_log_dt[i], s5_b_re[i], s5_b_im[i],
                                s5_c_re[i], s5_c_im[i], s5_d[i], s5_w_glu[i], s5_b_glu[i], conv_w[i])
        x_l = x_l + mod_l[2] * o_l
        h_l = rmsnorm(x_l, norm2_g[i]) * (1.0 + mod_l[4]) + mod_l[3]
        x_l = x_l + mod_l[5] * ec_moe(h_l, moe_w_router[i], moe_w_gate[i], moe_w_up[i], moe_w_down[i])

        if not last:
            x_c = x_c + mod_c[2] * o_c
            h_c = rmsnorm(x_c, norm2_g[i]) * (1.0 + mod_c[4]) + mod_c[3]
            x_c = x_c + mod_c[5] * ec_moe(h_c, moe_w_router[i], moe_w_gate[i], moe_w_up[i], moe_w_down[i])

    return rmsnorm(x_l, final_norm_g)
```

```python
import math
import numpy as np
from contextlib import ExitStack
import concourse.bass as bass
import concourse.mybir as mybir
from concourse.bass_utils import run_bass_kernel_spmd

F32 = mybir.dt.float32
BF16 = mybir.dt.bfloat16
I32 = mybir.dt.int32
AF = mybir.ActivationFunctionType
ALU = mybir.AluOpType
AX = mybir.AxisListType

D = 2048
T = 2304
NL = 2048
NX = 256
KD = 16
DEPTH = 2
TB = [(0, 512, 0), (512, 512, 0), (1024, 512, 0), (1536, 512, 0), (2048, 256, 1)]
NJ = 288
EPS = 1e-6
N_DMA_SEMS = 24
TWO_PI = 6.283185


class Buf:
    __slots__ = ("name", "w", "r")

    def __init__(self, name=""):
        self.name = name
        self.w = {}
        self.r = {}


class Sched:
    def __init__(self, nc, es):
        self.nc = nc
        self.es = es
        self.eng = {"pe": nc.tensor, "dve": nc.vector, "act": nc.scalar, "pool": nc.gpsimd, "sp": nc.sync}
        self.sems = {}
        self.cnt = {}
        for k in ["pe", "dve", "act", "pool"]:
            self.sems[k] = es.enter_context(nc.semaphore("s_" + k))
            self.cnt[k] = 0
        for i in range(N_DMA_SEMS):
            k = "d%d" % i
            self.sems[k] = es.enter_context(nc.semaphore("s_" + k))
            self.cnt[k] = 0
        self.dma_rr = 0
        self.waited = {e: {} for e in self.eng}
        self.bufs = []
        self.uid = 0

    def sbuf(self, name, shape, dt, es=None):
        self.uid += 1
        t = (es or self.es).enter_context(self.nc.sbuf_tensor("%s_%d" % (name, self.uid), list(shape), dt))
        b = Buf(name)
        self.bufs.append(b)
        return t, b

    def psum(self, name, shape, dt, es=None):
        self.uid += 1
        t = (es or self.es).enter_context(self.nc.psum_tensor("%s_%d" % (name, self.uid), list(shape), dt))
        b = Buf(name)
        self.bufs.append(b)
        return t, b

    def _wait(self, e, dep):
        sk, val, deng = dep
        if deng == e and e == "pe":
            return
        if self.waited[e].get(sk, 0) >= val:
            return
        self.eng[e].wait_ge(self.sems[sk], val)
        self.waited[e][sk] = val

    def _deps(self, e, reads, writes):
        for b in reads:
            for d in b.w.values():
                self._wait(e, d)
        for b in writes:
            for d in b.w.values():
                self._wait(e, d)
            for d in b.r.values():
                self._wait(e, d)

    def _commit(self, tag, reads, writes):
        for b in reads:
            b.r[tag[0]] = tag
        for b in writes:
            b.w = {tag[0]: tag}
            b.r = {}

    def op(self, e, fn, reads=(), writes=()):
        self._deps(e, reads, writes)
        ins = fn()
        self.cnt[e] += 1
        ins.then_inc(self.sems[e], 1)
        self._commit((e, self.cnt[e], e), reads, writes)
        return ins

    def seq(self, e, fns, reads=(), writes=()):
        self._deps(e, reads, writes)
        ins = None
        for i, fn in enumerate(fns):
            if i > 0:
                self.eng[e].wait_ge(self.sems[e], self.cnt[e])
                self.waited[e][e] = self.cnt[e]
            ins = fn()
            self.cnt[e] += 1
            ins.then_inc(self.sems[e], 1)
        self._commit((e, self.cnt[e], e), reads, writes)
        return ins

    def dma(self, q, out, in_, reads=(), writes=(), awrites=()):
        self._deps(q, reads, writes)
        sk = "d%d" % self.dma_rr
        self.dma_rr = (self.dma_rr + 1) % N_DMA_SEMS
        if self.cnt[sk] > 0:
            self._wait(q, (sk, self.cnt[sk], "dma"))
        ins = self.eng[q].dma_start(out=out, in_=in_)
        self.cnt[sk] += 16
        ins.then_inc(self.sems[sk], 16)
        self._commit((sk, self.cnt[sk], "dma"), reads, writes)
        for b in awrites:
            b.w[sk] = (sk, self.cnt[sk], "dma")
        return ins

    def barrier(self):
        for e in self.eng:
            for sk, c in self.cnt.items():
                if c > 0:
                    self._wait(e, (sk, c, "x"))
        for b in self.bufs:
            b.w = {}
            b.r = {}


class Ring:
    def __init__(self, S, es, name, shape, dt, n, psum=False):
        mk = S.psum if psum else S.sbuf
        self.items = [mk("%s%d" % (name, i), shape, dt, es=es) for i in range(n)]
        self.i = 0

    def next(self):
        it = self.items[self.i % len(self.items)]
        self.i += 1
        return it


class Ctx:
    pass


def build_program(stop_after=None, dump=()):
    nc = bass.Bass("TRN2", target_bir_lowering=False)
    C = Ctx()
    C.nc = nc
    C.dump = set(dump)
    C.stop_after = stop_after

    def din(name, shape, dt=F32):
        return nc.dram_tensor(name, list(shape), dt, kind="ExternalInput").ap()

    def dscr(name, shape, dt):
        kind = "ExternalOutput" if name in C.dump else "Internal"
        return nc.dram_tensor(name, list(shape), dt, kind=kind).ap()

    SPEC = {
        "xT0": [D, T],
        "cTp": [128, KD, 2],
        "w_ada": [DEPTH, D, 6 * D],
        "b_adaT": [DEPTH, 128, 96, 2],
        "g1T": [DEPTH, 128, KD, 2],
        "g2T": [DEPTH, 128, KD, 2],
        "gfT": [128, KD, 2],
        "w_in": [DEPTH, D, 3776],
        "w_in_sw": [DEPTH, D, 64],
        "w_out": [DEPTH, D, D],
        "sgu_g": [DEPTH, 1, 512],
        "sgu_wT": [DEPTH, 128, 4, 128],
        "sgu_b4": [DEPTH, 1, 2048],
        "qg": [DEPTH, 128, 3],
        "kvg": [DEPTH, 128, 2],
        "w_uq": [DEPTH, 384, 768],
        "w_uq_sw": [DEPTH, 384, 256],
        "w_ukv": [DEPTH, 256, 1024],
        "rope_cos": [64, T],
        "rope_sin": [64, T],
        "s5_pp": [DEPTH, 128, 3, 32],
        "s5_row": [DEPTH, 3, 4096],
        "s5_Bre": [DEPTH, 128, 4096],
        "s5_Bim": [DEPTH, 128, 4096],
        "s5_Cre": [DEPTH, 128, 4096],
        "s5_Cim": [DEPTH, 128, 4096],
        "s5_d": [DEPTH, 128, 4],
        "s5_tau": [1, 2 * T],
        "w_glu": [DEPTH, 512, 512],
        "b_glu": [DEPTH, 128, 4],
        "conv_wT": [DEPTH, 128, 4, 3],
        "w_router": [DEPTH, D, 16],
        "w_gate": [DEPTH, 16, D, 1024],
        "w_up": [DEPTH, 16, D, 1024],
        "w_down": [DEPTH, 16, 1024, D],
        "ident": [128, 128],
        "iota_j": [1, NJ],
        "iota_p3": [128, 3],
        "sel": [16, 16, 128],
    }

    class LazyIn(dict):
        def __missing__(self, k):
            v = din(k, SPEC[k])
            self[k] = v
            return v
    I = LazyIn()
    C.I = I
    C.outT = nc.dram_tensor("outT", [D, NL], F32, kind="ExternalOutput").ap()

    Sx = {}
    Sx["xA"] = dscr("xA", [D, T], F32)
    Sx["xB"] = dscr("xB", [D, T], F32)
    Sx["uTg"] = dscr("uTg", [512, T], BF16)
    Sx["v_tok"] = dscr("v_tok", [T, 512], BF16)
    Sx["cqT"] = dscr("cqT", [384, T], BF16)
    Sx["ckvT"] = dscr("ckvT", [256, T], BF16)
    Sx["krT"] = dscr("krT", [64, T], BF16)
    Sx["s5uT"] = dscr("s5uT", [512, T], BF16)
    Sx["s5u32"] = dscr("s5u32", [512, T], F32)
    Sx["catT"] = dscr("catT", [D, T], BF16)
    Sx["ys"] = dscr("ys", [16, NJ, D], BF16)
    Sx["dbg_mod"] = dscr("dbg_mod", [DEPTH, 128, 96, 2], F32)
    Sx["dbg_hT"] = dscr("dbg_hT", [D, T], BF16)
    Sx["dbg_aff"] = dscr("dbg_aff", [128, 18, 16], F32)
    Sx["dbg_posm"] = dscr("dbg_posm", [16, T], F32)
    Sx["dbg_g"] = dscr("dbg_g", [512, T], BF16)
    C.Sx = Sx

    with ExitStack() as es:
        S = Sched(nc, es)
        C.S = S
        _consts(C)
        phase_mod(C)
        S.barrier()
        x_in = I["xT0"]
        done = (stop_after == "mod")
        for li in range(DEPTH):
            if done:
                break
            x1 = Sx["xA"]
            x2 = Sx["xB"]
            for ph in (phase_in, phase_sgu, phase_mla, phase_s5, phase_out, phase_moe):
                if ph is phase_in:
                    ph(C, li, x_in)
                elif ph is phase_out:
                    ph(C, li, x_in, x1)
                elif ph is phase_moe:
                    ph(C, li, x1, x2)
                else:
                    ph(C, li)
                S.barrier()
                if stop_after == (li, ph.__name__) or (isinstance(stop_after, tuple) and len(stop_after) == 3 and stop_after[0] == li and ph is phase_in):
                    done = True
                    break
            if done:
                break
            x_in = x2
        if not done:
            phase_final(C, x_in)
        S.barrier()
    nc._used_inputs = set(I.keys())
    return nc


def _consts(C):
    S, nc, I = C.S, C.nc, C.I
    C.ones_bf, C.ones_bf_b = S.sbuf("ones_bf", [128, 128], BF16)
    S.op("pool", lambda: nc.gpsimd.memset(C.ones_bf[:], 1.0), writes=[C.ones_bf_b])
    C.eps_t, C.eps_b = S.sbuf("eps", [128, 1], F32)
    S.op("pool", lambda: nc.gpsimd.memset(C.eps_t[:], EPS), writes=[C.eps_b])
    C.one_f, C.one_f_b = S.sbuf("one_f", [128, 1], F32)
    S.op("pool", lambda: nc.gpsimd.memset(C.one_f[:], 1.0), writes=[C.one_f_b])
    C.hpi_t, C.hpi_b = S.sbuf("hpi", [128, 1], F32)
    S.op("pool", lambda: nc.gpsimd.memset(C.hpi_t[:], math.pi / 2), writes=[C.hpi_b])
    C.ident_f, C.ident_f_b = S.sbuf("ident_f", [128, 128], F32)
    S.dma("sp", C.ident_f[:], I["ident"][:, :], writes=[C.ident_f_b])
    C.ident_bf, C.ident_bf_b = S.sbuf("ident_bf", [128, 128], BF16)
    S.dma("pool", C.ident_bf[:], I["ident"][:, :], writes=[C.ident_bf_b])
    C.mod = []
    C.modalloc = []
    for li in range(DEPTH):
        C.modalloc.append((S.sbuf("mod%d" % li, [128, 96, 2], F32), S.sbuf("A1_%d" % li, [128, KD, 2], F32), S.sbuf("A2_%d" % li, [128, KD, 2], F32)))


def _dbg(C, name, src_ap, reads):
    if name in C.dump:
        C.S.dma("sp", C.Sx[name], src_ap, reads=reads)


def phase_mod(C):
    S, nc, I = C.S, C.nc, C.I
    with ExitStack() as ph:
        sc, scb = S.sbuf("sc", [128, KD, 2], F32, es=ph)
        S.dma("sp", sc[:], I["cTp"][:, :, :], writes=[scb])
        S.op("act", lambda: nc.scalar.activation(out=sc[:], in_=sc[:], func=AF.Silu), reads=[scb], writes=[scb])
        war = Ring(S, ph, "wa", [128, KD, 512], F32, 2)
        mps_, mpsb = S.psum("mps", [128, 512], F32, es=ph)
        mps = mps_[:, 0:192]
        for li in range(DEPTH):
            nxt = war.next()
            S.dma("sp", nxt[0][:], I["w_ada"][li, :, 0:512].rearrange("(k p) n -> p k n", p=128), writes=[nxt[1]])
            for nb in range(24):
                wa, wab = nxt
                if nb + 1 < 24:
                    nxt = war.next()
                    S.dma("sp", nxt[0][:], I["w_ada"][li, :, (nb + 1) * 512:(nb + 2) * 512].rearrange("(k p) n -> p k n", p=128),
                          writes=[nxt[1]])

                def mm():
                    ins = None
                    for fl in range(4):
                        fc = nb * 4 + fl
                        for k in range(KD):
                            ins = nc.tensor.matmul(mps[:, fc * 2:fc * 2 + 2], lhsT=wa[:, k, fl * 128:(fl + 1) * 128],
                                                   rhs=sc[:, k, :], start=(k == 0), stop=(k == KD - 1))
                    return ins
                S.op("pe", mm, reads=[wab, scb], writes=[mpsb])
            m = Ctx()
            modt, modb = C.modalloc[li][0]
            bt, btb = S.sbuf("badat", [128, 96, 2], F32, es=ph)
            S.dma("sp", bt[:], I["b_adaT"][li], writes=[btb])
            S.op("dve", lambda: nc.vector.tensor_tensor(modt[:].rearrange("p a b -> p (a b)"), mps_[:, 0:192], bt[:].rearrange("p a b -> p (a b)"), ALU.add),
                 reads=[mpsb, btb], writes=[modb])
            m.t, m.b = modt, modb
            for nm, gname, j in (("A1", "g1T", 1), ("A2", "g2T", 4)):
                gt, gtb = S.sbuf("g" + nm, [128, KD, 2], F32, es=ph)
                S.dma("sp", gt[:], I[gname][li], writes=[gtb])
                at, atb = C.modalloc[li][1 if nm == "A1" else 2]
                S.op("dve", lambda: nc.vector.scalar_tensor_tensor(at[:], modt[:, j * 16:(j + 1) * 16, :], 1.0, gt[:], ALU.add, ALU.mult),
                     reads=[modb, gtb], writes=[atb])
                setattr(m, nm, at)
                setattr(m, nm + "b", atb)
            C.mod.append(m)
            if "dbg_mod" in C.dump:
                S.dma("sp", C.Sx["dbg_mod"][li], modt[:], reads=[modb])


def modsl(m, j):
    return m.t[:, j * 16:(j + 1) * 16, :]


def norm_blocks(C, ph, x_dram, A_ap, A_b, B_ap, B_b, cb, blocks=TB):
    S, nc = C.S, C.nc
    xr = Ring(S, ph, "xblk", [128, KD, 512], F32, 2)
    sqt, sqb = S.sbuf("nsq", [128, KD, 512], BF16, es=ph)
    ssr = Ring(S, ph, "nss", [128, 512], F32, 2, psum=True)
    rs, rsb = S.sbuf("nrstd", [128, 512], F32, es=ph)
    for bi, (t0, n, lc) in enumerate(blocks):
        xb, xbb = xr.next()
        S.dma("sp", xb[:, :, :n], x_dram[:, t0:t0 + n].rearrange("(k p) t -> p k t", p=128), writes=[xbb])
        S.op("act", lambda: nc.scalar.activation(out=sqt[:, :, :n], in_=xb[:, :, :n], func=AF.Square), reads=[xbb], writes=[sqb])
        ss, ssb = ssr.next()

        def mm():
            ins = None
            for k in range(KD):
                ins = nc.tensor.matmul(ss[:, :n], lhsT=C.ones_bf[:, :], rhs=sqt[:, k, :n], start=(k == 0), stop=(k == KD - 1))
            return ins
        S.op("pe", mm, reads=[sqb, C.ones_bf_b], writes=[ssb])
        S.op("act", lambda: nc.scalar.activation(out=rs[:, :n], in_=ss[:, :n], func=AF.Sqrt, bias=C.eps_t[:, 0:1], scale=1.0 / D),
             reads=[ssb, C.eps_b], writes=[rsb])
        S.op("dve", lambda: nc.vector.reciprocal(rs[:, :n], rs[:, :n]), reads=[rsb], writes=[rsb])

        def nrm():
            ins = None
            for k in range(KD):
                ins = nc.vector.tensor_tensor(xb[:, k, :n], xb[:, k, :n], rs[:, :n], ALU.mult)
            return ins
        S.op("dve", nrm, reads=[xbb, rsb], writes=[xbb])

        def aff_act():
            ins = None
            for k in range(0, KD, 2):
                ins = nc.scalar.activation(out=xb[:, k, :n], in_=xb[:, k, :n], func=AF.Identity,
                                           bias=B_ap[:, k, lc:lc + 1], scale=A_ap[:, k, lc:lc + 1])
            return ins

        def aff_pool():
            ins = None
            for k in range(1, KD, 2):
                ins = nc.gpsimd.tensor_scalar(xb[:, k, :n], xb[:, k, :n], A_ap[:, k, lc:lc + 1], B_ap[:, k, lc:lc + 1], ALU.mult, ALU.add)
            return ins
        S.op("act", aff_act, reads=[xbb, A_b, B_b], writes=[xbb])
        S.op("pool", aff_pool, reads=[xbb, A_b, B_b], writes=[xbb])
        cb(bi, t0, n, lc, xb, xbb)


def phase_in(C, li, x_dram):
    S, nc, I, Sx = C.S, C.nc, C.I, C.Sx
    m = C.mod[li]
    with ExitStack() as ph:
        hT, hTb = S.sbuf("hT", [128, KD, T], BF16, es=ph)
        with ExitStack() as ph1:
            def cb(bi, t0, n, lc, xb, xbb):
                S.op("dve", lambda: nc.vector.tensor_copy(hT[:, :, t0:t0 + n], xb[:, :, :n]), reads=[xbb], writes=[hTb])
            norm_blocks(C, ph1, x_dram, m.A1, m.A1b, modsl(m, 0), m.b, cb)
        if "dbg_hT" in C.dump:
            S.dma("sp", Sx["dbg_hT"].rearrange("(k p) t -> p k t", p=128), hT[:], reads=[hTb])
        if C.stop_after == (li, "in", "norm"):
            return
        S.barrier()
        wr = Ring(S, ph, "wt", [128, KD, 512], BF16, 2)
        pr = Ring(S, ph, "pin", [128, 512], F32, 4, psum=True)
        win = I["w_in"][li]

        def wload(segs):
            wt, wtb = wr.next()
            for si, (src, c0, ncol, off) in enumerate(segs):
                S.dma("pool", wt[:, :, off:off + ncol], src[:, c0:c0 + ncol].rearrange("(k p) n -> p k n", p=128),
                      writes=[wtb] if si == 0 else [], awrites=[] if si == 0 else [wtb])
            return wt, wtb

        def proj_fm(wt, wtb, woff, M, t0, n):
            ps, psb = pr.next()

            def mm():
                ins = None
                for k in range(KD):
                    ins = nc.tensor.matmul(ps[:M, :n], lhsT=wt[:, k, woff:woff + M], rhs=hT[:, k, t0:t0 + n], start=(k == 0), stop=(k == KD - 1))
                return ins
            S.op("pe", mm, reads=[wtb, hTb], writes=[psb])
            return ps, psb

        obr = Ring(S, ph, "ob", [128, 512], BF16, 4)
        ofr = Ring(S, ph, "of", [128, 512], F32, 3)

        wt, wtb = wload([(win, 0, 512, 0)])
        for (t0, n, lc) in TB:
            for c in range(4):
                ps, psb = proj_fm(wt, wtb, c * 128, 128, t0, n)
                ob, obb = obr.next()
                S.op("act", lambda: nc.scalar.activation(out=ob[:, :n], in_=ps[:, :n], func=AF.Gelu), reads=[psb], writes=[obb])
                S.dma("sp", Sx["uTg"][c * 128:(c + 1) * 128, t0:t0 + n], ob[:, :n], reads=[obb])

        if C.stop_after == (li, "in", "A"):
            return
        wt, wtb = wload([(win, 512, 512, 0)])
        gs, gsb = S.sbuf("gs", [128, 512], F32, es=ph)
        S.dma("sp", gs[:], I["sgu_g"][li].to_broadcast([128, 512]), writes=[gsb])
        junk, junkb = S.sbuf("junk", [128, 512], BF16, es=ph)
        st, stb = S.sbuf("vst", [128, 2], F32, es=ph)
        for tt in range(18):
            ps, psb = pr.next()

            def mm():
                ins = None
                for k in range(KD):
                    ins = nc.tensor.matmul(ps[:, :], lhsT=hT[:, k, tt * 128:(tt + 1) * 128], rhs=wt[:, k, :], start=(k == 0), stop=(k == KD - 1))
                return ins
            S.op("pe", mm, reads=[wtb, hTb], writes=[psb])
            gv, gvb = ofr.next()
            S.op("act", lambda: nc.scalar.activation(out=gv[:, :], in_=ps[:, :], func=AF.Gelu), reads=[psb], writes=[gvb])
            S.op("act", lambda: nc.scalar.activation(out=junk[:, :], in_=gv[:, :], func=AF.Square, accum_out=st[:, 0:1]),
                 reads=[gvb], writes=[junkb, stb])
            S.op("act", lambda: nc.scalar.activation(out=st[:, 1:2], in_=st[:, 0:1], func=AF.Sqrt, bias=C.eps_t[:, 0:1], scale=1.0 / 512),
                 reads=[stb, C.eps_b], writes=[stb])
            S.op("dve", lambda: nc.vector.reciprocal(st[:, 1:2], st[:, 1:2]), reads=[stb], writes=[stb])
            ob, obb = obr.next()
            S.op("dve", lambda: nc.vector.scalar_tensor_tensor(ob[:, :], gv[:, :], st[:, 1:2], gs[:, :], ALU.mult, ALU.mult),
                 reads=[gvb, stb, gsb], writes=[obb])
            S.dma("sp", Sx["v_tok"][tt * 128:(tt + 1) * 128, :], ob[:, :], reads=[obb])

        if C.stop_after == (li, "in", "v"):
            return
        wt, wtb = wload([(win, 1024, 512, 0)])
        for (t0, n, lc) in TB:
            for c in range(4):
                ps, psb = proj_fm(wt, wtb, c * 128, 128, t0, n)
                ob, obb = obr.next()
                S.op("dve" if c % 2 else "act", (lambda: nc.vector.tensor_copy(ob[:, :n], ps[:, :n])) if c % 2 else
                     (lambda: nc.scalar.activation(out=ob[:, :n], in_=ps[:, :n], func=AF.Copy)), reads=[psb], writes=[obb])
                dst = Sx["cqT"][c * 128:(c + 1) * 128, t0:t0 + n] if c < 3 else Sx["ckvT"][0:128, t0:t0 + n]
                S.dma("sp", dst, ob[:, :n], reads=[obb])

        if C.stop_after == (li, "in", "B"):
            return
        wt, wtb = wload([(win, 1536, 192, 0), (I["w_in_sw"][li], 0, 64, 192)])
        rc, rcb = S.sbuf("ropec", [64, T], F32, es=ph)
        rsn, rsnb = S.sbuf("ropes", [64, T], F32, es=ph)
        S.dma("sp", rc[:], I["rope_cos"][:, :], writes=[rcb])
        S.dma("sp", rsn[:], I["rope_sin"][:, :], writes=[rsnb])
        for (t0, n, lc) in TB:
            ps, psb = proj_fm(wt, wtb, 0, 128, t0, n)
            ob, obb = obr.next()
            S.op("act", lambda: nc.scalar.activation(out=ob[:, :n], in_=ps[:, :n], func=AF.Copy), reads=[psb], writes=[obb])
            S.dma("sp", Sx["ckvT"][128:256, t0:t0 + n], ob[:, :n], reads=[obb])
            ps1, ps1b = proj_fm(wt, wtb, 128, 64, t0, n)
            ps2, ps2b = proj_fm(wt, wtb, 192, 64, t0, n)
            f1, f1b = ofr.next()
            f2, f2b = ofr.next()
            S.op("dve", lambda: nc.vector.tensor_tensor(f1[:64, :n], ps1[:64, :n], rc[:, t0:t0 + n], ALU.mult), reads=[ps1b, rcb], writes=[f1b])
            S.op("dve", lambda: nc.vector.tensor_tensor(f2[:64, :n], ps2[:64, :n], rsn[:, t0:t0 + n], ALU.mult), reads=[ps2b, rsnb], writes=[f2b])
            ob, obb = obr.next()
            S.op("pool", lambda: nc.gpsimd.tensor_tensor(ob[:64, :n], f1[:64, :n], f2[:64, :n], ALU.add), reads=[f1b, f2b], writes=[obb])
            S.dma("sp", Sx["krT"][:, t0:t0 + n], ob[:64, :n], reads=[obb])

        if C.stop_after == (li, "in", "C"):
            return
        wt, wtb = wload([(win, 1728, 512, 0)])
        for (t0, n, lc) in TB:
            for c in range(4):
                ps, psb = proj_fm(wt, wtb, c * 128, 128, t0, n)
                of, ofb = ofr.next()
                ob, obb = obr.next()
                S.op("dve", lambda: nc.vector.tensor_copy(of[:, :n], ps[:, :n]), reads=[psb], writes=[ofb])
                S.op("act", lambda: nc.scalar.activation(out=ob[:, :n], in_=of[:, :n], func=AF.Copy), reads=[ofb], writes=[obb])
                S.dma("sp", Sx["s5uT"][c * 128:(c + 1) * 128, t0:t0 + n], ob[:, :n], reads=[obb])
                S.dma("sp", Sx["s5u32"][c * 128:(c + 1) * 128, t0:t0 + n], of[:, :n], reads=[ofb])

        if C.stop_after == (li, "in", "D"):
            return
        cw, cwb = S.sbuf("convw", [128, 4, 3], F32, es=ph)
        S.dma("sp", cw[:], I["conv_wT"][li], writes=[cwb])
        ZW = 2307
        zr = Ring(S, ph, "zbuf", [128, ZW], F32, 2)
        yr = Ring(S, ph, "ybuf", [128, ZW], F32, 2)
        bgr = Ring(S, ph, "bgbuf", [128, T], BF16, 2)
        cor = Ring(S, ph, "cobuf", [128, T], BF16, 2)

        def zoff(t0):
            return t0 + 1 if t0 < NL else t0 + 2
        for c in range(4):
            wt, wtb = wload([(win, 2240 + c * 128, 128, 0), (win, 2752 + c * 128, 128, 128), (win, 3264 + c * 128, 128, 256)])
            zb, zbb = zr.next()
            yb, ybb = yr.next()
            bg, bgb = bgr.next()
            co, cob = cor.next()

            def zz():
                nc.gpsimd.memset(zb[:, 0:1], 0.0)
                nc.gpsimd.memset(zb[:, NL + 1:NL + 2], 0.0)
                return nc.gpsimd.memset(zb[:, ZW - 1:ZW], 0.0)
            S.op("pool", zz, writes=[zbb])
            for (t0, n, lc) in TB:
                psB, psBb = proj_fm(wt, wtb, 0, 128, t0, n)
                psC, psCb = proj_fm(wt, wtb, 128, 128, t0, n)
                psH, psHb = proj_fm(wt, wtb, 256, 128, t0, n)
                S.op("act", lambda: nc.scalar.activation(out=bg[:, t0:t0 + n], in_=psB[:, :n], func=AF.Copy), reads=[psBb], writes=[bgb])
                of, ofb = ofr.next()
                S.op("act", lambda: nc.scalar.activation(out=of[:, :n], in_=psC[:, :n], func=AF.Copy), reads=[psCb], writes=[ofb])
                zo = zoff(t0)
                S.op("dve", lambda: nc.vector.tensor_tensor(zb[:, zo:zo + n], psH[:, :n], of[:, :n], ALU.mult), reads=[psHb, ofb], writes=[zbb])

            S.seq("dve", [
                lambda: nc.vector.tensor_scalar(yb[:, 1:ZW - 1], zb[:, 1:ZW - 1], cw[:, c, 1:2], None, ALU.mult),
                lambda: nc.vector.scalar_tensor_tensor(yb[:, 1:ZW - 1], zb[:, 0:ZW - 2], cw[:, c, 0:1], yb[:, 1:ZW - 1], ALU.mult, ALU.add),
                lambda: nc.vector.scalar_tensor_tensor(yb[:, 1:ZW - 1], zb[:, 2:ZW], cw[:, c, 2:3], yb[:, 1:ZW - 1], ALU.mult, ALU.add),
            ], reads=[zbb, cwb], writes=[ybb])

            def gate():
                nc.gpsimd.tensor_tensor(co[:, 0:NL], yb[:, 1:NL + 1], bg[:, 0:NL], ALU.mult)
                return nc.gpsimd.tensor_tensor(co[:, NL:T], yb[:, NL + 2:ZW - 1], bg[:, NL:T], ALU.mult)
            S.op("pool", gate, reads=[ybb, bgb], writes=[cob])
            S.dma("sp", Sx["catT"][1536 + c * 128:1536 + (c + 1) * 128, :], co[:, :], reads=[cob])


def phase_sgu(C, li):
    S, nc, I, Sx = C.S, C.nc, C.I, C.Sx
    with ExitStack() as ph:
        ws, wsb = S.sbuf("wsT", [128, 4, 128], BF16, es=ph)
        S.dma("pool", ws[:], I["sgu_wT"][li], writes=[wsb])
        bsr, bsrb = S.sbuf("bsr", [128, 4, 512], F32, es=ph)
        S.dma("sp", bsr[:].rearrange("p a b -> p (a b)"), I["sgu_b4"][li].to_broadcast([128, 2048]), writes=[bsrb])
        vr = Ring(S, ph, "vt", [128, 4, 512], BF16, 2)
        ur = Ring(S, ph, "ut", [128, 4, 512], BF16, 2)
        pr = Ring(S, ph, "psg", [128, 512], F32, 4, psum=True)
        tr = Ring(S, ph, "tmp", [128, 512], F32, 3)
        orr = Ring(S, ph, "osg", [128, 512], BF16, 3)
        for (t0, n, lc) in TB:
            na = n // 128
            vt, vtb = vr.next()
            ut, utb = ur.next()
            S.dma("sp", vt[:, :na, :], Sx["v_tok"][t0:t0 + n, :].rearrange("(a q) c -> q a c", q=128), writes=[vtb])
            S.dma("sp", ut[:, :, :n], Sx["uTg"][:, t0:t0 + n].rearrange("(h c) t -> c h t", c=128), writes=[utb])
            for h in range(4):
                ps, psb = pr.next()

                def mm():
                    ins = None
                    for a in range(na):
                        ins = nc.tensor.matmul(ps[:, a * 128:(a + 1) * 128], lhsT=vt[:, a, h * 128:(h + 1) * 128], rhs=ws[:, h, :], start=True, stop=True)
                    return ins
                S.op("pe", mm, reads=[vtb, wsb], writes=[psb])
                tm, tmb = tr.next()
                S.op("dve", lambda: nc.vector.tensor_tensor(tm[:, :n], ps[:, :n], bsr[:, h, :n], ALU.add), reads=[psb, bsrb], writes=[tmb])
                ob, obb = orr.next()
                S.op("pool", lambda: nc.gpsimd.tensor_tensor(ob[:, :n], tm[:, :n], ut[:, h, :n], ALU.mult), reads=[tmb, utb], writes=[obb])
                S.dma("sp", Sx["catT"][h * 128:(h + 1) * 128, t0:t0 + n], ob[:, :n], reads=[obb])


def phase_mla(C, li):
    S, nc, I, Sx = C.S, C.nc, C.I, C.Sx
    SC = 192.0 ** -0.5
    with ExitStack() as ph:
        cq, cqb = S.sbuf("cq", [128, 3, T], BF16, es=ph)
        ckv, ckvb = S.sbuf("ckv", [128, 2, T], BF16, es=ph)
        kr, krb = S.sbuf("kr", [64, T], BF16, es=ph)
        S.dma("sp", cq[:], Sx["cqT"].rearrange("(c p) t -> p c t", p=128), writes=[cqb])
        S.dma("sp", ckv[:], Sx["ckvT"].rearrange("(c p) t -> p c t", p=128), writes=[ckvb])
        S.dma("sp", kr[:], Sx["krT"][:, :], writes=[krb])
        wuq, wuqb = S.sbuf("wuq", [128, 3, 768], BF16, es=ph)
        wuqs, wuqsb = S.sbuf("wuqs", [128, 3, 256], BF16, es=ph)
        wukv, wukvb = S.sbuf("wukv", [128, 2, 1024], BF16, es=ph)
        wv, wvb = S.sbuf("wv", [128, 2, 512], BF16, es=ph)
        rq, rqb = S.sbuf("rq", [128, T], F32, es=ph)
        rk, rkb = S.sbuf("rk", [128, T], F32, es=ph)
        rkt, rktb = S.sbuf("rkt", [128, 18], F32, es=ph)
        pr = Ring(S, ph, "pj", [128, 512], F32, 3, psum=True)
        pst_, pstb = S.psum("pst", [128, 512], F32, es=ph)
        pst = pst_[:, 0:18]
        with ExitStack() as wp:
            w32, w32b = S.sbuf("w32", [128, 3, 768], F32, es=wp)
            ws32, ws32b = S.sbuf("ws32", [128, 3, 256], F32, es=wp)
            wk32, wk32b = S.sbuf("wk32", [128, 2, 1024], F32, es=wp)
            qg, qgb = S.sbuf("qg", [128, 3], F32, es=wp)
            kg, kgb = S.sbuf("kg", [128, 2], F32, es=wp)
            S.dma("sp", w32[:], I["w_uq"][li].rearrange("(c p) n -> p c n", p=128), writes=[w32b])
            S.dma("sp", ws32[:], I["w_uq_sw"][li].rearrange("(c p) n -> p c n", p=128), writes=[ws32b])
            S.dma("sp", wk32[:], I["w_ukv"][li].rearrange("(c p) n -> p c n", p=128), writes=[wk32b])
            S.dma("sp", qg[:], I["qg"][li], writes=[qgb])
            S.dma("sp", kg[:], I["kvg"][li], writes=[kgb])

            def sc1():
                ins = None
                for c in range(3):
                    nc.vector.tensor_scalar(wuq[:, c, :], w32[:, c, :], qg[:, c:c + 1], None, ALU.mult)
                    ins = nc.vector.tensor_scalar(wuqs[:, c, :], ws32[:, c, :], qg[:, c:c + 1], None, ALU.mult)
                return ins
            S.op("dve", sc1, reads=[w32b, ws32b, qgb], writes=[wuqb, wuqsb])

            def sc2():
                ins = None
                for c in range(2):
                    ins = nc.vector.tensor_scalar(wukv[:, c, :], wk32[:, c, :], kg[:, c:c + 1], None, ALU.mult)
                return ins
            S.op("dve", sc2, reads=[wk32b, kgb], writes=[wukvb])
            S.op("dve", lambda: nc.vector.tensor_copy(wv[:].rearrange("p c (h x) -> p c h x", x=128),
                                                       wukv[:].rearrange("p c (h x) -> p c h x", x=256)[:, :, :, 128:256]),
                 reads=[wukvb], writes=[wvb])
            sqq, sqqb = S.sbuf("sqq", [128, 3, T], BF16, es=wp)
            sqk, sqkb = S.sbuf("sqk", [128, 2, T], BF16, es=wp)
            S.op("act", lambda: nc.scalar.activation(out=sqq[:], in_=cq[:], func=AF.Square), reads=[cqb], writes=[sqqb])
            S.op("act", lambda: nc.scalar.activation(out=sqk[:], in_=ckv[:], func=AF.Square), reads=[ckvb], writes=[sqkb])
            for (sq, sqb_, nch, rt, rtb) in ((sqq, sqqb, 3, rq, rqb), (sqk, sqkb, 2, rk, rkb)):
                for (t0, n, lc) in TB:
                    ps, psb = pr.next()

                    def mm():
                        ins = None
                        for c in range(nch):
                            ins = nc.tensor.matmul(ps[:, :n], lhsT=C.ones_bf[:, :], rhs=sq[:, c, t0:t0 + n], start=(c == 0), stop=(c == nch - 1))
                        return ins
                    S.op("pe", mm, reads=[sqb_, C.ones_bf_b], writes=[psb])
                    S.op("act", lambda: nc.scalar.activation(out=rt[:, t0:t0 + n], in_=ps[:, :n], func=AF.Sqrt, bias=C.eps_t[:, 0:1],
                                                             scale=1.0 / (128 * nch)), reads=[psb, C.eps_b], writes=[rtb])
            S.op("dve", lambda: nc.vector.reciprocal(rq[:], rq[:]), reads=[rqb], writes=[rqb])
            S.op("dve", lambda: nc.vector.reciprocal(rk[:], rk[:]), reads=[rkb], writes=[rkb])

            def mmt():
                ins = None
                for tt in range(18):
                    for c in range(2):
                        ins = nc.tensor.matmul(pst[:, tt:tt + 1], lhsT=sqk[:, c, tt * 128:(tt + 1) * 128], rhs=C.ones_bf[:, 0:1],
                                               start=(c == 0), stop=(c == 1))
                return ins
            S.op("pe", mmt, reads=[sqkb, C.ones_bf_b], writes=[pstb])
            S.op("act", lambda: nc.scalar.activation(out=rkt[:], in_=pst_[:, 0:18], func=AF.Sqrt, bias=C.eps_t[:, 0:1], scale=1.0 / 256),
                 reads=[pstb, C.eps_b], writes=[rktb])
            S.op("dve", lambda: nc.vector.reciprocal(rkt[:], rkt[:]), reads=[rktb], writes=[rktb])
            S.barrier()
        rc, rcb = S.sbuf("ropec", [64, T], F32, es=ph)
        rsn, rsnb = S.sbuf("ropes", [64, T], F32, es=ph)
        S.dma("sp", rc[:], I["rope_cos"][:, :], writes=[rcb])
        S.dma("sp", rsn[:], I["rope_sin"][:, :], writes=[rsnb])
        qn, qnb = S.sbuf("qn", [128, 4, T], BF16, es=ph)
        qr, qrb = S.sbuf("qr", [64, 4, T], BF16, es=ph)
        kn, knb = S.sbuf("kn", [128, 4, T], BF16, es=ph)
        vtok, vtokb = S.sbuf("vtok", [128, 18, 512], BF16, es=ph)
        fr = Ring(S, ph, "mf", [128, 512], F32, 4)

        def proj(wt, wtb, nch, c0, M, src, srcb, t0, n):
            ps, psb = pr.next()

            def mm():
                ins = None
                for c in range(nch):
                    ins = nc.tensor.matmul(ps[:M, :n], lhsT=wt[:, c, c0:c0 + M], rhs=src[:, c, t0:t0 + n], start=(c == 0), stop=(c == nch - 1))
                return ins
            S.op("pe", mm, reads=[wtb, srcb], writes=[psb])
            return ps, psb
        for h in range(4):
            for (t0, n, lc) in TB:
                ps, psb = proj(wuq, wuqb, 3, h * 192, 128, cq, cqb, t0, n)
                S.op("dve", lambda: nc.vector.scalar_tensor_tensor(qn[:, h, t0:t0 + n], ps[:, :n], SC, rq[:, t0:t0 + n], ALU.mult, ALU.mult),
                     reads=[psb, rqb], writes=[qnb])
                ps1, ps1b = proj(wuq, wuqb, 3, h * 192 + 128, 64, cq, cqb, t0, n)
                ps2, ps2b = proj(wuqs, wuqsb, 3, h * 64, 64, cq, cqb, t0, n)
                f1, f1b = fr.next()
                f2, f2b = fr.next()
                S.op("dve", lambda: nc.vector.tensor_tensor(f1[:64, :n], ps1[:64, :n], rc[:, t0:t0 + n], ALU.mult), reads=[ps1b, rcb], writes=[f1b])
                S.op("dve", lambda: nc.vector.tensor_tensor(f2[:64, :n], ps2[:64, :n], rsn[:, t0:t0 + n], ALU.mult), reads=[ps2b, rsnb], writes=[f2b])
                S.op("pool", lambda: nc.gpsimd.tensor_tensor(f1[:64, :n], f1[:64, :n], f2[:64, :n], ALU.add), reads=[f1b, f2b], writes=[f1b])
                S.op("dve", lambda: nc.vector.scalar_tensor_tensor(qr[:, h, t0:t0 + n], f1[:64, :n], SC, rq[:64, t0:t0 + n], ALU.mult, ALU.mult),
                     reads=[f1b, rqb], writes=[qrb])
                ps, psb = proj(wukv, wukvb, 2, h * 256, 128, ckv, ckvb, t0, n)
                S.op("dve", lambda: nc.vector.tensor_tensor(kn[:, h, t0:t0 + n], ps[:, :n], rk[:, t0:t0 + n], ALU.mult), reads=[psb, rkb], writes=[knb])
        for tt in range(18):
            ps, psb = pr.next()

            def mm():
                ins = None
                for c in range(2):
                    ins = nc.tensor.matmul(ps[:, :], lhsT=ckv[:, c, tt * 128:(tt + 1) * 128], rhs=wv[:, c, :], start=(c == 0), stop=(c == 1))
                return ins
            S.op("pe", mm, reads=[ckvb, wvb], writes=[psb])
            S.op("dve", lambda: nc.vector.tensor_scalar(vtok[:, tt, :], ps[:, :], rkt[:, tt:tt + 1], None, ALU.mult), reads=[psb, rktb], writes=[vtokb])
        opr = Ring(S, ph, "ops", [128, 512], F32, 2, psum=True)
        spr = Ring(S, ph, "sps", [128, 512], F32, 2, psum=True)
        ptr = Ring(S, ph, "pt", [128, 512], BF16, 3)
        rsr = Ring(S, ph, "rsm", [128, 512], F32, 2)
        atr = Ring(S, ph, "att", [128, 512], BF16, 2)
        for h in range(4):
            for (t0, n, lc) in TB:
                kts = list(range(18)) if lc == 0 else [16, 17]
                ops, opsb = opr.next()
                sps, spsb = spr.next()
                for i, kt in enumerate(kts):
                    st, stb = pr.next()

                    def mms():
                        nc.tensor.matmul(st[:, :n], lhsT=kn[:, h, kt * 128:(kt + 1) * 128], rhs=qn[:, h, t0:t0 + n], start=True, stop=False)
                        return nc.tensor.matmul(st[:, :n], lhsT=kr[:, kt * 128:(kt + 1) * 128], rhs=qr[:, h, t0:t0 + n], start=False, stop=True)
                    S.op("pe", mms, reads=[knb, qnb, krb, qrb], writes=[stb])
                    pt, ptb = ptr.next()
                    S.op("act", lambda: nc.scalar.activation(out=pt[:, :n], in_=st[:, :n], func=AF.Exp), reads=[stb], writes=[ptb])

                    def mmo():
                        nc.tensor.matmul(ops[:, :n], lhsT=vtok[:, kt, h * 128:(h + 1) * 128], rhs=pt[:, :n], start=(i == 0), stop=(i == len(kts) - 1))
                        return nc.tensor.matmul(sps[:, :n], lhsT=C.ones_bf[:, :], rhs=pt[:, :n], start=(i == 0), stop=(i == len(kts) - 1))
                    S.op("pe", mmo, reads=[vtokb, ptb, C.ones_bf_b], writes=[opsb, spsb])
                rs_, rsb_ = rsr.next()
                S.op("dve", lambda: nc.vector.reciprocal(rs_[:, :n], sps[:, :n]), reads=[spsb], writes=[rsb_])
                at, atb = atr.next()
                S.op("dve", lambda: nc.vector.tensor_tensor(at[:, :n], ops[:, :n], rs_[:, :n], ALU.mult), reads=[opsb, rsb_], writes=[atb])
                S.dma("sp", Sx["catT"][512 + h * 128:512 + (h + 1) * 128, t0:t0 + n], at[:, :n], reads=[atb])


def _s5_disc(C, es, are, aim, ldt, n, need_coef, tagb):
    S, nc = C.S, C.nc
    X = [S.sbuf("s5x%d" % i, [128, n], F32, es=es) for i in range(6)]
    KI = S.sbuf("s5ki", [128, n], I32, es=es)
    (x1, b1), (x2, b2), (x3, b3), (x4, b4), (x5, b5), (x6, b6) = X
    ki, kib = KI
    S.op("dve", lambda: nc.vector.tensor_scalar(are, are, -1e-4, None, ALU.min), reads=[tagb], writes=[tagb])
    ea = [
        lambda: nc.vector.tensor_scalar(ki[:], ldt, 1.0 / math.log(2.0), None, ALU.mult),
        lambda: nc.vector.tensor_copy(x3[:], ki[:]),
        lambda: nc.vector.scalar_tensor_tensor(x4[:], x3[:], -0.693145751953125, ldt, ALU.mult, ALU.add),
        lambda: nc.vector.scalar_tensor_tensor(x4[:], x3[:], -1.42860682030941723212e-6, x4[:], ALU.mult, ALU.add),
        lambda: nc.vector.tensor_scalar(x5[:], x4[:], 1.0 / 9.0, 1.0, ALU.mult, ALU.add),
    ]
    for j in range(8, 0, -1):
        ea.append(lambda: nc.vector.tensor_tensor(x5[:], x5[:], x4[:], ALU.mult))
        ea.append(lambda j=j: nc.vector.tensor_scalar(x5[:], x5[:], 1.0 / j, 1.0, ALU.mult, ALU.add))
    ea.append(lambda: nc.vector.tensor_scalar(ki[:], x3[:], 127.0, 8388608.0, ALU.add, ALU.mult))
    ea.append(lambda: nc.vector.tensor_tensor(ldt, x5[:], ki[:].bitcast(F32), ALU.mult))
    S.seq("dve", ea, reads=[tagb], writes=[tagb, kib, b3, b4, b5])
    S.op("dve", lambda: nc.vector.tensor_tensor(x1[:], are, ldt, ALU.mult), reads=[tagb], writes=[b1])
    S.op("act", lambda: nc.scalar.activation(out=x1[:], in_=x1[:], func=AF.Exp), reads=[b1], writes=[b1])
    S.op("dve", lambda: nc.vector.tensor_tensor(x2[:], aim, ldt, ALU.mult), reads=[tagb], writes=[b2])
    S.op("dve", lambda: nc.vector.tensor_scalar(x2[:], x2[:], 1.0 / (2 * math.pi), None, ALU.mult), reads=[b2], writes=[b2])
    if not need_coef:
        return {"r": (x1, b1), "f": (x2, b2)}
    S.op("dve", lambda: nc.vector.tensor_copy(ki[:], x2[:]), reads=[b2], writes=[kib])
    S.op("dve", lambda: nc.vector.tensor_tensor(x3[:], x2[:], ki[:], ALU.subtract), reads=[b2, kib], writes=[b3])
    S.op("act", lambda: nc.scalar.activation(out=x4[:], in_=x3[:], func=AF.Sin, scale=TWO_PI), reads=[b3], writes=[b4])
    S.op("dve", lambda: nc.vector.tensor_scalar(ki[:], x2[:], 0.25, None, ALU.add), reads=[b2], writes=[kib])
    S.op("dve", lambda: nc.vector.tensor_tensor(x3[:], x2[:], ki[:], ALU.subtract), reads=[b2, kib], writes=[b3])
    S.op("act", lambda: nc.scalar.activation(out=x5[:], in_=x3[:], func=AF.Sin, scale=TWO_PI, bias=C.hpi_t[:, 0:1]), reads=[b3, C.hpi_b], writes=[b5])
    S.op("dve", lambda: nc.vector.tensor_tensor(x5[:], x1[:], x5[:], ALU.mult), reads=[b1, b5], writes=[b5])
    S.op("dve", lambda: nc.vector.tensor_scalar(x5[:], x5[:], -1.0, None, ALU.add), reads=[b5], writes=[b5])
    S.op("dve", lambda: nc.vector.tensor_tensor(x4[:], x1[:], x4[:], ALU.mult), reads=[b1, b4], writes=[b4])
    S.op("dve", lambda: nc.vector.tensor_tensor(x1[:], are, are, ALU.mult), reads=[tagb], writes=[b1])
    S.op("dve", lambda: nc.vector.tensor_tensor(x3[:], aim, aim, ALU.mult), reads=[tagb], writes=[b3])
    S.op("dve", lambda: nc.vector.tensor_tensor(x1[:], x1[:], x3[:], ALU.add), reads=[b1, b3], writes=[b1])
    S.op("dve", lambda: nc.vector.reciprocal(x1[:], x1[:]), reads=[b1], writes=[b1])
    S.op("dve", lambda: nc.vector.tensor_tensor(x2[:], x5[:], are, ALU.mult), reads=[b5, tagb], writes=[b2])
    S.op("dve", lambda: nc.vector.tensor_tensor(x3[:], x4[:], aim, ALU.mult), reads=[b4, tagb], writes=[b3])
    S.op("dve", lambda: nc.vector.tensor_tensor(x2[:], x2[:], x3[:], ALU.add), reads=[b2, b3], writes=[b2])
    S.op("dve", lambda: nc.vector.tensor_tensor(x2[:], x2[:], x1[:], ALU.mult), reads=[b2, b1], writes=[b2])
    S.op("dve", lambda: nc.vector.tensor_tensor(x6[:], x4[:], are, ALU.mult), reads=[b4, tagb], writes=[b6])
    S.op("dve", lambda: nc.vector.tensor_tensor(x3[:], x5[:], aim, ALU.mult), reads=[b5, tagb], writes=[b3])
    S.op("dve", lambda: nc.vector.tensor_tensor(x6[:], x6[:], x3[:], ALU.subtract), reads=[b6, b3], writes=[b6])
    S.op("dve", lambda: nc.vector.tensor_tensor(x6[:], x6[:], x1[:], ALU.mult), reads=[b6, b1], writes=[b6])
    return {"cre": (x2, b2), "cim": (x6, b6), "t": [(x1, b1), (x3, b3), (x4, b4), (x5, b5)]}


def phase_s5(C, li):
    S, nc, I, Sx = C.S, C.nc, C.I, C.Sx
    with ExitStack() as ph:
        BbR, BbRb = S.sbuf("BbR", [128, 32, 128], BF16, es=ph)
        BbI, BbIb = S.sbuf("BbI", [128, 32, 128], BF16, es=ph)
        CR, CRb = S.sbuf("CR", [128, 32, 128], BF16, es=ph)
        CIn, CInb = S.sbuf("CIn", [128, 32, 128], BF16, es=ph)
        pp, ppb = S.sbuf("s5pp", [128, 3, 32], F32, es=ph)
        S.dma("sp", pp[:], I["s5_pp"][li], writes=[ppb])
        dpp = _s5_disc(C, ph, pp[:, 0, :], pp[:, 1, :], pp[:, 2, :], 32, False, ppb)
        rpp, rppb = dpp["r"]
        fpp, fppb = dpp["f"]
        for half in range(2):
            with ExitStack() as wp:
                rows, rowsb = S.sbuf("s5rows", [128, 3, 2048], F32, es=wp)
                for j in range(3):
                    S.dma("sp", rows[:, j, :], I["s5_row"][li, j:j + 1, half * 2048:(half + 1) * 2048].to_broadcast([128, 2048]),
                          writes=[rowsb] if j == 0 else [], awrites=[] if j == 0 else [rowsb])
                dr = _s5_disc(C, wp, rows[:, 0, :], rows[:, 1, :], rows[:, 2, :], 2048, True, rowsb)
                cre, creb = dr["cre"]
                cim, cimb = dr["cim"]
                (t1, t1b), (t2, t2b), (t3, t3b), (t4, t4b) = dr["t"]
                S.dma("sp", t1[:], I["s5_Bre"][li, :, half * 2048:(half + 1) * 2048], writes=[t1b])
                S.dma("sp", t2[:], I["s5_Bim"][li, :, half * 2048:(half + 1) * 2048], writes=[t2b])
                osl = slice(half * 16, (half + 1) * 16)
                S.op("dve", lambda: nc.vector.tensor_tensor(t3[:], cre[:], t1[:], ALU.mult), reads=[creb, t1b], writes=[t3b])
                S.op("pool", lambda: nc.gpsimd.tensor_tensor(t4[:], cim[:], t2[:], ALU.mult), reads=[cimb, t2b], writes=[t4b])
                S.op("dve", lambda: nc.vector.tensor_tensor(BbR[:, osl, :].rearrange("p a b -> p (a b)"), t3[:], t4[:], ALU.subtract),
                     reads=[t3b, t4b], writes=[BbRb])
                S.op("dve", lambda: nc.vector.tensor_tensor(t3[:], cre[:], t2[:], ALU.mult), reads=[creb, t2b], writes=[t3b])
                S.op("pool", lambda: nc.gpsimd.tensor_tensor(t4[:], cim[:], t1[:], ALU.mult), reads=[cimb, t1b], writes=[t4b])
                S.op("dve", lambda: nc.vector.tensor_tensor(BbI[:, osl, :].rearrange("p a b -> p (a b)"), t3[:], t4[:], ALU.add),
                     reads=[t3b, t4b], writes=[BbIb])
                S.dma("sp", t1[:], I["s5_Cre"][li, :, half * 2048:(half + 1) * 2048], reads=[], writes=[t1b])
                S.dma("sp", t2[:], I["s5_Cim"][li, :, half * 2048:(half + 1) * 2048], reads=[], writes=[t2b])
                S.op("act", lambda: nc.scalar.activation(out=CR[:, osl, :].rearrange("p a b -> p (a b)"), in_=t1[:], func=AF.Copy), reads=[t1b], writes=[CRb])
                S.op("act", lambda: nc.scalar.activation(out=CIn[:, osl, :].rearrange("p a b -> p (a b)"), in_=t2[:], func=AF.Copy, scale=-1.0),
                     reads=[t2b], writes=[CInb])
                S.barrier()
        ubf, ubfb = S.sbuf("ubf", [128, 4, T], BF16, es=ph)
        S.dma("sp", ubf[:], Sx["s5uT"].rearrange("(c p) t -> p c t", p=128), writes=[ubfb])
        tau, taub = S.sbuf("tau", [128, 2, T], F32, es=ph)
        S.dma("sp", tau[:].rearrange("p a b -> p (a b)"), I["s5_tau"][0:1, :].to_broadcast([128, 2 * T]), writes=[taub])
        gT, gTb = S.sbuf("gT", [128, 4, T], BF16, es=ph)
        cosT, cosb = S.sbuf("cosT", [128, T], F32, es=ph)
        sinT, sinb = S.sbuf("sinT", [128, T], F32, es=ph)
        ki, kib = S.sbuf("ki", [128, T], I32, es=ph)
        bre, breb = S.sbuf("bre", [128, T], F32, es=ph)
        bim, bimb = S.sbuf("bim", [128, T], F32, es=ph)
        sre, sreb = S.sbuf("sre", [128, T], F32, es=ph)
        sim, simb = S.sbuf("sim", [128, T], F32, es=ph)
        srb, srbb = S.sbuf("srb", [128, T], BF16, es=ph)
        sib, sibb = S.sbuf("sib", [128, T], BF16, es=ph)
        dsk, dskb = S.sbuf("dsk", [128, 4], F32, es=ph)
        S.dma("sp", dsk[:], I["s5_d"][li], writes=[dskb])
        yps = [S.psum("yps%d" % i, [128, 512], F32, es=ph) for i in range(5)]
        bur = Ring(S, ph, "bu", [128, 512], F32, 3, psum=True)
        tr = Ring(S, ph, "s5t", [128, 512], F32, 4)
        for ct in range(4):
            for d in range(2):
                for ns in range(4):
                    col = (d * 4 + ct) * 4 + ns
                    fcol = fpp[:, col:col + 1]
                    S.op("dve", lambda: nc.vector.tensor_scalar(ki[:], tau[:, d, :], fcol, None, ALU.mult), reads=[taub, fppb], writes=[kib])
                    S.op("dve", lambda: nc.vector.scalar_tensor_tensor(sinT[:], tau[:, d, :], fcol, ki[:], ALU.mult, ALU.subtract),
                         reads=[taub, fppb, kib], writes=[sinb])
                    S.op("act", lambda: nc.scalar.activation(out=sinT[:], in_=sinT[:], func=AF.Sin, scale=TWO_PI), reads=[sinb], writes=[sinb])
                    S.op("dve", lambda: nc.vector.tensor_scalar(ki[:], tau[:, d, :], fcol, 0.25, ALU.mult, ALU.add), reads=[taub, fppb], writes=[kib])
                    S.op("dve", lambda: nc.vector.scalar_tensor_tensor(cosT[:], tau[:, d, :], fcol, ki[:], ALU.mult, ALU.subtract),
                         reads=[taub, fppb, kib], writes=[cosb])
                    S.op("act", lambda: nc.scalar.activation(out=cosT[:], in_=cosT[:], func=AF.Sin, scale=TWO_PI, bias=C.hpi_t[:, 0:1]),
                         reads=[cosb, C.hpi_b], writes=[cosb])
                    for (t0, n, lc) in TB:
                        pr_, prb_ = bur.next()
                        pi_, pib_ = bur.next()
                        S.op("pe", lambda: nc.tensor.matmul(pr_[:, :n], lhsT=BbR[:, col, :], rhs=ubf[:, ct, t0:t0 + n], start=True, stop=True),
                             reads=[BbRb, ubfb], writes=[prb_])
                        S.op("pe", lambda: nc.tensor.matmul(pi_[:, :n], lhsT=BbI[:, col, :], rhs=ubf[:, ct, t0:t0 + n], start=True, stop=True),
                             reads=[BbIb, ubfb], writes=[pib_])
                        a1, a1b = tr.next()
                        a2, a2b = tr.next()
                        S.op("dve", lambda: nc.vector.tensor_tensor(a1[:, :n], pr_[:, :n], cosT[:, t0:t0 + n], ALU.mult), reads=[prb_, cosb], writes=[a1b])
                        S.op("dve", lambda: nc.vector.tensor_tensor(a2[:, :n], pi_[:, :n], sinT[:, t0:t0 + n], ALU.mult), reads=[pib_, sinb], writes=[a2b])
                        S.op("pool", lambda: nc.gpsimd.tensor_tensor(bre[:, t0:t0 + n], a1[:, :n], a2[:, :n], ALU.add), reads=[a1b, a2b], writes=[breb])
                        a3, a3b = tr.next()
                        a4, a4b = tr.next()
                        S.op("dve", lambda: nc.vector.tensor_tensor(a3[:, :n], pi_[:, :n], cosT[:, t0:t0 + n], ALU.mult), reads=[pib_, cosb], writes=[a3b])
                        S.op("dve", lambda: nc.vector.tensor_tensor(a4[:, :n], pr_[:, :n], sinT[:, t0:t0 + n], ALU.mult), reads=[prb_, sinb], writes=[a4b])
                        S.op("pool", lambda: nc.gpsimd.tensor_tensor(bim[:, t0:t0 + n], a3[:, :n], a4[:, :n], ALU.subtract), reads=[a3b, a4b], writes=[bimb])
                    rdec = rpp[:, col:col + 1]

                    sq_ = []
                    for (src, dst) in ((bre, sre), (bim, sim)):
                        if d == 0:
                            sq_.append(lambda src=src, dst=dst: nc.vector.tensor_tensor_scan(dst[:, NL:T], rdec.to_broadcast([128, NX]), src[:, NL:T], 0.0, ALU.mult, ALU.add))
                        else:
                            sq_.append(lambda src=src, dst=dst: nc.vector.tensor_tensor_scan(dst[:, NL:T][:, ::-1], rdec.to_broadcast([128, NX]), src[:, NL:T][:, ::-1], 0.0, ALU.mult, ALU.add))
                    for (src, dst) in ((bre, sre), (bim, sim)):
                        if d == 0:
                            sq_.append(lambda src=src, dst=dst: nc.vector.tensor_tensor_scan(dst[:, 0:NL], rdec.to_broadcast([128, NL]), src[:, 0:NL], dst[:, T - 1:T], ALU.mult, ALU.add))
                        else:
                            sq_.append(lambda src=src, dst=dst: nc.vector.tensor_tensor_scan(dst[:, 0:NL][:, ::-1], rdec.to_broadcast([128, NL]), src[:, 0:NL][:, ::-1],
                                                                                             dst[:, NL:NL + 1], ALU.mult, ALU.add))
                    S.seq("dve", sq_, reads=[breb, bimb, rppb], writes=[sreb, simb])
                    S.op("dve", lambda: nc.vector.tensor_tensor(bre[:], sre[:], cosT[:], ALU.mult), reads=[sreb, cosb], writes=[breb])
                    S.op("pool", lambda: nc.gpsimd.tensor_tensor(bim[:], sim[:], sinT[:], ALU.mult), reads=[simb, sinb], writes=[bimb])
                    S.op("pool", lambda: nc.gpsimd.tensor_tensor(srb[:], bre[:], bim[:], ALU.subtract), reads=[breb, bimb], writes=[srbb])
                    S.op("dve", lambda: nc.vector.tensor_tensor(bre[:], sre[:], sinT[:], ALU.mult), reads=[sreb, sinb], writes=[breb])
                    S.op("dve", lambda: nc.vector.tensor_tensor(bim[:], sim[:], cosT[:], ALU.mult), reads=[simb, cosb], writes=[bimb])
                    S.op("pool", lambda: nc.gpsimd.tensor_tensor(sib[:], bre[:], bim[:], ALU.add), reads=[breb, bimb], writes=[sibb])
                    first = (d == 0 and ns == 0)
                    last = (d == 1 and ns == 3)
                    for bi, (t0, n, lc) in enumerate(TB):
                        yp, ypb = yps[bi]

                        def rd():
                            nc.tensor.matmul(yp[:, :n], lhsT=CR[:, col, :], rhs=srb[:, t0:t0 + n], start=first, stop=False)
                            return nc.tensor.matmul(yp[:, :n], lhsT=CIn[:, col, :], rhs=sib[:, t0:t0 + n], start=False, stop=last)
                        S.op("pe", rd, reads=[CRb, CInb, srbb, sibb], writes=[ypb])
            S.dma("sp", bre[:], Sx["s5u32"][ct * 128:(ct + 1) * 128, :], writes=[breb])
            for bi, (t0, n, lc) in enumerate(TB):
                yp, ypb = yps[bi]
                a1, a1b = tr.next()
                S.op("dve", lambda: nc.vector.scalar_tensor_tensor(a1[:, :n], bre[:, t0:t0 + n], dsk[:, ct:ct + 1], yp[:, :n], ALU.mult, ALU.add),
                     reads=[breb, dskb, ypb], writes=[a1b])
                S.op("act", lambda: nc.scalar.activation(out=gT[:, ct, t0:t0 + n], in_=a1[:, :n], func=AF.Gelu), reads=[a1b], writes=[gTb])
        if "dbg_g" in C.dump:
            S.dma("sp", Sx["dbg_g"].rearrange("(c p) t -> p c t", p=128), gT[:], reads=[gTb])
        wgl, wglb = S.sbuf("wglu", [128, 4, 512], BF16, es=ph)
        S.dma("pool", wgl[:], I["w_glu"][li].rearrange("(c p) f -> p c f", p=128), writes=[wglb])
        bgl, bglb = S.sbuf("bglu", [128, 4], F32, es=ph)
        S.dma("sp", bgl[:], I["b_glu"][li], writes=[bglb])
        obr = Ring(S, ph, "s5o", [128, 512], BF16, 3)
        for fch in range(4):
            for (t0, n, lc) in TB:
                ps, psb = bur.next()

                def mm():
                    ins = None
                    for c in range(4):
                        ins = nc.tensor.matmul(ps[:, :n], lhsT=wgl[:, c, fch * 128:(fch + 1) * 128], rhs=gT[:, c, t0:t0 + n], start=(c == 0), stop=(c == 3))
                    return ins
                S.op("pe", mm, reads=[wglb, gTb], writes=[psb])
                a1, a1b = tr.next()
                S.op("act", lambda: nc.scalar.activation(out=a1[:, :n], in_=ps[:, :n], func=AF.Sigmoid, bias=bgl[:, fch:fch + 1]), reads=[psb, bglb], writes=[a1b])
                ob, obb = obr.next()
                S.op("dve", lambda: nc.vector.tensor_tensor(ob[:, :n], a1[:, :n], gT[:, fch, t0:t0 + n], ALU.mult), reads=[a1b, gTb], writes=[obb])
                S.dma("sp", Sx["catT"][1024 + fch * 128:1024 + (fch + 1) * 128, t0:t0 + n], ob[:, :n], reads=[obb])


def phase_out(C, li, x_in, x_out):
    S, nc, I, Sx = C.S, C.nc, C.I, C.Sx
    m = C.mod[li]
    with ExitStack() as ph:
        cat, catb = S.sbuf("cat", [128, KD, T], BF16, es=ph)
        for k4 in range(4):
            S.dma("sp", cat[:, k4 * 4:(k4 + 1) * 4, :], Sx["catT"][k4 * 512:(k4 + 1) * 512, :].rearrange("(k p) t -> p k t", p=128),
                  writes=[catb] if k4 == 0 else [], awrites=[] if k4 == 0 else [catb])
        wr = Ring(S, ph, "wo", [128, KD, 512], BF16, 2)
        pr = Ring(S, ph, "po", [128, 512], F32, 4, psum=True)
        xr = Ring(S, ph, "xo", [128, 512], F32, 3)
        orr = Ring(S, ph, "oo", [128, 512], F32, 3)
        nxt = wr.next()
        S.dma("pool", nxt[0][:], I["w_out"][li][:, 0:512].rearrange("(k p) n -> p k n", p=128), writes=[nxt[1]])
        for nb in range(4):
            w, wb = nxt
            if nb < 3:
                nxt = wr.next()
                S.dma("pool", nxt[0][:], I["w_out"][li][:, (nb + 1) * 512:(nb + 2) * 512].rearrange("(k p) n -> p k n", p=128), writes=[nxt[1]])
            for dl in range(4):
                dch = nb * 4 + dl
                for (t0, n, lc) in TB:
                    xt, xtb = xr.next()
                    S.dma("sp", xt[:, :n], x_in[dch * 128:(dch + 1) * 128, t0:t0 + n], writes=[xtb])
                    ps, psb = pr.next()

                    def mm():
                        ins = None
                        for k in range(KD):
                            ins = nc.tensor.matmul(ps[:, :n], lhsT=w[:, k, dl * 128:(dl + 1) * 128], rhs=cat[:, k, t0:t0 + n], start=(k == 0), stop=(k == KD - 1))
                        return ins
                    S.op("pe", mm, reads=[wb, catb], writes=[psb])
                    o, ob = orr.next()
                    S.op("dve", lambda: nc.vector.scalar_tensor_tensor(o[:, :n], ps[:, :n], m.t[:, 32 + dch, lc:lc + 1], xt[:, :n], ALU.mult, ALU.add),
                         reads=[psb, m.b, xtb], writes=[ob])
                    S.dma("sp", x_out[dch * 128:(dch + 1) * 128, t0:t0 + n], o[:, :n], reads=[ob])


def phase_moe(C, li, x_in, x_out):
    S, nc, I, Sx = C.S, C.nc, C.I, C.Sx
    m = C.mod[li]
    with ExitStack() as ph:
        posmT, posmTb = S.sbuf("posmT", [16, T], F32, es=ph)
        with ExitStack() as ph8:
            h2tok, h2tokb = S.sbuf("h2tok", [128, 18, D], BF16, es=ph8)
            aff3, aff3b = S.sbuf("aff3", [128, 18, 16, 3], BF16, es=ph8)
            posm_tok, posm_tokb = S.sbuf("posm_tok", [128, 18, 16], F32, es=ph8)
            with ExitStack() as p7:
                wrt, wrtb = S.sbuf("wrt", [128, KD, 16], F32, es=p7)
                S.dma("sp", wrt[:], I["w_router"][li].rearrange("(k p) e -> p k e", p=128), writes=[wrtb])
                aff, affb = S.sbuf("aff", [128, 18, 16], F32, es=p7)
                lg_, lgb = S.psum("lg", [128, 512], F32, es=p7)
                lg = lg_[:, 0:288].rearrange("p (a b) -> p a b", b=16)
                with ExitStack() as p7a:
                    hb, hbb = S.sbuf("hb", [128, KD, 512], BF16, es=p7a)
                    ptr = Ring(S, p7a, "pT", [128, 1024], BF16, 2, psum=True)

                    def cb(bi, t0, n, lc, hf, hfb):
                        S.op("pool", lambda: nc.gpsimd.tensor_copy(hb[:, :, :n], hf[:, :, :n]), reads=[hfb], writes=[hbb])
                        for a in range(n // 128):
                            tt = t0 // 128 + a

                            def mm():
                                ins = None
                                for k in range(KD):
                                    ins = nc.tensor.matmul(lg[:, tt, :], lhsT=hf[:, k, a * 128:(a + 1) * 128], rhs=wrt[:, k, :], start=(k == 0), stop=(k == KD - 1))
                                return ins
                            S.op("pe", mm, reads=[hfb, wrtb], writes=[lgb])
                            for kq in range(4):
                                pT, pTb = ptr.next()

                                def tp():
                                    ins = None
                                    for j in range(4):
                                        ins = nc.tensor.transpose(pT[:, j * 128:(j + 1) * 128], hb[:, kq * 4 + j, a * 128:(a + 1) * 128], C.ident_bf[:, :])
                                    return ins
                                S.op("pe", tp, reads=[hbb, C.ident_bf_b], writes=[pTb])
                                if kq % 2:
                                    S.op("act", lambda: nc.scalar.activation(out=h2tok[:, tt, kq * 512:(kq + 1) * 512], in_=pT[:, 0:512], func=AF.Copy),
                                         reads=[pTb], writes=[h2tokb])
                                else:
                                    S.op("dve", lambda: nc.vector.tensor_copy(h2tok[:, tt, kq * 512:(kq + 1) * 512], pT[:, 0:512]), reads=[pTb], writes=[h2tokb])
                    norm_blocks(C, p7a, x_in, m.A2, m.A2b, modsl(m, 3), m.b, cb)
                    S.barrier()
                mx, mxb = S.sbuf("mx", [128, 18], F32, es=p7)
                S.op("dve", lambda: nc.vector.tensor_reduce(mx[:], lg, AX.X, ALU.max), reads=[lgb], writes=[mxb])
                S.op("dve", lambda: nc.vector.tensor_tensor(aff[:], lg, mx[:].unsqueeze(2).to_broadcast([128, 18, 16]), ALU.subtract),
                     reads=[lgb, mxb], writes=[affb])
                S.op("act", lambda: nc.scalar.activation(out=aff[:], in_=aff[:], func=AF.Exp), reads=[affb], writes=[affb])
                S.op("dve", lambda: nc.vector.tensor_reduce(mx[:], aff[:], AX.X, ALU.add), reads=[affb], writes=[mxb])
                S.op("dve", lambda: nc.vector.reciprocal(mx[:], mx[:]), reads=[mxb], writes=[mxb])
                S.op("dve", lambda: nc.vector.tensor_tensor(aff[:], aff[:], mx[:].unsqueeze(2).to_broadcast([128, 18, 16]), ALU.mult),
                     reads=[affb, mxb], writes=[affb])
                if "dbg_aff" in C.dump:
                    S.dma("sp", Sx["dbg_aff"], aff[:], reads=[affb])
                r1, r1b = S.sbuf("r1", [128, 18, 16], F32, es=p7)
                S.op("dve", lambda: nc.vector.tensor_copy(aff3[:, :, :, 0], aff[:]), reads=[affb], writes=[aff3b])
                S.op("dve", lambda: nc.vector.tensor_tensor(r1[:], aff[:], aff3[:, :, :, 0], ALU.subtract), reads=[affb, aff3b], writes=[r1b])
                S.op("dve", lambda: nc.vector.tensor_copy(aff3[:, :, :, 1], r1[:]), reads=[r1b], writes=[aff3b])
                S.op("dve", lambda: nc.vector.tensor_tensor(r1[:], r1[:], aff3[:, :, :, 1], ALU.subtract), reads=[r1b, aff3b], writes=[r1b])
                S.op("dve", lambda: nc.vector.tensor_copy(aff3[:, :, :, 2], r1[:]), reads=[r1b], writes=[aff3b])
                affT, affTb = S.sbuf("affT", [16, T], F32, es=p7)
                work, workb = S.sbuf("work", [16, T], F32, es=p7)
                pa_, pab = S.psum("pa", [128, 512], F32, es=p7)
                pa = pa_[0:16, :]
                for (t0, n, lc) in TB:
                    def tpa():
                        ins = None
                        for a in range(n // 128):
                            ins = nc.tensor.transpose(pa[:, a * 128:(a + 1) * 128], aff[:, t0 // 128 + a, :], C.ident_f[:, :])
                        return ins
                    S.op("pe", tpa, reads=[affb, C.ident_f_b], writes=[pab])
                    S.op("dve", lambda: nc.vector.tensor_copy(affT[:, t0:t0 + n], pa[:, :n]), reads=[pab], writes=[affTb])
                S.op("dve", lambda: nc.vector.tensor_copy(work[:], affT[:]), reads=[affTb], writes=[workb])
                m8, m8b = S.sbuf("m8", [16, 16], F32, es=p7)

                tk = []
                for (lo, hi, rounds, oc) in ((0, NL, 32, 0), (NL, T, 4, 8)):
                    for r in range(rounds):
                        tk.append(lambda lo=lo, hi=hi, oc=oc: nc.vector.max(m8[:, oc:oc + 8], work[:, lo:hi]))
                        if r < rounds - 1:
                            tk.append(lambda lo=lo, hi=hi, oc=oc: nc.vector.match_replace(work[:, lo:hi], m8[:, oc:oc + 8], work[:, lo:hi], -1.0))
                S.seq("dve", tk, reads=[workb], writes=[workb, m8b])
                mk, mkb = S.sbuf("mk", [16, T], F32, es=p7)

                S.seq("dve", [
                    lambda: nc.vector.tensor_scalar(mk[:, 0:NL], affT[:, 0:NL], m8[:, 7:8], None, ALU.is_ge),
                    lambda: nc.vector.tensor_scalar(mk[:, NL:T], affT[:, NL:T], m8[:, 15:16], None, ALU.is_ge),
                    lambda: nc.vector.tensor_tensor_scan(work[:, 0:NL], C.one_f[:16, 0:1].to_broadcast([16, NL]), mk[:, 0:NL], 0.0, ALU.mult, ALU.add),
                    lambda: nc.vector.tensor_tensor_scan(work[:, NL:T], C.one_f[:16, 0:1].to_broadcast([16, NX]), mk[:, NL:T], 0.0, ALU.mult, ALU.add),
                    lambda: nc.vector.tensor_scalar(work[:, NL:T], work[:, NL:T], 256.0, None, ALU.add),
                    lambda: nc.vector.tensor_tensor(work[:], work[:], mk[:], ALU.mult),
                    lambda: nc.vector.tensor_scalar(posmT[:], work[:], -1.0, None, ALU.add),
                ], reads=[affTb, m8b, C.one_f_b, workb], writes=[mkb, workb, posmTb])
                if "dbg_posm" in C.dump:
                    S.dma("sp", Sx["dbg_posm"], posmT[:], reads=[posmTb])
                pp__, ppb_ = S.psum("ppm", [128, 512], F32, es=p7)
                pp_ = pp__[:, 0:288].rearrange("p (a b) -> p a b", b=16)

                def tpp():
                    ins = None
                    for tt in range(18):
                        ins = nc.tensor.transpose(pp_[:, tt, :], posmT[:, tt * 128:(tt + 1) * 128], C.ident_f[:16, :16])
                    return ins
                S.op("pe", tpp, reads=[posmTb, C.ident_f_b], writes=[ppb_])
                S.op("dve", lambda: nc.vector.tensor_copy(posm_tok[:], pp_), reads=[ppb_], writes=[posm_tokb])
                S.barrier()
            with ExitStack() as p8:
                ioj, iojb = S.sbuf("ioj", [128, NJ], F32, es=p8)
                S.dma("sp", ioj[:], I["iota_j"][0:1, :].to_broadcast([128, NJ]), writes=[iojb])
                Se, Seb = S.sbuf("Se", [128, 18, NJ], BF16, es=p8)
                xsr = Ring(S, p8, "xs", [128, KD, NJ], BF16, 2)
                hidr = Ring(S, p8, "hid", [128, 8, NJ], BF16, 2)
                ysb_, ysbb = S.sbuf("ysb", [128, 3, D], BF16, es=p8)
                wring = Ring(S, p8, "wu", [128, 4096], BF16, 6)
                pr = Ring(S, p8, "pe8", [128, 512], F32, 6, psum=True)
                tap_, tapb = S.psum("tap", [128, 512], F32, es=p8)
                tap = tap_[:, 0:9]
                ta, tab = S.sbuf("ta", [128, 3], F32, es=p8)
                sgr = Ring(S, p8, "sg", [128, NJ], F32, 3)

                def units(e):
                    u = []
                    for fq in range(4):
                        u.append(("g", fq, I["w_gate"][li, e][:, fq * 256:(fq + 1) * 256].rearrange("(k p) f -> p k f", p=128)))
                        u.append(("u", fq, I["w_up"][li, e][:, fq * 256:(fq + 1) * 256].rearrange("(k p) f -> p k f", p=128)))
                    for dq in range(4):
                        u.append(("d", dq, I["w_down"][li, e][:, dq * 512:(dq + 1) * 512].rearrange("(c p) d -> p c d", p=128)))
                    return u
                allu = [(e,) + u for e in range(16) for u in units(e)]
                loaded = {}

                def issue(i):
                    if i >= len(allu):
                        return
                    e, kind, idx, src = allu[i]
                    wt, wtb = wring.next()
                    if kind == "d":
                        S.dma("pool", wt[:].rearrange("p (c d) -> p c d", c=8), src, writes=[wtb])
                    else:
                        S.dma("pool", wt[:].rearrange("p (k f) -> p k f", k=KD), src, writes=[wtb])
                    loaded[i] = (wt, wtb)
                PRE = 4
                for i in range(PRE):
                    issue(i)
                ui = 0
                for e in range(16):
                    def mkS():
                        ins = None
                        for tt in range(18):
                            ins = nc.vector.tensor_scalar(Se[:, tt, :], ioj[:, :], posm_tok[:, tt, e:e + 1], None, ALU.is_equal)
                        return ins
                    S.op("dve", mkS, reads=[iojb, posm_tokb], writes=[Seb])

                    def mta():
                        ins = None
                        for jc, (tts, M) in enumerate(((range(16), 128), (range(16), 128), ((16, 17), 32))):
                            tts = list(tts)
                            for ii, tt in enumerate(tts):
                                ins = nc.tensor.matmul(tap[:M, jc * 3:(jc + 1) * 3], lhsT=Se[:, tt, jc * 128:jc * 128 + M], rhs=aff3[:, tt, e, :],
                                                       start=(ii == 0), stop=(ii == len(tts) - 1))
                        return ins
                    S.op("pe", mta, reads=[Seb, aff3b], writes=[tapb])
                    S.op("dve", lambda: nc.vector.tensor_reduce(ta[:], tap_[:, 0:9].rearrange("p (a b) -> p a b", b=3), AX.X, ALU.add), reads=[tapb], writes=[tab])
                    xs, xsb = xsr.next()
                    for k in range(KD):
                        ps, psb = pr.next()

                        def gm():
                            ins = None
                            for tt in range(16):
                                ins = nc.tensor.matmul(ps[:, 0:256], lhsT=h2tok[:, tt, k * 128:(k + 1) * 128], rhs=Se[:, tt, 0:256], start=(tt == 0), stop=(tt == 15))
                            for tt in (16, 17):
                                ins = nc.tensor.matmul(ps[:, 256:NJ], lhsT=h2tok[:, tt, k * 128:(k + 1) * 128], rhs=Se[:, tt, 256:NJ], start=(tt == 16), stop=(tt == 17))
                            return ins
                        S.op("pe", gm, reads=[h2tokb, Seb], writes=[psb])
                        if k % 2:
                            S.op("act", lambda: nc.scalar.activation(out=xs[:, k, :], in_=ps[:, :NJ], func=AF.Copy), reads=[psb], writes=[xsb])
                        else:
                            S.op("dve", lambda: nc.vector.tensor_copy(xs[:, k, :], ps[:, :NJ]), reads=[psb], writes=[xsb])
                    hid, hidb = hidr.next()
                    for fq in range(4):
                        wg, wgb = loaded.pop(ui)
                        wu, wub = loaded.pop(ui + 1)
                        ui += 2
                        wg3 = wg[:].rearrange("p (k f) -> p k f", k=KD)
                        wu3 = wu[:].rearrange("p (k f) -> p k f", k=KD)
                        for fcl in range(2):
                            fc = fq * 2 + fcl
                            pg, pgb = pr.next()
                            pu, pub = pr.next()

                            def mg():
                                ins = None
                                for k in range(KD):
                                    ins = nc.tensor.matmul(pg[:, :NJ], lhsT=wg3[:, k, fcl * 128:(fcl + 1) * 128], rhs=xs[:, k, :], start=(k == 0), stop=(k == KD - 1))
                                return ins

                            def mu():
                                ins = None
                                for k in range(KD):
                                    ins = nc.tensor.matmul(pu[:, :NJ], lhsT=wu3[:, k, fcl * 128:(fcl + 1) * 128], rhs=xs[:, k, :], start=(k == 0), stop=(k == KD - 1))
                                return ins
                            S.op("pe", mg, reads=[wgb, xsb], writes=[pgb])
                            S.op("pe", mu, reads=[wub, xsb], writes=[pub])
                            sg, sgb = sgr.next()
                            S.op("act", lambda: nc.scalar.activation(out=sg[:, :], in_=pg[:, :NJ], func=AF.Silu), reads=[pgb], writes=[sgb])
                            S.op("dve", lambda: nc.vector.tensor_tensor(hid[:, fc, :], sg[:, :], pu[:, :NJ], ALU.mult), reads=[sgb, pub], writes=[hidb])
                        issue(ui - 2 + PRE)
                        issue(ui - 1 + PRE)
                    for dq in range(4):
                        wd, wdb = loaded.pop(ui)
                        ui += 1
                        wd3 = wd[:].rearrange("p (c d) -> p c d", c=8)
                        for jc, M in enumerate((128, 128, 32)):
                            ps, psb = pr.next()

                            def md():
                                ins = None
                                for fc in range(8):
                                    ins = nc.tensor.matmul(ps[:M, :], lhsT=hid[:, fc, jc * 128:jc * 128 + M], rhs=wd3[:, fc, :], start=(fc == 0), stop=(fc == 7))
                                return ins
                            S.op("pe", md, reads=[hidb, wdb], writes=[psb])
                            if jc == 1:
                                S.op("act", lambda: nc.scalar.activation(out=ysb_[:M, jc, dq * 512:(dq + 1) * 512], in_=ps[:M, :], func=AF.Copy, scale=ta[:M, jc:jc + 1]),
                                     reads=[psb, tab], writes=[ysbb])
                            else:
                                S.op("dve", lambda: nc.vector.tensor_scalar(ysb_[:M, jc, dq * 512:(dq + 1) * 512], ps[:M, :], ta[:M, jc:jc + 1], None, ALU.mult),
                                     reads=[psb, tab], writes=[ysbb])
                        issue(ui - 1 + PRE)
                    S.dma("sp", Sx["ys"][e, 0:256, :].rearrange("(c j) d -> j c d", j=128), ysb_[:, 0:2, :], reads=[ysbb])
                    S.dma("sp", Sx["ys"][e, 256:NJ, :], ysb_[:32, 2, :], reads=[ysbb])
                S.barrier()
        with ExitStack() as p9:
            ysh, yshb = S.sbuf("ysh", [128, 16, 3, 1024], BF16, es=p9)
            ST, STb = S.sbuf("ST", [128, 16, 2, 512], BF16, es=p9)
            selt, seltb = S.sbuf("selt", [16, 16, 128], F32, es=p9)
            S.dma("sp", selt[:], I["sel"][:, :, :], writes=[seltb])
            ip3, ip3b = S.sbuf("ip3", [128, 3], F32, es=p9)
            S.dma("sp", ip3[:], I["iota_p3"][:, :], writes=[ip3b])
            bcr = Ring(S, p9, "bc", [128, 512], F32, 3, psum=True)
            pr = Ring(S, p9, "p9", [128, 512], F32, 4, psum=True)
            xr = Ring(S, p9, "x9", [128, 512], F32, 3)
            orr = Ring(S, p9, "o9", [128, 512], F32, 3)
            for dh in range(2):
                for jc in range(2):
                    S.dma("sp", ysh[:, :, jc, :], Sx["ys"][:, jc * 128:(jc + 1) * 128, dh * 1024:(dh + 1) * 1024].rearrange("e j d -> j e d"),
                          writes=[yshb] if jc == 0 else [], awrites=[] if jc == 0 else [yshb])
                S.dma("sp", ysh[:32, :, 2, :], Sx["ys"][:, 256:NJ, dh * 1024:(dh + 1) * 1024].rearrange("e j d -> j e d"), awrites=[yshb])
                for (t0, n, lc) in TB:
                    for e in range(16):
                        bc, bcb = bcr.next()
                        S.op("pe", lambda: nc.tensor.matmul(bc[:, :n], lhsT=selt[:, e, :], rhs=posmT[:, t0:t0 + n], start=True, stop=True),
                             reads=[seltb, posmTb], writes=[bcb])
                        if lc == 0:
                            S.op("dve", lambda: nc.vector.tensor_scalar(ST[:, e, 0, :n], bc[:, :n], ip3[:, 0:1], None, ALU.is_equal), reads=[bcb, ip3b], writes=[STb])
                            S.op("pool" if False else "dve", lambda: nc.vector.tensor_scalar(ST[:, e, 1, :n], bc[:, :n], ip3[:, 1:2], None, ALU.is_equal),
                                 reads=[bcb, ip3b], writes=[STb])
                        else:
                            S.op("dve", lambda: nc.vector.tensor_scalar(ST[:32, e, 0, :n], bc[:32, :n], ip3[:32, 2:3], None, ALU.is_equal), reads=[bcb, ip3b], writes=[STb])
                    for dl in range(8):
                        dch = dh * 8 + dl
                        xt, xtb = xr.next()
                        S.dma("sp", xt[:, :n], x_in[dch * 128:(dch + 1) * 128, t0:t0 + n], writes=[xtb])
                        ps, psb = pr.next()

                        def msc():
                            ins = None
                            if lc == 0:
                                for e in range(16):
                                    for jc in range(2):
                                        ins = nc.tensor.matmul(ps[:, :n], lhsT=ysh[:, e, jc, dl * 128:(dl + 1) * 128], rhs=ST[:, e, jc, :n],
                                                               start=(e == 0 and jc == 0), stop=(e == 15 and jc == 1))
                            else:
                                for e in range(16):
                                    ins = nc.tensor.matmul(ps[:, :n], lhsT=ysh[:32, e, 2, dl * 128:(dl + 1) * 128], rhs=ST[:32, e, 0, :n],
                                                           start=(e == 0), stop=(e == 15))
                            return ins
                        S.op("pe", msc, reads=[yshb, STb], writes=[psb])
                        o, ob = orr.next()
                        S.op("dve", lambda: nc.vector.scalar_tensor_tensor(o[:, :n], ps[:, :n], m.t[:, 80 + dch, lc:lc + 1], xt[:, :n], ALU.mult, ALU.add),
                             reads=[psb, m.b, xtb], writes=[ob])
                        S.dma("sp", x_out[dch * 128:(dch + 1) * 128, t0:t0 + n], o[:, :n], reads=[ob])


def phase_final(C, x_in):
    S, nc, I = C.S, C.nc, C.I
    with ExitStack() as ph:
        gf, gfb = S.sbuf("gf", [128, KD, 2], F32, es=ph)
        S.dma("sp", gf[:], I["gfT"][:, :, :], writes=[gfb])
        zs, zsb = S.sbuf("zs", [128, KD, 2], F32, es=ph)
        S.op("pool", lambda: nc.gpsimd.memset(zs[:], 0.0), writes=[zsb])

        def cb(bi, t0, n, lc, hf, hfb):
            S.dma("sp", C.outT[:, t0:t0 + n].rearrange("(k p) t -> p k t", p=128), hf[:, :, :n], reads=[hfb])
        norm_blocks(C, ph, x_in, gf, gfb, zs, zsb, cb, blocks=TB[:4])


def _prep_shared(inp):
    f = np.float32
    L = DEPTH
    sh = {}
    sh["w_ada"] = np.ascontiguousarray(inp["w_ada"], dtype=f)
    sh["b_adaT"] = np.ascontiguousarray(np.repeat(inp["b_ada"].reshape(L, 96, 128).transpose(0, 2, 1)[..., None], 2, axis=-1), dtype=f)
    for nm, src in (("g1T", "norm1_g"), ("g2T", "norm2_g")):
        sh[nm] = np.ascontiguousarray(np.repeat(inp[src].reshape(L, KD, 128).transpose(0, 2, 1)[..., None], 2, axis=-1), dtype=f)
    sh["gfT"] = np.ascontiguousarray(np.repeat(inp["final_norm_g"].reshape(KD, 128).T[..., None], 2, axis=-1), dtype=f)
    sh["w_in"] = np.ascontiguousarray(inp["w_in"], dtype=f)
    perm = np.array([(r // 32) * 32 + ((r % 32) + 16) % 32 for r in range(64)])
    sh["w_in_sw"] = np.ascontiguousarray(inp["w_in"][:, :, 1664:1728][:, :, perm], dtype=f)
    sh["w_out"] = np.ascontiguousarray(inp["w_out"], dtype=f)
    sh["sgu_g"] = np.ascontiguousarray(inp["sgu_norm_g"].reshape(L, 1, 512), dtype=f)
    sh["sgu_wT"] = np.ascontiguousarray(inp["sgu_w"].transpose(0, 3, 1, 2), dtype=f)
    sh["sgu_b4"] = np.ascontiguousarray(np.repeat(inp["sgu_b"][:, :, None, :], 4, axis=2).reshape(L, 1, 2048), dtype=f)
    sh["qg"] = np.ascontiguousarray(inp["mla_q_norm_g"].reshape(L, 3, 128).transpose(0, 2, 1), dtype=f)
    sh["kvg"] = np.ascontiguousarray(inp["mla_kv_norm_g"].reshape(L, 2, 128).transpose(0, 2, 1), dtype=f)
    sh["w_uq"] = np.ascontiguousarray(inp["mla_w_uq"], dtype=f)
    sh["w_uq_sw"] = np.ascontiguousarray(np.concatenate([inp["mla_w_uq"][:, :, h * 192 + 128 + perm] for h in range(4)], axis=-1), dtype=f)
    sh["w_ukv"] = np.ascontiguousarray(inp["mla_w_ukv"], dtype=f)
    t = np.arange(NL)
    row_id = (t // 64).astype(f)
    col_id = (t % 64).astype(f)
    inv_freq = (f(10000.0) ** (-np.arange(16, dtype=f) / f(16))).astype(f)
    cosT = np.ones((64, T), f)
    sinT = np.zeros((64, T), f)
    for r in range(64):
        pos = row_id if r < 32 else col_id
        ang = (pos * inv_freq[r % 16]).astype(f)
        cosT[r, :NL] = np.cos(ang)
        sinT[r, :NL] = np.sin(ang) * (-1.0 if (r % 32) < 16 else 1.0)
    sh["rope_cos"] = cosT
    sh["rope_sin"] = sinT

    def pp(a):
        return a.reshape(L, 2, 4, 8, 4, 16).transpose(0, 3, 5, 1, 2, 4).reshape(L, 128, 32)

    def rowl(a):
        return a.reshape(L, 2, 4, 8, 4, 16).transpose(0, 1, 2, 4, 3, 5).reshape(L, 4096)
    ldt_full = np.repeat(inp["s5_log_dt"][..., None], 64, axis=-1)
    sh["s5_pp"] = np.ascontiguousarray(np.stack([pp(inp["s5_a_re"]), pp(inp["s5_a_im"]), pp(ldt_full)], axis=2), dtype=f)
    sh["s5_row"] = np.ascontiguousarray(np.stack([rowl(inp["s5_a_re"]), rowl(inp["s5_a_im"]), rowl(ldt_full)], axis=1), dtype=f)

    def bblk(b):
        o = np.zeros((L, 8, 16, 2, 4, 4, 8, 16), f)
        bb = b.reshape(L, 2, 4, 8, 4, 16, 16)
        for g in range(8):
            o[:, g, :, :, :, :, g, :] = bb[:, :, :, g].transpose(0, 4, 1, 2, 3, 5)[:, :, :, :, :, :] if False else \
                np.transpose(bb[:, :, :, g], (0, 5, 1, 2, 3, 4))
        return o.reshape(L, 128, 4096)

    def cblk(c):
        o = np.zeros((L, 8, 16, 2, 4, 4, 8, 16), f)
        cc = c.reshape(L, 2, 4, 8, 16, 4, 16)
        for g in range(8):
            o[:, g, :, :, :, :, g, :] = np.transpose(cc[:, :, :, g], (0, 5, 1, 2, 4, 3))
        return o.reshape(L, 128, 4096)
    sh["s5_Bre"] = bblk(inp["s5_b_re"])
    sh["s5_Bim"] = bblk(inp["s5_b_im"])
    sh["s5_Cre"] = cblk(inp["s5_c_re"])
    sh["s5_Cim"] = cblk(inp["s5_c_im"])
    sh["s5_d"] = np.ascontiguousarray(inp["s5_d"].reshape(L, 4, 128).transpose(0, 2, 1), dtype=f)
    tau = np.zeros((2, T), f)
    tau[0, NL:] = np.arange(NX)
    tau[0, :NL] = NX + np.arange(NL)
    tau[1, NL:] = NX - 1 - np.arange(NX)
    tau[1, :NL] = NX + (NL - 1 - np.arange(NL))
    sh["s5_tau"] = tau.reshape(1, 2 * T)
    sh["w_glu"] = np.ascontiguousarray(inp["s5_w_glu"], dtype=f)
    sh["b_glu"] = np.ascontiguousarray(inp["s5_b_glu"].reshape(L, 4, 128).transpose(0, 2, 1), dtype=f)
    sh["conv_wT"] = np.ascontiguousarray(inp["conv_w"].reshape(L, 3, 4, 128).transpose(0, 3, 2, 1), dtype=f)
    sh["w_router"] = np.ascontiguousarray(inp["moe_w_router"], dtype=f)
    sh["w_gate"] = np.ascontiguousarray(inp["moe_w_gate"], dtype=f)
    sh["w_up"] = np.ascontiguousarray(inp["moe_w_up"], dtype=f)
    sh["w_down"] = np.ascontiguousarray(inp["moe_w_down"], dtype=f)
    sh["ident"] = np.eye(128, dtype=f)
    sh["iota_j"] = np.arange(NJ, dtype=f).reshape(1, NJ)
    sh["iota_p3"] = (np.arange(128, dtype=f)[:, None] + np.array([0, 128, 256], f)[None, :]).astype(f)
    sel = np.zeros((16, 16, 128), f)
    for e in range(16):
        sel[e, e, :] = 1.0
    sh["sel"] = sel
    return sh


def _prep_core(inp, b):
    f = np.float32
    d = {}
    d["xT0"] = np.ascontiguousarray(np.concatenate([inp["x"][b].T, inp["ctx"][b].T], axis=1), dtype=f)
    c2 = np.stack([inp["c"][b], inp["c_ctx"]], axis=0)
    d["cTp"] = np.ascontiguousarray(c2.reshape(2, KD, 128).transpose(2, 1, 0), dtype=f)
    return d


_NC_CACHE = {}


def used_inputs(nc_I, m):
    return {k: v for k, v in m.items() if k in nc_I}


def kernel(**inputs):
    inp = {k: np.asarray(v) for k, v in inputs.items()}
    B = inp["x"].shape[0]
    if "full" not in _NC_CACHE:
        _NC_CACHE["full"] = build_program()
    nc = _NC_CACHE["full"]
    sh = _prep_shared(inp)
    in_maps = []
    for b in range(B):
        m = dict(sh)
        m.update(_prep_core(inp, b))
        in_maps.append({k: v for k, v in m.items() if k in nc._used_inputs})
    res = run_bass_kernel_spmd(nc, in_maps, core_ids=list(range(B)))
    out = np.stack([np.asarray(res.results[b]["outT"]).T for b in range(B)], axis=0)
    return np.ascontiguousarray(out, dtype=np.float32)
```

```python
import math
import numpy as np
from contextlib import ExitStack
import concourse.bass as bass
import concourse.mybir as mybir
from concourse.bass_utils import run_bass_kernel_spmd

F32 = mybir.dt.float32
BF16 = mybir.dt.bfloat16
I32 = mybir.dt.int32
AF = mybir.ActivationFunctionType
ALU = mybir.AluOpType
AX = mybir.AxisListType

D = 2048
T = 2304
NL = 2048
NX = 256
KD = 16
DEPTH = 2
TB = [(0, 512, 0), (512, 512, 0), (1024, 512, 0), (1536, 512, 0), (2048, 256, 1)]
NJ = 288
EPS = 1e-6
N_DMA_SEMS = 24
TWO_PI = 6.283185


class Buf:
    __slots__ = ("name", "w", "r")

    def __init__(self, name=""):
        self.name = name
        self.w = {}
        self.r = {}


class Sched:
    def __init__(self, nc, es):
        self.nc = nc
        self.es = es
        self.eng = {"pe": nc.tensor, "dve": nc.vector, "act": nc.scalar, "pool": nc.gpsimd, "sp": nc.sync}
        self.sems = {}
        self.cnt = {}
        for k in ["pe", "dve", "act", "pool"]:
            self.sems[k] = es.enter_context(nc.semaphore("s_" + k))
            self.cnt[k] = 0
        for i in range(N_DMA_SEMS):
            k = "d%d" % i
            self.sems[k] = es.enter_context(nc.semaphore("s_" + k))
            self.cnt[k] = 0
        self.dma_rr = 0
        self.waited = {e: {} for e in self.eng}
        self.bufs = []
        self.uid = 0

    def sbuf(self, name, shape, dt, es=None):
        self.uid += 1
        t = (es or self.es).enter_context(self.nc.sbuf_tensor("%s_%d" % (name, self.uid), list(shape), dt))
        b = Buf(name)
        self.bufs.append(b)
        return t, b

    def psum(self, name, shape, dt, es=None):
        self.uid += 1
        t = (es or self.es).enter_context(self.nc.psum_tensor("%s_%d" % (name, self.uid), list(shape), dt))
        b = Buf(name)
        self.bufs.append(b)
        return t, b

    def _wait(self, e, dep):
        sk, val, deng = dep
        if deng == e and e == "pe":
            return
        if self.waited[e].get(sk, 0) >= val:
            return
        self.eng[e].wait_ge(self.sems[sk], val)
        self.waited[e][sk] = val

    def _deps(self, e, reads, writes):
        for b in reads:
            for d in b.w.values():
                self._wait(e, d)
        for b in writes:
            for d in b.w.values():
                self._wait(e, d)
            for d in b.r.values():
                self._wait(e, d)

    def _commit(self, tag, reads, writes):
        for b in reads:
            b.r[tag[0]] = tag
        for b in writes:
            b.w = {tag[0]: tag}
            b.r = {}

    def op(self, e, fn, reads=(), writes=()):
        self._deps(e, reads, writes)
        ins = fn()
        self.cnt[e] += 1
        ins.then_inc(self.sems[e], 1)
        self._commit((e, self.cnt[e], e), reads, writes)
        return ins

    def seq(self, e, fns, reads=(), writes=()):
        self._deps(e, reads, writes)
        ins = None
        for i, fn in enumerate(fns):
            if i > 0:
                self.eng[e].wait_ge(self.sems[e], self.cnt[e])
                self.waited[e][e] = self.cnt[e]
            ins = fn()
            self.cnt[e] += 1
            ins.then_inc(self.sems[e], 1)
        self._commit((e, self.cnt[e], e), reads, writes)
        return ins

    def dma(self, q, out, in_, reads=(), writes=(), awrites=()):
        self._deps(q, reads, writes)
        sk = "d%d" % self.dma_rr
        self.dma_rr = (self.dma_rr + 1) % N_DMA_SEMS
        if self.cnt[sk] > 0:
            self._wait(q, (sk, self.cnt[sk], "dma"))
        ins = self.eng[q].dma_start(out=out, in_=in_)
        self.cnt[sk] += 16
        ins.then_inc(self.sems[sk], 16)
        self._commit((sk, self.cnt[sk], "dma"), reads, writes)
        for b in awrites:
            b.w[sk] = (sk, self.cnt[sk], "dma")
        return ins

    def barrier(self):
        for e in self.eng:
            for sk, c in self.cnt.items():
                if c > 0:
                    self._wait(e, (sk, c, "x"))
        for b in self.bufs:
            b.w = {}
            b.r = {}


class Ring:
    def __init__(self, S, es, name, shape, dt, n, psum=False):
        mk = S.psum if psum else S.sbuf
        self.items = [mk("%s%d" % (name, i), shape, dt, es=es) for i in range(n)]
        self.i = 0

    def next(self):
        it = self.items[self.i % len(self.items)]
        self.i += 1
        return it


class Ctx:
    pass


def build_program(stop_after=None, dump=()):
    nc = bass.Bass("TRN2", target_bir_lowering=False)
    C = Ctx()
    C.nc = nc
    C.dump = set(dump)
    C.stop_after = stop_after

    def din(name, shape, dt=F32):
        return nc.dram_tensor(name, list(shape), dt, kind="ExternalInput").ap()

    def dscr(name, shape, dt):
        kind = "ExternalOutput" if name in C.dump else "Internal"
        return nc.dram_tensor(name, list(shape), dt, kind=kind).ap()

    SPEC = {
        "xT0": [D, T],
        "cTp": [128, KD, 2],
        "w_ada": [DEPTH, D, 6 * D],
        "b_adaT": [DEPTH, 128, 96, 2],
        "g1T": [DEPTH, 128, KD, 2],
        "g2T": [DEPTH, 128, KD, 2],
        "gfT": [128, KD, 2],
        "w_in": [DEPTH, D, 3776],
        "w_in_sw": [DEPTH, D, 64],
        "w_out": [DEPTH, D, D],
        "sgu_g": [DEPTH, 1, 512],
        "sgu_wT": [DEPTH, 128, 4, 128],
        "sgu_b4": [DEPTH, 1, 2048],
        "qg": [DEPTH, 128, 3],
        "kvg": [DEPTH, 128, 2],
        "w_uq": [DEPTH, 384, 768],
        "w_uq_sw": [DEPTH, 384, 256],
        "w_ukv": [DEPTH, 256, 1024],
        "rope_cos": [64, T],
        "rope_sin": [64, T],
        "s5_pp": [DEPTH, 128, 3, 32],
        "s5_row": [DEPTH, 3, 4096],
        "s5_Bre": [DEPTH, 128, 4096],
        "s5_Bim": [DEPTH, 128, 4096],
        "s5_Cre": [DEPTH, 128, 4096],
        "s5_Cim": [DEPTH, 128, 4096],
        "s5_d": [DEPTH, 128, 4],
        "s5_tau": [1, 2 * T],
        "w_glu": [DEPTH, 512, 512],
        "b_glu": [DEPTH, 128, 4],
        "conv_wT": [DEPTH, 128, 4, 3],
        "w_router": [DEPTH, D, 16],
        "w_gate": [DEPTH, 16, D, 1024],
        "w_up": [DEPTH, 16, D, 1024],
        "w_down": [DEPTH, 16, 1024, D],
        "ident": [128, 128],
        "iota_j": [1, NJ],
        "iota_p3": [128, 3],
        "sel": [16, 16, 128],
    }

    class LazyIn(dict):
        def __missing__(self, k):
            v = din(k, SPEC[k])
            self[k] = v
            return v
    I = LazyIn()
    C.I = I
    C.outT = nc.dram_tensor("outT", [D, NL], F32, kind="ExternalOutput").ap()

    Sx = {}
    Sx["xA"] = dscr("xA", [D, T], F32)
    Sx["xB"] = dscr("xB", [D, T], F32)
    Sx["uTg"] = dscr("uTg", [512, T], BF16)
    Sx["v_tok"] = dscr("v_tok", [T, 512], BF16)
    Sx["cqT"] = dscr("cqT", [384, T], BF16)
    Sx["ckvT"] = dscr("ckvT", [256, T], BF16)
    Sx["krT"] = dscr("krT", [64, T], BF16)
    Sx["s5uT"] = dscr("s5uT", [512, T], BF16)
    Sx["s5u32"] = dscr("s5u32", [512, T], F32)
    Sx["catT"] = dscr("catT", [D, T], BF16)
    Sx["ys"] = dscr("ys", [16, NJ, D], BF16)
    Sx["dbg_mod"] = dscr("dbg_mod", [DEPTH, 128, 96, 2], F32)
    Sx["dbg_hT"] = dscr("dbg_hT", [D, T], BF16)
    Sx["dbg_aff"] = dscr("dbg_aff", [128, 18, 16], F32)
    Sx["dbg_posm"] = dscr("dbg_posm", [16, T], F32)
    Sx["dbg_g"] = dscr("dbg_g", [512, T], BF16)
    C.Sx = Sx

    with ExitStack() as es:
        S = Sched(nc, es)
        C.S = S
        _consts(C)
        phase_mod(C)
        S.barrier()
        x_in = I["xT0"]
        done = (stop_after == "mod")
        for li in range(DEPTH):
            if done:
                break
            x1 = Sx["xA"]
            x2 = Sx["xB"]
            for ph in (phase_in, phase_sgu, phase_mla, phase_s5, phase_out, phase_moe):
                if ph is phase_in:
                    ph(C, li, x_in)
                elif ph is phase_out:
                    ph(C, li, x_in, x1)
                elif ph is phase_moe:
                    ph(C, li, x1, x2)
                else:
                    ph(C, li)
                S.barrier()
                if stop_after == (li, ph.__name__) or (isinstance(stop_after, tuple) and len(stop_after) == 3 and stop_after[0] == li and ph is phase_in):
                    done = True
                    break
            if done:
                break
            x_in = x2
        if not done:
            phase_final(C, x_in)
        S.barrier()
    nc._used_inputs = set(I.keys())
    return nc


def _consts(C):
    S, nc, I = C.S, C.nc, C.I
    C.ones_bf, C.ones_bf_b = S.sbuf("ones_bf", [128, 128], BF16)
    S.op("pool", lambda: nc.gpsimd.memset(C.ones_bf[:], 1.0), writes=[C.ones_bf_b])
    C.eps_t, C.eps_b = S.sbuf("eps", [128, 1], F32)
    S.op("pool", lambda: nc.gpsimd.memset(C.eps_t[:], EPS), writes=[C.eps_b])
    C.one_f, C.one_f_b = S.sbuf("one_f", [128, 1], F32)
    S.op("pool", lambda: nc.gpsimd.memset(C.one_f[:], 1.0), writes=[C.one_f_b])
    C.hpi_t, C.hpi_b = S.sbuf("hpi", [128, 1], F32)
    S.op("pool", lambda: nc.gpsimd.memset(C.hpi_t[:], math.pi / 2), writes=[C.hpi_b])
    C.ident_f, C.ident_f_b = S.sbuf("ident_f", [128, 128], F32)
    S.dma("sp", C.ident_f[:], I["ident"][:, :], writes=[C.ident_f_b])
    C.ident_bf, C.ident_bf_b = S.sbuf("ident_bf", [128, 128], BF16)
    S.dma("pool", C.ident_bf[:], I["ident"][:, :], writes=[C.ident_bf_b])
    C.mod = []
    C.modalloc = []
    for li in range(DEPTH):
        C.modalloc.append((S.sbuf("mod%d" % li, [128, 96, 2], F32), S.sbuf("A1_%d" % li, [128, KD, 2], F32), S.sbuf("A2_%d" % li, [128, KD, 2], F32)))


def _dbg(C, name, src_ap, reads):
    if name in C.dump:
        C.S.dma("sp", C.Sx[name], src_ap, reads=reads)


def phase_mod(C):
    S, nc, I = C.S, C.nc, C.I
    with ExitStack() as ph:
        sc, scb = S.sbuf("sc", [128, KD, 2], F32, es=ph)
        S.dma("sp", sc[:], I["cTp"][:, :, :], writes=[scb])
        S.op("act", lambda: nc.scalar.activation(out=sc[:], in_=sc[:], func=AF.Silu), reads=[scb], writes=[scb])
        war = Ring(S, ph, "wa", [128, KD, 512], F32, 2)
        mps_, mpsb = S.psum("mps", [128, 512], F32, es=ph)
        mps = mps_[:, 0:192]
        for li in range(DEPTH):
            nxt = war.next()
            S.dma("sp", nxt[0][:], I["w_ada"][li, :, 0:512].rearrange("(k p) n -> p k n", p=128), writes=[nxt[1]])
            for nb in range(24):
                wa, wab = nxt
                if nb + 1 < 24:
                    nxt = war.next()
                    S.dma("sp", nxt[0][:], I["w_ada"][li, :, (nb + 1) * 512:(nb + 2) * 512].rearrange("(k p) n -> p k n", p=128),
                          writes=[nxt[1]])

                def mm():
                    ins = None
                    for fl in range(4):
                        fc = nb * 4 + fl
                        for k in range(KD):
                            ins = nc.tensor.matmul(mps[:, fc * 2:fc * 2 + 2], lhsT=wa[:, k, fl * 128:(fl + 1) * 128],
                                                   rhs=sc[:, k, :], start=(k == 0), stop=(k == KD - 1))
                    return ins
                S.op("pe", mm, reads=[wab, scb], writes=[mpsb])
            m = Ctx()
            modt, modb = C.modalloc[li][0]
            bt, btb = S.sbuf("badat", [128, 96, 2], F32, es=ph)
            S.dma("sp", bt[:], I["b_adaT"][li], writes=[btb])
            S.op("dve", lambda: nc.vector.tensor_tensor(modt[:].rearrange("p a b -> p (a b)"), mps_[:, 0:192], bt[:].rearrange("p a b -> p (a b)"), ALU.add),
                 reads=[mpsb, btb], writes=[modb])
            m.t, m.b = modt, modb
            for nm, gname, j in (("A1", "g1T", 1), ("A2", "g2T", 4)):
                gt, gtb = S.sbuf("g" + nm, [128, KD, 2], F32, es=ph)
                S.dma("sp", gt[:], I[gname][li], writes=[gtb])
                at, atb = C.modalloc[li][1 if nm == "A1" else 2]
                S.op("dve", lambda: nc.vector.scalar_tensor_tensor(at[:], modt[:, j * 16:(j + 1) * 16, :], 1.0, gt[:], ALU.add, ALU.mult),
                     reads=[modb, gtb], writes=[atb])
                setattr(m, nm, at)
                setattr(m, nm + "b", atb)
            C.mod.append(m)
            if "dbg_mod" in C.dump:
                S.dma("sp", C.Sx["dbg_mod"][li], modt[:], reads=[modb])


def modsl(m, j):
    return m.t[:, j * 16:(j + 1) * 16, :]


def norm_blocks(C, ph, x_dram, A_ap, A_b, B_ap, B_b, cb, blocks=TB):
    S, nc = C.S, C.nc
    xr = Ring(S, ph, "xblk", [128, KD, 512], F32, 2)
    sqt, sqb = S.sbuf("nsq", [128, KD, 512], BF16, es=ph)
    ssr = Ring(S, ph, "nss", [128, 512], F32, 2, psum=True)
    rs, rsb = S.sbuf("nrstd", [128, 512], F32, es=ph)
    for bi, (t0, n, lc) in enumerate(blocks):
        xb, xbb = xr.next()
        S.dma("sp", xb[:, :, :n], x_dram[:, t0:t0 + n].rearrange("(k p) t -> p k t", p=128), writes=[xbb])
        S.op("act", lambda: nc.scalar.activation(out=sqt[:, :, :n], in_=xb[:, :, :n], func=AF.Square), reads=[xbb], writes=[sqb])
        ss, ssb = ssr.next()

        def mm():
            ins = None
            for k in range(KD):
                ins = nc.tensor.matmul(ss[:, :n], lhsT=C.ones_bf[:, :], rhs=sqt[:, k, :n], start=(k == 0), stop=(k == KD - 1))
            return ins
        S.op("pe", mm, reads=[sqb, C.ones_bf_b], writes=[ssb])
        S.op("act", lambda: nc.scalar.activation(out=rs[:, :n], in_=ss[:, :n], func=AF.Sqrt, bias=C.eps_t[:, 0:1], scale=1.0 / D),
             reads=[ssb, C.eps_b], writes=[rsb])
        S.op("dve", lambda: nc.vector.reciprocal(rs[:, :n], rs[:, :n]), reads=[rsb], writes=[rsb])

        def nrm():
            ins = None
            for k in range(KD):
                ins = nc.vector.tensor_tensor(xb[:, k, :n], xb[:, k, :n], rs[:, :n], ALU.mult)
            return ins
        S.op("dve", nrm, reads=[xbb, rsb], writes=[xbb])

        def aff_act():
            ins = None
            for k in range(0, KD, 2):
                ins = nc.scalar.activation(out=xb[:, k, :n], in_=xb[:, k, :n], func=AF.Identity,
                                           bias=B_ap[:, k, lc:lc + 1], scale=A_ap[:, k, lc:lc + 1])
            return ins

        def aff_pool():
            ins = None
            for k in range(1, KD, 2):
                ins = nc.gpsimd.tensor_scalar(xb[:, k, :n], xb[:, k, :n], A_ap[:, k, lc:lc + 1], B_ap[:, k, lc:lc + 1], ALU.mult, ALU.add)
            return ins
        S.op("act", aff_act, reads=[xbb, A_b, B_b], writes=[xbb])
        S.op("pool", aff_pool, reads=[xbb, A_b, B_b], writes=[xbb])
        cb(bi, t0, n, lc, xb, xbb)


def phase_in(C, li, x_dram):
    S, nc, I, Sx = C.S, C.nc, C.I, C.Sx
    m = C.mod[li]
    with ExitStack() as ph:
        hT, hTb = S.sbuf("hT", [128, KD, T], BF16, es=ph)
        with ExitStack() as ph1:
            def cb(bi, t0, n, lc, xb, xbb):
                S.op("dve", lambda: nc.vector.tensor_copy(hT[:, :, t0:t0 + n], xb[:, :, :n]), reads=[xbb], writes=[hTb])
            norm_blocks(C, ph1, x_dram, m.A1, m.A1b, modsl(m, 0), m.b, cb)
        if "dbg_hT" in C.dump:
            S.dma("sp", Sx["dbg_hT"].rearrange("(k p) t -> p k t", p=128), hT[:], reads=[hTb])
        if C.stop_after == (li, "in", "norm"):
            return
        S.barrier()
        wr = Ring(S, ph, "wt", [128, KD, 512], BF16, 2)
        pr = Ring(S, ph, "pin", [128, 512], F32, 4, psum=True)
        win = I["w_in"][li]

        def wload(segs):
            wt, wtb = wr.next()
            for si, (src, c0, ncol, off) in enumerate(segs):
                S.dma("pool", wt[:, :, off:off + ncol], src[:, c0:c0 + ncol].rearrange("(k p) n -> p k n", p=128),
                      writes=[wtb] if si == 0 else [], awrites=[] if si == 0 else [wtb])
            return wt, wtb

        def proj_fm(wt, wtb, woff, M, t0, n):
            ps, psb = pr.next()

            def mm():
                ins = None
                for k in range(KD):
                    ins = nc.tensor.matmul(ps[:M, :n], lhsT=wt[:, k, woff:woff + M], rhs=hT[:, k, t0:t0 + n], start=(k == 0), stop=(k == KD - 1))
                return ins
            S.op("pe", mm, reads=[wtb, hTb], writes=[psb])
            return ps, psb

        obr = Ring(S, ph, "ob", [128, 512], BF16, 4)
        ofr = Ring(S, ph, "of", [128, 512], F32, 3)

        wt, wtb = wload([(win, 0, 512, 0)])
        for (t0, n, lc) in TB:
            for c in range(4):
                ps, psb = proj_fm(wt, wtb, c * 128, 128, t0, n)
                ob, obb = obr.next()
                S.op("act", lambda: nc.scalar.activation(out=ob[:, :n], in_=ps[:, :n], func=AF.Gelu), reads=[psb], writes=[obb])
                S.dma("sp", Sx["uTg"][c * 128:(c + 1) * 128, t0:t0 + n], ob[:, :n], reads=[obb])

        if C.stop_after == (li, "in", "A"):
            return
        wt, wtb = wload([(win, 512, 512, 0)])
        gs, gsb = S.sbuf("gs", [128, 512], F32, es=ph)
        S.dma("sp", gs[:], I["sgu_g"][li].to_broadcast([128, 512]), writes=[gsb])
        junk, junkb = S.sbuf("junk", [128, 512], BF16, es=ph)
        st, stb = S.sbuf("vst", [128, 2], F32, es=ph)
        for tt in range(18):
            ps, psb = pr.next()

            def mm():
                ins = None
                for k in range(KD):
                    ins = nc.tensor.matmul(ps[:, :], lhsT=hT[:, k, tt * 128:(tt + 1) * 128], rhs=wt[:, k, :], start=(k == 0), stop=(k == KD - 1))
                return ins
            S.op("pe", mm, reads=[wtb, hTb], writes=[psb])
            gv, gvb = ofr.next()
            S.op("act", lambda: nc.scalar.activation(out=gv[:, :], in_=ps[:, :], func=AF.Gelu), reads=[psb], writes=[gvb])
            S.op("act", lambda: nc.scalar.activation(out=junk[:, :], in_=gv[:, :], func=AF.Square, accum_out=st[:, 0:1]),
                 reads=[gvb], writes=[junkb, stb])
            S.op("act", lambda: nc.scalar.activation(out=st[:, 1:2], in_=st[:, 0:1], func=AF.Sqrt, bias=C.eps_t[:, 0:1], scale=1.0 / 512),
                 reads=[stb, C.eps_b], writes=[stb])
            S.op("dve", lambda: nc.vector.reciprocal(st[:, 1:2], st[:, 1:2]), reads=[stb], writes=[stb])
            ob, obb = obr.next()
            S.op("dve", lambda: nc.vector.scalar_tensor_tensor(ob[:, :], gv[:, :], st[:, 1:2], gs[:, :], ALU.mult, ALU.mult),
                 reads=[gvb, stb, gsb], writes=[obb])
            S.dma("sp", Sx["v_tok"][tt * 128:(tt + 1) * 128, :], ob[:, :], reads=[obb])

        if C.stop_after == (li, "in", "v"):
            return
        wt, wtb = wload([(win, 1024, 512, 0)])
        for (t0, n, lc) in TB:
            for c in range(4):
                ps, psb = proj_fm(wt, wtb, c * 128, 128, t0, n)
                ob, obb = obr.next()
                S.op("dve" if c % 2 else "act", (lambda: nc.vector.tensor_copy(ob[:, :n], ps[:, :n])) if c % 2 else
                     (lambda: nc.scalar.activation(out=ob[:, :n], in_=ps[:, :n], func=AF.Copy)), reads=[psb], writes=[obb])
                dst = Sx["cqT"][c * 128:(c + 1) * 128, t0:t0 + n] if c < 3 else Sx["ckvT"][0:128, t0:t0 + n]
                S.dma("sp", dst, ob[:, :n], reads=[obb])

        if C.stop_after == (li, "in", "B"):
            return
        wt, wtb = wload([(win, 1536, 192, 0), (I["w_in_sw"][li], 0, 64, 192)])
        rc, rcb = S.sbuf("ropec", [64, T], F32, es=ph)
        rsn, rsnb = S.sbuf("ropes", [64, T], F32, es=ph)
        S.dma("sp", rc[:], I["rope_cos"][:, :], writes=[rcb])
        S.dma("sp", rsn[:], I["rope_sin"][:, :], writes=[rsnb])
        for (t0, n, lc) in TB:
            ps, psb = proj_fm(wt, wtb, 0, 128, t0, n)
            ob, obb = obr.next()
            S.op("act", lambda: nc.scalar.activation(out=ob[:, :n], in_=ps[:, :n], func=AF.Copy), reads=[psb], writes=[obb])
            S.dma("sp", Sx["ckvT"][128:256, t0:t0 + n], ob[:, :n], reads=[obb])
            ps1, ps1b = proj_fm(wt, wtb, 128, 64, t0, n)
            ps2, ps2b = proj_fm(wt, wtb, 192, 64, t0, n)
            f1, f1b = ofr.next()
            f2, f2b = ofr.next()
            S.op("dve", lambda: nc.vector.tensor_tensor(f1[:64, :n], ps1[:64, :n], rc[:, t0:t0 + n], ALU.mult), reads=[ps1b, rcb], writes=[f1b])
            S.op("dve", lambda: nc.vector.tensor_tensor(f2[:64, :n], ps2[:64, :n], rsn[:, t0:t0 + n], ALU.mult), reads=[ps2b, rsnb], writes=[f2b])
            ob, obb = obr.next()
            S.op("pool", lambda: nc.gpsimd.tensor_tensor(ob[:64, :n], f1[:64, :n], f2[:64, :n], ALU.add), reads=[f1b, f2b], writes=[obb])
            S.dma("sp", Sx["krT"][:, t0:t0 + n], ob[:64, :n], reads=[obb])

        if C.stop_after == (li, "in", "C"):
            return
        wt, wtb = wload([(win, 1728, 512, 0)])
        for (t0, n, lc) in TB:
            for c in range(4):
                ps, psb = proj_fm(wt, wtb, c * 128, 128, t0, n)
                of, ofb = ofr.next()
                ob, obb = obr.next()
                S.op("dve", lambda: nc.vector.tensor_copy(of[:, :n], ps[:, :n]), reads=[psb], writes=[ofb])
                S.op("act", lambda: nc.scalar.activation(out=ob[:, :n], in_=of[:, :n], func=AF.Copy), reads=[ofb], writes=[obb])
                S.dma("sp", Sx["s5uT"][c * 128:(c + 1) * 128, t0:t0 + n], ob[:, :n], reads=[obb])
                S.dma("sp", Sx["s5u32"][c * 128:(c + 1) * 128, t0:t0 + n], of[:, :n], reads=[ofb])

        if C.stop_after == (li, "in", "D"):
            return
        cw, cwb = S.sbuf("convw", [128, 4, 3], F32, es=ph)
        S.dma("sp", cw[:], I["conv_wT"][li], writes=[cwb])
        ZW = 2307
        zr = Ring(S, ph, "zbuf", [128, ZW], F32, 2)
        yr = Ring(S, ph, "ybuf", [128, ZW], F32, 2)
        bgr = Ring(S, ph, "bgbuf", [128, T], BF16, 2)
        cor = Ring(S, ph, "cobuf", [128, T], BF16, 2)

        def zoff(t0):
            return t0 + 1 if t0 < NL else t0 + 2
        for c in range(4):
            wt, wtb = wload([(win, 2240 + c * 128, 128, 0), (win, 2752 + c * 128, 128, 128), (win, 3264 + c * 128, 128, 256)])
            zb, zbb = zr.next()
            yb, ybb = yr.next()
            bg, bgb = bgr.next()
            co, cob = cor.next()

            def zz():
                nc.gpsimd.memset(zb[:, 0:1], 0.0)
                nc.gpsimd.memset(zb[:, NL + 1:NL + 2], 0.0)
                return nc.gpsimd.memset(zb[:, ZW - 1:ZW], 0.0)
            S.op("pool", zz, writes=[zbb])
            for (t0, n, lc) in TB:
                psB, psBb = proj_fm(wt, wtb, 0, 128, t0, n)
                psC, psCb = proj_fm(wt, wtb, 128, 128, t0, n)
                psH, psHb = proj_fm(wt, wtb, 256, 128, t0, n)
                S.op("act", lambda: nc.scalar.activation(out=bg[:, t0:t0 + n], in_=psB[:, :n], func=AF.Copy), reads=[psBb], writes=[bgb])
                of, ofb = ofr.next()
                S.op("act", lambda: nc.scalar.activation(out=of[:, :n], in_=psC[:, :n], func=AF.Copy), reads=[psCb], writes=[ofb])
                zo = zoff(t0)
                S.op("dve", lambda: nc.vector.tensor_tensor(zb[:, zo:zo + n], psH[:, :n], of[:, :n], ALU.mult), reads=[psHb, ofb], writes=[zbb])

            S.seq("dve", [
                lambda: nc.vector.tensor_scalar(yb[:, 1:ZW - 1], zb[:, 1:ZW - 1], cw[:, c, 1:2], None, ALU.mult),
                lambda: nc.vector.scalar_tensor_tensor(yb[:, 1:ZW - 1], zb[:, 0:ZW - 2], cw[:, c, 0:1], yb[:, 1:ZW - 1], ALU.mult, ALU.add),
                lambda: nc.vector.scalar_tensor_tensor(yb[:, 1:ZW - 1], zb[:, 2:ZW], cw[:, c, 2:3], yb[:, 1:ZW - 1], ALU.mult, ALU.add),
            ], reads=[zbb, cwb], writes=[ybb])

            def gate():
                nc.gpsimd.tensor_tensor(co[:, 0:NL], yb[:, 1:NL + 1], bg[:, 0:NL], ALU.mult)
                return nc.gpsimd.tensor_tensor(co[:, NL:T], yb[:, NL + 2:ZW - 1], bg[:, NL:T], ALU.mult)
            S.op("pool", gate, reads=[ybb, bgb], writes=[cob])
            S.dma("sp", Sx["catT"][1536 + c * 128:1536 + (c + 1) * 128, :], co[:, :], reads=[cob])


def phase_sgu(C, li):
    S, nc, I, Sx = C.S, C.nc, C.I, C.Sx
    with ExitStack() as ph:
        ws, wsb = S.sbuf("wsT", [128, 4, 128], BF16, es=ph)
        S.dma("pool", ws[:], I["sgu_wT"][li], writes=[wsb])
        bsr, bsrb = S.sbuf("bsr", [128, 4, 512], F32, es=ph)
        S.dma("sp", bsr[:].rearrange("p a b -> p (a b)"), I["sgu_b4"][li].to_broadcast([128, 2048]), writes=[bsrb])
        vr = Ring(S, ph, "vt", [128, 4, 512], BF16, 2)
        ur = Ring(S, ph, "ut", [128, 4, 512], BF16, 2)
        pr = Ring(S, ph, "psg", [128, 512], F32, 4, psum=True)
        tr = Ring(S, ph, "tmp", [128, 512], F32, 3)
        orr = Ring(S, ph, "osg", [128, 512], BF16, 3)
        for (t0, n, lc) in TB:
            na = n // 128
            vt, vtb = vr.next()
            ut, utb = ur.next()
            S.dma("sp", vt[:, :na, :], Sx["v_tok"][t0:t0 + n, :].rearrange("(a q) c -> q a c", q=128), writes=[vtb])
            S.dma("sp", ut[:, :, :n], Sx["uTg"][:, t0:t0 + n].rearrange("(h c) t -> c h t", c=128), writes=[utb])
            for h in range(4):
                ps, psb = pr.next()

                def mm():
                    ins = None
                    for a in range(na):
                        ins = nc.tensor.matmul(ps[:, a * 128:(a + 1) * 128], lhsT=vt[:, a, h * 128:(h + 1) * 128], rhs=ws[:, h, :], start=True, stop=True)
                    return ins
                S.op("pe", mm, reads=[vtb, wsb], writes=[psb])
                tm, tmb = tr.next()
                S.op("dve", lambda: nc.vector.tensor_tensor(tm[:, :n], ps[:, :n], bsr[:, h, :n], ALU.add), reads=[psb, bsrb], writes=[tmb])
                ob, obb = orr.next()
                S.op("pool", lambda: nc.gpsimd.tensor_tensor(ob[:, :n], tm[:, :n], ut[:, h, :n], ALU.mult), reads=[tmb, utb], writes=[obb])
                S.dma("sp", Sx["catT"][h * 128:(h + 1) * 128, t0:t0 + n], ob[:, :n], reads=[obb])


def phase_mla(C, li):
    S, nc, I, Sx = C.S, C.nc, C.I, C.Sx
    SC = 192.0 ** -0.5
    with ExitStack() as ph:
        cq, cqb = S.sbuf("cq", [128, 3, T], BF16, es=ph)
        ckv, ckvb = S.sbuf("ckv", [128, 2, T], BF16, es=ph)
        kr, krb = S.sbuf("kr", [64, T], BF16, es=ph)
        S.dma("sp", cq[:], Sx["cqT"].rearrange("(c p) t -> p c t", p=128), writes=[cqb])
        S.dma("sp", ckv[:], Sx["ckvT"].rearrange("(c p) t -> p c t", p=128), writes=[ckvb])
        S.dma("sp", kr[:], Sx["krT"][:, :], writes=[krb])
        wuq, wuqb = S.sbuf("wuq", [128, 3, 768], BF16, es=ph)
        wuqs, wuqsb = S.sbuf("wuqs", [128, 3, 256], BF16, es=ph)
        wukv, wukvb = S.sbuf("wukv", [128, 2, 1024], BF16, es=ph)
        wv, wvb = S.sbuf("wv", [128, 2, 512], BF16, es=ph)
        rq, rqb = S.sbuf("rq", [128, T], F32, es=ph)
        rk, rkb = S.sbuf("rk", [128, T], F32, es=ph)
        rkt, rktb = S.sbuf("rkt", [128, 18], F32, es=ph)
        pr = Ring(S, ph, "pj", [128, 512], F32, 3, psum=True)
        pst_, pstb = S.psum("pst", [128, 512], F32, es=ph)
        pst = pst_[:, 0:18]
        with ExitStack() as wp:
            w32, w32b = S.sbuf("w32", [128, 3, 768], F32, es=wp)
            ws32, ws32b = S.sbuf("ws32", [128, 3, 256], F32, es=wp)
            wk32, wk32b = S.sbuf("wk32", [128, 2, 1024], F32, es=wp)
            qg, qgb = S.sbuf("qg", [128, 3], F32, es=wp)
            kg, kgb = S.sbuf("kg", [128, 2], F32, es=wp)
            S.dma("sp", w32[:], I["w_uq"][li].rearrange("(c p) n -> p c n", p=128), writes=[w32b])
            S.dma("sp", ws32[:], I["w_uq_sw"][li].rearrange("(c p) n -> p c n", p=128), writes=[ws32b])
            S.dma("sp", wk32[:], I["w_ukv"][li].rearrange("(c p) n -> p c n", p=128), writes=[wk32b])
            S.dma("sp", qg[:], I["qg"][li], writes=[qgb])
            S.dma("sp", kg[:], I["kvg"][li], writes=[kgb])

            def sc1():
                ins = None
                for c in range(3):
                    nc.vector.tensor_scalar(wuq[:, c, :], w32[:, c, :], qg[:, c:c + 1], None, ALU.mult)
                    ins = nc.vector.tensor_scalar(wuqs[:, c, :], ws32[:, c, :], qg[:, c:c + 1], None, ALU.mult)
                return ins
            S.op("dve", sc1, reads=[w32b, ws32b, qgb], writes=[wuqb, wuqsb])

            def sc2():
                ins = None
                for c in range(2):
                    ins = nc.vector.tensor_scalar(wukv[:, c, :], wk32[:, c, :], kg[:, c:c + 1], None, ALU.mult)
                return ins
            S.op("dve", sc2, reads=[wk32b, kgb], writes=[wukvb])
            S.op("dve", lambda: nc.vector.tensor_copy(wv[:].rearrange("p c (h x) -> p c h x", x=128),
                                                       wukv[:].rearrange("p c (h x) -> p c h x", x=256)[:, :, :, 128:256]),
                 reads=[wukvb], writes=[wvb])
            sqq, sqqb = S.sbuf("sqq", [128, 3, T], BF16, es=wp)
            sqk, sqkb = S.sbuf("sqk", [128, 2, T], BF16, es=wp)
            S.op("act", lambda: nc.scalar.activation(out=sqq[:], in_=cq[:], func=AF.Square), reads=[cqb], writes=[sqqb])
            S.op("act", lambda: nc.scalar.activation(out=sqk[:], in_=ckv[:], func=AF.Square), reads=[ckvb], writes=[sqkb])
            for (sq, sqb_, nch, rt, rtb) in ((sqq, sqqb, 3, rq, rqb), (sqk, sqkb, 2, rk, rkb)):
                for (t0, n, lc) in TB:
                    ps, psb = pr.next()

                    def mm():
                        ins = None
                        for c in range(nch):
                            ins = nc.tensor.matmul(ps[:, :n], lhsT=C.ones_bf[:, :], rhs=sq[:, c, t0:t0 + n], start=(c == 0), stop=(c == nch - 1))
                        return ins
                    S.op("pe", mm, reads=[sqb_, C.ones_bf_b], writes=[psb])
                    S.op("act", lambda: nc.scalar.activation(out=rt[:, t0:t0 + n], in_=ps[:, :n], func=AF.Sqrt, bias=C.eps_t[:, 0:1],
                                                             scale=1.0 / (128 * nch)), reads=[psb, C.eps_b], writes=[rtb])
            S.op("dve", lambda: nc.vector.reciprocal(rq[:], rq[:]), reads=[rqb], writes=[rqb])
            S.op("dve", lambda: nc.vector.reciprocal(rk[:], rk[:]), reads=[rkb], writes=[rkb])

            def mmt():
                ins = None
                for tt in range(18):
                    for c in range(2):
                        ins = nc.tensor.matmul(pst[:, tt:tt + 1], lhsT=sqk[:, c, tt * 128:(tt + 1) * 128], rhs=C.ones_bf[:, 0:1],
                                               start=(c == 0), stop=(c == 1))
                return ins
            S.op("pe", mmt, reads=[sqkb, C.ones_bf_b], writes=[pstb])
            S.op("act", lambda: nc.scalar.activation(out=rkt[:], in_=pst_[:, 0:18], func=AF.Sqrt, bias=C.eps_t[:, 0:1], scale=1.0 / 256),
                 reads=[pstb, C.eps_b], writes=[rktb])
            S.op("dve", lambda: nc.vector.reciprocal(rkt[:], rkt[:]), reads=[rktb], writes=[rktb])
            S.barrier()
        rc, rcb = S.sbuf("ropec", [64, T], F32, es=ph)
        rsn, rsnb = S.sbuf("ropes", [64, T], F32, es=ph)
        S.dma("sp", rc[:], I["rope_cos"][:, :], writes=[rcb])
        S.dma("sp", rsn[:], I["rope_sin"][:, :], writes=[rsnb])
        qn, qnb = S.sbuf("qn", [128, 4, T], BF16, es=ph)
        qr, qrb = S.sbuf("qr", [64, 4, T], BF16, es=ph)
        kn, knb = S.sbuf("kn", [128, 4, T], BF16, es=ph)
        vtok, vtokb = S.sbuf("vtok", [128, 18, 512], BF16, es=ph)
        fr = Ring(S, ph, "mf", [128, 512], F32, 4)

        def proj(wt, wtb, nch, c0, M, src, srcb, t0, n):
            ps, psb = pr.next()

            def mm():
                ins = None
                for c in range(nch):
                    ins = nc.tensor.matmul(ps[:M, :n], lhsT=wt[:, c, c0:c0 + M], rhs=src[:, c, t0:t0 + n], start=(c == 0), stop=(c == nch - 1))
                return ins
            S.op("pe", mm, reads=[wtb, srcb], writes=[psb])
            return ps, psb
        for h in range(4):
            for (t0, n, lc) in TB:
                ps, psb = proj(wuq, wuqb, 3, h * 192, 128, cq, cqb, t0, n)
                S.op("dve", lambda: nc.vector.scalar_tensor_tensor(qn[:, h, t0:t0 + n], ps[:, :n], SC, rq[:, t0:t0 + n], ALU.mult, ALU.mult),
                     reads=[psb, rqb], writes=[qnb])
                ps1, ps1b = proj(wuq, wuqb, 3, h * 192 + 128, 64, cq, cqb, t0, n)
                ps2, ps2b = proj(wuqs, wuqsb, 3, h * 64, 64, cq, cqb, t0, n)
                f1, f1b = fr.next()
                f2, f2b = fr.next()
                S.op("dve", lambda: nc.vector.tensor_tensor(f1[:64, :n], ps1[:64, :n], rc[:, t0:t0 + n], ALU.mult), reads=[ps1b, rcb], writes=[f1b])
                S.op("dve", lambda: nc.vector.tensor_tensor(f2[:64, :n], ps2[:64, :n], rsn[:, t0:t0 + n], ALU.mult), reads=[ps2b, rsnb], writes=[f2b])
                S.op("pool", lambda: nc.gpsimd.tensor_tensor(f1[:64, :n], f1[:64, :n], f2[:64, :n], ALU.add), reads=[f1b, f2b], writes=[f1b])
                S.op("dve", lambda: nc.vector.scalar_tensor_tensor(qr[:, h, t0:t0 + n], f1[:64, :n], SC, rq[:64, t0:t0 + n], ALU.mult, ALU.mult),
                     reads=[f1b, rqb], writes=[qrb])
                ps, psb = proj(wukv, wukvb, 2, h * 256, 128, ckv, ckvb, t0, n)
                S.op("dve", lambda: nc.vector.tensor_tensor(kn[:, h, t0:t0 + n], ps[:, :n], rk[:, t0:t0 + n], ALU.mult), reads=[psb, rkb], writes=[knb])
        for tt in range(18):
            ps, psb = pr.next()

            def mm():
                ins = None
                for c in range(2):
                    ins = nc.tensor.matmul(ps[:, :], lhsT=ckv[:, c, tt * 128:(tt + 1) * 128], rhs=wv[:, c, :], start=(c == 0), stop=(c == 1))
                return ins
            S.op("pe", mm, reads=[ckvb, wvb], writes=[psb])
            S.op("dve", lambda: nc.vector.tensor_scalar(vtok[:, tt, :], ps[:, :], rkt[:, tt:tt + 1], None, ALU.mult), reads=[psb, rktb], writes=[vtokb])
        opr = Ring(S, ph, "ops", [128, 512], F32, 2, psum=True)
        spr = Ring(S, ph, "sps", [128, 512], F32, 2, psum=True)
        ptr = Ring(S, ph, "pt", [128, 512], BF16, 3)
        rsr = Ring(S, ph, "rsm", [128, 512], F32, 2)
        atr = Ring(S, ph, "att", [128, 512], BF16, 2)
        for h in range(4):
            for (t0, n, lc) in TB:
                kts = list(range(18)) if lc == 0 else [16, 17]
                ops, opsb = opr.next()
                sps, spsb = spr.next()
                for i, kt in enumerate(kts):
                    st, stb = pr.next()

                    def mms():
                        nc.tensor.matmul(st[:, :n], lhsT=kn[:, h, kt * 128:(kt + 1) * 128], rhs=qn[:, h, t0:t0 + n], start=True, stop=False)
                        return nc.tensor.matmul(st[:, :n], lhsT=kr[:, kt * 128:(kt + 1) * 128], rhs=qr[:, h, t0:t0 + n], start=False, stop=True)
                    S.op("pe", mms, reads=[knb, qnb, krb, qrb], writes=[stb])
                    pt, ptb = ptr.next()
                    S.op("act", lambda: nc.scalar.activation(out=pt[:, :n], in_=st[:, :n], func=AF.Exp), reads=[stb], writes=[ptb])

                    def mmo():
                        nc.tensor.matmul(ops[:, :n], lhsT=vtok[:, kt, h * 128:(h + 1) * 128], rhs=pt[:, :n], start=(i == 0), stop=(i == len(kts) - 1))
                        return nc.tensor.matmul(sps[:, :n], lhsT=C.ones_bf[:, :], rhs=pt[:, :n], start=(i == 0), stop=(i == len(kts) - 1))
                    S.op("pe", mmo, reads=[vtokb, ptb, C.ones_bf_b], writes=[opsb, spsb])
                rs_, rsb_ = rsr.next()
                S.op("dve", lambda: nc.vector.reciprocal(rs_[:, :n], sps[:, :n]), reads=[spsb], writes=[rsb_])
                at, atb = atr.next()
                S.op("dve", lambda: nc.vector.tensor_tensor(at[:, :n], ops[:, :n], rs_[:, :n], ALU.mult), reads=[opsb, rsb_], writes=[atb])
                S.dma("sp", Sx["catT"][512 + h * 128:512 + (h + 1) * 128, t0:t0 + n], at[:, :n], reads=[atb])


def _s5_disc(C, es, are, aim, ldt, n, need_coef, tagb):
    S, nc = C.S, C.nc
    X = [S.sbuf("s5x%d" % i, [128, n], F32, es=es) for i in range(6)]
    KI = S.sbuf("s5ki", [128, n], I32, es=es)
    (x1, b1), (x2, b2), (x3, b3), (x4, b4), (x5, b5), (x6, b6) = X
    ki, kib = KI
    S.op("dve", lambda: nc.vector.tensor_scalar(are, are, -1e-4, None, ALU.min), reads=[tagb], writes=[tagb])
    ea = [
        lambda: nc.vector.tensor_scalar(ki[:], ldt, 1.0 / math.log(2.0), None, ALU.mult),
        lambda: nc.vector.tensor_copy(x3[:], ki[:]),
        lambda: nc.vector.scalar_tensor_tensor(x4[:], x3[:], -0.693145751953125, ldt, ALU.mult, ALU.add),
        lambda: nc.vector.scalar_tensor_tensor(x4[:], x3[:], -1.42860682030941723212e-6, x4[:], ALU.mult, ALU.add),
        lambda: nc.vector.tensor_scalar(x5[:], x4[:], 1.0 / 9.0, 1.0, ALU.mult, ALU.add),
    ]
    for j in range(8, 0, -1):
        ea.append(lambda: nc.vector.tensor_tensor(x5[:], x5[:], x4[:], ALU.mult))
        ea.append(lambda j=j: nc.vector.tensor_scalar(x5[:], x5[:], 1.0 / j, 1.0, ALU.mult, ALU.add))
    ea.append(lambda: nc.vector.tensor_scalar(ki[:], x3[:], 127.0, 8388608.0, ALU.add, ALU.mult))
    ea.append(lambda: nc.vector.tensor_tensor(ldt, x5[:], ki[:].bitcast(F32), ALU.mult))
    S.seq("dve", ea, reads=[tagb], writes=[tagb, kib, b3, b4, b5])
    S.op("dve", lambda: nc.vector.tensor_tensor(x1[:], are, ldt, ALU.mult), reads=[tagb], writes=[b1])
    S.op("act", lambda: nc.scalar.activation(out=x1[:], in_=x1[:], func=AF.Exp), reads=[b1], writes=[b1])
    S.op("dve", lambda: nc.vector.tensor_tensor(x2[:], aim, ldt, ALU.mult), reads=[tagb], writes=[b2])
    S.op("dve", lambda: nc.vector.tensor_scalar(x2[:], x2[:], 1.0 / (2 * math.pi), None, ALU.mult), reads=[b2], writes=[b2])
    if not need_coef:
        return {"r": (x1, b1), "f": (x2, b2)}
    S.op("dve", lambda: nc.vector.tensor_copy(ki[:], x2[:]), reads=[b2], writes=[kib])
    S.op("dve", lambda: nc.vector.tensor_tensor(x3[:], x2[:], ki[:], ALU.subtract), reads=[b2, kib], writes=[b3])
    S.op("act", lambda: nc.scalar.activation(out=x4[:], in_=x3[:], func=AF.Sin, scale=TWO_PI), reads=[b3], writes=[b4])
    S.op("dve", lambda: nc.vector.tensor_scalar(ki[:], x2[:], 0.25, None, ALU.add), reads=[b2], writes=[kib])
    S.op("dve", lambda: nc.vector.tensor_tensor(x3[:], x2[:], ki[:], ALU.subtract), reads=[b2, kib], writes=[b3])
    S.op("act", lambda: nc.scalar.activation(out=x5[:], in_=x3[:], func=AF.Sin, scale=TWO_PI, bias=C.hpi_t[:, 0:1]), reads=[b3, C.hpi_b], writes=[b5])
    S.op("dve", lambda: nc.vector.tensor_tensor(x5[:], x1[:], x5[:], ALU.mult), reads=[b1, b5], writes=[b5])
    S.op("dve", lambda: nc.vector.tensor_scalar(x5[:], x5[:], -1.0, None, ALU.add), reads=[b5], writes=[b5])
    S.op("dve", lambda: nc.vector.tensor_tensor(x4[:], x1[:], x4[:], ALU.mult), reads=[b1, b4], writes=[b4])
    S.op("dve", lambda: nc.vector.tensor_tensor(x1[:], are, are, ALU.mult), reads=[tagb], writes=[b1])
    S.op("dve", lambda: nc.vector.tensor_tensor(x3[:], aim, aim, ALU.mult), reads=[tagb], writes=[b3])
    S.op("dve", lambda: nc.vector.tensor_tensor(x1[:], x1[:], x3[:], ALU.add), reads=[b1, b3], writes=[b1])
    S.op("dve", lambda: nc.vector.reciprocal(x1[:], x1[:]), reads=[b1], writes=[b1])
    S.op("dve", lambda: nc.vector.tensor_tensor(x2[:], x5[:], are, ALU.mult), reads=[b5, tagb], writes=[b2])
    S.op("dve", lambda: nc.vector.tensor_tensor(x3[:], x4[:], aim, ALU.mult), reads=[b4, tagb], writes=[b3])
    S.op("dve", lambda: nc.vector.tensor_tensor(x2[:], x2[:], x3[:], ALU.add), reads=[b2, b3], writes=[b2])
    S.op("dve", lambda: nc.vector.tensor_tensor(x2[:], x2[:], x1[:], ALU.mult), reads=[b2, b1], writes=[b2])
    S.op("dve", lambda: nc.vector.tensor_tensor(x6[:], x4[:], are, ALU.mult), reads=[b4, tagb], writes=[b6])
    S.op("dve", lambda: nc.vector.tensor_tensor(x3[:], x5[:], aim, ALU.mult), reads=[b5, tagb], writes=[b3])
    S.op("dve", lambda: nc.vector.tensor_tensor(x6[:], x6[:], x3[:], ALU.subtract), reads=[b6, b3], writes=[b6])
    S.op("dve", lambda: nc.vector.tensor_tensor(x6[:], x6[:], x1[:], ALU.mult), reads=[b6, b1], writes=[b6])
    return {"cre": (x2, b2), "cim": (x6, b6), "t": [(x1, b1), (x3, b3), (x4, b4), (x5, b5)]}


def phase_s5(C, li):
    S, nc, I, Sx = C.S, C.nc, C.I, C.Sx
    with ExitStack() as ph:
        BbR, BbRb = S.sbuf("BbR", [128, 32, 128], BF16, es=ph)
        BbI, BbIb = S.sbuf("BbI", [128, 32, 128], BF16, es=ph)
        CR, CRb = S.sbuf("CR", [128, 32, 128], BF16, es=ph)
        CIn, CInb = S.sbuf("CIn", [128, 32, 128], BF16, es=ph)
        pp, ppb = S.sbuf("s5pp", [128, 3, 32], F32, es=ph)
        S.dma("sp", pp[:], I["s5_pp"][li], writes=[ppb])
        dpp = _s5_disc(C, ph, pp[:, 0, :], pp[:, 1, :], pp[:, 2, :], 32, False, ppb)
        rpp, rppb = dpp["r"]
        fpp, fppb = dpp["f"]
        for half in range(2):
            with ExitStack() as wp:
                rows, rowsb = S.sbuf("s5rows", [128, 3, 2048], F32, es=wp)
                for j in range(3):
                    S.dma("sp", rows[:, j, :], I["s5_row"][li, j:j + 1, half * 2048:(half + 1) * 2048].to_broadcast([128, 2048]),
                          writes=[rowsb] if j == 0 else [], awrites=[] if j == 0 else [rowsb])
                dr = _s5_disc(C, wp, rows[:, 0, :], rows[:, 1, :], rows[:, 2, :], 2048, True, rowsb)
                cre, creb = dr["cre"]
                cim, cimb = dr["cim"]
                (t1, t1b), (t2, t2b), (t3, t3b), (t4, t4b) = dr["t"]
                S.dma("sp", t1[:], I["s5_Bre"][li, :, half * 2048:(half + 1) * 2048], writes=[t1b])
                S.dma("sp", t2[:], I["s5_Bim"][li, :, half * 2048:(half + 1) * 2048], writes=[t2b])
                osl = slice(half * 16, (half + 1) * 16)
                S.op("dve", lambda: nc.vector.tensor_tensor(t3[:], cre[:], t1[:], ALU.mult), reads=[creb, t1b], writes=[t3b])
                S.op("pool", lambda: nc.gpsimd.tensor_tensor(t4[:], cim[:], t2[:], ALU.mult), reads=[cimb, t2b], writes=[t4b])
                S.op("dve", lambda: nc.vector.tensor_tensor(BbR[:, osl, :].rearrange("p a b -> p (a b)"), t3[:], t4[:], ALU.subtract),
                     reads=[t3b, t4b], writes=[BbRb])
                S.op("dve", lambda: nc.vector.tensor_tensor(t3[:], cre[:], t2[:], ALU.mult), reads=[creb, t2b], writes=[t3b])
                S.op("pool", lambda: nc.gpsimd.tensor_tensor(t4[:], cim[:], t1[:], ALU.mult), reads=[cimb, t1b], writes=[t4b])
                S.op("dve", lambda: nc.vector.tensor_tensor(BbI[:, osl, :].rearrange("p a b -> p (a b)"), t3[:], t4[:], ALU.add),
                     reads=[t3b, t4b], writes=[BbIb])
                S.dma("sp", t1[:], I["s5_Cre"][li, :, half * 2048:(half + 1) * 2048], reads=[], writes=[t1b])
                S.dma("sp", t2[:], I["s5_Cim"][li, :, half * 2048:(half + 1) * 2048], reads=[], writes=[t2b])
                S.op("act", lambda: nc.scalar.activation(out=CR[:, osl, :].rearrange("p a b -> p (a b)"), in_=t1[:], func=AF.Copy), reads=[t1b], writes=[CRb])
                S.op("act", lambda: nc.scalar.activation(out=CIn[:, osl, :].rearrange("p a b -> p (a b)"), in_=t2[:], func=AF.Copy, scale=-1.0),
                     reads=[t2b], writes=[CInb])
                S.barrier()
        TA = 1536
        mst = ExitStack()
        ubr = Ring(S, mst, "ubf", [128, T], BF16, 2)
        tau, taub = S.sbuf("tau", [128, 2, T], F32, es=mst)
        S.dma("sp", tau[:].rearrange("p a b -> p (a b)"), I["s5_tau"][0:1, :].to_broadcast([128, 2 * T]), writes=[taub])

        def two(name, dt):
            t, ba = S.sbuf(name, [128, T], dt, es=mst)
            bb = Buf(name + "B")
            S.bufs.append(bb)
            return t, (ba, bb)
        tabr = [(two("cosT%d" % i, F32), two("sinT%d" % i, F32)) for i in range(2)]
        kis, kisb = S.sbuf("kis", [128, T], I32, es=mst)
        kic, kicb = S.sbuf("kic", [128, T], I32, es=mst)
        evr, evrb = two("evr", F32)
        evi, evib = two("evi", F32)
        bre, breb = two("bre", F32)
        bim, bimb = two("bim", F32)
        sre, sreb = two("sre", F32)
        sim, simb = two("sim", F32)
        srb, srbb = two("srb", BF16)
        sib, sibb = two("sib", BF16)
        dsk, dskb = S.sbuf("dsk", [128, 4], F32, es=mst)
        S.dma("sp", dsk[:], I["s5_d"][li], writes=[dskb])
        yps = [S.psum("yps%d" % i, [128, 512], F32, es=mst) for i in range(5)]
        bur = Ring(S, mst, "bu", [128, 512], F32, 3, psum=True)
        tr = Ring(S, mst, "s5t", [128, 512], F32, 4)
        gor = Ring(S, mst, "s5g", [128, 512], BF16, 3)
        iters = [(ct, d, ns) for ct in range(4) for d in range(2) for ns in range(4)]

        def ew(o, ob, x, xb, y, yb, op):
            S.op("dve", lambda: nc.vector.tensor_tensor(o[:], x[:], y[:], op), reads=list(xb) + list(yb), writes=list(ob))

        def colof(it):
            ct, d, ns = it
            return (d * 4 + ct) * 4 + ns
        tabs = {}

        def tab_pool(i):
            pass

        def tab_rest(i):
            ct, d, ns = iters[i]
            fcol = fpp[:, colof(iters[i]):colof(iters[i]) + 1]
            (cosT, cosb), (sinT, sinb) = tabr[i % 2]
            S.op("dve", lambda: nc.vector.tensor_scalar(kis[:], tau[:, d, :], fcol, None, ALU.mult), reads=[taub, fppb], writes=[kisb])
            S.op("dve", lambda: nc.vector.scalar_tensor_tensor(sinT[:], tau[:, d, :], fcol, kis[:], ALU.mult, ALU.subtract),
                 reads=[taub, fppb, kisb], writes=list(sinb))
            S.op("act", lambda: nc.scalar.activation(out=cosT[:], in_=sinT[:], func=AF.Abs), reads=list(sinb), writes=list(cosb))
            S.op("act", lambda: nc.scalar.activation(out=sinT[:], in_=sinT[:], func=AF.Sin, scale=TWO_PI), reads=list(sinb) + list(cosb), writes=list(sinb))
            S.op("act", lambda: nc.scalar.activation(out=cosT[:], in_=cosT[:], func=AF.Sin, scale=-TWO_PI, bias=C.hpi_t[:, 0:1]),
                 reads=list(cosb) + [C.hpi_b], writes=list(cosb))
            tabs[i] = (cosT, cosb, sinT, sinb)
        tab_pool(0)
        tab_rest(0)
        ub_next = ubr.next()
        S.dma("sp", ub_next[0][:], Sx["s5uT"][0:128, :], writes=[ub_next[1]])
        for idx, (ct, d, ns) in enumerate(iters):
            col = colof((ct, d, ns))
            if d == 0 and ns == 0:
                ubf, ubfb = ub_next
                if ct < 3:
                    ub_next = ubr.next()
                    S.dma("sp", ub_next[0][:], Sx["s5uT"][(ct + 1) * 128:(ct + 2) * 128, :], writes=[ub_next[1]])
            cosT, cosb, sinT, sinb = tabs.pop(idx)
            if idx + 1 < len(iters):
                tab_pool(idx + 1)
            for (t0, n, lc) in TB:
                part = 0 if t0 < TA else 1
                pr_, prb_ = bur.next()
                pi_, pib_ = bur.next()
                S.op("pe", lambda: nc.tensor.matmul(pr_[:, :n], lhsT=BbR[:, col, :], rhs=ubf[:, t0:t0 + n], start=True, stop=True),
                     reads=[BbRb, ubfb], writes=[prb_])
                S.op("pe", lambda: nc.tensor.matmul(pi_[:, :n], lhsT=BbI[:, col, :], rhs=ubf[:, t0:t0 + n], start=True, stop=True),
                     reads=[BbIb, ubfb], writes=[pib_])
                S.op("act", lambda: nc.scalar.activation(out=evr[:, t0:t0 + n], in_=pr_[:, :n], func=AF.Copy), reads=[prb_], writes=[evrb[part]])
                S.op("act", lambda: nc.scalar.activation(out=evi[:, t0:t0 + n], in_=pi_[:, :n], func=AF.Copy), reads=[pib_], writes=[evib[part]])
            ew(bre, breb, evr, evrb, cosT, cosb, ALU.mult)
            ew(sre, sreb, evi, evib, sinT, sinb, ALU.mult)
            ew(bim, bimb, evi, evib, cosT, cosb, ALU.mult)
            ew(sim, simb, evr, evrb, sinT, sinb, ALU.mult)
            ew(bre, breb, bre, breb, sre, sreb, ALU.add)
            ew(bim, bimb, bim, bimb, sim, simb, ALU.subtract)
            if idx + 1 < len(iters):
                tab_rest(idx + 1)
            rdec = rpp[:, col:col + 1]
            sq_ = []
            for (src, dst) in ((bre, sre), (bim, sim)):
                if d == 0:
                    sq_.append(lambda src=src, dst=dst: nc.vector.tensor_tensor_scan(dst[:, NL:T], rdec.to_broadcast([128, NX]), src[:, NL:T], 0.0, ALU.mult, ALU.add))
                else:
                    sq_.append(lambda src=src, dst=dst: nc.vector.tensor_tensor_scan(dst[:, NL:T][:, ::-1], rdec.to_broadcast([128, NX]), src[:, NL:T][:, ::-1], 0.0, ALU.mult, ALU.add))
            for (src, dst) in ((bre, sre), (bim, sim)):
                if d == 0:
                    sq_.append(lambda src=src, dst=dst: nc.vector.tensor_tensor_scan(dst[:, 0:NL], rdec.to_broadcast([128, NL]), src[:, 0:NL], dst[:, T - 1:T], ALU.mult, ALU.add))
                else:
                    sq_.append(lambda src=src, dst=dst: nc.vector.tensor_tensor_scan(dst[:, 0:NL][:, ::-1], rdec.to_broadcast([128, NL]), src[:, 0:NL][:, ::-1],
                                                                                     dst[:, NL:NL + 1], ALU.mult, ALU.add))
            S.seq("dve", sq_, reads=list(breb) + list(bimb) + [rppb], writes=list(sreb) + list(simb))
            ew(bre, breb, sre, sreb, cosT, cosb, ALU.mult)
            ew(bim, bimb, sim, simb, sinT, sinb, ALU.mult)
            ew(srb, srbb, bre, breb, bim, bimb, ALU.subtract)
            ew(bre, breb, sre, sreb, sinT, sinb, ALU.mult)
            ew(bim, bimb, sim, simb, cosT, cosb, ALU.mult)
            ew(sib, sibb, bre, breb, bim, bimb, ALU.add)
            first = (d == 0 and ns == 0)
            last = (d == 1 and ns == 3)
            for bi, (t0, n, lc) in enumerate(TB):
                part = 0 if t0 < TA else 1
                yp, ypb = yps[bi]

                def rd():
                    nc.tensor.matmul(yp[:, :n], lhsT=CR[:, col, :], rhs=srb[:, t0:t0 + n], start=first, stop=False)
                    return nc.tensor.matmul(yp[:, :n], lhsT=CIn[:, col, :], rhs=sib[:, t0:t0 + n], start=False, stop=last)
                S.op("pe", rd, reads=[CRb, CInb, srbb[part], sibb[part]], writes=[ypb])
            if last:
                for bi, (t0, n, lc) in enumerate(TB):
                    yp, ypb = yps[bi]
                    u32, u32b = tr.next()
                    S.dma("sp", u32[:, :n], Sx["s5u32"][ct * 128:(ct + 1) * 128, t0:t0 + n], writes=[u32b])
                    a1, a1b = tr.next()
                    S.op("dve", lambda: nc.vector.scalar_tensor_tensor(a1[:, :n], u32[:, :n], dsk[:, ct:ct + 1], yp[:, :n], ALU.mult, ALU.add),
                         reads=[u32b, dskb, ypb], writes=[a1b])
                    go, gob = gor.next()
                    S.op("act", lambda: nc.scalar.activation(out=go[:, :n], in_=a1[:, :n], func=AF.Gelu), reads=[a1b], writes=[gob])
                    S.dma("sp", Sx["dbg_g"][ct * 128:(ct + 1) * 128, t0:t0 + n], go[:, :n], reads=[gob])
        S.barrier()
        mst.close()
        gT, gTb = S.sbuf("gT", [128, 4, T], BF16, es=ph)
        S.dma("sp", gT[:], Sx["dbg_g"].rearrange("(c p) t -> p c t", p=128), writes=[gTb])
        wgl, wglb = S.sbuf("wglu", [128, 4, 512], BF16, es=ph)
        S.dma("pool", wgl[:], I["w_glu"][li].rearrange("(c p) f -> p c f", p=128), writes=[wglb])
        bgl, bglb = S.sbuf("bglu", [128, 4], F32, es=ph)
        S.dma("sp", bgl[:], I["b_glu"][li], writes=[bglb])
        obr = Ring(S, ph, "s5o", [128, 512], BF16, 3)
        for fch in range(4):
            for (t0, n, lc) in TB:
                ps, psb = bur.next()

                def mm():
                    ins = None
                    for c in range(4):
                        ins = nc.tensor.matmul(ps[:, :n], lhsT=wgl[:, c, fch * 128:(fch + 1) * 128], rhs=gT[:, c, t0:t0 + n], start=(c == 0), stop=(c == 3))
                    return ins
                S.op("pe", mm, reads=[wglb, gTb], writes=[psb])
                a1, a1b = tr.next()
                S.op("act", lambda: nc.scalar.activation(out=a1[:, :n], in_=ps[:, :n], func=AF.Sigmoid, bias=bgl[:, fch:fch + 1]), reads=[psb, bglb], writes=[a1b])
                ob, obb = obr.next()
                S.op("dve", lambda: nc.vector.tensor_tensor(ob[:, :n], a1[:, :n], gT[:, fch, t0:t0 + n], ALU.mult), reads=[a1b, gTb], writes=[obb])
                S.dma("sp", Sx["catT"][1024 + fch * 128:1024 + (fch + 1) * 128, t0:t0 + n], ob[:, :n], reads=[obb])


def phase_out(C, li, x_in, x_out):
    S, nc, I, Sx = C.S, C.nc, C.I, C.Sx
    m = C.mod[li]
    with ExitStack() as ph:
        cat, catb = S.sbuf("cat", [128, KD, T], BF16, es=ph)
        for k4 in range(4):
            S.dma("sp", cat[:, k4 * 4:(k4 + 1) * 4, :], Sx["catT"][k4 * 512:(k4 + 1) * 512, :].rearrange("(k p) t -> p k t", p=128),
                  writes=[catb] if k4 == 0 else [], awrites=[] if k4 == 0 else [catb])
        wr = Ring(S, ph, "wo", [128, KD, 512], BF16, 2)
        pr = Ring(S, ph, "po", [128, 512], F32, 4, psum=True)
        xr = Ring(S, ph, "xo", [128, 512], F32, 3)
        orr = Ring(S, ph, "oo", [128, 512], F32, 3)
        nxt = wr.next()
        S.dma("pool", nxt[0][:], I["w_out"][li][:, 0:512].rearrange("(k p) n -> p k n", p=128), writes=[nxt[1]])
        for nb in range(4):
            w, wb = nxt
            if nb < 3:
                nxt = wr.next()
                S.dma("pool", nxt[0][:], I["w_out"][li][:, (nb + 1) * 512:(nb + 2) * 512].rearrange("(k p) n -> p k n", p=128), writes=[nxt[1]])
            for dl in range(4):
                dch = nb * 4 + dl
                for (t0, n, lc) in TB:
                    xt, xtb = xr.next()
                    S.dma("sp", xt[:, :n], x_in[dch * 128:(dch + 1) * 128, t0:t0 + n], writes=[xtb])
                    ps, psb = pr.next()

                    def mm():
                        ins = None
                        for k in range(KD):
                            ins = nc.tensor.matmul(ps[:, :n], lhsT=w[:, k, dl * 128:(dl + 1) * 128], rhs=cat[:, k, t0:t0 + n], start=(k == 0), stop=(k == KD - 1))
                        return ins
                    S.op("pe", mm, reads=[wb, catb], writes=[psb])
                    o, ob = orr.next()
                    S.op("dve", lambda: nc.vector.scalar_tensor_tensor(o[:, :n], ps[:, :n], m.t[:, 32 + dch, lc:lc + 1], xt[:, :n], ALU.mult, ALU.add),
                         reads=[psb, m.b, xtb], writes=[ob])
                    S.dma("sp", x_out[dch * 128:(dch + 1) * 128, t0:t0 + n], o[:, :n], reads=[ob])


def phase_moe(C, li, x_in, x_out):
    S, nc, I, Sx = C.S, C.nc, C.I, C.Sx
    m = C.mod[li]
    with ExitStack() as ph:
        posmT, posmTb = S.sbuf("posmT", [16, T], F32, es=ph)
        with ExitStack() as ph8:
            h2tok, h2tokb = S.sbuf("h2tok", [128, 18, D], BF16, es=ph8)
            aff3, aff3b = S.sbuf("aff3", [128, 18, 16, 3], BF16, es=ph8)
            posm_tok, posm_tokb = S.sbuf("posm_tok", [128, 18, 16], F32, es=ph8)
            with ExitStack() as p7:
                wrt, wrtb = S.sbuf("wrt", [128, KD, 16], F32, es=p7)
                S.dma("sp", wrt[:], I["w_router"][li].rearrange("(k p) e -> p k e", p=128), writes=[wrtb])
                aff, affb = S.sbuf("aff", [128, 18, 16], F32, es=p7)
                lg_, lgb = S.psum("lg", [128, 512], F32, es=p7)
                lg = lg_[:, 0:288].rearrange("p (a b) -> p a b", b=16)
                with ExitStack() as p7a:
                    hb, hbb = S.sbuf("hb", [128, KD, 512], BF16, es=p7a)
                    ptr = Ring(S, p7a, "pT", [128, 1024], BF16, 2, psum=True)

                    def cb(bi, t0, n, lc, hf, hfb):
                        S.op("pool", lambda: nc.gpsimd.tensor_copy(hb[:, :, :n], hf[:, :, :n]), reads=[hfb], writes=[hbb])
                        for a in range(n // 128):
                            tt = t0 // 128 + a

                            def mm():
                                ins = None
                                for k in range(KD):
                                    ins = nc.tensor.matmul(lg[:, tt, :], lhsT=hf[:, k, a * 128:(a + 1) * 128], rhs=wrt[:, k, :], start=(k == 0), stop=(k == KD - 1))
                                return ins
                            S.op("pe", mm, reads=[hfb, wrtb], writes=[lgb])
                            for kq in range(4):
                                pT, pTb = ptr.next()

                                def tp():
                                    ins = None
                                    for j in range(4):
                                        ins = nc.tensor.transpose(pT[:, j * 128:(j + 1) * 128], hb[:, kq * 4 + j, a * 128:(a + 1) * 128], C.ident_bf[:, :])
                                    return ins
                                S.op("pe", tp, reads=[hbb, C.ident_bf_b], writes=[pTb])
                                if kq % 2:
                                    S.op("act", lambda: nc.scalar.activation(out=h2tok[:, tt, kq * 512:(kq + 1) * 512], in_=pT[:, 0:512], func=AF.Copy),
                                         reads=[pTb], writes=[h2tokb])
                                else:
                                    S.op("dve", lambda: nc.vector.tensor_copy(h2tok[:, tt, kq * 512:(kq + 1) * 512], pT[:, 0:512]), reads=[pTb], writes=[h2tokb])
                    norm_blocks(C, p7a, x_in, m.A2, m.A2b, modsl(m, 3), m.b, cb)
                    S.barrier()
                mx, mxb = S.sbuf("mx", [128, 18], F32, es=p7)
                S.op("dve", lambda: nc.vector.tensor_reduce(mx[:], lg, AX.X, ALU.max), reads=[lgb], writes=[mxb])
                S.op("dve", lambda: nc.vector.tensor_tensor(aff[:], lg, mx[:].unsqueeze(2).to_broadcast([128, 18, 16]), ALU.subtract),
                     reads=[lgb, mxb], writes=[affb])
                S.op("act", lambda: nc.scalar.activation(out=aff[:], in_=aff[:], func=AF.Exp), reads=[affb], writes=[affb])
                S.op("dve", lambda: nc.vector.tensor_reduce(mx[:], aff[:], AX.X, ALU.add), reads=[affb], writes=[mxb])
                S.op("dve", lambda: nc.vector.reciprocal(mx[:], mx[:]), reads=[mxb], writes=[mxb])
                S.op("dve", lambda: nc.vector.tensor_tensor(aff[:], aff[:], mx[:].unsqueeze(2).to_broadcast([128, 18, 16]), ALU.mult),
                     reads=[affb, mxb], writes=[affb])
                if "dbg_aff" in C.dump:
                    S.dma("sp", Sx["dbg_aff"], aff[:], reads=[affb])
                r1, r1b = S.sbuf("r1", [128, 18, 16], F32, es=p7)
                S.op("dve", lambda: nc.vector.tensor_copy(aff3[:, :, :, 0], aff[:]), reads=[affb], writes=[aff3b])
                S.op("dve", lambda: nc.vector.tensor_tensor(r1[:], aff[:], aff3[:, :, :, 0], ALU.subtract), reads=[affb, aff3b], writes=[r1b])
                S.op("dve", lambda: nc.vector.tensor_copy(aff3[:, :, :, 1], r1[:]), reads=[r1b], writes=[aff3b])
                S.op("dve", lambda: nc.vector.tensor_tensor(r1[:], r1[:], aff3[:, :, :, 1], ALU.subtract), reads=[r1b, aff3b], writes=[r1b])
                S.op("dve", lambda: nc.vector.tensor_copy(aff3[:, :, :, 2], r1[:]), reads=[r1b], writes=[aff3b])
                affT, affTb = S.sbuf("affT", [16, T], F32, es=p7)
                work, workb = S.sbuf("work", [16, T], F32, es=p7)
                pa_, pab = S.psum("pa", [128, 512], F32, es=p7)
                pa = pa_[0:16, :]
                for (t0, n, lc) in TB:
                    def tpa():
                        ins = None
                        for a in range(n // 128):
                            ins = nc.tensor.transpose(pa[:, a * 128:(a + 1) * 128], aff[:, t0 // 128 + a, :], C.ident_f[:, :])
                        return ins
                    S.op("pe", tpa, reads=[affb, C.ident_f_b], writes=[pab])
                    S.op("dve", lambda: nc.vector.tensor_copy(affT[:, t0:t0 + n], pa[:, :n]), reads=[pab], writes=[affTb])
                S.op("dve", lambda: nc.vector.tensor_copy(work[:], affT[:]), reads=[affTb], writes=[workb])
                m8, m8b = S.sbuf("m8", [16, 16], F32, es=p7)

                tk = []
                for (lo, hi, rounds, oc) in ((0, NL, 32, 0), (NL, T, 4, 8)):
                    for r in range(rounds):
                        tk.append(lambda lo=lo, hi=hi, oc=oc: nc.vector.max(m8[:, oc:oc + 8], work[:, lo:hi]))
                        if r < rounds - 1:
                            tk.append(lambda lo=lo, hi=hi, oc=oc: nc.vector.match_replace(work[:, lo:hi], m8[:, oc:oc + 8], work[:, lo:hi], -1.0))
                S.seq("dve", tk, reads=[workb], writes=[workb, m8b])
                mk, mkb = S.sbuf("mk", [16, T], F32, es=p7)

                S.seq("dve", [
                    lambda: nc.vector.tensor_scalar(mk[:, 0:NL], affT[:, 0:NL], m8[:, 7:8], None, ALU.is_ge),
                    lambda: nc.vector.tensor_scalar(mk[:, NL:T], affT[:, NL:T], m8[:, 15:16], None, ALU.is_ge),
                    lambda: nc.vector.tensor_tensor_scan(work[:, 0:NL], C.one_f[:16, 0:1].to_broadcast([16, NL]), mk[:, 0:NL], 0.0, ALU.mult, ALU.add),
                    lambda: nc.vector.tensor_tensor_scan(work[:, NL:T], C.one_f[:16, 0:1].to_broadcast([16, NX]), mk[:, NL:T], 0.0, ALU.mult, ALU.add),
                    lambda: nc.vector.tensor_scalar(work[:, NL:T], work[:, NL:T], 256.0, None, ALU.add),
                    lambda: nc.vector.tensor_tensor(work[:], work[:], mk[:], ALU.mult),
                    lambda: nc.vector.tensor_scalar(posmT[:], work[:], -1.0, None, ALU.add),
                ], reads=[affTb, m8b, C.one_f_b, workb], writes=[mkb, workb, posmTb])
                if "dbg_posm" in C.dump:
                    S.dma("sp", Sx["dbg_posm"], posmT[:], reads=[posmTb])
                pp__, ppb_ = S.psum("ppm", [128, 512], F32, es=p7)
                pp_ = pp__[:, 0:288].rearrange("p (a b) -> p a b", b=16)

                def tpp():
                    ins = None
                    for tt in range(18):
                        ins = nc.tensor.transpose(pp_[:, tt, :], posmT[:, tt * 128:(tt + 1) * 128], C.ident_f[:16, :16])
                    return ins
                S.op("pe", tpp, reads=[posmTb, C.ident_f_b], writes=[ppb_])
                S.op("dve", lambda: nc.vector.tensor_copy(posm_tok[:], pp_), reads=[ppb_], writes=[posm_tokb])
                S.barrier()
            with ExitStack() as p8:
                ioj, iojb = S.sbuf("ioj", [128, NJ], F32, es=p8)
                S.dma("sp", ioj[:], I["iota_j"][0:1, :].to_broadcast([128, NJ]), writes=[iojb])
                Se, Seb = S.sbuf("Se", [128, 18, NJ], BF16, es=p8)
                xsr = Ring(S, p8, "xs", [128, KD, NJ], BF16, 2)
                hidr = Ring(S, p8, "hid", [128, 8, NJ], BF16, 2)
                ysb_, ysbb = S.sbuf("ysb", [128, 3, D], BF16, es=p8)
                wring = Ring(S, p8, "wu", [128, 4096], BF16, 6)
                pr = Ring(S, p8, "pe8", [128, 512], F32, 6, psum=True)
                tap_, tapb = S.psum("tap", [128, 512], F32, es=p8)
                tap = tap_[:, 0:9]
                ta, tab = S.sbuf("ta", [128, 3], F32, es=p8)
                sgr = Ring(S, p8, "sg", [128, NJ], F32, 3)

                def units(e):
                    u = []
                    for fq in range(4):
                        u.append(("g", fq, I["w_gate"][li, e][:, fq * 256:(fq + 1) * 256].rearrange("(k p) f -> p k f", p=128)))
                        u.append(("u", fq, I["w_up"][li, e][:, fq * 256:(fq + 1) * 256].rearrange("(k p) f -> p k f", p=128)))
                    for dq in range(4):
                        u.append(("d", dq, I["w_down"][li, e][:, dq * 512:(dq + 1) * 512].rearrange("(c p) d -> p c d", p=128)))
                    return u
                allu = [(e,) + u for e in range(16) for u in units(e)]
                loaded = {}

                def issue(i):
                    if i >= len(allu):
                        return
                    e, kind, idx, src = allu[i]
                    wt, wtb = wring.next()
                    if kind == "d":
                        S.dma("pool", wt[:].rearrange("p (c d) -> p c d", c=8), src, writes=[wtb])
                    else:
                        S.dma("pool", wt[:].rearrange("p (k f) -> p k f", k=KD), src, writes=[wtb])
                    loaded[i] = (wt, wtb)
                PRE = 4
                for i in range(PRE):
                    issue(i)
                ui = 0
                for e in range(16):
                    def mkS():
                        ins = None
                        for tt in range(18):
                            ins = nc.vector.tensor_scalar(Se[:, tt, :], ioj[:, :], posm_tok[:, tt, e:e + 1], None, ALU.is_equal)
                        return ins
                    S.op("dve", mkS, reads=[iojb, posm_tokb], writes=[Seb])

                    def mta():
                        ins = None
                        for jc, (tts, M) in enumerate(((range(16), 128), (range(16), 128), ((16, 17), 32))):
                            tts = list(tts)
                            for ii, tt in enumerate(tts):
                                ins = nc.tensor.matmul(tap[:M, jc * 3:(jc + 1) * 3], lhsT=Se[:, tt, jc * 128:jc * 128 + M], rhs=aff3[:, tt, e, :],
                                                       start=(ii == 0), stop=(ii == len(tts) - 1))
                        return ins
                    S.op("pe", mta, reads=[Seb, aff3b], writes=[tapb])
                    S.op("dve", lambda: nc.vector.tensor_reduce(ta[:], tap_[:, 0:9].rearrange("p (a b) -> p a b", b=3), AX.X, ALU.add), reads=[tapb], writes=[tab])
                    xs, xsb = xsr.next()
                    for k in range(KD):
                        ps, psb = pr.next()

                        def gm():
                            ins = None
                            for tt in range(16):
                                ins = nc.tensor.matmul(ps[:, 0:256], lhsT=h2tok[:, tt, k * 128:(k + 1) * 128], rhs=Se[:, tt, 0:256], start=(tt == 0), stop=(tt == 15))
                            for tt in (16, 17):
                                ins = nc.tensor.matmul(ps[:, 256:NJ], lhsT=h2tok[:, tt, k * 128:(k + 1) * 128], rhs=Se[:, tt, 256:NJ], start=(tt == 16), stop=(tt == 17))
                            return ins
                        S.op("pe", gm, reads=[h2tokb, Seb], writes=[psb])
                        if k % 2:
                            S.op("act", lambda: nc.scalar.activation(out=xs[:, k, :], in_=ps[:, :NJ], func=AF.Copy), reads=[psb], writes=[xsb])
                        else:
                            S.op("dve", lambda: nc.vector.tensor_copy(xs[:, k, :], ps[:, :NJ]), reads=[psb], writes=[xsb])
                    hid, hidb = hidr.next()
                    for fq in range(4):
                        wg, wgb = loaded.pop(ui)
                        wu, wub = loaded.pop(ui + 1)
                        ui += 2
                        wg3 = wg[:].rearrange("p (k f) -> p k f", k=KD)
                        wu3 = wu[:].rearrange("p (k f) -> p k f", k=KD)
                        for fcl in range(2):
                            fc = fq * 2 + fcl
                            pg, pgb = pr.next()
                            pu, pub = pr.next()

                            def mg():
                                ins = None
                                for k in range(KD):
                                    ins = nc.tensor.matmul(pg[:, :NJ], lhsT=wg3[:, k, fcl * 128:(fcl + 1) * 128], rhs=xs[:, k, :], start=(k == 0), stop=(k == KD - 1))
                                return ins

                            def mu():
                                ins = None
                                for k in range(KD):
                                    ins = nc.tensor.matmul(pu[:, :NJ], lhsT=wu3[:, k, fcl * 128:(fcl + 1) * 128], rhs=xs[:, k, :], start=(k == 0), stop=(k == KD - 1))
                                return ins
                            S.op("pe", mg, reads=[wgb, xsb], writes=[pgb])
                            S.op("pe", mu, reads=[wub, xsb], writes=[pub])
                            sg, sgb = sgr.next()
                            S.op("act", lambda: nc.scalar.activation(out=sg[:, :], in_=pg[:, :NJ], func=AF.Silu), reads=[pgb], writes=[sgb])
                            S.op("dve", lambda: nc.vector.tensor_tensor(hid[:, fc, :], sg[:, :], pu[:, :NJ], ALU.mult), reads=[sgb, pub], writes=[hidb])
                        issue(ui - 2 + PRE)
                        issue(ui - 1 + PRE)
                    for dq in range(4):
                        wd, wdb = loaded.pop(ui)
                        ui += 1
                        wd3 = wd[:].rearrange("p (c d) -> p c d", c=8)
                        for jc, M in enumerate((128, 128, 32)):
                            ps, psb = pr.next()

                            def md():
                                ins = None
                                for fc in range(8):
                                    ins = nc.tensor.matmul(ps[:M, :], lhsT=hid[:, fc, jc * 128:jc * 128 + M], rhs=wd3[:, fc, :], start=(fc == 0), stop=(fc == 7))
                                return ins
                            S.op("pe", md, reads=[hidb, wdb], writes=[psb])
                            if jc == 1:
                                S.op("act", lambda: nc.scalar.activation(out=ysb_[:M, jc, dq * 512:(dq + 1) * 512], in_=ps[:M, :], func=AF.Copy, scale=ta[:M, jc:jc + 1]),
                                     reads=[psb, tab], writes=[ysbb])
                            else:
                                S.op("dve", lambda: nc.vector.tensor_scalar(ysb_[:M, jc, dq * 512:(dq + 1) * 512], ps[:M, :], ta[:M, jc:jc + 1], None, ALU.mult),
                                     reads=[psb, tab], writes=[ysbb])
                        issue(ui - 1 + PRE)
                    S.dma("sp", Sx["ys"][e, 0:256, :].rearrange("(c j) d -> j c d", j=128), ysb_[:, 0:2, :], reads=[ysbb])
                    S.dma("sp", Sx["ys"][e, 256:NJ, :], ysb_[:32, 2, :], reads=[ysbb])
                S.barrier()
        with ExitStack() as p9:
            ysh, yshb = S.sbuf("ysh", [128, 16, 3, 1024], BF16, es=p9)
            ST, STb = S.sbuf("ST", [128, 16, 2, 512], BF16, es=p9)
            selt, seltb = S.sbuf("selt", [16, 16, 128], F32, es=p9)
            S.dma("sp", selt[:], I["sel"][:, :, :], writes=[seltb])
            ip3, ip3b = S.sbuf("ip3", [128, 3], F32, es=p9)
            S.dma("sp", ip3[:], I["iota_p3"][:, :], writes=[ip3b])
            bcr = Ring(S, p9, "bc", [128, 512], F32, 3, psum=True)
            pr = Ring(S, p9, "p9", [128, 512], F32, 4, psum=True)
            xr = Ring(S, p9, "x9", [128, 512], F32, 3)
            orr = Ring(S, p9, "o9", [128, 512], F32, 3)
            for dh in range(2):
                for jc in range(2):
                    S.dma("sp", ysh[:, :, jc, :], Sx["ys"][:, jc * 128:(jc + 1) * 128, dh * 1024:(dh + 1) * 1024].rearrange("e j d -> j e d"),
                          writes=[yshb] if jc == 0 else [], awrites=[] if jc == 0 else [yshb])
                S.dma("sp", ysh[:32, :, 2, :], Sx["ys"][:, 256:NJ, dh * 1024:(dh + 1) * 1024].rearrange("e j d -> j e d"), awrites=[yshb])
                for (t0, n, lc) in TB:
                    for e in range(16):
                        bc, bcb = bcr.next()
                        S.op("pe", lambda: nc.tensor.matmul(bc[:, :n], lhsT=selt[:, e, :], rhs=posmT[:, t0:t0 + n], start=True, stop=True),
                             reads=[seltb, posmTb], writes=[bcb])
                        if lc == 0:
                            S.op("dve", lambda: nc.vector.tensor_scalar(ST[:, e, 0, :n], bc[:, :n], ip3[:, 0:1], None, ALU.is_equal), reads=[bcb, ip3b], writes=[STb])
                            S.op("pool" if False else "dve", lambda: nc.vector.tensor_scalar(ST[:, e, 1, :n], bc[:, :n], ip3[:, 1:2], None, ALU.is_equal),
                                 reads=[bcb, ip3b], writes=[STb])
                        else:
                            S.op("dve", lambda: nc.vector.tensor_scalar(ST[:32, e, 0, :n], bc[:32, :n], ip3[:32, 2:3], None, ALU.is_equal), reads=[bcb, ip3b], writes=[STb])
                    for dl in range(8):
                        dch = dh * 8 + dl
                        xt, xtb = xr.next()
                        S.dma("sp", xt[:, :n], x_in[dch * 128:(dch + 1) * 128, t0:t0 + n], writes=[xtb])
                        ps, psb = pr.next()

                        def msc():
                            ins = None
                            if lc == 0:
                                for e in range(16):
                                    for jc in range(2):
                                        ins = nc.tensor.matmul(ps[:, :n], lhsT=ysh[:, e, jc, dl * 128:(dl + 1) * 128], rhs=ST[:, e, jc, :n],
                                                               start=(e == 0 and jc == 0), stop=(e == 15 and jc == 1))
                            else:
                                for e in range(16):
                                    ins = nc.tensor.matmul(ps[:, :n], lhsT=ysh[:32, e, 2, dl * 128:(dl + 1) * 128], rhs=ST[:32, e, 0, :n],
                                                           start=(e == 0), stop=(e == 15))
                            return ins
                        S.op("pe", msc, reads=[yshb, STb], writes=[psb])
                        o, ob = orr.next()
                        S.op("dve", lambda: nc.vector.scalar_tensor_tensor(o[:, :n], ps[:, :n], m.t[:, 80 + dch, lc:lc + 1], xt[:, :n], ALU.mult, ALU.add),
                             reads=[psb, m.b, xtb], writes=[ob])
                        S.dma("sp", x_out[dch * 128:(dch + 1) * 128, t0:t0 + n], o[:, :n], reads=[ob])


def phase_final(C, x_in):
    S, nc, I = C.S, C.nc, C.I
    with ExitStack() as ph:
        gf, gfb = S.sbuf("gf", [128, KD, 2], F32, es=ph)
        S.dma("sp", gf[:], I["gfT"][:, :, :], writes=[gfb])
        zs, zsb = S.sbuf("zs", [128, KD, 2], F32, es=ph)
        S.op("pool", lambda: nc.gpsimd.memset(zs[:], 0.0), writes=[zsb])

        def cb(bi, t0, n, lc, hf, hfb):
            S.dma("sp", C.outT[:, t0:t0 + n].rearrange("(k p) t -> p k t", p=128), hf[:, :, :n], reads=[hfb])
        norm_blocks(C, ph, x_in, gf, gfb, zs, zsb, cb, blocks=TB[:4])


def _prep_shared(inp):
    f = np.float32
    L = DEPTH
    sh = {}
    sh["w_ada"] = np.ascontiguousarray(inp["w_ada"], dtype=f)
    sh["b_adaT"] = np.ascontiguousarray(np.repeat(inp["b_ada"].reshape(L, 96, 128).transpose(0, 2, 1)[..., None], 2, axis=-1), dtype=f)
    for nm, src in (("g1T", "norm1_g"), ("g2T", "norm2_g")):
        sh[nm] = np.ascontiguousarray(np.repeat(inp[src].reshape(L, KD, 128).transpose(0, 2, 1)[..., None], 2, axis=-1), dtype=f)
    sh["gfT"] = np.ascontiguousarray(np.repeat(inp["final_norm_g"].reshape(KD, 128).T[..., None], 2, axis=-1), dtype=f)
    sh["w_in"] = np.ascontiguousarray(inp["w_in"], dtype=f)
    perm = np.array([(r // 32) * 32 + ((r % 32) + 16) % 32 for r in range(64)])
    sh["w_in_sw"] = np.ascontiguousarray(inp["w_in"][:, :, 1664:1728][:, :, perm], dtype=f)
    sh["w_out"] = np.ascontiguousarray(inp["w_out"], dtype=f)
    sh["sgu_g"] = np.ascontiguousarray(inp["sgu_norm_g"].reshape(L, 1, 512), dtype=f)
    sh["sgu_wT"] = np.ascontiguousarray(inp["sgu_w"].transpose(0, 3, 1, 2), dtype=f)
    sh["sgu_b4"] = np.ascontiguousarray(np.repeat(inp["sgu_b"][:, :, None, :], 4, axis=2).reshape(L, 1, 2048), dtype=f)
    sh["qg"] = np.ascontiguousarray(inp["mla_q_norm_g"].reshape(L, 3, 128).transpose(0, 2, 1), dtype=f)
    sh["kvg"] = np.ascontiguousarray(inp["mla_kv_norm_g"].reshape(L, 2, 128).transpose(0, 2, 1), dtype=f)
    sh["w_uq"] = np.ascontiguousarray(inp["mla_w_uq"], dtype=f)
    sh["w_uq_sw"] = np.ascontiguousarray(np.concatenate([inp["mla_w_uq"][:, :, h * 192 + 128 + perm] for h in range(4)], axis=-1), dtype=f)
    sh["w_ukv"] = np.ascontiguousarray(inp["mla_w_ukv"], dtype=f)
    t = np.arange(NL)
    row_id = (t // 64).astype(f)
    col_id = (t % 64).astype(f)
    inv_freq = (f(10000.0) ** (-np.arange(16, dtype=f) / f(16))).astype(f)
    cosT = np.ones((64, T), f)
    sinT = np.zeros((64, T), f)
    for r in range(64):
        pos = row_id if r < 32 else col_id
        ang = (pos * inv_freq[r % 16]).astype(f)
        cosT[r, :NL] = np.cos(ang)
        sinT[r, :NL] = np.sin(ang) * (-1.0 if (r % 32) < 16 else 1.0)
    sh["rope_cos"] = cosT
    sh["rope_sin"] = sinT

    def pp(a):
        return a.reshape(L, 2, 4, 8, 4, 16).transpose(0, 3, 5, 1, 2, 4).reshape(L, 128, 32)

    def rowl(a):
        return a.reshape(L, 2, 4, 8, 4, 16).transpose(0, 1, 2, 4, 3, 5).reshape(L, 4096)
    ldt_full = np.repeat(inp["s5_log_dt"][..., None], 64, axis=-1)
    sh["s5_pp"] = np.ascontiguousarray(np.stack([pp(inp["s5_a_re"]), pp(inp["s5_a_im"]), pp(ldt_full)], axis=2), dtype=f)
    sh["s5_row"] = np.ascontiguousarray(np.stack([rowl(inp["s5_a_re"]), rowl(inp["s5_a_im"]), rowl(ldt_full)], axis=1), dtype=f)

    def bblk(b):
        o = np.zeros((L, 8, 16, 2, 4, 4, 8, 16), f)
        bb = b.reshape(L, 2, 4, 8, 4, 16, 16)
        for g in range(8):
            o[:, g, :, :, :, :, g, :] = bb[:, :, :, g].transpose(0, 4, 1, 2, 3, 5)[:, :, :, :, :, :] if False else \
                np.transpose(bb[:, :, :, g], (0, 5, 1, 2, 3, 4))
        return o.reshape(L, 128, 4096)

    def cblk(c):
        o = np.zeros((L, 8, 16, 2, 4, 4, 8, 16), f)
        cc = c.reshape(L, 2, 4, 8, 16, 4, 16)
        for g in range(8):
            o[:, g, :, :, :, :, g, :] = np.transpose(cc[:, :, :, g], (0, 5, 1, 2, 4, 3))
        return o.reshape(L, 128, 4096)
    sh["s5_Bre"] = bblk(inp["s5_b_re"])
    sh["s5_Bim"] = bblk(inp["s5_b_im"])
    sh["s5_Cre"] = cblk(inp["s5_c_re"])
    sh["s5_Cim"] = cblk(inp["s5_c_im"])
    sh["s5_d"] = np.ascontiguousarray(inp["s5_d"].reshape(L, 4, 128).transpose(0, 2, 1), dtype=f)
    tau = np.zeros((2, T), f)
    tau[0, NL:] = np.arange(NX)
    tau[0, :NL] = NX + np.arange(NL)
    tau[1, NL:] = NX - 1 - np.arange(NX)
    tau[1, :NL] = NX + (NL - 1 - np.arange(NL))
    sh["s5_tau"] = tau.reshape(1, 2 * T)
    sh["w_glu"] = np.ascontiguousarray(inp["s5_w_glu"], dtype=f)
    sh["b_glu"] = np.ascontiguousarray(inp["s5_b_glu"].reshape(L, 4, 128).transpose(0, 2, 1), dtype=f)
    sh["conv_wT"] = np.ascontiguousarray(inp["conv_w"].reshape(L, 3, 4, 128).transpose(0, 3, 2, 1), dtype=f)
    sh["w_router"] = np.ascontiguousarray(inp["moe_w_router"], dtype=f)
    sh["w_gate"] = np.ascontiguousarray(inp["moe_w_gate"], dtype=f)
    sh["w_up"] = np.ascontiguousarray(inp["moe_w_up"], dtype=f)
    sh["w_down"] = np.ascontiguousarray(inp["moe_w_down"], dtype=f)
    sh["ident"] = np.eye(128, dtype=f)
    sh["iota_j"] = np.arange(NJ, dtype=f).reshape(1, NJ)
    sh["iota_p3"] = (np.arange(128, dtype=f)[:, None] + np.array([0, 128, 256], f)[None, :]).astype(f)
    sel = np.zeros((16, 16, 128), f)
    for e in range(16):
        sel[e, e, :] = 1.0
    sh["sel"] = sel
    return sh


def _prep_core(inp, b):
    f = np.float32
    d = {}
    d["xT0"] = np.ascontiguousarray(np.concatenate([inp["x"][b].T, inp["ctx"][b].T], axis=1), dtype=f)
    c2 = np.stack([inp["c"][b], inp["c_ctx"]], axis=0)
    d["cTp"] = np.ascontiguousarray(c2.reshape(2, KD, 128).transpose(2, 1, 0), dtype=f)
    return d


_NC_CACHE = {}


def used_inputs(nc_I, m):
    return {k: v for k, v in m.items() if k in nc_I}


def kernel(**inputs):
    inp = {k: np.asarray(v) for k, v in inputs.items()}
    B = inp["x"].shape[0]
    if "full" not in _NC_CACHE:
        _NC_CACHE["full"] = build_program()
    nc = _NC_CACHE["full"]
    sh = _prep_shared(inp)
    in_maps = []
    for b in range(B):
        m = dict(sh)
        m.update(_prep_core(inp, b))
        in_maps.append({k: v for k, v in m.items() if k in nc._used_inputs})
    res = run_bass_kernel_spmd(nc, in_maps, core_ids=list(range(B)))
    out = np.stack([np.asarray(res.results[b]["outT"]).T for b in range(B)], axis=0)
    return np.ascontiguousarray(out, dtype=np.float32)
```

```python
import math
import numpy as np
from contextlib import ExitStack
import concourse.bass as bass
import concourse.mybir as mybir
from concourse.bass_utils import run_bass_kernel_spmd

F32 = mybir.dt.float32
BF16 = mybir.dt.bfloat16
I32 = mybir.dt.int32
AF = mybir.ActivationFunctionType
ALU = mybir.AluOpType
AX = mybir.AxisListType

D = 2048
T = 2304
NL = 2048
NX = 256
KD = 16
DEPTH = 2
TB = [(0, 512, 0), (512, 512, 0), (1024, 512, 0), (1536, 512, 0), (2048, 256, 1)]
NJ = 288
EPS = 1e-6
N_DMA_SEMS = 24
TWO_PI = 6.283185


class Buf:
    __slots__ = ("name", "w", "r")

    def __init__(self, name=""):
        self.name = name
        self.w = {}
        self.r = {}


class Sched:
    def __init__(self, nc, es):
        self.nc = nc
        self.es = es
        self.eng = {"pe": nc.tensor, "dve": nc.vector, "act": nc.scalar, "pool": nc.gpsimd, "sp": nc.sync}
        self.sems = {}
        self.cnt = {}
        for k in ["pe", "dve", "act", "pool"]:
            self.sems[k] = es.enter_context(nc.semaphore("s_" + k))
            self.cnt[k] = 0
        for i in range(N_DMA_SEMS):
            k = "d%d" % i
            self.sems[k] = es.enter_context(nc.semaphore("s_" + k))
            self.cnt[k] = 0
        self.dma_rr = 0
        self.waited = {e: {} for e in self.eng}
        self.bufs = []
        self.uid = 0

    def sbuf(self, name, shape, dt, es=None):
        self.uid += 1
        t = (es or self.es).enter_context(self.nc.sbuf_tensor("%s_%d" % (name, self.uid), list(shape), dt))
        b = Buf(name)
        self.bufs.append(b)
        return t, b

    def psum(self, name, shape, dt, es=None):
        self.uid += 1
        t = (es or self.es).enter_context(self.nc.psum_tensor("%s_%d" % (name, self.uid), list(shape), dt))
        b = Buf(name)
        self.bufs.append(b)
        return t, b

    def _wait(self, e, dep):
        sk, val, deng = dep
        if deng == e and e == "pe":
            return
        if self.waited[e].get(sk, 0) >= val:
            return
        self.eng[e].wait_ge(self.sems[sk], val)
        self.waited[e][sk] = val

    def _deps(self, e, reads, writes):
        for b in reads:
            for d in b.w.values():
                self._wait(e, d)
        for b in writes:
            for d in b.w.values():
                self._wait(e, d)
            for d in b.r.values():
                self._wait(e, d)

    def _commit(self, tag, reads, writes):
        for b in reads:
            b.r[tag[0]] = tag
        for b in writes:
            b.w = {tag[0]: tag}
            b.r = {}

    def op(self, e, fn, reads=(), writes=()):
        self._deps(e, reads, writes)
        ins = fn()
        self.cnt[e] += 1
        ins.then_inc(self.sems[e], 1)
        self._commit((e, self.cnt[e], e), reads, writes)
        return ins

    def seq(self, e, fns, reads=(), writes=()):
        self._deps(e, reads, writes)
        ins = None
        for i, fn in enumerate(fns):
            if i > 0:
                self.eng[e].wait_ge(self.sems[e], self.cnt[e])
                self.waited[e][e] = self.cnt[e]
            ins = fn()
            self.cnt[e] += 1
            ins.then_inc(self.sems[e], 1)
        self._commit((e, self.cnt[e], e), reads, writes)
        return ins

    def dma(self, q, out, in_, reads=(), writes=(), awrites=()):
        self._deps(q, reads, writes)
        sk = "d%d" % self.dma_rr
        self.dma_rr = (self.dma_rr + 1) % N_DMA_SEMS
        if self.cnt[sk] > 0:
            self._wait(q, (sk, self.cnt[sk], "dma"))
        ins = self.eng[q].dma_start(out=out, in_=in_)
        self.cnt[sk] += 16
        ins.then_inc(self.sems[sk], 16)
        self._commit((sk, self.cnt[sk], "dma"), reads, writes)
        for b in awrites:
            b.w[sk] = (sk, self.cnt[sk], "dma")
        return ins

    def barrier(self):
        for e in self.eng:
            for sk, c in self.cnt.items():
                if c > 0:
                    self._wait(e, (sk, c, "x"))
        for b in self.bufs:
            b.w = {}
            b.r = {}


class Ring:
    def __init__(self, S, es, name, shape, dt, n, psum=False):
        mk = S.psum if psum else S.sbuf
        self.items = [mk("%s%d" % (name, i), shape, dt, es=es) for i in range(n)]
        self.i = 0

    def next(self):
        it = self.items[self.i % len(self.items)]
        self.i += 1
        return it


class Ctx:
    pass


def build_program(stop_after=None, dump=()):
    nc = bass.Bass("TRN2", target_bir_lowering=False)
    C = Ctx()
    C.nc = nc
    C.dump = set(dump)
    C.stop_after = stop_after

    def din(name, shape, dt=F32):
        return nc.dram_tensor(name, list(shape), dt, kind="ExternalInput").ap()

    def dscr(name, shape, dt):
        kind = "ExternalOutput" if name in C.dump else "Internal"
        return nc.dram_tensor(name, list(shape), dt, kind=kind).ap()

    SPEC = {
        "xT0": [D, T],
        "cTp": [128, KD, 2],
        "w_ada": [DEPTH, D, 6 * D],
        "b_adaT": [DEPTH, 128, 96, 2],
        "g1T": [DEPTH, 128, KD, 2],
        "g2T": [DEPTH, 128, KD, 2],
        "gfT": [128, KD, 2],
        "w_in": [DEPTH, D, 3776],
        "w_in_sw": [DEPTH, D, 64],
        "w_out": [DEPTH, D, D],
        "sgu_g": [DEPTH, 1, 512],
        "sgu_wT": [DEPTH, 128, 4, 128],
        "sgu_b4": [DEPTH, 1, 2048],
        "qg": [DEPTH, 128, 3],
        "kvg": [DEPTH, 128, 2],
        "w_uq": [DEPTH, 384, 768],
        "w_uq_sw": [DEPTH, 384, 256],
        "w_ukv": [DEPTH, 256, 1024],
        "rope_cos": [64, T],
        "rope_sin": [64, T],
        "s5_pp": [DEPTH, 128, 3, 32],
        "s5_row": [DEPTH, 3, 4096],
        "s5_Bre": [DEPTH, 128, 4096],
        "s5_Bim": [DEPTH, 128, 4096],
        "s5_Cre": [DEPTH, 128, 4096],
        "s5_Cim": [DEPTH, 128, 4096],
        "s5_d": [DEPTH, 128, 4],
        "s5_tau": [1, 2 * T],
        "w_glu": [DEPTH, 512, 512],
        "b_glu": [DEPTH, 128, 4],
        "conv_wT": [DEPTH, 128, 4, 3],
        "w_router": [DEPTH, D, 16],
        "w_gate": [DEPTH, 16, D, 1024],
        "w_up": [DEPTH, 16, D, 1024],
        "w_down": [DEPTH, 16, 1024, D],
        "ident": [128, 128],
        "iota_j": [1, NJ],
        "iota_p3": [128, 3],
        "sel": [16, 16, 128],
    }

    class LazyIn(dict):
        def __missing__(self, k):
            v = din(k, SPEC[k])
            self[k] = v
            return v
    I = LazyIn()
    C.I = I
    C.outT = nc.dram_tensor("outT", [D, NL], F32, kind="ExternalOutput").ap()

    Sx = {}
    Sx["xA"] = dscr("xA", [D, T], F32)
    Sx["xB"] = dscr("xB", [D, T], F32)
    Sx["uTg"] = dscr("uTg", [512, T], BF16)
    Sx["v_tok"] = dscr("v_tok", [T, 512], BF16)
    Sx["cqT"] = dscr("cqT", [384, T], BF16)
    Sx["ckvT"] = dscr("ckvT", [256, T], BF16)
    Sx["krT"] = dscr("krT", [64, T], BF16)
    Sx["s5uT"] = dscr("s5uT", [512, T], BF16)
    Sx["s5u32"] = dscr("s5u32", [512, T], F32)
    Sx["catT"] = dscr("catT", [D, T], BF16)
    Sx["ys"] = dscr("ys", [16, NJ, D], BF16)
    Sx["dbg_mod"] = dscr("dbg_mod", [DEPTH, 128, 96, 2], F32)
    Sx["dbg_hT"] = dscr("dbg_hT", [D, T], BF16)
    Sx["dbg_aff"] = dscr("dbg_aff", [128, 18, 16], F32)
    Sx["dbg_posm"] = dscr("dbg_posm", [16, T], F32)
    Sx["dbg_g"] = dscr("dbg_g", [512, T], BF16)
    C.Sx = Sx

    with ExitStack() as es:
        S = Sched(nc, es)
        C.S = S
        _consts(C)
        phase_mod(C)
        S.barrier()
        x_in = I["xT0"]
        done = (stop_after == "mod")
        for li in range(DEPTH):
            if done:
                break
            x1 = Sx["xA"]
            x2 = Sx["xB"]
            for ph in (phase_in, phase_sgu, phase_mla, phase_s5, phase_out, phase_moe):
                if ph is phase_in:
                    ph(C, li, x_in)
                elif ph is phase_out:
                    ph(C, li, x_in, x1)
                elif ph is phase_moe:
                    ph(C, li, x1, x2)
                else:
                    ph(C, li)
                S.barrier()
                if stop_after == (li, ph.__name__) or (isinstance(stop_after, tuple) and len(stop_after) == 3 and stop_after[0] == li and ph is phase_in):
                    done = True
                    break
            if done:
                break
            x_in = x2
        if not done:
            phase_final(C, x_in)
        S.barrier()
    nc._used_inputs = set(I.keys())
    return nc


def _consts(C):
    S, nc, I = C.S, C.nc, C.I
    C.ones_bf, C.ones_bf_b = S.sbuf("ones_bf", [128, 128], BF16)
    S.op("pool", lambda: nc.gpsimd.memset(C.ones_bf[:], 1.0), writes=[C.ones_bf_b])
    C.eps_t, C.eps_b = S.sbuf("eps", [128, 1], F32)
    S.op("pool", lambda: nc.gpsimd.memset(C.eps_t[:], EPS), writes=[C.eps_b])
    C.one_f, C.one_f_b = S.sbuf("one_f", [128, 1], F32)
    S.op("pool", lambda: nc.gpsimd.memset(C.one_f[:], 1.0), writes=[C.one_f_b])
    C.hpi_t, C.hpi_b = S.sbuf("hpi", [128, 1], F32)
    S.op("pool", lambda: nc.gpsimd.memset(C.hpi_t[:], math.pi / 2), writes=[C.hpi_b])
    C.ident_f, C.ident_f_b = S.sbuf("ident_f", [128, 128], F32)
    S.dma("sp", C.ident_f[:], I["ident"][:, :], writes=[C.ident_f_b])
    C.ident_bf, C.ident_bf_b = S.sbuf("ident_bf", [128, 128], BF16)
    S.dma("pool", C.ident_bf[:], I["ident"][:, :], writes=[C.ident_bf_b])
    C.mod = []
    C.modalloc = []
    for li in range(DEPTH):
        C.modalloc.append((S.sbuf("mod%d" % li, [128, 96, 2], F32), S.sbuf("A1_%d" % li, [128, KD, 2], F32), S.sbuf("A2_%d" % li, [128, KD, 2], F32)))


def _dbg(C, name, src_ap, reads):
    if name in C.dump:
        C.S.dma("sp", C.Sx[name], src_ap, reads=reads)


def phase_mod(C):
    S, nc, I = C.S, C.nc, C.I
    with ExitStack() as ph:
        sc, scb = S.sbuf("sc", [128, KD, 2], F32, es=ph)
        S.dma("sp", sc[:], I["cTp"][:, :, :], writes=[scb])
        S.op("act", lambda: nc.scalar.activation(out=sc[:], in_=sc[:], func=AF.Silu), reads=[scb], writes=[scb])
        war = Ring(S, ph, "wa", [128, KD, 512], F32, 2)
        mps_, mpsb = S.psum("mps", [128, 512], F32, es=ph)
        mps = mps_[:, 0:192]
        rowr = Ring(S, ph, "mrow", [128, 512], F32, 2, psum=True)
        mrow, mrowb = S.sbuf("mrow_sb", [2, 6 * D], F32, es=ph)
        for li in range(DEPTH):
            nxt = war.next()
            S.dma("sp", nxt[0][:], I["w_ada"][li, :, 0:512].rearrange("(k p) n -> p k n", p=128), writes=[nxt[1]])
            for nb in range(24):
                wa, wab = nxt
                if nb + 1 < 24:
                    nxt = war.next()
                    S.dma("sp", nxt[0][:], I["w_ada"][li, :, (nb + 1) * 512:(nb + 2) * 512].rearrange("(k p) n -> p k n", p=128),
                          writes=[nxt[1]])
                pr_, prb_ = rowr.next()

                def mm():
                    ins = None
                    for k in range(KD):
                        ins = nc.tensor.matmul(pr_[0:2, :], lhsT=sc[:, k, :], rhs=wa[:, k, :], start=(k == 0), stop=(k == KD - 1))
                    return ins
                S.op("pe", mm, reads=[wab, scb], writes=[prb_])
                S.op("act", lambda: nc.scalar.activation(out=mrow[0:2, nb * 512:(nb + 1) * 512], in_=pr_[0:2, :], func=AF.Copy), reads=[prb_], writes=[mrowb])

            def tps():
                ins = None
                for fc in range(96):
                    ins = nc.tensor.transpose(mps_[:, fc * 2:fc * 2 + 2], mrow[0:2, fc * 128:(fc + 1) * 128], C.ident_f[0:2, 0:2])
                return ins
            S.op("pe", tps, reads=[mrowb, C.ident_f_b], writes=[mpsb])
            m = Ctx()
            modt, modb = C.modalloc[li][0]
            bt, btb = S.sbuf("badat", [128, 96, 2], F32, es=ph)
            S.dma("sp", bt[:], I["b_adaT"][li], writes=[btb])
            S.op("dve", lambda: nc.vector.tensor_tensor(modt[:].rearrange("p a b -> p (a b)"), mps_[:, 0:192], bt[:].rearrange("p a b -> p (a b)"), ALU.add),
                 reads=[mpsb, btb], writes=[modb])
            m.t, m.b = modt, modb
            for nm, gname, j in (("A1", "g1T", 1), ("A2", "g2T", 4)):
                gt, gtb = S.sbuf("g" + nm, [128, KD, 2], F32, es=ph)
                S.dma("sp", gt[:], I[gname][li], writes=[gtb])
                at, atb = C.modalloc[li][1 if nm == "A1" else 2]
                S.op("dve", lambda: nc.vector.scalar_tensor_tensor(at[:], modt[:, j * 16:(j + 1) * 16, :], 1.0, gt[:], ALU.add, ALU.mult),
                     reads=[modb, gtb], writes=[atb])
                setattr(m, nm, at)
                setattr(m, nm + "b", atb)
            C.mod.append(m)
            if "dbg_mod" in C.dump:
                S.dma("sp", C.Sx["dbg_mod"][li], modt[:], reads=[modb])


def modsl(m, j):
    return m.t[:, j * 16:(j + 1) * 16, :]


def norm_blocks(C, ph, x_dram, A_ap, A_b, B_ap, B_b, cb, blocks=TB):
    S, nc = C.S, C.nc
    xr = Ring(S, ph, "xblk", [128, KD, 512], F32, 2)
    sqt, sqb = S.sbuf("nsq", [128, KD, 512], BF16, es=ph)
    ssr = Ring(S, ph, "nss", [128, 512], F32, 2, psum=True)
    rs, rsb = S.sbuf("nrstd", [128, 512], F32, es=ph)
    for bi, (t0, n, lc) in enumerate(blocks):
        xb, xbb = xr.next()
        S.dma("sp", xb[:, :, :n], x_dram[:, t0:t0 + n].rearrange("(k p) t -> p k t", p=128), writes=[xbb])
        S.op("act", lambda: nc.scalar.activation(out=sqt[:, :, :n], in_=xb[:, :, :n], func=AF.Square), reads=[xbb], writes=[sqb])
        ss, ssb = ssr.next()

        def mm():
            ins = None
            for k in range(KD):
                ins = nc.tensor.matmul(ss[:, :n], lhsT=C.ones_bf[:, :], rhs=sqt[:, k, :n], start=(k == 0), stop=(k == KD - 1))
            return ins
        S.op("pe", mm, reads=[sqb, C.ones_bf_b], writes=[ssb])
        S.op("act", lambda: nc.scalar.activation(out=rs[:, :n], in_=ss[:, :n], func=AF.Sqrt, bias=C.eps_t[:, 0:1], scale=1.0 / D),
             reads=[ssb, C.eps_b], writes=[rsb])
        S.op("dve", lambda: nc.vector.reciprocal(rs[:, :n], rs[:, :n]), reads=[rsb], writes=[rsb])

        def nrm():
            ins = None
            for k in range(KD):
                ins = nc.vector.tensor_tensor(xb[:, k, :n], xb[:, k, :n], rs[:, :n], ALU.mult)
            return ins
        S.op("dve", nrm, reads=[xbb, rsb], writes=[xbb])

        def aff_act():
            ins = None
            for k in range(0, KD, 2):
                ins = nc.scalar.activation(out=xb[:, k, :n], in_=xb[:, k, :n], func=AF.Identity,
                                           bias=B_ap[:, k, lc:lc + 1], scale=A_ap[:, k, lc:lc + 1])
            return ins

        def aff_pool():
            ins = None
            for k in range(1, KD, 2):
                ins = nc.gpsimd.tensor_scalar(xb[:, k, :n], xb[:, k, :n], A_ap[:, k, lc:lc + 1], B_ap[:, k, lc:lc + 1], ALU.mult, ALU.add)
            return ins
        S.op("act", aff_act, reads=[xbb, A_b, B_b], writes=[xbb])
        S.op("pool", aff_pool, reads=[xbb, A_b, B_b], writes=[xbb])
        cb(bi, t0, n, lc, xb, xbb)


def phase_in(C, li, x_dram):
    S, nc, I, Sx = C.S, C.nc, C.I, C.Sx
    m = C.mod[li]
    with ExitStack() as ph:
        hT, hTb = S.sbuf("hT", [128, KD, T], BF16, es=ph)
        with ExitStack() as ph1:
            def cb(bi, t0, n, lc, xb, xbb):
                S.op("dve", lambda: nc.vector.tensor_copy(hT[:, :, t0:t0 + n], xb[:, :, :n]), reads=[xbb], writes=[hTb])
            norm_blocks(C, ph1, x_dram, m.A1, m.A1b, modsl(m, 0), m.b, cb)
        if "dbg_hT" in C.dump:
            S.dma("sp", Sx["dbg_hT"].rearrange("(k p) t -> p k t", p=128), hT[:], reads=[hTb])
        if C.stop_after == (li, "in", "norm"):
            return
        S.barrier()
        wr = Ring(S, ph, "wt", [128, KD, 512], BF16, 2)
        pr = Ring(S, ph, "pin", [128, 512], F32, 4, psum=True)
        win = I["w_in"][li]

        def wload(segs):
            wt, wtb = wr.next()
            for si, (src, c0, ncol, off) in enumerate(segs):
                S.dma("pool", wt[:, :, off:off + ncol], src[:, c0:c0 + ncol].rearrange("(k p) n -> p k n", p=128),
                      writes=[wtb] if si == 0 else [], awrites=[] if si == 0 else [wtb])
            return wt, wtb

        def proj_fm(wt, wtb, woff, M, t0, n):
            ps, psb = pr.next()

            def mm():
                ins = None
                for k in range(KD):
                    ins = nc.tensor.matmul(ps[:M, :n], lhsT=wt[:, k, woff:woff + M], rhs=hT[:, k, t0:t0 + n], start=(k == 0), stop=(k == KD - 1))
                return ins
            S.op("pe", mm, reads=[wtb, hTb], writes=[psb])
            return ps, psb

        obr = Ring(S, ph, "ob", [128, 512], BF16, 4)
        ofr = Ring(S, ph, "of", [128, 512], F32, 3)

        wt, wtb = wload([(win, 0, 512, 0)])
        for (t0, n, lc) in TB:
            for c in range(4):
                ps, psb = proj_fm(wt, wtb, c * 128, 128, t0, n)
                ob, obb = obr.next()
                S.op("act", lambda: nc.scalar.activation(out=ob[:, :n], in_=ps[:, :n], func=AF.Gelu), reads=[psb], writes=[obb])
                S.dma("sp", Sx["uTg"][c * 128:(c + 1) * 128, t0:t0 + n], ob[:, :n], reads=[obb])

        if C.stop_after == (li, "in", "A"):
            return
        wt, wtb = wload([(win, 512, 512, 0)])
        gs, gsb = S.sbuf("gs", [128, 512], F32, es=ph)
        S.dma("sp", gs[:], I["sgu_g"][li].to_broadcast([128, 512]), writes=[gsb])
        junk, junkb = S.sbuf("junk", [128, 512], BF16, es=ph)
        st, stb = S.sbuf("vst", [128, 2], F32, es=ph)
        for tt in range(18):
            ps, psb = pr.next()

            def mm():
                ins = None
                for k in range(KD):
                    ins = nc.tensor.matmul(ps[:, :], lhsT=hT[:, k, tt * 128:(tt + 1) * 128], rhs=wt[:, k, :], start=(k == 0), stop=(k == KD - 1))
                return ins
            S.op("pe", mm, reads=[wtb, hTb], writes=[psb])
            gv, gvb = ofr.next()
            S.op("act", lambda: nc.scalar.activation(out=gv[:, :], in_=ps[:, :], func=AF.Gelu), reads=[psb], writes=[gvb])
            S.op("act", lambda: nc.scalar.activation(out=junk[:, :], in_=gv[:, :], func=AF.Square, accum_out=st[:, 0:1]),
                 reads=[gvb], writes=[junkb, stb])
            S.op("act", lambda: nc.scalar.activation(out=st[:, 1:2], in_=st[:, 0:1], func=AF.Sqrt, bias=C.eps_t[:, 0:1], scale=1.0 / 512),
                 reads=[stb, C.eps_b], writes=[stb])
            S.op("dve", lambda: nc.vector.reciprocal(st[:, 1:2], st[:, 1:2]), reads=[stb], writes=[stb])
            ob, obb = obr.next()
            S.op("dve", lambda: nc.vector.scalar_tensor_tensor(ob[:, :], gv[:, :], st[:, 1:2], gs[:, :], ALU.mult, ALU.mult),
                 reads=[gvb, stb, gsb], writes=[obb])
            S.dma("sp", Sx["v_tok"][tt * 128:(tt + 1) * 128, :], ob[:, :], reads=[obb])

        if C.stop_after == (li, "in", "v"):
            return
        wt, wtb = wload([(win, 1024, 512, 0)])
        for (t0, n, lc) in TB:
            for c in range(4):
                ps, psb = proj_fm(wt, wtb, c * 128, 128, t0, n)
                ob, obb = obr.next()
                S.op("dve" if c % 2 else "act", (lambda: nc.vector.tensor_copy(ob[:, :n], ps[:, :n])) if c % 2 else
                     (lambda: nc.scalar.activation(out=ob[:, :n], in_=ps[:, :n], func=AF.Copy)), reads=[psb], writes=[obb])
                dst = Sx["cqT"][c * 128:(c + 1) * 128, t0:t0 + n] if c < 3 else Sx["ckvT"][0:128, t0:t0 + n]
                S.dma("sp", dst, ob[:, :n], reads=[obb])

        if C.stop_after == (li, "in", "B"):
            return
        wt, wtb = wload([(win, 1536, 192, 0), (I["w_in_sw"][li], 0, 64, 192)])
        rc, rcb = S.sbuf("ropec", [64, T], F32, es=ph)
        rsn, rsnb = S.sbuf("ropes", [64, T], F32, es=ph)
        S.dma("sp", rc[:], I["rope_cos"][:, :], writes=[rcb])
        S.dma("sp", rsn[:], I["rope_sin"][:, :], writes=[rsnb])
        for (t0, n, lc) in TB:
            ps, psb = proj_fm(wt, wtb, 0, 128, t0, n)
            ob, obb = obr.next()
            S.op("act", lambda: nc.scalar.activation(out=ob[:, :n], in_=ps[:, :n], func=AF.Copy), reads=[psb], writes=[obb])
            S.dma("sp", Sx["ckvT"][128:256, t0:t0 + n], ob[:, :n], reads=[obb])
            ps1, ps1b = proj_fm(wt, wtb, 128, 64, t0, n)
            ps2, ps2b = proj_fm(wt, wtb, 192, 64, t0, n)
            f1, f1b = ofr.next()
            f2, f2b = ofr.next()
            S.op("dve", lambda: nc.vector.tensor_tensor(f1[:64, :n], ps1[:64, :n], rc[:, t0:t0 + n], ALU.mult), reads=[ps1b, rcb], writes=[f1b])
            S.op("dve", lambda: nc.vector.tensor_tensor(f2[:64, :n], ps2[:64, :n], rsn[:, t0:t0 + n], ALU.mult), reads=[ps2b, rsnb], writes=[f2b])
            ob, obb = obr.next()
            S.op("pool", lambda: nc.gpsimd.tensor_tensor(ob[:64, :n], f1[:64, :n], f2[:64, :n], ALU.add), reads=[f1b, f2b], writes=[obb])
            S.dma("sp", Sx["krT"][:, t0:t0 + n], ob[:64, :n], reads=[obb])

        if C.stop_after == (li, "in", "C"):
            return
        wt, wtb = wload([(win, 1728, 512, 0)])
        for (t0, n, lc) in TB:
            for c in range(4):
                ps, psb = proj_fm(wt, wtb, c * 128, 128, t0, n)
                of, ofb = ofr.next()
                ob, obb = obr.next()
                S.op("dve", lambda: nc.vector.tensor_copy(of[:, :n], ps[:, :n]), reads=[psb], writes=[ofb])
                S.op("act", lambda: nc.scalar.activation(out=ob[:, :n], in_=of[:, :n], func=AF.Copy), reads=[ofb], writes=[obb])
                S.dma("sp", Sx["s5uT"][c * 128:(c + 1) * 128, t0:t0 + n], ob[:, :n], reads=[obb])
                S.dma("sp", Sx["s5u32"][c * 128:(c + 1) * 128, t0:t0 + n], of[:, :n], reads=[ofb])

        if C.stop_after == (li, "in", "D"):
            return
        cw, cwb = S.sbuf("convw", [128, 4, 3], F32, es=ph)
        S.dma("sp", cw[:], I["conv_wT"][li], writes=[cwb])
        ZW = 2307
        zr = Ring(S, ph, "zbuf", [128, ZW], F32, 2)
        yr = Ring(S, ph, "ybuf", [128, ZW], F32, 2)
        bgr = Ring(S, ph, "bgbuf", [128, T], BF16, 2)
        cor = Ring(S, ph, "cobuf", [128, T], BF16, 2)

        def zoff(t0):
            return t0 + 1 if t0 < NL else t0 + 2
        for c in range(4):
            wt, wtb = wload([(win, 2240 + c * 128, 128, 0), (win, 2752 + c * 128, 128, 128), (win, 3264 + c * 128, 128, 256)])
            zb, zbb = zr.next()
            yb, ybb = yr.next()
            bg, bgb = bgr.next()
            co, cob = cor.next()

            def zz():
                nc.gpsimd.memset(zb[:, 0:1], 0.0)
                nc.gpsimd.memset(zb[:, NL + 1:NL + 2], 0.0)
                return nc.gpsimd.memset(zb[:, ZW - 1:ZW], 0.0)
            S.op("pool", zz, writes=[zbb])
            for (t0, n, lc) in TB:
                psB, psBb = proj_fm(wt, wtb, 0, 128, t0, n)
                psC, psCb = proj_fm(wt, wtb, 128, 128, t0, n)
                psH, psHb = proj_fm(wt, wtb, 256, 128, t0, n)
                S.op("act", lambda: nc.scalar.activation(out=bg[:, t0:t0 + n], in_=psB[:, :n], func=AF.Copy), reads=[psBb], writes=[bgb])
                of, ofb = ofr.next()
                S.op("act", lambda: nc.scalar.activation(out=of[:, :n], in_=psC[:, :n], func=AF.Copy), reads=[psCb], writes=[ofb])
                zo = zoff(t0)
                S.op("dve", lambda: nc.vector.tensor_tensor(zb[:, zo:zo + n], psH[:, :n], of[:, :n], ALU.mult), reads=[psHb, ofb], writes=[zbb])

            S.seq("dve", [
                lambda: nc.vector.tensor_scalar(yb[:, 1:ZW - 1], zb[:, 1:ZW - 1], cw[:, c, 1:2], None, ALU.mult),
                lambda: nc.vector.scalar_tensor_tensor(yb[:, 1:ZW - 1], zb[:, 0:ZW - 2], cw[:, c, 0:1], yb[:, 1:ZW - 1], ALU.mult, ALU.add),
                lambda: nc.vector.scalar_tensor_tensor(yb[:, 1:ZW - 1], zb[:, 2:ZW], cw[:, c, 2:3], yb[:, 1:ZW - 1], ALU.mult, ALU.add),
            ], reads=[zbb, cwb], writes=[ybb])

            def gate():
                nc.gpsimd.tensor_tensor(co[:, 0:NL], yb[:, 1:NL + 1], bg[:, 0:NL], ALU.mult)
                return nc.gpsimd.tensor_tensor(co[:, NL:T], yb[:, NL + 2:ZW - 1], bg[:, NL:T], ALU.mult)
            S.op("pool", gate, reads=[ybb, bgb], writes=[cob])
            S.dma("sp", Sx["catT"][1536 + c * 128:1536 + (c + 1) * 128, :], co[:, :], reads=[cob])


def phase_sgu(C, li):
    S, nc, I, Sx = C.S, C.nc, C.I, C.Sx
    with ExitStack() as ph:
        ws, wsb = S.sbuf("wsT", [128, 4, 128], BF16, es=ph)
        S.dma("pool", ws[:], I["sgu_wT"][li], writes=[wsb])
        bsr, bsrb = S.sbuf("bsr", [128, 4, 512], F32, es=ph)
        S.dma("sp", bsr[:].rearrange("p a b -> p (a b)"), I["sgu_b4"][li].to_broadcast([128, 2048]), writes=[bsrb])
        vr = Ring(S, ph, "vt", [128, 4, 512], BF16, 2)
        ur = Ring(S, ph, "ut", [128, 4, 512], BF16, 2)
        pr = Ring(S, ph, "psg", [128, 512], F32, 4, psum=True)
        tr = Ring(S, ph, "tmp", [128, 512], F32, 3)
        orr = Ring(S, ph, "osg", [128, 512], BF16, 3)
        for (t0, n, lc) in TB:
            na = n // 128
            vt, vtb = vr.next()
            ut, utb = ur.next()
            S.dma("sp", vt[:, :na, :], Sx["v_tok"][t0:t0 + n, :].rearrange("(a q) c -> q a c", q=128), writes=[vtb])
            S.dma("sp", ut[:, :, :n], Sx["uTg"][:, t0:t0 + n].rearrange("(h c) t -> c h t", c=128), writes=[utb])
            for h in range(4):
                ps, psb = pr.next()

                def mm():
                    ins = None
                    for a in range(na):
                        ins = nc.tensor.matmul(ps[:, a * 128:(a + 1) * 128], lhsT=vt[:, a, h * 128:(h + 1) * 128], rhs=ws[:, h, :], start=True, stop=True)
                    return ins
                S.op("pe", mm, reads=[vtb, wsb], writes=[psb])
                tm, tmb = tr.next()
                S.op("dve", lambda: nc.vector.tensor_tensor(tm[:, :n], ps[:, :n], bsr[:, h, :n], ALU.add), reads=[psb, bsrb], writes=[tmb])
                ob, obb = orr.next()
                S.op("pool", lambda: nc.gpsimd.tensor_tensor(ob[:, :n], tm[:, :n], ut[:, h, :n], ALU.mult), reads=[tmb, utb], writes=[obb])
                S.dma("sp", Sx["catT"][h * 128:(h + 1) * 128, t0:t0 + n], ob[:, :n], reads=[obb])


def phase_mla(C, li):
    S, nc, I, Sx = C.S, C.nc, C.I, C.Sx
    SC = 192.0 ** -0.5
    with ExitStack() as ph:
        cq, cqb = S.sbuf("cq", [128, 3, T], BF16, es=ph)
        ckv, ckvb = S.sbuf("ckv", [128, 2, T], BF16, es=ph)
        kr, krb = S.sbuf("kr", [64, T], BF16, es=ph)
        S.dma("sp", cq[:], Sx["cqT"].rearrange("(c p) t -> p c t", p=128), writes=[cqb])
        S.dma("sp", ckv[:], Sx["ckvT"].rearrange("(c p) t -> p c t", p=128), writes=[ckvb])
        S.dma("sp", kr[:], Sx["krT"][:, :], writes=[krb])
        wuq, wuqb = S.sbuf("wuq", [128, 3, 768], BF16, es=ph)
        wuqs, wuqsb = S.sbuf("wuqs", [128, 3, 256], BF16, es=ph)
        wukv, wukvb = S.sbuf("wukv", [128, 2, 1024], BF16, es=ph)
        wv, wvb = S.sbuf("wv", [128, 2, 512], BF16, es=ph)
        rq, rqb = S.sbuf("rq", [128, T], F32, es=ph)
        rk, rkb = S.sbuf("rk", [128, T], F32, es=ph)
        rkt, rktb = S.sbuf("rkt", [128, 18], F32, es=ph)
        pr = Ring(S, ph, "pj", [128, 512], F32, 3, psum=True)
        pst_, pstb = S.psum("pst", [128, 512], F32, es=ph)
        pst = pst_[:, 0:18]
        with ExitStack() as wp:
            w32, w32b = S.sbuf("w32", [128, 3, 768], F32, es=wp)
            ws32, ws32b = S.sbuf("ws32", [128, 3, 256], F32, es=wp)
            wk32, wk32b = S.sbuf("wk32", [128, 2, 1024], F32, es=wp)
            qg, qgb = S.sbuf("qg", [128, 3], F32, es=wp)
            kg, kgb = S.sbuf("kg", [128, 2], F32, es=wp)
            S.dma("sp", w32[:], I["w_uq"][li].rearrange("(c p) n -> p c n", p=128), writes=[w32b])
            S.dma("sp", ws32[:], I["w_uq_sw"][li].rearrange("(c p) n -> p c n", p=128), writes=[ws32b])
            S.dma("sp", wk32[:], I["w_ukv"][li].rearrange("(c p) n -> p c n", p=128), writes=[wk32b])
            S.dma("sp", qg[:], I["qg"][li], writes=[qgb])
            S.dma("sp", kg[:], I["kvg"][li], writes=[kgb])

            def sc1():
                ins = None
                for c in range(3):
                    nc.vector.tensor_scalar(wuq[:, c, :], w32[:, c, :], qg[:, c:c + 1], None, ALU.mult)
                    ins = nc.vector.tensor_scalar(wuqs[:, c, :], ws32[:, c, :], qg[:, c:c + 1], None, ALU.mult)
                return ins
            S.op("dve", sc1, reads=[w32b, ws32b, qgb], writes=[wuqb, wuqsb])

            def sc2():
                ins = None
                for c in range(2):
                    ins = nc.vector.tensor_scalar(wukv[:, c, :], wk32[:, c, :], kg[:, c:c + 1], None, ALU.mult)
                return ins
            S.op("dve", sc2, reads=[wk32b, kgb], writes=[wukvb])
            S.op("dve", lambda: nc.vector.tensor_copy(wv[:].rearrange("p c (h x) -> p c h x", x=128),
                                                       wukv[:].rearrange("p c (h x) -> p c h x", x=256)[:, :, :, 128:256]),
                 reads=[wukvb], writes=[wvb])
            sqq, sqqb = S.sbuf("sqq", [128, 3, T], BF16, es=wp)
            sqk, sqkb = S.sbuf("sqk", [128, 2, T], BF16, es=wp)
            S.op("act", lambda: nc.scalar.activation(out=sqq[:], in_=cq[:], func=AF.Square), reads=[cqb], writes=[sqqb])
            S.op("act", lambda: nc.scalar.activation(out=sqk[:], in_=ckv[:], func=AF.Square), reads=[ckvb], writes=[sqkb])
            for (sq, sqb_, nch, rt, rtb) in ((sqq, sqqb, 3, rq, rqb), (sqk, sqkb, 2, rk, rkb)):
                for (t0, n, lc) in TB:
                    ps, psb = pr.next()

                    def mm():
                        ins = None
                        for c in range(nch):
                            ins = nc.tensor.matmul(ps[:, :n], lhsT=C.ones_bf[:, :], rhs=sq[:, c, t0:t0 + n], start=(c == 0), stop=(c == nch - 1))
                        return ins
                    S.op("pe", mm, reads=[sqb_, C.ones_bf_b], writes=[psb])
                    S.op("act", lambda: nc.scalar.activation(out=rt[:, t0:t0 + n], in_=ps[:, :n], func=AF.Sqrt, bias=C.eps_t[:, 0:1],
                                                             scale=1.0 / (128 * nch)), reads=[psb, C.eps_b], writes=[rtb])
            S.op("dve", lambda: nc.vector.reciprocal(rq[:], rq[:]), reads=[rqb], writes=[rqb])
            S.op("dve", lambda: nc.vector.reciprocal(rk[:], rk[:]), reads=[rkb], writes=[rkb])

            def mmt():
                ins = None
                for tt in range(18):
                    for c in range(2):
                        ins = nc.tensor.matmul(pst[:, tt:tt + 1], lhsT=sqk[:, c, tt * 128:(tt + 1) * 128], rhs=C.ones_bf[:, 0:1],
                                               start=(c == 0), stop=(c == 1))
                return ins
            S.op("pe", mmt, reads=[sqkb, C.ones_bf_b], writes=[pstb])
            S.op("act", lambda: nc.scalar.activation(out=rkt[:], in_=pst_[:, 0:18], func=AF.Sqrt, bias=C.eps_t[:, 0:1], scale=1.0 / 256),
                 reads=[pstb, C.eps_b], writes=[rktb])
            S.op("dve", lambda: nc.vector.reciprocal(rkt[:], rkt[:]), reads=[rktb], writes=[rktb])
            S.barrier()
        rc, rcb = S.sbuf("ropec", [64, T], F32, es=ph)
        rsn, rsnb = S.sbuf("ropes", [64, T], F32, es=ph)
        S.dma("sp", rc[:], I["rope_cos"][:, :], writes=[rcb])
        S.dma("sp", rsn[:], I["rope_sin"][:, :], writes=[rsnb])
        qn, qnb = S.sbuf("qn", [128, 4, T], BF16, es=ph)
        qr, qrb = S.sbuf("qr", [64, 4, T], BF16, es=ph)
        kn, knb = S.sbuf("kn", [128, 4, T], BF16, es=ph)
        vtok, vtokb = S.sbuf("vtok", [128, 18, 512], BF16, es=ph)
        fr = Ring(S, ph, "mf", [128, 512], F32, 4)

        def proj(wt, wtb, nch, c0, M, src, srcb, t0, n):
            ps, psb = pr.next()

            def mm():
                ins = None
                for c in range(nch):
                    ins = nc.tensor.matmul(ps[:M, :n], lhsT=wt[:, c, c0:c0 + M], rhs=src[:, c, t0:t0 + n], start=(c == 0), stop=(c == nch - 1))
                return ins
            S.op("pe", mm, reads=[wtb, srcb], writes=[psb])
            return ps, psb
        for h in range(4):
            for (t0, n, lc) in TB:
                ps, psb = proj(wuq, wuqb, 3, h * 192, 128, cq, cqb, t0, n)
                S.op("dve", lambda: nc.vector.scalar_tensor_tensor(qn[:, h, t0:t0 + n], ps[:, :n], SC, rq[:, t0:t0 + n], ALU.mult, ALU.mult),
                     reads=[psb, rqb], writes=[qnb])
                ps1, ps1b = proj(wuq, wuqb, 3, h * 192 + 128, 64, cq, cqb, t0, n)
                ps2, ps2b = proj(wuqs, wuqsb, 3, h * 64, 64, cq, cqb, t0, n)
                f1, f1b = fr.next()
                f2, f2b = fr.next()
                S.op("dve", lambda: nc.vector.tensor_tensor(f1[:64, :n], ps1[:64, :n], rc[:, t0:t0 + n], ALU.mult), reads=[ps1b, rcb], writes=[f1b])
                S.op("dve", lambda: nc.vector.tensor_tensor(f2[:64, :n], ps2[:64, :n], rsn[:, t0:t0 + n], ALU.mult), reads=[ps2b, rsnb], writes=[f2b])
                S.op("pool", lambda: nc.gpsimd.tensor_tensor(f1[:64, :n], f1[:64, :n], f2[:64, :n], ALU.add), reads=[f1b, f2b], writes=[f1b])
                S.op("dve", lambda: nc.vector.scalar_tensor_tensor(qr[:, h, t0:t0 + n], f1[:64, :n], SC, rq[:64, t0:t0 + n], ALU.mult, ALU.mult),
                     reads=[f1b, rqb], writes=[qrb])
                ps, psb = proj(wukv, wukvb, 2, h * 256, 128, ckv, ckvb, t0, n)
                S.op("dve", lambda: nc.vector.tensor_tensor(kn[:, h, t0:t0 + n], ps[:, :n], rk[:, t0:t0 + n], ALU.mult), reads=[psb, rkb], writes=[knb])
        for tt in range(18):
            ps, psb = pr.next()

            def mm():
                ins = None
                for c in range(2):
                    ins = nc.tensor.matmul(ps[:, :], lhsT=ckv[:, c, tt * 128:(tt + 1) * 128], rhs=wv[:, c, :], start=(c == 0), stop=(c == 1))
                return ins
            S.op("pe", mm, reads=[ckvb, wvb], writes=[psb])
            S.op("dve", lambda: nc.vector.tensor_scalar(vtok[:, tt, :], ps[:, :], rkt[:, tt:tt + 1], None, ALU.mult), reads=[psb, rktb], writes=[vtokb])
        opr = Ring(S, ph, "ops", [128, 512], F32, 2, psum=True)
        spr = Ring(S, ph, "sps", [128, 512], F32, 2, psum=True)
        ptr = Ring(S, ph, "pt", [128, 512], BF16, 3)
        rsr = Ring(S, ph, "rsm", [128, 512], F32, 2)
        atr = Ring(S, ph, "att", [128, 512], BF16, 2)
        for h in range(4):
            for (t0, n, lc) in TB:
                kts = list(range(18)) if lc == 0 else [16, 17]
                ops, opsb = opr.next()
                sps, spsb = spr.next()
                pend = None
                nk = len(kts)

                def pv(pd):
                    kt_, pt_, ptb_, i_ = pd

                    def mmo():
                        nc.tensor.matmul(ops[:, :n], lhsT=vtok[:, kt_, h * 128:(h + 1) * 128], rhs=pt_[:, :n], start=(i_ == 0), stop=(i_ == nk - 1))
                        return nc.tensor.matmul(sps[:, :n], lhsT=C.ones_bf[:, :], rhs=pt_[:, :n], start=(i_ == 0), stop=(i_ == nk - 1))
                    S.op("pe", mmo, reads=[vtokb, ptb_, C.ones_bf_b], writes=[opsb, spsb])
                for i, kt in enumerate(kts):
                    st, stb = pr.next()

                    def mms():
                        nc.tensor.matmul(st[:, :n], lhsT=kn[:, h, kt * 128:(kt + 1) * 128], rhs=qn[:, h, t0:t0 + n], start=True, stop=False)
                        return nc.tensor.matmul(st[:, :n], lhsT=kr[:, kt * 128:(kt + 1) * 128], rhs=qr[:, h, t0:t0 + n], start=False, stop=True)
                    S.op("pe", mms, reads=[knb, qnb, krb, qrb], writes=[stb])
                    pt, ptb = ptr.next()
                    S.op("act", lambda: nc.scalar.activation(out=pt[:, :n], in_=st[:, :n], func=AF.Exp), reads=[stb], writes=[ptb])
                    if pend is not None:
                        pv(pend)
                    pend = (kt, pt, ptb, i)
                pv(pend)
                rs_, rsb_ = rsr.next()
                S.op("dve", lambda: nc.vector.reciprocal(rs_[:, :n], sps[:, :n]), reads=[spsb], writes=[rsb_])
                at, atb = atr.next()
                S.op("dve", lambda: nc.vector.tensor_tensor(at[:, :n], ops[:, :n], rs_[:, :n], ALU.mult), reads=[opsb, rsb_], writes=[atb])
                S.dma("sp", Sx["catT"][512 + h * 128:512 + (h + 1) * 128, t0:t0 + n], at[:, :n], reads=[atb])


def _s5_disc(C, es, are, aim, ldt, n, need_coef, tagb):
    S, nc = C.S, C.nc
    X = [S.sbuf("s5x%d" % i, [128, n], F32, es=es) for i in range(6)]
    KI = S.sbuf("s5ki", [128, n], I32, es=es)
    (x1, b1), (x2, b2), (x3, b3), (x4, b4), (x5, b5), (x6, b6) = X
    ki, kib = KI
    S.op("dve", lambda: nc.vector.tensor_scalar(are, are, -1e-4, None, ALU.min), reads=[tagb], writes=[tagb])
    ea = [
        lambda: nc.vector.tensor_scalar(ki[:], ldt, 1.0 / math.log(2.0), None, ALU.mult),
        lambda: nc.vector.tensor_copy(x3[:], ki[:]),
        lambda: nc.vector.scalar_tensor_tensor(x4[:], x3[:], -0.693145751953125, ldt, ALU.mult, ALU.add),
        lambda: nc.vector.scalar_tensor_tensor(x4[:], x3[:], -1.42860682030941723212e-6, x4[:], ALU.mult, ALU.add),
        lambda: nc.vector.tensor_scalar(x5[:], x4[:], 1.0 / 9.0, 1.0, ALU.mult, ALU.add),
    ]
    for j in range(8, 0, -1):
        ea.append(lambda: nc.vector.tensor_tensor(x5[:], x5[:], x4[:], ALU.mult))
        ea.append(lambda j=j: nc.vector.tensor_scalar(x5[:], x5[:], 1.0 / j, 1.0, ALU.mult, ALU.add))
    ea.append(lambda: nc.vector.tensor_scalar(ki[:], x3[:], 127.0, 8388608.0, ALU.add, ALU.mult))
    ea.append(lambda: nc.vector.tensor_tensor(ldt, x5[:], ki[:].bitcast(F32), ALU.mult))
    S.seq("dve", ea, reads=[tagb], writes=[tagb, kib, b3, b4, b5])
    S.op("dve", lambda: nc.vector.tensor_tensor(x1[:], are, ldt, ALU.mult), reads=[tagb], writes=[b1])
    S.op("act", lambda: nc.scalar.activation(out=x1[:], in_=x1[:], func=AF.Exp), reads=[b1], writes=[b1])
    S.op("dve", lambda: nc.vector.tensor_tensor(x2[:], aim, ldt, ALU.mult), reads=[tagb], writes=[b2])
    S.op("dve", lambda: nc.vector.tensor_scalar(x2[:], x2[:], 1.0 / (2 * math.pi), None, ALU.mult), reads=[b2], writes=[b2])
    if not need_coef:
        return {"r": (x1, b1), "f": (x2, b2)}
    S.op("dve", lambda: nc.vector.tensor_copy(ki[:], x2[:]), reads=[b2], writes=[kib])
    S.op("dve", lambda: nc.vector.tensor_tensor(x3[:], x2[:], ki[:], ALU.subtract), reads=[b2, kib], writes=[b3])
    S.op("act", lambda: nc.scalar.activation(out=x4[:], in_=x3[:], func=AF.Sin, scale=TWO_PI), reads=[b3], writes=[b4])
    S.op("dve", lambda: nc.vector.tensor_scalar(ki[:], x2[:], 0.25, None, ALU.add), reads=[b2], writes=[kib])
    S.op("dve", lambda: nc.vector.tensor_tensor(x3[:], x2[:], ki[:], ALU.subtract), reads=[b2, kib], writes=[b3])
    S.op("act", lambda: nc.scalar.activation(out=x5[:], in_=x3[:], func=AF.Sin, scale=TWO_PI, bias=C.hpi_t[:, 0:1]), reads=[b3, C.hpi_b], writes=[b5])
    S.op("dve", lambda: nc.vector.tensor_tensor(x5[:], x1[:], x5[:], ALU.mult), reads=[b1, b5], writes=[b5])
    S.op("dve", lambda: nc.vector.tensor_scalar(x5[:], x5[:], -1.0, None, ALU.add), reads=[b5], writes=[b5])
    S.op("dve", lambda: nc.vector.tensor_tensor(x4[:], x1[:], x4[:], ALU.mult), reads=[b1, b4], writes=[b4])
    S.op("dve", lambda: nc.vector.tensor_tensor(x1[:], are, are, ALU.mult), reads=[tagb], writes=[b1])
    S.op("dve", lambda: nc.vector.tensor_tensor(x3[:], aim, aim, ALU.mult), reads=[tagb], writes=[b3])
    S.op("dve", lambda: nc.vector.tensor_tensor(x1[:], x1[:], x3[:], ALU.add), reads=[b1, b3], writes=[b1])
    S.op("dve", lambda: nc.vector.reciprocal(x1[:], x1[:]), reads=[b1], writes=[b1])
    S.op("dve", lambda: nc.vector.tensor_tensor(x2[:], x5[:], are, ALU.mult), reads=[b5, tagb], writes=[b2])
    S.op("dve", lambda: nc.vector.tensor_tensor(x3[:], x4[:], aim, ALU.mult), reads=[b4, tagb], writes=[b3])
    S.op("dve", lambda: nc.vector.tensor_tensor(x2[:], x2[:], x3[:], ALU.add), reads=[b2, b3], writes=[b2])
    S.op("dve", lambda: nc.vector.tensor_tensor(x2[:], x2[:], x1[:], ALU.mult), reads=[b2, b1], writes=[b2])
    S.op("dve", lambda: nc.vector.tensor_tensor(x6[:], x4[:], are, ALU.mult), reads=[b4, tagb], writes=[b6])
    S.op("dve", lambda: nc.vector.tensor_tensor(x3[:], x5[:], aim, ALU.mult), reads=[b5, tagb], writes=[b3])
    S.op("dve", lambda: nc.vector.tensor_tensor(x6[:], x6[:], x3[:], ALU.subtract), reads=[b6, b3], writes=[b6])
    S.op("dve", lambda: nc.vector.tensor_tensor(x6[:], x6[:], x1[:], ALU.mult), reads=[b6, b1], writes=[b6])
    return {"cre": (x2, b2), "cim": (x6, b6), "t": [(x1, b1), (x3, b3), (x4, b4), (x5, b5)]}


def phase_s5(C, li):
    S, nc, I, Sx = C.S, C.nc, C.I, C.Sx
    with ExitStack() as ph:
        BbR, BbRb = S.sbuf("BbR", [128, 32, 128], BF16, es=ph)
        BbI, BbIb = S.sbuf("BbI", [128, 32, 128], BF16, es=ph)
        CR, CRb = S.sbuf("CR", [128, 32, 128], BF16, es=ph)
        CIn, CInb = S.sbuf("CIn", [128, 32, 128], BF16, es=ph)
        CRn, CRnb = S.sbuf("CRn", [128, 32, 128], BF16, es=ph)
        pp, ppb = S.sbuf("s5pp", [128, 3, 32], F32, es=ph)
        S.dma("sp", pp[:], I["s5_pp"][li], writes=[ppb])
        dpp = _s5_disc(C, ph, pp[:, 0, :], pp[:, 1, :], pp[:, 2, :], 32, False, ppb)
        rpp, rppb = dpp["r"]
        fpp, fppb = dpp["f"]
        with ExitStack() as wp:
            pp2, pp2b = S.sbuf("s5pp2", [128, 3, 32], F32, es=wp)
            S.dma("sp", pp2[:], I["s5_pp"][li], writes=[pp2b])
            dc = _s5_disc(C, wp, pp2[:, 0, :], pp2[:, 1, :], pp2[:, 2, :], 32, True, pp2b)
            cre, creb = dc["cre"]
            cim, cimb = dc["cim"]
            onesf, onesfb = S.sbuf("onesf", [128, 128], F32, es=wp)
            S.op("pool", lambda: nc.gpsimd.memset(onesf[:], 1.0), writes=[onesfb])
            Bre, Breb = S.sbuf("Bre32", [128, 4096], F32, es=wp)
            Bim, Bimb = S.sbuf("Bim32", [128, 4096], F32, es=wp)
            Cre, Creb = S.sbuf("Cre32", [128, 4096], F32, es=wp)
            Cim, Cimb = S.sbuf("Cim32", [128, 4096], F32, es=wp)
            S.dma("sp", Bre[:], I["s5_Bre"][li], writes=[Breb])
            S.dma("sp", Bim[:], I["s5_Bim"][li], writes=[Bimb])
            S.dma("sp", Cre[:], I["s5_Cre"][li], writes=[Creb])
            S.dma("sp", Cim[:], I["s5_Cim"][li], writes=[Cimb])
            S.op("act", lambda: nc.scalar.activation(out=CR[:].rearrange("p a b -> p (a b)"), in_=Cre[:], func=AF.Copy), reads=[Creb], writes=[CRb])
            S.op("act", lambda: nc.scalar.activation(out=CIn[:].rearrange("p a b -> p (a b)"), in_=Cim[:], func=AF.Copy, scale=-1.0), reads=[Cimb], writes=[CInb])
            S.op("act", lambda: nc.scalar.activation(out=CRn[:].rearrange("p a b -> p (a b)"), in_=Cre[:], func=AF.Copy, scale=-1.0), reads=[Creb], writes=[CRnb])
            dgr = Ring(S, wp, "dg", [128, 4, 128], F32, 4)
            rpr = Ring(S, wp, "rp", [128, 512], F32, 4, psum=True)
            t3r = Ring(S, wp, "s5p3", [128, 512], F32, 2)
            t4r = Ring(S, wp, "s5p4", [128, 512], F32, 2)
            for g4 in range(8):
                reps = []
                for (v, vb) in ((cre, creb), (cim, cimb)):
                    dg, dgb = dgr.next()

                    def mkd():
                        ins = None
                        for j in range(4):
                            ins = nc.vector.tensor_scalar(dg[:, j, :], C.ident_f[:, :], v[:, g4 * 4 + j:g4 * 4 + j + 1], None, ALU.mult)
                        return ins
                    S.op("dve", mkd, reads=[vb, C.ident_f_b], writes=[dgb])
                    rp, rpb = rpr.next()

                    def mmr():
                        ins = None
                        for j in range(4):
                            ins = nc.tensor.matmul(rp[:, j * 128:(j + 1) * 128], lhsT=onesf[:, :], rhs=dg[:, j, :], start=True, stop=True)
                        return ins
                    S.op("pe", mmr, reads=[onesfb, dgb], writes=[rpb])
                    reps.append((rp, rpb))
                (rcr, rcrb), (rci, rcib) = reps
                sl = slice(g4 * 512, (g4 + 1) * 512)
                osl = slice(g4 * 4, (g4 + 1) * 4)
                t3, t3b = t3r.next()
                t4, t4b = t4r.next()
                S.op("dve", lambda: nc.vector.tensor_tensor(t3[:], rcr[:, :], Bre[:, sl], ALU.mult), reads=[rcrb, Breb], writes=[t3b])
                S.op("dve", lambda: nc.vector.tensor_tensor(t4[:], rci[:, :], Bim[:, sl], ALU.mult), reads=[rcib, Bimb], writes=[t4b])
                S.op("dve", lambda: nc.vector.tensor_tensor(BbR[:, osl, :].rearrange("p a b -> p (a b)"), t3[:], t4[:], ALU.subtract),
                     reads=[t3b, t4b], writes=[BbRb])
                t3, t3b = t3r.next()
                t4, t4b = t4r.next()
                S.op("dve", lambda: nc.vector.tensor_tensor(t3[:], rcr[:, :], Bim[:, sl], ALU.mult), reads=[rcrb, Bimb], writes=[t3b])
                S.op("dve", lambda: nc.vector.tensor_tensor(t4[:], rci[:, :], Bre[:, sl], ALU.mult), reads=[rcib, Breb], writes=[t4b])
                S.op("dve", lambda: nc.vector.tensor_tensor(BbI[:, osl, :].rearrange("p a b -> p (a b)"), t3[:], t4[:], ALU.add),
                     reads=[t3b, t4b], writes=[BbIb])
            S.barrier()
        TA = 1536
        mst = ExitStack()
        ubr = Ring(S, mst, "ubf", [128, T], BF16, 2)
        tau, taub = S.sbuf("tau", [128, 2, T], F32, es=mst)
        S.dma("sp", tau[:].rearrange("p a b -> p (a b)"), I["s5_tau"][0:1, :].to_broadcast([128, 2 * T]), writes=[taub])

        def two(name, dt):
            t, ba = S.sbuf(name, [128, T], dt, es=mst)
            bb = Buf(name + "B")
            S.bufs.append(bb)
            return t, (ba, bb)
        tabr = [(two("cosT%d" % i, F32), two("sinT%d" % i, F32)) for i in range(2)]
        kis, kisb = S.sbuf("kis", [128, T], I32, es=mst)
        evr, evrb = two("evr", F32)
        evi, evib = two("evi", F32)
        bre, breb = two("bre", F32)
        bim, bimb = two("bim", F32)
        sre, sreb = two("sre", F32)
        sim, simb = two("sim", F32)
        pa, pab = two("pa", BF16)
        pb, pbb = two("pb", BF16)
        pc, pcb = two("pc", BF16)
        pd, pdb = two("pd", BF16)
        dsk, dskb = S.sbuf("dsk", [128, 4], F32, es=mst)
        S.dma("sp", dsk[:], I["s5_d"][li], writes=[dskb])
        yps = [S.psum("yps%d" % i, [128, 512], F32, es=mst) for i in range(5)]
        bur = Ring(S, mst, "bu", [128, 512], F32, 3, psum=True)
        tr = Ring(S, mst, "s5t", [128, 512], F32, 4)
        gor = Ring(S, mst, "s5g", [128, 512], BF16, 3)
        iters = [(ct, d, ns) for ct in range(4) for d in range(2) for ns in range(4)]

        def ew(o, ob, x, xb, y, yb, op):
            S.op("dve", lambda: nc.vector.tensor_tensor(o[:], x[:], y[:], op), reads=list(xb) + list(yb), writes=list(ob))

        def colof(it):
            ct, d, ns = it
            return (d * 4 + ct) * 4 + ns
        tabs = {}

        def tab_pool(i):
            pass

        def tab_rest(i):
            ct, d, ns = iters[i]
            fcol = fpp[:, colof(iters[i]):colof(iters[i]) + 1]
            (cosT, cosb), (sinT, sinb) = tabr[i % 2]
            S.op("dve", lambda: nc.vector.tensor_scalar(kis[:], tau[:, d, :], fcol, None, ALU.mult), reads=[taub, fppb], writes=[kisb])
            S.op("dve", lambda: nc.vector.scalar_tensor_tensor(sinT[:], tau[:, d, :], fcol, kis[:], ALU.mult, ALU.subtract),
                 reads=[taub, fppb, kisb], writes=list(sinb))
            S.op("act", lambda: nc.scalar.activation(out=cosT[:], in_=sinT[:], func=AF.Abs), reads=list(sinb), writes=list(cosb))
            S.op("act", lambda: nc.scalar.activation(out=sinT[:], in_=sinT[:], func=AF.Sin, scale=TWO_PI), reads=list(sinb) + list(cosb), writes=list(sinb))
            S.op("act", lambda: nc.scalar.activation(out=cosT[:], in_=cosT[:], func=AF.Sin, scale=-TWO_PI, bias=C.hpi_t[:, 0:1]),
                 reads=list(cosb) + [C.hpi_b], writes=list(cosb))
            tabs[i] = (cosT, cosb, sinT, sinb)
        tab_pool(0)
        tab_rest(0)
        ub_next = ubr.next()
        S.dma("sp", ub_next[0][:], Sx["s5uT"][0:128, :], writes=[ub_next[1]])
        for idx, (ct, d, ns) in enumerate(iters):
            col = colof((ct, d, ns))
            if d == 0 and ns == 0:
                ubf, ubfb = ub_next
                if ct < 3:
                    ub_next = ubr.next()
                    S.dma("sp", ub_next[0][:], Sx["s5uT"][(ct + 1) * 128:(ct + 2) * 128, :], writes=[ub_next[1]])
            cosT, cosb, sinT, sinb = tabs.pop(idx)
            if idx + 1 < len(iters):
                tab_pool(idx + 1)
            for (t0, n, lc) in TB:
                part = 0 if t0 < TA else 1
                pr_, prb_ = bur.next()
                pi_, pib_ = bur.next()
                S.op("pe", lambda: nc.tensor.matmul(pr_[:, :n], lhsT=BbR[:, col, :], rhs=ubf[:, t0:t0 + n], start=True, stop=True),
                     reads=[BbRb, ubfb], writes=[prb_])
                S.op("pe", lambda: nc.tensor.matmul(pi_[:, :n], lhsT=BbI[:, col, :], rhs=ubf[:, t0:t0 + n], start=True, stop=True),
                     reads=[BbIb, ubfb], writes=[pib_])
                S.op("act", lambda: nc.scalar.activation(out=evr[:, t0:t0 + n], in_=pr_[:, :n], func=AF.Copy), reads=[prb_], writes=[evrb[part]])
                S.op("act", lambda: nc.scalar.activation(out=evi[:, t0:t0 + n], in_=pi_[:, :n], func=AF.Copy), reads=[pib_], writes=[evib[part]])
            ew(bre, breb, evr, evrb, cosT, cosb, ALU.mult)
            ew(sre, sreb, evi, evib, sinT, sinb, ALU.mult)
            ew(bim, bimb, evi, evib, cosT, cosb, ALU.mult)
            ew(sim, simb, evr, evrb, sinT, sinb, ALU.mult)
            ew(bre, breb, bre, breb, sre, sreb, ALU.add)
            ew(bim, bimb, bim, bimb, sim, simb, ALU.subtract)
            if idx + 1 < len(iters):
                tab_rest(idx + 1)
            rdec = rpp[:, col:col + 1]
            sq_ = []
            for (src, dst) in ((bre, sre), (bim, sim)):
                if d == 0:
                    sq_.append(lambda src=src, dst=dst: nc.vector.tensor_tensor_scan(dst[:, NL:T], rdec.to_broadcast([128, NX]), src[:, NL:T], 0.0, ALU.mult, ALU.add))
                else:
                    sq_.append(lambda src=src, dst=dst: nc.vector.tensor_tensor_scan(dst[:, NL:T][:, ::-1], rdec.to_broadcast([128, NX]), src[:, NL:T][:, ::-1], 0.0, ALU.mult, ALU.add))
            for (src, dst) in ((bre, sre), (bim, sim)):
                if d == 0:
                    sq_.append(lambda src=src, dst=dst: nc.vector.tensor_tensor_scan(dst[:, 0:NL], rdec.to_broadcast([128, NL]), src[:, 0:NL], dst[:, T - 1:T], ALU.mult, ALU.add))
                else:
                    sq_.append(lambda src=src, dst=dst: nc.vector.tensor_tensor_scan(dst[:, 0:NL][:, ::-1], rdec.to_broadcast([128, NL]), src[:, 0:NL][:, ::-1],
                                                                                     dst[:, NL:NL + 1], ALU.mult, ALU.add))
            S.seq("dve", sq_, reads=list(breb) + list(bimb) + [rppb], writes=list(sreb) + list(simb))
            ew(pa, pab, sre, sreb, cosT, cosb, ALU.mult)
            ew(pb, pbb, sim, simb, sinT, sinb, ALU.mult)
            ew(pc, pcb, sre, sreb, sinT, sinb, ALU.mult)
            ew(pd, pdb, sim, simb, cosT, cosb, ALU.mult)
            first = (d == 0 and ns == 0)
            last = (d == 1 and ns == 3)
            for bi, (t0, n, lc) in enumerate(TB):
                yp, ypb = yps[bi]

                def rd():
                    nc.tensor.matmul(yp[:, :n], lhsT=CR[:, col, :], rhs=pa[:, t0:t0 + n], start=first, stop=False)
                    nc.tensor.matmul(yp[:, :n], lhsT=CRn[:, col, :], rhs=pb[:, t0:t0 + n], start=False, stop=False)
                    nc.tensor.matmul(yp[:, :n], lhsT=CIn[:, col, :], rhs=pc[:, t0:t0 + n], start=False, stop=False)
                    return nc.tensor.matmul(yp[:, :n], lhsT=CIn[:, col, :], rhs=pd[:, t0:t0 + n], start=False, stop=last)
                S.op("pe", rd, reads=[CRb, CRnb, CInb] + list(pab) + list(pbb) + list(pcb) + list(pdb), writes=[ypb])
            if last:
                for bi, (t0, n, lc) in enumerate(TB):
                    yp, ypb = yps[bi]
                    u32, u32b = tr.next()
                    S.dma("sp", u32[:, :n], Sx["s5u32"][ct * 128:(ct + 1) * 128, t0:t0 + n], writes=[u32b])
                    a1, a1b = tr.next()
                    S.op("dve", lambda: nc.vector.scalar_tensor_tensor(a1[:, :n], u32[:, :n], dsk[:, ct:ct + 1], yp[:, :n], ALU.mult, ALU.add),
                         reads=[u32b, dskb, ypb], writes=[a1b])
                    go, gob = gor.next()
                    S.op("act", lambda: nc.scalar.activation(out=go[:, :n], in_=a1[:, :n], func=AF.Gelu), reads=[a1b], writes=[gob])
                    S.dma("sp", Sx["dbg_g"][ct * 128:(ct + 1) * 128, t0:t0 + n], go[:, :n], reads=[gob])
        S.barrier()
        mst.close()
        gT, gTb = S.sbuf("gT", [128, 4, T], BF16, es=ph)
        S.dma("sp", gT[:], Sx["dbg_g"].rearrange("(c p) t -> p c t", p=128), writes=[gTb])
        wgl, wglb = S.sbuf("wglu", [128, 4, 512], BF16, es=ph)
        S.dma("pool", wgl[:], I["w_glu"][li].rearrange("(c p) f -> p c f", p=128), writes=[wglb])
        bgl, bglb = S.sbuf("bglu", [128, 4], F32, es=ph)
        S.dma("sp", bgl[:], I["b_glu"][li], writes=[bglb])
        obr = Ring(S, ph, "s5o", [128, 512], BF16, 3)
        for fch in range(4):
            for (t0, n, lc) in TB:
                ps, psb = bur.next()

                def mm():
                    ins = None
                    for c in range(4):
                        ins = nc.tensor.matmul(ps[:, :n], lhsT=wgl[:, c, fch * 128:(fch + 1) * 128], rhs=gT[:, c, t0:t0 + n], start=(c == 0), stop=(c == 3))
                    return ins
                S.op("pe", mm, reads=[wglb, gTb], writes=[psb])
                a1, a1b = tr.next()
                S.op("act", lambda: nc.scalar.activation(out=a1[:, :n], in_=ps[:, :n], func=AF.Sigmoid, bias=bgl[:, fch:fch + 1]), reads=[psb, bglb], writes=[a1b])
                ob, obb = obr.next()
                S.op("dve", lambda: nc.vector.tensor_tensor(ob[:, :n], a1[:, :n], gT[:, fch, t0:t0 + n], ALU.mult), reads=[a1b, gTb], writes=[obb])
                S.dma("sp", Sx["catT"][1024 + fch * 128:1024 + (fch + 1) * 128, t0:t0 + n], ob[:, :n], reads=[obb])


def phase_out(C, li, x_in, x_out):
    S, nc, I, Sx = C.S, C.nc, C.I, C.Sx
    m = C.mod[li]
    with ExitStack() as ph:
        cat, catb = S.sbuf("cat", [128, KD, T], BF16, es=ph)
        for k4 in range(4):
            S.dma("sp", cat[:, k4 * 4:(k4 + 1) * 4, :], Sx["catT"][k4 * 512:(k4 + 1) * 512, :].rearrange("(k p) t -> p k t", p=128),
                  writes=[catb] if k4 == 0 else [], awrites=[] if k4 == 0 else [catb])
        wr = Ring(S, ph, "wo", [128, KD, 512], BF16, 2)
        pr = Ring(S, ph, "po", [128, 512], F32, 4, psum=True)
        xr = Ring(S, ph, "xo", [128, 512], F32, 3)
        orr = Ring(S, ph, "oo", [128, 512], F32, 3)
        nxt = wr.next()
        S.dma("pool", nxt[0][:], I["w_out"][li][:, 0:512].rearrange("(k p) n -> p k n", p=128), writes=[nxt[1]])
        for nb in range(4):
            w, wb = nxt
            if nb < 3:
                nxt = wr.next()
                S.dma("pool", nxt[0][:], I["w_out"][li][:, (nb + 1) * 512:(nb + 2) * 512].rearrange("(k p) n -> p k n", p=128), writes=[nxt[1]])
            for dl in range(4):
                dch = nb * 4 + dl
                for (t0, n, lc) in TB:
                    xt, xtb = xr.next()
                    S.dma("sp", xt[:, :n], x_in[dch * 128:(dch + 1) * 128, t0:t0 + n], writes=[xtb])
                    ps, psb = pr.next()

                    def mm():
                        ins = None
                        for k in range(KD):
                            ins = nc.tensor.matmul(ps[:, :n], lhsT=w[:, k, dl * 128:(dl + 1) * 128], rhs=cat[:, k, t0:t0 + n], start=(k == 0), stop=(k == KD - 1))
                        return ins
                    S.op("pe", mm, reads=[wb, catb], writes=[psb])
                    o, ob = orr.next()
                    S.op("dve", lambda: nc.vector.scalar_tensor_tensor(o[:, :n], ps[:, :n], m.t[:, 32 + dch, lc:lc + 1], xt[:, :n], ALU.mult, ALU.add),
                         reads=[psb, m.b, xtb], writes=[ob])
                    S.dma("sp", x_out[dch * 128:(dch + 1) * 128, t0:t0 + n], o[:, :n], reads=[ob])


def phase_moe(C, li, x_in, x_out):
    S, nc, I, Sx = C.S, C.nc, C.I, C.Sx
    m = C.mod[li]
    with ExitStack() as ph:
        posmT, posmTb = S.sbuf("posmT", [16, T], F32, es=ph)
        posmB, posmBb = S.sbuf("posmB", [16, T], BF16, es=ph)
        with ExitStack() as ph8:
            h2tok, h2tokb = S.sbuf("h2tok", [128, 18, D], BF16, es=ph8)
            aff3, aff3b = S.sbuf("aff3", [128, 18, 16, 3], BF16, es=ph8)
            posm_tok, posm_tokb = S.sbuf("posm_tok", [128, 18, 16], F32, es=ph8)
            with ExitStack() as p7:
                wrt, wrtb = S.sbuf("wrt", [128, KD, 16], F32, es=p7)
                S.dma("sp", wrt[:], I["w_router"][li].rearrange("(k p) e -> p k e", p=128), writes=[wrtb])
                aff, affb = S.sbuf("aff", [128, 18, 16], F32, es=p7)
                lg_, lgb = S.psum("lg", [128, 512], F32, es=p7)
                lg = lg_[:, 0:288].rearrange("p (a b) -> p a b", b=16)
                with ExitStack() as p7a:
                    hb, hbb = S.sbuf("hb", [128, KD, 512], BF16, es=p7a)
                    ptr = Ring(S, p7a, "pT", [128, 1024], BF16, 2, psum=True)

                    def cb(bi, t0, n, lc, hf, hfb):
                        S.op("pool", lambda: nc.gpsimd.tensor_copy(hb[:, :, :n], hf[:, :, :n]), reads=[hfb], writes=[hbb])
                        for a in range(n // 128):
                            tt = t0 // 128 + a

                            def mm():
                                ins = None
                                for k in range(KD):
                                    ins = nc.tensor.matmul(lg[:, tt, :], lhsT=hf[:, k, a * 128:(a + 1) * 128], rhs=wrt[:, k, :], start=(k == 0), stop=(k == KD - 1))
                                return ins
                            S.op("pe", mm, reads=[hfb, wrtb], writes=[lgb])
                            for kq in range(4):
                                pT, pTb = ptr.next()

                                def tp():
                                    ins = None
                                    for j in range(4):
                                        ins = nc.tensor.transpose(pT[:, j * 128:(j + 1) * 128], hb[:, kq * 4 + j, a * 128:(a + 1) * 128], C.ident_bf[:, :])
                                    return ins
                                S.op("pe", tp, reads=[hbb, C.ident_bf_b], writes=[pTb])
                                if kq % 2:
                                    S.op("act", lambda: nc.scalar.activation(out=h2tok[:, tt, kq * 512:(kq + 1) * 512], in_=pT[:, 0:512], func=AF.Copy),
                                         reads=[pTb], writes=[h2tokb])
                                else:
                                    S.op("dve", lambda: nc.vector.tensor_copy(h2tok[:, tt, kq * 512:(kq + 1) * 512], pT[:, 0:512]), reads=[pTb], writes=[h2tokb])
                    norm_blocks(C, p7a, x_in, m.A2, m.A2b, modsl(m, 3), m.b, cb)
                    S.barrier()
                mx, mxb = S.sbuf("mx", [128, 18], F32, es=p7)
                S.op("dve", lambda: nc.vector.tensor_reduce(mx[:], lg, AX.X, ALU.max), reads=[lgb], writes=[mxb])
                S.op("dve", lambda: nc.vector.tensor_tensor(aff[:], lg, mx[:].unsqueeze(2).to_broadcast([128, 18, 16]), ALU.subtract),
                     reads=[lgb, mxb], writes=[affb])
                S.op("act", lambda: nc.scalar.activation(out=aff[:], in_=aff[:], func=AF.Exp), reads=[affb], writes=[affb])
                S.op("dve", lambda: nc.vector.tensor_reduce(mx[:], aff[:], AX.X, ALU.add), reads=[affb], writes=[mxb])
                S.op("dve", lambda: nc.vector.reciprocal(mx[:], mx[:]), reads=[mxb], writes=[mxb])
                S.op("dve", lambda: nc.vector.tensor_tensor(aff[:], aff[:], mx[:].unsqueeze(2).to_broadcast([128, 18, 16]), ALU.mult),
                     reads=[affb, mxb], writes=[affb])
                if "dbg_aff" in C.dump:
                    S.dma("sp", Sx["dbg_aff"], aff[:], reads=[affb])
                r1, r1b = S.sbuf("r1", [128, 18, 16], F32, es=p7)
                S.op("dve", lambda: nc.vector.tensor_copy(aff3[:, :, :, 0], aff[:]), reads=[affb], writes=[aff3b])
                S.op("dve", lambda: nc.vector.tensor_tensor(r1[:], aff[:], aff3[:, :, :, 0], ALU.subtract), reads=[affb, aff3b], writes=[r1b])
                S.op("dve", lambda: nc.vector.tensor_copy(aff3[:, :, :, 1], r1[:]), reads=[r1b], writes=[aff3b])
                S.op("dve", lambda: nc.vector.tensor_tensor(r1[:], r1[:], aff3[:, :, :, 1], ALU.subtract), reads=[r1b, aff3b], writes=[r1b])
                S.op("dve", lambda: nc.vector.tensor_copy(aff3[:, :, :, 2], r1[:]), reads=[r1b], writes=[aff3b])
                affT, affTb = S.sbuf("affT", [16, T], F32, es=p7)
                work, workb = S.sbuf("work", [16, T], F32, es=p7)
                pa_, pab = S.psum("pa", [128, 512], F32, es=p7)
                pa = pa_[0:16, :]
                for (t0, n, lc) in TB:
                    def tpa():
                        ins = None
                        for a in range(n // 128):
                            ins = nc.tensor.transpose(pa[:, a * 128:(a + 1) * 128], aff[:, t0 // 128 + a, :], C.ident_f[:, :])
                        return ins
                    S.op("pe", tpa, reads=[affb, C.ident_f_b], writes=[pab])
                    S.op("dve", lambda: nc.vector.tensor_copy(affT[:, t0:t0 + n], pa[:, :n]), reads=[pab], writes=[affTb])
                S.op("dve", lambda: nc.vector.tensor_copy(work[:], affT[:]), reads=[affTb], writes=[workb])
                m8, m8b = S.sbuf("m8", [16, 16], F32, es=p7)

                tk = []
                for (lo, hi, rounds, oc) in ((0, NL, 32, 0), (NL, T, 4, 8)):
                    for r in range(rounds):
                        tk.append(lambda lo=lo, hi=hi, oc=oc: nc.vector.max(m8[:, oc:oc + 8], work[:, lo:hi]))
                        if r < rounds - 1:
                            tk.append(lambda lo=lo, hi=hi, oc=oc: nc.vector.match_replace(work[:, lo:hi], m8[:, oc:oc + 8], work[:, lo:hi], -1.0))
                S.seq("dve", tk, reads=[workb], writes=[workb, m8b])
                mk, mkb = S.sbuf("mk", [16, T], F32, es=p7)

                S.seq("dve", [
                    lambda: nc.vector.tensor_scalar(mk[:, 0:NL], affT[:, 0:NL], m8[:, 7:8], None, ALU.is_ge),
                    lambda: nc.vector.tensor_scalar(mk[:, NL:T], affT[:, NL:T], m8[:, 15:16], None, ALU.is_ge),
                    lambda: nc.vector.tensor_tensor_scan(work[:, 0:NL], C.one_f[:16, 0:1].to_broadcast([16, NL]), mk[:, 0:NL], 0.0, ALU.mult, ALU.add),
                    lambda: nc.vector.tensor_tensor_scan(work[:, NL:T], C.one_f[:16, 0:1].to_broadcast([16, NX]), mk[:, NL:T], 0.0, ALU.mult, ALU.add),
                    lambda: nc.vector.tensor_scalar(work[:, NL:T], work[:, NL:T], 256.0, None, ALU.add),
                    lambda: nc.vector.tensor_tensor(work[:], work[:], mk[:], ALU.mult),
                    lambda: nc.vector.tensor_scalar(posmT[:], work[:], -1.0, None, ALU.add),
                ], reads=[affTb, m8b, C.one_f_b, workb], writes=[mkb, workb, posmTb])
                S.seq("dve", [
                    lambda: nc.vector.tensor_copy(posmB[:, 0:NL], posmT[:, 0:NL]),
                    lambda: nc.vector.tensor_scalar(posmB[:, NL:T], posmT[:, NL:T], -256.0, None, ALU.add),
                ], reads=[posmTb], writes=[posmBb])
                if "dbg_posm" in C.dump:
                    S.dma("sp", Sx["dbg_posm"], posmT[:], reads=[posmTb])
                pp__, ppb_ = S.psum("ppm", [128, 512], F32, es=p7)
                pp_ = pp__[:, 0:288].rearrange("p (a b) -> p a b", b=16)

                def tpp():
                    ins = None
                    for tt in range(18):
                        ins = nc.tensor.transpose(pp_[:, tt, :], posmT[:, tt * 128:(tt + 1) * 128], C.ident_f[:16, :16])
                    return ins
                S.op("pe", tpp, reads=[posmTb, C.ident_f_b], writes=[ppb_])
                S.op("dve", lambda: nc.vector.tensor_copy(posm_tok[:], pp_), reads=[ppb_], writes=[posm_tokb])
                S.barrier()
            with ExitStack() as p8:
                ioj, iojb = S.sbuf("ioj", [128, NJ], F32, es=p8)
                S.dma("sp", ioj[:], I["iota_j"][0:1, :].to_broadcast([128, NJ]), writes=[iojb])
                Se, Seb = S.sbuf("Se", [128, 18, NJ], BF16, es=p8)
                xsr = Ring(S, p8, "xs", [128, KD, NJ], BF16, 2)
                hidr = Ring(S, p8, "hid", [128, 8, NJ], BF16, 2)
                ysb_, ysbb = S.sbuf("ysb", [128, 3, D], BF16, es=p8)
                wring = Ring(S, p8, "wu", [128, 4096], BF16, 6)
                pr = Ring(S, p8, "pe8", [128, 512], F32, 6, psum=True)
                tap_, tapb = S.psum("tap", [128, 512], F32, es=p8)
                tap = tap_[:, 0:9]
                ta, tab = S.sbuf("ta", [128, 3], F32, es=p8)
                sgr = Ring(S, p8, "sg", [128, NJ], F32, 3)

                def units(e):
                    u = []
                    for fq in range(4):
                        u.append(("g", fq, I["w_gate"][li, e][:, fq * 256:(fq + 1) * 256].rearrange("(k p) f -> p k f", p=128)))
                        u.append(("u", fq, I["w_up"][li, e][:, fq * 256:(fq + 1) * 256].rearrange("(k p) f -> p k f", p=128)))
                    for dq in range(4):
                        u.append(("d", dq, I["w_down"][li, e][:, dq * 512:(dq + 1) * 512].rearrange("(c p) d -> p c d", p=128)))
                    return u
                allu = [(e,) + u for e in range(16) for u in units(e)]
                loaded = {}

                def issue(i):
                    if i >= len(allu):
                        return
                    e, kind, idx, src = allu[i]
                    wt, wtb = wring.next()
                    if kind == "d":
                        S.dma("pool", wt[:].rearrange("p (c d) -> p c d", c=8), src, writes=[wtb])
                    else:
                        S.dma("pool", wt[:].rearrange("p (k f) -> p k f", k=KD), src, writes=[wtb])
                    loaded[i] = (wt, wtb)
                PRE = 4
                for i in range(PRE):
                    issue(i)
                ui = 0
                for e in range(16):
                    def mkS():
                        ins = None
                        for tt in range(18):
                            ins = nc.vector.tensor_scalar(Se[:, tt, :], ioj[:, :], posm_tok[:, tt, e:e + 1], None, ALU.is_equal)
                        return ins
                    S.op("dve", mkS, reads=[iojb, posm_tokb], writes=[Seb])

                    def mta():
                        ins = None
                        for jc, (tts, M) in enumerate(((range(16), 128), (range(16), 128), ((16, 17), 32))):
                            tts = list(tts)
                            for ii, tt in enumerate(tts):
                                ins = nc.tensor.matmul(tap[:M, jc * 3:(jc + 1) * 3], lhsT=Se[:, tt, jc * 128:jc * 128 + M], rhs=aff3[:, tt, e, :],
                                                       start=(ii == 0), stop=(ii == len(tts) - 1))
                        return ins
                    S.op("pe", mta, reads=[Seb, aff3b], writes=[tapb])
                    S.op("dve", lambda: nc.vector.tensor_reduce(ta[:], tap_[:, 0:9].rearrange("p (a b) -> p a b", b=3), AX.X, ALU.add), reads=[tapb], writes=[tab])
                    xs, xsb = xsr.next()
                    for k in range(KD):
                        ps, psb = pr.next()

                        def gm():
                            ins = None
                            for tt in range(16):
                                ins = nc.tensor.matmul(ps[:, 0:256], lhsT=h2tok[:, tt, k * 128:(k + 1) * 128], rhs=Se[:, tt, 0:256], start=(tt == 0), stop=(tt == 15))
                            for tt in (16, 17):
                                ins = nc.tensor.matmul(ps[:, 256:NJ], lhsT=h2tok[:, tt, k * 128:(k + 1) * 128], rhs=Se[:, tt, 256:NJ], start=(tt == 16), stop=(tt == 17))
                            return ins
                        S.op("pe", gm, reads=[h2tokb, Seb], writes=[psb])
                        if k % 2:
                            S.op("act", lambda: nc.scalar.activation(out=xs[:, k, :], in_=ps[:, :NJ], func=AF.Copy), reads=[psb], writes=[xsb])
                        else:
                            S.op("dve", lambda: nc.vector.tensor_copy(xs[:, k, :], ps[:, :NJ]), reads=[psb], writes=[xsb])
                    hid, hidb = hidr.next()
                    for fq in range(4):
                        wg, wgb = loaded.pop(ui)
                        wu, wub = loaded.pop(ui + 1)
                        ui += 2
                        wg3 = wg[:].rearrange("p (k f) -> p k f", k=KD)
                        wu3 = wu[:].rearrange("p (k f) -> p k f", k=KD)
                        for fcl in range(2):
                            fc = fq * 2 + fcl
                            pg, pgb = pr.next()
                            pu, pub = pr.next()

                            def mg():
                                ins = None
                                for k in range(KD):
                                    ins = nc.tensor.matmul(pg[:, :NJ], lhsT=wg3[:, k, fcl * 128:(fcl + 1) * 128], rhs=xs[:, k, :], start=(k == 0), stop=(k == KD - 1))
                                return ins

                            def mu():
                                ins = None
                                for k in range(KD):
                                    ins = nc.tensor.matmul(pu[:, :NJ], lhsT=wu3[:, k, fcl * 128:(fcl + 1) * 128], rhs=xs[:, k, :], start=(k == 0), stop=(k == KD - 1))
                                return ins
                            S.op("pe", mg, reads=[wgb, xsb], writes=[pgb])
                            S.op("pe", mu, reads=[wub, xsb], writes=[pub])
                            sg, sgb = sgr.next()
                            S.op("act", lambda: nc.scalar.activation(out=sg[:, :], in_=pg[:, :NJ], func=AF.Silu), reads=[pgb], writes=[sgb])
                            S.op("dve", lambda: nc.vector.tensor_tensor(hid[:, fc, :], sg[:, :], pu[:, :NJ], ALU.mult), reads=[sgb, pub], writes=[hidb])
                        issue(ui - 2 + PRE)
                        issue(ui - 1 + PRE)
                    for dq in range(4):
                        wd, wdb = loaded.pop(ui)
                        ui += 1
                        wd3 = wd[:].rearrange("p (c d) -> p c d", c=8)
                        for jc, M in enumerate((128, 128, 32)):
                            ps, psb = pr.next()

                            def md():
                                ins = None
                                for fc in range(8):
                                    ins = nc.tensor.matmul(ps[:M, :], lhsT=hid[:, fc, jc * 128:jc * 128 + M], rhs=wd3[:, fc, :], start=(fc == 0), stop=(fc == 7))
                                return ins
                            S.op("pe", md, reads=[hidb, wdb], writes=[psb])
                            if jc == 1:
                                S.op("act", lambda: nc.scalar.activation(out=ysb_[:M, jc, dq * 512:(dq + 1) * 512], in_=ps[:M, :], func=AF.Copy, scale=ta[:M, jc:jc + 1]),
                                     reads=[psb, tab], writes=[ysbb])
                            else:
                                S.op("dve", lambda: nc.vector.tensor_scalar(ysb_[:M, jc, dq * 512:(dq + 1) * 512], ps[:M, :], ta[:M, jc:jc + 1], None, ALU.mult),
                                     reads=[psb, tab], writes=[ysbb])
                        issue(ui - 1 + PRE)
                    S.dma("sp", Sx["ys"][e, 0:256, :].rearrange("(c j) d -> j c d", j=128), ysb_[:, 0:2, :], reads=[ysbb])
                    S.dma("sp", Sx["ys"][e, 256:NJ, :], ysb_[:32, 2, :], reads=[ysbb])
                S.barrier()
        with ExitStack() as p9:
            ysh, yshb = S.sbuf("ysh", [128, 16, 3, 1024], BF16, es=p9)
            STr = Ring(S, p9, "ST", [128, 16, 2, 512], BF16, 2)
            selt, seltb = S.sbuf("selt", [16, 16, 128], BF16, es=p9)
            S.dma("pool", selt[:], I["sel"][:, :, :], writes=[seltb])
            ip3, ip3b = S.sbuf("ip3", [128, 3], F32, es=p9)
            S.dma("sp", ip3[:], I["iota_p3"][:, :], writes=[ip3b])
            bcr = Ring(S, p9, "bc", [128, 512], F32, 3, psum=True)
            pr = Ring(S, p9, "p9", [128, 512], F32, 4, psum=True)
            xr = Ring(S, p9, "x9", [128, 512], F32, 3)
            orr = Ring(S, p9, "o9", [128, 512], F32, 3)
            for dh in range(2):
                for jc in range(2):
                    S.dma("sp", ysh[:, :, jc, :], Sx["ys"][:, jc * 128:(jc + 1) * 128, dh * 1024:(dh + 1) * 1024].rearrange("e j d -> j e d"),
                          writes=[yshb] if jc == 0 else [], awrites=[] if jc == 0 else [yshb])
                S.dma("sp", ysh[:32, :, 2, :], Sx["ys"][:, 256:NJ, dh * 1024:(dh + 1) * 1024].rearrange("e j d -> j e d"), awrites=[yshb])
                for (t0, n, lc) in TB:
                    ST, STb = STr.next()
                    for e in range(16):
                        bc, bcb = bcr.next()
                        S.op("pe", lambda: nc.tensor.matmul(bc[:, :n], lhsT=selt[:, e, :], rhs=posmB[:, t0:t0 + n], start=True, stop=True),
                             reads=[seltb, posmBb], writes=[bcb])
                        if lc == 0:
                            S.op("dve", lambda: nc.vector.tensor_scalar(ST[:, e, 0, :n], bc[:, :n], ip3[:, 0:1], None, ALU.is_equal), reads=[bcb, ip3b], writes=[STb])
                            S.op("pool" if False else "dve", lambda: nc.vector.tensor_scalar(ST[:, e, 1, :n], bc[:, :n], ip3[:, 1:2], None, ALU.is_equal),
                                 reads=[bcb, ip3b], writes=[STb])
                        else:
                            S.op("dve", lambda: nc.vector.tensor_scalar(ST[:32, e, 0, :n], bc[:32, :n], ip3[:32, 0:1], None, ALU.is_equal), reads=[bcb, ip3b], writes=[STb])
                    for dl in range(8):
                        dch = dh * 8 + dl
                        xt, xtb = xr.next()
                        S.dma("sp", xt[:, :n], x_in[dch * 128:(dch + 1) * 128, t0:t0 + n], writes=[xtb])
                        ps, psb = pr.next()

                        def msc():
                            ins = None
                            if lc == 0:
                                for e in range(16):
                                    for jc in range(2):
                                        ins = nc.tensor.matmul(ps[:, :n], lhsT=ysh[:, e, jc, dl * 128:(dl + 1) * 128], rhs=ST[:, e, jc, :n],
                                                               start=(e == 0 and jc == 0), stop=(e == 15 and jc == 1))
                            else:
                                for e in range(16):
                                    ins = nc.tensor.matmul(ps[:, :n], lhsT=ysh[:32, e, 2, dl * 128:(dl + 1) * 128], rhs=ST[:32, e, 0, :n],
                                                           start=(e == 0), stop=(e == 15))
                            return ins
                        S.op("pe", msc, reads=[yshb, STb], writes=[psb])
                        o, ob = orr.next()
                        S.op("dve", lambda: nc.vector.scalar_tensor_tensor(o[:, :n], ps[:, :n], m.t[:, 80 + dch, lc:lc + 1], xt[:, :n], ALU.mult, ALU.add),
                             reads=[psb, m.b, xtb], writes=[ob])
                        S.dma("sp", x_out[dch * 128:(dch + 1) * 128, t0:t0 + n], o[:, :n], reads=[ob])


def phase_final(C, x_in):
    S, nc, I = C.S, C.nc, C.I
    with ExitStack() as ph:
        gf, gfb = S.sbuf("gf", [128, KD, 2], F32, es=ph)
        S.dma("sp", gf[:], I["gfT"][:, :, :], writes=[gfb])
        zs, zsb = S.sbuf("zs", [128, KD, 2], F32, es=ph)
        S.op("pool", lambda: nc.gpsimd.memset(zs[:], 0.0), writes=[zsb])

        def cb(bi, t0, n, lc, hf, hfb):
            S.dma("sp", C.outT[:, t0:t0 + n].rearrange("(k p) t -> p k t", p=128), hf[:, :, :n], reads=[hfb])
        norm_blocks(C, ph, x_in, gf, gfb, zs, zsb, cb, blocks=TB[:4])


def _prep_shared(inp):
    f = np.float32
    L = DEPTH
    sh = {}
    sh["w_ada"] = np.ascontiguousarray(inp["w_ada"], dtype=f)
    sh["b_adaT"] = np.ascontiguousarray(np.repeat(inp["b_ada"].reshape(L, 96, 128).transpose(0, 2, 1)[..., None], 2, axis=-1), dtype=f)
    for nm, src in (("g1T", "norm1_g"), ("g2T", "norm2_g")):
        sh[nm] = np.ascontiguousarray(np.repeat(inp[src].reshape(L, KD, 128).transpose(0, 2, 1)[..., None], 2, axis=-1), dtype=f)
    sh["gfT"] = np.ascontiguousarray(np.repeat(inp["final_norm_g"].reshape(KD, 128).T[..., None], 2, axis=-1), dtype=f)
    sh["w_in"] = np.ascontiguousarray(inp["w_in"], dtype=f)
    perm = np.array([(r // 32) * 32 + ((r % 32) + 16) % 32 for r in range(64)])
    sh["w_in_sw"] = np.ascontiguousarray(inp["w_in"][:, :, 1664:1728][:, :, perm], dtype=f)
    sh["w_out"] = np.ascontiguousarray(inp["w_out"], dtype=f)
    sh["sgu_g"] = np.ascontiguousarray(inp["sgu_norm_g"].reshape(L, 1, 512), dtype=f)
    sh["sgu_wT"] = np.ascontiguousarray(inp["sgu_w"].transpose(0, 3, 1, 2), dtype=f)
    sh["sgu_b4"] = np.ascontiguousarray(np.repeat(inp["sgu_b"][:, :, None, :], 4, axis=2).reshape(L, 1, 2048), dtype=f)
    sh["qg"] = np.ascontiguousarray(inp["mla_q_norm_g"].reshape(L, 3, 128).transpose(0, 2, 1), dtype=f)
    sh["kvg"] = np.ascontiguousarray(inp["mla_kv_norm_g"].reshape(L, 2, 128).transpose(0, 2, 1), dtype=f)
    sh["w_uq"] = np.ascontiguousarray(inp["mla_w_uq"], dtype=f)
    sh["w_uq_sw"] = np.ascontiguousarray(np.concatenate([inp["mla_w_uq"][:, :, h * 192 + 128 + perm] for h in range(4)], axis=-1), dtype=f)
    sh["w_ukv"] = np.ascontiguousarray(inp["mla_w_ukv"], dtype=f)
    t = np.arange(NL)
    row_id = (t // 64).astype(f)
    col_id = (t % 64).astype(f)
    inv_freq = (f(10000.0) ** (-np.arange(16, dtype=f) / f(16))).astype(f)
    cosT = np.ones((64, T), f)
    sinT = np.zeros((64, T), f)
    for r in range(64):
        pos = row_id if r < 32 else col_id
        ang = (pos * inv_freq[r % 16]).astype(f)
        cosT[r, :NL] = np.cos(ang)
        sinT[r, :NL] = np.sin(ang) * (-1.0 if (r % 32) < 16 else 1.0)
    sh["rope_cos"] = cosT
    sh["rope_sin"] = sinT

    def pp(a):
        return a.reshape(L, 2, 4, 8, 4, 16).transpose(0, 3, 5, 1, 2, 4).reshape(L, 128, 32)

    def rowl(a):
        return a.reshape(L, 2, 4, 8, 4, 16).transpose(0, 1, 2, 4, 3, 5).reshape(L, 4096)
    ldt_full = np.repeat(inp["s5_log_dt"][..., None], 64, axis=-1)
    sh["s5_pp"] = np.ascontiguousarray(np.stack([pp(inp["s5_a_re"]), pp(inp["s5_a_im"]), pp(ldt_full)], axis=2), dtype=f)
    sh["s5_row"] = np.ascontiguousarray(np.stack([rowl(inp["s5_a_re"]), rowl(inp["s5_a_im"]), rowl(ldt_full)], axis=1), dtype=f)

    def bblk(b):
        o = np.zeros((L, 8, 16, 2, 4, 4, 8, 16), f)
        bb = b.reshape(L, 2, 4, 8, 4, 16, 16)
        for g in range(8):
            o[:, g, :, :, :, :, g, :] = bb[:, :, :, g].transpose(0, 4, 1, 2, 3, 5)[:, :, :, :, :, :] if False else \
                np.transpose(bb[:, :, :, g], (0, 5, 1, 2, 3, 4))
        return o.reshape(L, 128, 4096)

    def cblk(c):
        o = np.zeros((L, 8, 16, 2, 4, 4, 8, 16), f)
        cc = c.reshape(L, 2, 4, 8, 16, 4, 16)
        for g in range(8):
            o[:, g, :, :, :, :, g, :] = np.transpose(cc[:, :, :, g], (0, 5, 1, 2, 4, 3))
        return o.reshape(L, 128, 4096)
    sh["s5_Bre"] = bblk(inp["s5_b_re"])
    sh["s5_Bim"] = bblk(inp["s5_b_im"])
    sh["s5_Cre"] = cblk(inp["s5_c_re"])
    sh["s5_Cim"] = cblk(inp["s5_c_im"])
    sh["s5_d"] = np.ascontiguousarray(inp["s5_d"].reshape(L, 4, 128).transpose(0, 2, 1), dtype=f)
    tau = np.zeros((2, T), f)
    tau[0, NL:] = np.arange(NX)
    tau[0, :NL] = NX + np.arange(NL)
    tau[1, NL:] = NX - 1 - np.arange(NX)
    tau[1, :NL] = NX + (NL - 1 - np.arange(NL))
    sh["s5_tau"] = tau.reshape(1, 2 * T)
    sh["w_glu"] = np.ascontiguousarray(inp["s5_w_glu"], dtype=f)
    sh["b_glu"] = np.ascontiguousarray(inp["s5_b_glu"].reshape(L, 4, 128).transpose(0, 2, 1), dtype=f)
    sh["conv_wT"] = np.ascontiguousarray(inp["conv_w"].reshape(L, 3, 4, 128).transpose(0, 3, 2, 1), dtype=f)
    sh["w_router"] = np.ascontiguousarray(inp["moe_w_router"], dtype=f)
    sh["w_gate"] = np.ascontiguousarray(inp["moe_w_gate"], dtype=f)
    sh["w_up"] = np.ascontiguousarray(inp["moe_w_up"], dtype=f)
    sh["w_down"] = np.ascontiguousarray(inp["moe_w_down"], dtype=f)
    sh["ident"] = np.eye(128, dtype=f)
    sh["iota_j"] = np.arange(NJ, dtype=f).reshape(1, NJ)
    sh["iota_p3"] = (np.arange(128, dtype=f)[:, None] + np.array([0, 128, 256], f)[None, :]).astype(f)
    sel = np.zeros((16, 16, 128), f)
    for e in range(16):
        sel[e, e, :] = 1.0
    sh["sel"] = sel
    return sh


def _prep_core(inp, b):
    f = np.float32
    d = {}
    d["xT0"] = np.ascontiguousarray(np.concatenate([inp["x"][b].T, inp["ctx"][b].T], axis=1), dtype=f)
    c2 = np.stack([inp["c"][b], inp["c_ctx"]], axis=0)
    d["cTp"] = np.ascontiguousarray(c2.reshape(2, KD, 128).transpose(2, 1, 0), dtype=f)
    return d


_NC_CACHE = {}


def used_inputs(nc_I, m):
    return {k: v for k, v in m.items() if k in nc_I}


def kernel(**inputs):
    inp = {k: np.asarray(v) for k, v in inputs.items()}
    B = inp["x"].shape[0]
    if "full" not in _NC_CACHE:
        _NC_CACHE["full"] = build_program()
    nc = _NC_CACHE["full"]
    sh = _prep_shared(inp)
    in_maps = []
    for b in range(B):
        m = dict(sh)
        m.update(_prep_core(inp, b))
        in_maps.append({k: v for k, v in m.items() if k in nc._used_inputs})
    res = run_bass_kernel_spmd(nc, in_maps, core_ids=list(range(B)))
    out = np.stack([np.asarray(res.results[b]["outT"]).T for b in range(B)], axis=0)
    return np.ascontiguousarray(out, dtype=np.float32)
```

```python
import math
import numpy as np
from contextlib import ExitStack
import concourse.bass as bass
import concourse.mybir as mybir
from concourse.bass_utils import run_bass_kernel_spmd

F32 = mybir.dt.float32
BF16 = mybir.dt.bfloat16
I32 = mybir.dt.int32
AF = mybir.ActivationFunctionType
ALU = mybir.AluOpType
AX = mybir.AxisListType

D = 2048
T = 2304
NL = 2048
NX = 256
KD = 16
DEPTH = 2
TB = [(0, 512, 0), (512, 512, 0), (1024, 512, 0), (1536, 512, 0), (2048, 256, 1)]
NJ = 288
EPS = 1e-6
N_DMA_SEMS = 24
TWO_PI = 6.283185


class Buf:
    __slots__ = ("name", "w", "r")

    def __init__(self, name=""):
        self.name = name
        self.w = {}
        self.r = {}


class Sched:
    def __init__(self, nc, es):
        self.nc = nc
        self.es = es
        self.eng = {"pe": nc.tensor, "dve": nc.vector, "act": nc.scalar, "pool": nc.gpsimd, "sp": nc.sync}
        self.sems = {}
        self.cnt = {}
        for k in ["pe", "dve", "act", "pool"]:
            self.sems[k] = es.enter_context(nc.semaphore("s_" + k))
            self.cnt[k] = 0
        for i in range(N_DMA_SEMS):
            k = "d%d" % i
            self.sems[k] = es.enter_context(nc.semaphore("s_" + k))
            self.cnt[k] = 0
        self.dma_rr = 0
        self.waited = {e: {} for e in self.eng}
        self.bufs = []
        self.uid = 0

    def sbuf(self, name, shape, dt, es=None):
        self.uid += 1
        t = (es or self.es).enter_context(self.nc.sbuf_tensor("%s_%d" % (name, self.uid), list(shape), dt))
        b = Buf(name)
        self.bufs.append(b)
        return t, b

    def psum(self, name, shape, dt, es=None):
        self.uid += 1
        t = (es or self.es).enter_context(self.nc.psum_tensor("%s_%d" % (name, self.uid), list(shape), dt))
        b = Buf(name)
        self.bufs.append(b)
        return t, b

    def _wait(self, e, dep):
        sk, val, deng = dep
        if deng == e and e == "pe":
            return
        if self.waited[e].get(sk, 0) >= val:
            return
        self.eng[e].wait_ge(self.sems[sk], val)
        self.waited[e][sk] = val

    def _deps(self, e, reads, writes):
        for b in reads:
            for d in b.w.values():
                self._wait(e, d)
        for b in writes:
            for d in b.w.values():
                self._wait(e, d)
            for d in b.r.values():
                self._wait(e, d)

    def _commit(self, tag, reads, writes):
        for b in reads:
            b.r[tag[0]] = tag
        for b in writes:
            b.w = {tag[0]: tag}
            b.r = {}

    def op(self, e, fn, reads=(), writes=()):
        self._deps(e, reads, writes)
        ins = fn()
        self.cnt[e] += 1
        ins.then_inc(self.sems[e], 1)
        self._commit((e, self.cnt[e], e), reads, writes)
        return ins

    def seq(self, e, fns, reads=(), writes=()):
        self._deps(e, reads, writes)
        ins = None
        for i, fn in enumerate(fns):
            if i > 0:
                self.eng[e].wait_ge(self.sems[e], self.cnt[e])
                self.waited[e][e] = self.cnt[e]
            ins = fn()
            self.cnt[e] += 1
            ins.then_inc(self.sems[e], 1)
        self._commit((e, self.cnt[e], e), reads, writes)
        return ins

    def dma(self, q, out, in_, reads=(), writes=(), awrites=()):
        self._deps(q, reads, writes)
        sk = "d%d" % self.dma_rr
        self.dma_rr = (self.dma_rr + 1) % N_DMA_SEMS
        if self.cnt[sk] > 0:
            self._wait(q, (sk, self.cnt[sk], "dma"))
        ins = self.eng[q].dma_start(out=out, in_=in_)
        self.cnt[sk] += 16
        ins.then_inc(self.sems[sk], 16)
        self._commit((sk, self.cnt[sk], "dma"), reads, writes)
        for b in awrites:
            b.w[sk] = (sk, self.cnt[sk], "dma")
        return ins

    def barrier(self):
        for e in self.eng:
            for sk, c in self.cnt.items():
                if c > 0:
                    self._wait(e, (sk, c, "x"))
        for b in self.bufs:
            b.w = {}
            b.r = {}


class Ring:
    def __init__(self, S, es, name, shape, dt, n, psum=False):
        mk = S.psum if psum else S.sbuf
        self.items = [mk("%s%d" % (name, i), shape, dt, es=es) for i in range(n)]
        self.i = 0

    def next(self):
        it = self.items[self.i % len(self.items)]
        self.i += 1
        return it


class Ctx:
    pass


def build_program(stop_after=None, dump=()):
    nc = bass.Bass("TRN2", target_bir_lowering=False)
    C = Ctx()
    C.nc = nc
    C.dump = set(dump)
    C.stop_after = stop_after

    def din(name, shape, dt=F32):
        return nc.dram_tensor(name, list(shape), dt, kind="ExternalInput").ap()

    def dscr(name, shape, dt):
        kind = "ExternalOutput" if name in C.dump else "Internal"
        return nc.dram_tensor(name, list(shape), dt, kind=kind).ap()

    SPEC = {
        "xT0": [D, T],
        "cTp": [128, KD, 2],
        "w_ada": [DEPTH, D, 6 * D],
        "b_adaT": [DEPTH, 128, 96, 2],
        "g1T": [DEPTH, 128, KD, 2],
        "g2T": [DEPTH, 128, KD, 2],
        "gfT": [128, KD, 2],
        "w_in": [DEPTH, D, 3776],
        "w_in_sw": [DEPTH, D, 64],
        "w_out": [DEPTH, D, D],
        "sgu_g": [DEPTH, 1, 512],
        "sgu_wT": [DEPTH, 128, 4, 128],
        "sgu_b4": [DEPTH, 1, 2048],
        "qg": [DEPTH, 128, 3],
        "kvg": [DEPTH, 128, 2],
        "w_uq": [DEPTH, 384, 768],
        "w_uq_sw": [DEPTH, 384, 256],
        "w_ukv": [DEPTH, 256, 1024],
        "rope_cos": [64, T],
        "rope_sin": [64, T],
        "s5_pp": [DEPTH, 128, 3, 32],
        "s5_row": [DEPTH, 3, 4096],
        "s5_Bre": [DEPTH, 128, 4096],
        "s5_Bim": [DEPTH, 128, 4096],
        "s5_Cre": [DEPTH, 128, 4096],
        "s5_Cim": [DEPTH, 128, 4096],
        "s5_d": [DEPTH, 128, 4],
        "s5_tau": [1, 2 * T],
        "w_glu": [DEPTH, 512, 512],
        "b_glu": [DEPTH, 128, 4],
        "conv_wT": [DEPTH, 128, 4, 3],
        "w_router": [DEPTH, D, 16],
        "w_gate": [DEPTH, 16, D, 1024],
        "w_up": [DEPTH, 16, D, 1024],
        "w_down": [DEPTH, 16, 1024, D],
        "ident": [128, 128],
        "iota_j": [1, NJ],
        "iota_p3": [128, 3],
        "sel": [16, 16, 128],
    }

    class LazyIn(dict):
        def __missing__(self, k):
            v = din(k, SPEC[k])
            self[k] = v
            return v
    I = LazyIn()
    C.I = I
    C.outT = nc.dram_tensor("outT", [D, NL], F32, kind="ExternalOutput").ap()

    Sx = {}
    Sx["xA"] = dscr("xA", [D, T], F32)
    Sx["xB"] = dscr("xB", [D, T], F32)
    Sx["uTg"] = dscr("uTg", [512, T], BF16)
    Sx["v_tok"] = dscr("v_tok", [T, 512], BF16)
    Sx["cqT"] = dscr("cqT", [384, T], BF16)
    Sx["ckvT"] = dscr("ckvT", [256, T], BF16)
    Sx["krT"] = dscr("krT", [64, T], BF16)
    Sx["s5uT"] = dscr("s5uT", [512, T], BF16)
    Sx["s5u32"] = dscr("s5u32", [512, T], F32)
    Sx["catT"] = dscr("catT", [D, T], BF16)
    Sx["ys"] = dscr("ys", [16, NJ, D], BF16)
    Sx["dbg_mod"] = dscr("dbg_mod", [DEPTH, 128, 96, 2], F32)
    Sx["dbg_hT"] = dscr("dbg_hT", [D, T], BF16)
    Sx["dbg_aff"] = dscr("dbg_aff", [128, 18, 16], F32)
    Sx["dbg_posm"] = dscr("dbg_posm", [16, T], F32)
    Sx["dbg_g"] = dscr("dbg_g", [512, T], BF16)
    C.Sx = Sx

    with ExitStack() as es:
        S = Sched(nc, es)
        C.S = S
        _consts(C)
        phase_mod(C)
        S.barrier()
        x_in = I["xT0"]
        done = (stop_after == "mod")
        for li in range(DEPTH):
            if done:
                break
            x1 = Sx["xA"]
            x2 = Sx["xB"]
            for ph in (phase_in, phase_sgu, phase_mla, phase_s5, phase_out, phase_moe):
                if ph is phase_in:
                    ph(C, li, x_in)
                elif ph is phase_out:
                    ph(C, li, x_in, x1)
                elif ph is phase_moe:
                    ph(C, li, x1, x2)
                else:
                    ph(C, li)
                S.barrier()
                if stop_after == (li, ph.__name__) or (isinstance(stop_after, tuple) and len(stop_after) == 3 and stop_after[0] == li and ph is phase_in):
                    done = True
                    break
            if done:
                break
            x_in = x2
        if not done:
            phase_final(C, x_in)
        S.barrier()
    nc._used_inputs = set(I.keys())
    return nc


def _consts(C):
    S, nc, I = C.S, C.nc, C.I
    C.ones_bf, C.ones_bf_b = S.sbuf("ones_bf", [128, 128], BF16)
    S.op("pool", lambda: nc.gpsimd.memset(C.ones_bf[:], 1.0), writes=[C.ones_bf_b])
    C.eps_t, C.eps_b = S.sbuf("eps", [128, 1], F32)
    S.op("pool", lambda: nc.gpsimd.memset(C.eps_t[:], EPS), writes=[C.eps_b])
    C.one_f, C.one_f_b = S.sbuf("one_f", [128, 1], F32)
    S.op("pool", lambda: nc.gpsimd.memset(C.one_f[:], 1.0), writes=[C.one_f_b])
    C.hpi_t, C.hpi_b = S.sbuf("hpi", [128, 1], F32)
    S.op("pool", lambda: nc.gpsimd.memset(C.hpi_t[:], math.pi / 2), writes=[C.hpi_b])
    C.ident_f, C.ident_f_b = S.sbuf("ident_f", [128, 128], F32)
    S.dma("sp", C.ident_f[:], I["ident"][:, :], writes=[C.ident_f_b])
    C.ident_bf, C.ident_bf_b = S.sbuf("ident_bf", [128, 128], BF16)
    S.dma("pool", C.ident_bf[:], I["ident"][:, :], writes=[C.ident_bf_b])
    C.mod = []
    C.modalloc = []
    for li in range(DEPTH):
        C.modalloc.append((S.sbuf("mod%d" % li, [128, 96, 2], F32), S.sbuf("A1_%d" % li, [128, KD, 2], F32), S.sbuf("A2_%d" % li, [128, KD, 2], F32)))


def _dbg(C, name, src_ap, reads):
    if name in C.dump:
        C.S.dma("sp", C.Sx[name], src_ap, reads=reads)


def phase_mod(C):
    S, nc, I = C.S, C.nc, C.I
    with ExitStack() as ph:
        sc, scb = S.sbuf("sc", [128, KD, 2], F32, es=ph)
        S.dma("sp", sc[:], I["cTp"][:, :, :], writes=[scb])
        S.op("act", lambda: nc.scalar.activation(out=sc[:], in_=sc[:], func=AF.Silu), reads=[scb], writes=[scb])
        war = Ring(S, ph, "wa", [128, KD, 512], F32, 2)
        mps_, mpsb = S.psum("mps", [128, 512], F32, es=ph)
        mps = mps_[:, 0:192]
        rowr = Ring(S, ph, "mrow", [128, 512], F32, 2, psum=True)
        mrow, mrowb = S.sbuf("mrow_sb", [2, 6 * D], F32, es=ph)
        for li in range(DEPTH):
            nxt = war.next()
            S.dma("sp", nxt[0][:], I["w_ada"][li, :, 0:512].rearrange("(k p) n -> p k n", p=128), writes=[nxt[1]])
            for nb in range(24):
                wa, wab = nxt
                if nb + 1 < 24:
                    nxt = war.next()
                    S.dma("sp", nxt[0][:], I["w_ada"][li, :, (nb + 1) * 512:(nb + 2) * 512].rearrange("(k p) n -> p k n", p=128),
                          writes=[nxt[1]])
                pr_, prb_ = rowr.next()

                def mm():
                    ins = None
                    for k in range(KD):
                        ins = nc.tensor.matmul(pr_[0:2, :], lhsT=sc[:, k, :], rhs=wa[:, k, :], start=(k == 0), stop=(k == KD - 1))
                    return ins
                S.op("pe", mm, reads=[wab, scb], writes=[prb_])
                S.op("act", lambda: nc.scalar.activation(out=mrow[0:2, nb * 512:(nb + 1) * 512], in_=pr_[0:2, :], func=AF.Copy), reads=[prb_], writes=[mrowb])

            def tps():
                ins = None
                for fc in range(96):
                    ins = nc.tensor.transpose(mps_[:, fc * 2:fc * 2 + 2], mrow[0:2, fc * 128:(fc + 1) * 128], C.ident_f[0:2, 0:2])
                return ins
            S.op("pe", tps, reads=[mrowb, C.ident_f_b], writes=[mpsb])
            m = Ctx()
            modt, modb = C.modalloc[li][0]
            bt, btb = S.sbuf("badat", [128, 96, 2], F32, es=ph)
            S.dma("sp", bt[:], I["b_adaT"][li], writes=[btb])
            S.op("dve", lambda: nc.vector.tensor_tensor(modt[:].rearrange("p a b -> p (a b)"), mps_[:, 0:192], bt[:].rearrange("p a b -> p (a b)"), ALU.add),
                 reads=[mpsb, btb], writes=[modb])
            m.t, m.b = modt, modb
            for nm, gname, j in (("A1", "g1T", 1), ("A2", "g2T", 4)):
                gt, gtb = S.sbuf("g" + nm, [128, KD, 2], F32, es=ph)
                S.dma("sp", gt[:], I[gname][li], writes=[gtb])
                at, atb = C.modalloc[li][1 if nm == "A1" else 2]
                S.op("dve", lambda: nc.vector.scalar_tensor_tensor(at[:], modt[:, j * 16:(j + 1) * 16, :], 1.0, gt[:], ALU.add, ALU.mult),
                     reads=[modb, gtb], writes=[atb])
                setattr(m, nm, at)
                setattr(m, nm + "b", atb)
            C.mod.append(m)
            if "dbg_mod" in C.dump:
                S.dma("sp", C.Sx["dbg_mod"][li], modt[:], reads=[modb])


def modsl(m, j):
    return m.t[:, j * 16:(j + 1) * 16, :]


def norm_blocks(C, ph, x_dram, A_ap, A_b, B_ap, B_b, cb, blocks=TB):
    S, nc = C.S, C.nc
    xr = Ring(S, ph, "xblk", [128, KD, 512], F32, 2)
    sqt, sqb = S.sbuf("nsq", [128, KD, 512], BF16, es=ph)
    ssr = Ring(S, ph, "nss", [128, 512], F32, 2, psum=True)
    rs, rsb = S.sbuf("nrstd", [128, 512], F32, es=ph)
    for bi, (t0, n, lc) in enumerate(blocks):
        xb, xbb = xr.next()
        S.dma("sp", xb[:, :, :n], x_dram[:, t0:t0 + n].rearrange("(k p) t -> p k t", p=128), writes=[xbb])
        S.op("act", lambda: nc.scalar.activation(out=sqt[:, :, :n], in_=xb[:, :, :n], func=AF.Square), reads=[xbb], writes=[sqb])
        ss, ssb = ssr.next()

        def mm():
            ins = None
            for k in range(KD):
                ins = nc.tensor.matmul(ss[:, :n], lhsT=C.ones_bf[:, :], rhs=sqt[:, k, :n], start=(k == 0), stop=(k == KD - 1))
            return ins
        S.op("pe", mm, reads=[sqb, C.ones_bf_b], writes=[ssb])
        S.op("act", lambda: nc.scalar.activation(out=rs[:, :n], in_=ss[:, :n], func=AF.Sqrt, bias=C.eps_t[:, 0:1], scale=1.0 / D),
             reads=[ssb, C.eps_b], writes=[rsb])
        S.op("dve", lambda: nc.vector.reciprocal(rs[:, :n], rs[:, :n]), reads=[rsb], writes=[rsb])

        def nrm():
            ins = None
            for k in range(KD):
                ins = nc.vector.tensor_tensor(xb[:, k, :n], xb[:, k, :n], rs[:, :n], ALU.mult)
            return ins
        S.op("dve", nrm, reads=[xbb, rsb], writes=[xbb])

        def aff_act():
            ins = None
            for k in range(0, KD, 2):
                ins = nc.scalar.activation(out=xb[:, k, :n], in_=xb[:, k, :n], func=AF.Identity,
                                           bias=B_ap[:, k, lc:lc + 1], scale=A_ap[:, k, lc:lc + 1])
            return ins

        def aff_pool():
            ins = None
            for k in range(1, KD, 2):
                ins = nc.gpsimd.tensor_scalar(xb[:, k, :n], xb[:, k, :n], A_ap[:, k, lc:lc + 1], B_ap[:, k, lc:lc + 1], ALU.mult, ALU.add)
            return ins
        S.op("act", aff_act, reads=[xbb, A_b, B_b], writes=[xbb])
        S.op("pool", aff_pool, reads=[xbb, A_b, B_b], writes=[xbb])
        cb(bi, t0, n, lc, xb, xbb)


def phase_in(C, li, x_dram):
    S, nc, I, Sx = C.S, C.nc, C.I, C.Sx
    m = C.mod[li]
    with ExitStack() as ph:
        hT, hTb = S.sbuf("hT", [128, KD, T], BF16, es=ph)
        with ExitStack() as ph1:
            def cb(bi, t0, n, lc, xb, xbb):
                S.op("dve", lambda: nc.vector.tensor_copy(hT[:, :, t0:t0 + n], xb[:, :, :n]), reads=[xbb], writes=[hTb])
            norm_blocks(C, ph1, x_dram, m.A1, m.A1b, modsl(m, 0), m.b, cb)
        if "dbg_hT" in C.dump:
            S.dma("sp", Sx["dbg_hT"].rearrange("(k p) t -> p k t", p=128), hT[:], reads=[hTb])
        if C.stop_after == (li, "in", "norm"):
            return
        S.barrier()
        wr = Ring(S, ph, "wt", [128, KD, 512], BF16, 2)
        pr = Ring(S, ph, "pin", [128, 512], F32, 4, psum=True)
        win = I["w_in"][li]

        def wload(segs):
            wt, wtb = wr.next()
            for si, (src, c0, ncol, off) in enumerate(segs):
                S.dma("pool", wt[:, :, off:off + ncol], src[:, c0:c0 + ncol].rearrange("(k p) n -> p k n", p=128),
                      writes=[wtb] if si == 0 else [], awrites=[] if si == 0 else [wtb])
            return wt, wtb

        def proj_fm(wt, wtb, woff, M, t0, n):
            ps, psb = pr.next()

            def mm():
                ins = None
                for k in range(KD):
                    ins = nc.tensor.matmul(ps[:M, :n], lhsT=wt[:, k, woff:woff + M], rhs=hT[:, k, t0:t0 + n], start=(k == 0), stop=(k == KD - 1))
                return ins
            S.op("pe", mm, reads=[wtb, hTb], writes=[psb])
            return ps, psb

        obr = Ring(S, ph, "ob", [128, 512], BF16, 4)
        ofr = Ring(S, ph, "of", [128, 512], F32, 3)

        wt, wtb = wload([(win, 0, 512, 0)])
        for (t0, n, lc) in TB:
            for c in range(4):
                ps, psb = proj_fm(wt, wtb, c * 128, 128, t0, n)
                ob, obb = obr.next()
                S.op("act", lambda: nc.scalar.activation(out=ob[:, :n], in_=ps[:, :n], func=AF.Gelu), reads=[psb], writes=[obb])
                S.dma("sp", Sx["uTg"][c * 128:(c + 1) * 128, t0:t0 + n], ob[:, :n], reads=[obb])

        if C.stop_after == (li, "in", "A"):
            return
        wt, wtb = wload([(win, 512, 512, 0)])
        gs, gsb = S.sbuf("gs", [128, 512], F32, es=ph)
        S.dma("sp", gs[:], I["sgu_g"][li].to_broadcast([128, 512]), writes=[gsb])
        junk, junkb = S.sbuf("junk", [128, 512], BF16, es=ph)
        st, stb = S.sbuf("vst", [128, 2], F32, es=ph)
        for tt in range(18):
            ps, psb = pr.next()

            def mm():
                ins = None
                for k in range(KD):
                    ins = nc.tensor.matmul(ps[:, :], lhsT=hT[:, k, tt * 128:(tt + 1) * 128], rhs=wt[:, k, :], start=(k == 0), stop=(k == KD - 1))
                return ins
            S.op("pe", mm, reads=[wtb, hTb], writes=[psb])
            gv, gvb = ofr.next()
            S.op("act", lambda: nc.scalar.activation(out=gv[:, :], in_=ps[:, :], func=AF.Gelu), reads=[psb], writes=[gvb])
            S.op("act", lambda: nc.scalar.activation(out=junk[:, :], in_=gv[:, :], func=AF.Square, accum_out=st[:, 0:1]),
                 reads=[gvb], writes=[junkb, stb])
            S.op("act", lambda: nc.scalar.activation(out=st[:, 1:2], in_=st[:, 0:1], func=AF.Sqrt, bias=C.eps_t[:, 0:1], scale=1.0 / 512),
                 reads=[stb, C.eps_b], writes=[stb])
            S.op("dve", lambda: nc.vector.reciprocal(st[:, 1:2], st[:, 1:2]), reads=[stb], writes=[stb])
            ob, obb = obr.next()
            S.op("dve", lambda: nc.vector.scalar_tensor_tensor(ob[:, :], gv[:, :], st[:, 1:2], gs[:, :], ALU.mult, ALU.mult),
                 reads=[gvb, stb, gsb], writes=[obb])
            S.dma("sp", Sx["v_tok"][tt * 128:(tt + 1) * 128, :], ob[:, :], reads=[obb])

        if C.stop_after == (li, "in", "v"):
            return
        wt, wtb = wload([(win, 1024, 512, 0)])
        for (t0, n, lc) in TB:
            for c in range(4):
                ps, psb = proj_fm(wt, wtb, c * 128, 128, t0, n)
                ob, obb = obr.next()
                S.op("dve" if c % 2 else "act", (lambda: nc.vector.tensor_copy(ob[:, :n], ps[:, :n])) if c % 2 else
                     (lambda: nc.scalar.activation(out=ob[:, :n], in_=ps[:, :n], func=AF.Copy)), reads=[psb], writes=[obb])
                dst = Sx["cqT"][c * 128:(c + 1) * 128, t0:t0 + n] if c < 3 else Sx["ckvT"][0:128, t0:t0 + n]
                S.dma("sp", dst, ob[:, :n], reads=[obb])

        if C.stop_after == (li, "in", "B"):
            return
        wt, wtb = wload([(win, 1536, 192, 0), (I["w_in_sw"][li], 0, 64, 192)])
        rc, rcb = S.sbuf("ropec", [64, T], F32, es=ph)
        rsn, rsnb = S.sbuf("ropes", [64, T], F32, es=ph)
        S.dma("sp", rc[:], I["rope_cos"][:, :], writes=[rcb])
        S.dma("sp", rsn[:], I["rope_sin"][:, :], writes=[rsnb])
        for (t0, n, lc) in TB:
            ps, psb = proj_fm(wt, wtb, 0, 128, t0, n)
            ob, obb = obr.next()
            S.op("act", lambda: nc.scalar.activation(out=ob[:, :n], in_=ps[:, :n], func=AF.Copy), reads=[psb], writes=[obb])
            S.dma("sp", Sx["ckvT"][128:256, t0:t0 + n], ob[:, :n], reads=[obb])
            ps1, ps1b = proj_fm(wt, wtb, 128, 64, t0, n)
            ps2, ps2b = proj_fm(wt, wtb, 192, 64, t0, n)
            f1, f1b = ofr.next()
            f2, f2b = ofr.next()
            S.op("dve", lambda: nc.vector.tensor_tensor(f1[:64, :n], ps1[:64, :n], rc[:, t0:t0 + n], ALU.mult), reads=[ps1b, rcb], writes=[f1b])
            S.op("dve", lambda: nc.vector.tensor_tensor(f2[:64, :n], ps2[:64, :n], rsn[:, t0:t0 + n], ALU.mult), reads=[ps2b, rsnb], writes=[f2b])
            ob, obb = obr.next()
            S.op("pool", lambda: nc.gpsimd.tensor_tensor(ob[:64, :n], f1[:64, :n], f2[:64, :n], ALU.add), reads=[f1b, f2b], writes=[obb])
            S.dma("sp", Sx["krT"][:, t0:t0 + n], ob[:64, :n], reads=[obb])

        if C.stop_after == (li, "in", "C"):
            return
        wt, wtb = wload([(win, 1728, 512, 0)])
        for (t0, n, lc) in TB:
            for c in range(4):
                ps, psb = proj_fm(wt, wtb, c * 128, 128, t0, n)
                of, ofb = ofr.next()
                ob, obb = obr.next()
                S.op("dve", lambda: nc.vector.tensor_copy(of[:, :n], ps[:, :n]), reads=[psb], writes=[ofb])
                S.op("act", lambda: nc.scalar.activation(out=ob[:, :n], in_=of[:, :n], func=AF.Copy), reads=[ofb], writes=[obb])
                S.dma("sp", Sx["s5uT"][c * 128:(c + 1) * 128, t0:t0 + n], ob[:, :n], reads=[obb])
                S.dma("sp", Sx["s5u32"][c * 128:(c + 1) * 128, t0:t0 + n], of[:, :n], reads=[ofb])

        if C.stop_after == (li, "in", "D"):
            return
        cw, cwb = S.sbuf("convw", [128, 4, 3], F32, es=ph)
        S.dma("sp", cw[:], I["conv_wT"][li], writes=[cwb])
        ZW = 2307
        zr = Ring(S, ph, "zbuf", [128, ZW], F32, 2)
        yr = Ring(S, ph, "ybuf", [128, ZW], F32, 2)
        bgr = Ring(S, ph, "bgbuf", [128, T], BF16, 2)
        cor = Ring(S, ph, "cobuf", [128, T], BF16, 2)

        def zoff(t0):
            return t0 + 1 if t0 < NL else t0 + 2
        for c in range(4):
            wt, wtb = wload([(win, 2240 + c * 128, 128, 0), (win, 2752 + c * 128, 128, 128), (win, 3264 + c * 128, 128, 256)])
            zb, zbb = zr.next()
            yb, ybb = yr.next()
            bg, bgb = bgr.next()
            co, cob = cor.next()

            def zz():
                nc.gpsimd.memset(zb[:, 0:1], 0.0)
                nc.gpsimd.memset(zb[:, NL + 1:NL + 2], 0.0)
                return nc.gpsimd.memset(zb[:, ZW - 1:ZW], 0.0)
            S.op("pool", zz, writes=[zbb])
            for (t0, n, lc) in TB:
                psB, psBb = proj_fm(wt, wtb, 0, 128, t0, n)
                psC, psCb = proj_fm(wt, wtb, 128, 128, t0, n)
                psH, psHb = proj_fm(wt, wtb, 256, 128, t0, n)
                S.op("act", lambda: nc.scalar.activation(out=bg[:, t0:t0 + n], in_=psB[:, :n], func=AF.Copy), reads=[psBb], writes=[bgb])
                of, ofb = ofr.next()
                S.op("act", lambda: nc.scalar.activation(out=of[:, :n], in_=psC[:, :n], func=AF.Copy), reads=[psCb], writes=[ofb])
                zo = zoff(t0)
                S.op("dve", lambda: nc.vector.tensor_tensor(zb[:, zo:zo + n], psH[:, :n], of[:, :n], ALU.mult), reads=[psHb, ofb], writes=[zbb])

            S.seq("dve", [
                lambda: nc.vector.tensor_scalar(yb[:, 1:ZW - 1], zb[:, 1:ZW - 1], cw[:, c, 1:2], None, ALU.mult),
                lambda: nc.vector.scalar_tensor_tensor(yb[:, 1:ZW - 1], zb[:, 0:ZW - 2], cw[:, c, 0:1], yb[:, 1:ZW - 1], ALU.mult, ALU.add),
                lambda: nc.vector.scalar_tensor_tensor(yb[:, 1:ZW - 1], zb[:, 2:ZW], cw[:, c, 2:3], yb[:, 1:ZW - 1], ALU.mult, ALU.add),
            ], reads=[zbb, cwb], writes=[ybb])

            def gate():
                nc.gpsimd.tensor_tensor(co[:, 0:NL], yb[:, 1:NL + 1], bg[:, 0:NL], ALU.mult)
                return nc.gpsimd.tensor_tensor(co[:, NL:T], yb[:, NL + 2:ZW - 1], bg[:, NL:T], ALU.mult)
            S.op("pool", gate, reads=[ybb, bgb], writes=[cob])
            S.dma("sp", Sx["catT"][1536 + c * 128:1536 + (c + 1) * 128, :], co[:, :], reads=[cob])


def phase_sgu(C, li):
    S, nc, I, Sx = C.S, C.nc, C.I, C.Sx
    with ExitStack() as ph:
        ws, wsb = S.sbuf("wsT", [128, 4, 128], BF16, es=ph)
        S.dma("pool", ws[:], I["sgu_wT"][li], writes=[wsb])
        bsr, bsrb = S.sbuf("bsr", [128, 4, 512], F32, es=ph)
        S.dma("sp", bsr[:].rearrange("p a b -> p (a b)"), I["sgu_b4"][li].to_broadcast([128, 2048]), writes=[bsrb])
        vr = Ring(S, ph, "vt", [128, 4, 512], BF16, 2)
        ur = Ring(S, ph, "ut", [128, 4, 512], BF16, 2)
        pr = Ring(S, ph, "psg", [128, 512], F32, 4, psum=True)
        tr = Ring(S, ph, "tmp", [128, 512], F32, 3)
        orr = Ring(S, ph, "osg", [128, 512], BF16, 3)
        for (t0, n, lc) in TB:
            na = n // 128
            vt, vtb = vr.next()
            ut, utb = ur.next()
            S.dma("sp", vt[:, :na, :], Sx["v_tok"][t0:t0 + n, :].rearrange("(a q) c -> q a c", q=128), writes=[vtb])
            S.dma("sp", ut[:, :, :n], Sx["uTg"][:, t0:t0 + n].rearrange("(h c) t -> c h t", c=128), writes=[utb])
            for h in range(4):
                ps, psb = pr.next()

                def mm():
                    ins = None
                    for a in range(na):
                        ins = nc.tensor.matmul(ps[:, a * 128:(a + 1) * 128], lhsT=vt[:, a, h * 128:(h + 1) * 128], rhs=ws[:, h, :], start=True, stop=True)
                    return ins
                S.op("pe", mm, reads=[vtb, wsb], writes=[psb])
                tm, tmb = tr.next()
                S.op("dve", lambda: nc.vector.tensor_tensor(tm[:, :n], ps[:, :n], bsr[:, h, :n], ALU.add), reads=[psb, bsrb], writes=[tmb])
                ob, obb = orr.next()
                S.op("pool", lambda: nc.gpsimd.tensor_tensor(ob[:, :n], tm[:, :n], ut[:, h, :n], ALU.mult), reads=[tmb, utb], writes=[obb])
                S.dma("sp", Sx["catT"][h * 128:(h + 1) * 128, t0:t0 + n], ob[:, :n], reads=[obb])


def phase_mla(C, li):
    S, nc, I, Sx = C.S, C.nc, C.I, C.Sx
    SC = 192.0 ** -0.5
    with ExitStack() as ph:
        cq, cqb = S.sbuf("cq", [128, 3, T], BF16, es=ph)
        ckv, ckvb = S.sbuf("ckv", [128, 2, T], BF16, es=ph)
        kr, krb = S.sbuf("kr", [64, T], BF16, es=ph)
        S.dma("sp", cq[:], Sx["cqT"].rearrange("(c p) t -> p c t", p=128), writes=[cqb])
        S.dma("sp", ckv[:], Sx["ckvT"].rearrange("(c p) t -> p c t", p=128), writes=[ckvb])
        S.dma("sp", kr[:], Sx["krT"][:, :], writes=[krb])
        wuq, wuqb = S.sbuf("wuq", [128, 3, 768], BF16, es=ph)
        wuqs, wuqsb = S.sbuf("wuqs", [128, 3, 256], BF16, es=ph)
        wukv, wukvb = S.sbuf("wukv", [128, 2, 1024], BF16, es=ph)
        wv, wvb = S.sbuf("wv", [128, 2, 512], BF16, es=ph)
        rq, rqb = S.sbuf("rq", [128, T], F32, es=ph)
        rk, rkb = S.sbuf("rk", [128, T], F32, es=ph)
        rkt, rktb = S.sbuf("rkt", [128, 18], F32, es=ph)
        pr = Ring(S, ph, "pj", [128, 512], F32, 3, psum=True)
        pst_, pstb = S.psum("pst", [128, 512], F32, es=ph)
        pst = pst_[:, 0:18]
        with ExitStack() as wp:
            w32, w32b = S.sbuf("w32", [128, 3, 768], F32, es=wp)
            ws32, ws32b = S.sbuf("ws32", [128, 3, 256], F32, es=wp)
            wk32, wk32b = S.sbuf("wk32", [128, 2, 1024], F32, es=wp)
            qg, qgb = S.sbuf("qg", [128, 3], F32, es=wp)
            kg, kgb = S.sbuf("kg", [128, 2], F32, es=wp)
            S.dma("sp", w32[:], I["w_uq"][li].rearrange("(c p) n -> p c n", p=128), writes=[w32b])
            S.dma("sp", ws32[:], I["w_uq_sw"][li].rearrange("(c p) n -> p c n", p=128), writes=[ws32b])
            S.dma("sp", wk32[:], I["w_ukv"][li].rearrange("(c p) n -> p c n", p=128), writes=[wk32b])
            S.dma("sp", qg[:], I["qg"][li], writes=[qgb])
            S.dma("sp", kg[:], I["kvg"][li], writes=[kgb])

            def sc1():
                ins = None
                for c in range(3):
                    nc.vector.tensor_scalar(wuq[:, c, :], w32[:, c, :], qg[:, c:c + 1], None, ALU.mult)
                    ins = nc.vector.tensor_scalar(wuqs[:, c, :], ws32[:, c, :], qg[:, c:c + 1], None, ALU.mult)
                return ins
            S.op("dve", sc1, reads=[w32b, ws32b, qgb], writes=[wuqb, wuqsb])

            def sc2():
                ins = None
                for c in range(2):
                    ins = nc.vector.tensor_scalar(wukv[:, c, :], wk32[:, c, :], kg[:, c:c + 1], None, ALU.mult)
                return ins
            S.op("dve", sc2, reads=[wk32b, kgb], writes=[wukvb])
            S.op("dve", lambda: nc.vector.tensor_copy(wv[:].rearrange("p c (h x) -> p c h x", x=128),
                                                       wukv[:].rearrange("p c (h x) -> p c h x", x=256)[:, :, :, 128:256]),
                 reads=[wukvb], writes=[wvb])
            sqq, sqqb = S.sbuf("sqq", [128, 3, T], BF16, es=wp)
            sqk, sqkb = S.sbuf("sqk", [128, 2, T], BF16, es=wp)
            S.op("act", lambda: nc.scalar.activation(out=sqq[:], in_=cq[:], func=AF.Square), reads=[cqb], writes=[sqqb])
            S.op("act", lambda: nc.scalar.activation(out=sqk[:], in_=ckv[:], func=AF.Square), reads=[ckvb], writes=[sqkb])
            for (sq, sqb_, nch, rt, rtb) in ((sqq, sqqb, 3, rq, rqb), (sqk, sqkb, 2, rk, rkb)):
                for (t0, n, lc) in TB:
                    ps, psb = pr.next()

                    def mm():
                        ins = None
                        for c in range(nch):
                            ins = nc.tensor.matmul(ps[:, :n], lhsT=C.ones_bf[:, :], rhs=sq[:, c, t0:t0 + n], start=(c == 0), stop=(c == nch - 1))
                        return ins
                    S.op("pe", mm, reads=[sqb_, C.ones_bf_b], writes=[psb])
                    S.op("act", lambda: nc.scalar.activation(out=rt[:, t0:t0 + n], in_=ps[:, :n], func=AF.Sqrt, bias=C.eps_t[:, 0:1],
                                                             scale=1.0 / (128 * nch)), reads=[psb, C.eps_b], writes=[rtb])
            S.op("dve", lambda: nc.vector.reciprocal(rq[:], rq[:]), reads=[rqb], writes=[rqb])
            S.op("dve", lambda: nc.vector.reciprocal(rk[:], rk[:]), reads=[rkb], writes=[rkb])

            def mmt():
                ins = None
                for tt in range(18):
                    for c in range(2):
                        ins = nc.tensor.matmul(pst[:, tt:tt + 1], lhsT=sqk[:, c, tt * 128:(tt + 1) * 128], rhs=C.ones_bf[:, 0:1],
                                               start=(c == 0), stop=(c == 1))
                return ins
            S.op("pe", mmt, reads=[sqkb, C.ones_bf_b], writes=[pstb])
            S.op("act", lambda: nc.scalar.activation(out=rkt[:], in_=pst_[:, 0:18], func=AF.Sqrt, bias=C.eps_t[:, 0:1], scale=1.0 / 256),
                 reads=[pstb, C.eps_b], writes=[rktb])
            S.op("dve", lambda: nc.vector.reciprocal(rkt[:], rkt[:]), reads=[rktb], writes=[rktb])
            S.barrier()
        rc, rcb = S.sbuf("ropec", [64, T], F32, es=ph)
        rsn, rsnb = S.sbuf("ropes", [64, T], F32, es=ph)
        S.dma("sp", rc[:], I["rope_cos"][:, :], writes=[rcb])
        S.dma("sp", rsn[:], I["rope_sin"][:, :], writes=[rsnb])
        qn, qnb = S.sbuf("qn", [128, 4, T], BF16, es=ph)
        qr, qrb = S.sbuf("qr", [64, 4, T], BF16, es=ph)
        kn, knb = S.sbuf("kn", [128, 4, T], BF16, es=ph)
        vtok, vtokb = S.sbuf("vtok", [128, 18, 512], BF16, es=ph)
        fr = Ring(S, ph, "mf", [128, 512], F32, 4)

        def proj(wt, wtb, nch, c0, M, src, srcb, t0, n):
            ps, psb = pr.next()

            def mm():
                ins = None
                for c in range(nch):
                    ins = nc.tensor.matmul(ps[:M, :n], lhsT=wt[:, c, c0:c0 + M], rhs=src[:, c, t0:t0 + n], start=(c == 0), stop=(c == nch - 1))
                return ins
            S.op("pe", mm, reads=[wtb, srcb], writes=[psb])
            return ps, psb
        for h in range(4):
            for (t0, n, lc) in TB:
                ps, psb = proj(wuq, wuqb, 3, h * 192, 128, cq, cqb, t0, n)
                S.op("dve", lambda: nc.vector.scalar_tensor_tensor(qn[:, h, t0:t0 + n], ps[:, :n], SC, rq[:, t0:t0 + n], ALU.mult, ALU.mult),
                     reads=[psb, rqb], writes=[qnb])
                ps1, ps1b = proj(wuq, wuqb, 3, h * 192 + 128, 64, cq, cqb, t0, n)
                ps2, ps2b = proj(wuqs, wuqsb, 3, h * 64, 64, cq, cqb, t0, n)
                f1, f1b = fr.next()
                f2, f2b = fr.next()
                S.op("dve", lambda: nc.vector.tensor_tensor(f1[:64, :n], ps1[:64, :n], rc[:, t0:t0 + n], ALU.mult), reads=[ps1b, rcb], writes=[f1b])
                S.op("dve", lambda: nc.vector.tensor_tensor(f2[:64, :n], ps2[:64, :n], rsn[:, t0:t0 + n], ALU.mult), reads=[ps2b, rsnb], writes=[f2b])
                S.op("pool", lambda: nc.gpsimd.tensor_tensor(f1[:64, :n], f1[:64, :n], f2[:64, :n], ALU.add), reads=[f1b, f2b], writes=[f1b])
                S.op("dve", lambda: nc.vector.scalar_tensor_tensor(qr[:, h, t0:t0 + n], f1[:64, :n], SC, rq[:64, t0:t0 + n], ALU.mult, ALU.mult),
                     reads=[f1b, rqb], writes=[qrb])
                ps, psb = proj(wukv, wukvb, 2, h * 256, 128, ckv, ckvb, t0, n)
                S.op("dve", lambda: nc.vector.tensor_tensor(kn[:, h, t0:t0 + n], ps[:, :n], rk[:, t0:t0 + n], ALU.mult), reads=[psb, rkb], writes=[knb])
        for tt in range(18):
            ps, psb = pr.next()

            def mm():
                ins = None
                for c in range(2):
                    ins = nc.tensor.matmul(ps[:, :], lhsT=ckv[:, c, tt * 128:(tt + 1) * 128], rhs=wv[:, c, :], start=(c == 0), stop=(c == 1))
                return ins
            S.op("pe", mm, reads=[ckvb, wvb], writes=[psb])
            S.op("dve", lambda: nc.vector.tensor_scalar(vtok[:, tt, :], ps[:, :], rkt[:, tt:tt + 1], None, ALU.mult), reads=[psb, rktb], writes=[vtokb])
        opr = Ring(S, ph, "ops", [128, 512], F32, 2, psum=True)
        spr = Ring(S, ph, "sps", [128, 512], F32, 2, psum=True)
        ptr = Ring(S, ph, "pt", [128, 512], BF16, 3)
        rsr = Ring(S, ph, "rsm", [128, 512], F32, 2)
        atr = Ring(S, ph, "att", [128, 512], BF16, 2)
        for h in range(4):
            for (t0, n, lc) in TB:
                kts = list(range(18)) if lc == 0 else [16, 17]
                ops, opsb = opr.next()
                sps, spsb = spr.next()
                pend = None
                nk = len(kts)

                def pv(pd):
                    kt_, pt_, ptb_, i_ = pd

                    def mmo():
                        nc.tensor.matmul(ops[:, :n], lhsT=vtok[:, kt_, h * 128:(h + 1) * 128], rhs=pt_[:, :n], start=(i_ == 0), stop=(i_ == nk - 1))
                        return nc.tensor.matmul(sps[:, :n], lhsT=C.ones_bf[:, :], rhs=pt_[:, :n], start=(i_ == 0), stop=(i_ == nk - 1))
                    S.op("pe", mmo, reads=[vtokb, ptb_, C.ones_bf_b], writes=[opsb, spsb])
                for i, kt in enumerate(kts):
                    st, stb = pr.next()

                    def mms():
                        nc.tensor.matmul(st[:, :n], lhsT=kn[:, h, kt * 128:(kt + 1) * 128], rhs=qn[:, h, t0:t0 + n], start=True, stop=False)
                        return nc.tensor.matmul(st[:, :n], lhsT=kr[:, kt * 128:(kt + 1) * 128], rhs=qr[:, h, t0:t0 + n], start=False, stop=True)
                    S.op("pe", mms, reads=[knb, qnb, krb, qrb], writes=[stb])
                    pt, ptb = ptr.next()
                    S.op("act", lambda: nc.scalar.activation(out=pt[:, :n], in_=st[:, :n], func=AF.Exp), reads=[stb], writes=[ptb])
                    if pend is not None:
                        pv(pend)
                    pend = (kt, pt, ptb, i)
                pv(pend)
                rs_, rsb_ = rsr.next()
                S.op("dve", lambda: nc.vector.reciprocal(rs_[:, :n], sps[:, :n]), reads=[spsb], writes=[rsb_])
                at, atb = atr.next()
                S.op("dve", lambda: nc.vector.tensor_tensor(at[:, :n], ops[:, :n], rs_[:, :n], ALU.mult), reads=[opsb, rsb_], writes=[atb])
                S.dma("sp", Sx["catT"][512 + h * 128:512 + (h + 1) * 128, t0:t0 + n], at[:, :n], reads=[atb])


def _s5_disc(C, es, are, aim, ldt, n, need_coef, tagb):
    S, nc = C.S, C.nc
    X = [S.sbuf("s5x%d" % i, [128, n], F32, es=es) for i in range(6)]
    KI = S.sbuf("s5ki", [128, n], I32, es=es)
    (x1, b1), (x2, b2), (x3, b3), (x4, b4), (x5, b5), (x6, b6) = X
    ki, kib = KI
    S.op("dve", lambda: nc.vector.tensor_scalar(are, are, -1e-4, None, ALU.min), reads=[tagb], writes=[tagb])
    ea = [
        lambda: nc.vector.tensor_scalar(ki[:], ldt, 1.0 / math.log(2.0), None, ALU.mult),
        lambda: nc.vector.tensor_copy(x3[:], ki[:]),
        lambda: nc.vector.scalar_tensor_tensor(x4[:], x3[:], -0.693145751953125, ldt, ALU.mult, ALU.add),
        lambda: nc.vector.scalar_tensor_tensor(x4[:], x3[:], -1.42860682030941723212e-6, x4[:], ALU.mult, ALU.add),
        lambda: nc.vector.tensor_scalar(x5[:], x4[:], 1.0 / 9.0, 1.0, ALU.mult, ALU.add),
    ]
    for j in range(8, 0, -1):
        ea.append(lambda: nc.vector.tensor_tensor(x5[:], x5[:], x4[:], ALU.mult))
        ea.append(lambda j=j: nc.vector.tensor_scalar(x5[:], x5[:], 1.0 / j, 1.0, ALU.mult, ALU.add))
    ea.append(lambda: nc.vector.tensor_scalar(ki[:], x3[:], 127.0, 8388608.0, ALU.add, ALU.mult))
    ea.append(lambda: nc.vector.tensor_tensor(ldt, x5[:], ki[:].bitcast(F32), ALU.mult))
    S.seq("dve", ea, reads=[tagb], writes=[tagb, kib, b3, b4, b5])
    S.op("dve", lambda: nc.vector.tensor_tensor(x1[:], are, ldt, ALU.mult), reads=[tagb], writes=[b1])
    S.op("act", lambda: nc.scalar.activation(out=x1[:], in_=x1[:], func=AF.Exp), reads=[b1], writes=[b1])
    S.op("dve", lambda: nc.vector.tensor_tensor(x2[:], aim, ldt, ALU.mult), reads=[tagb], writes=[b2])
    S.op("dve", lambda: nc.vector.tensor_scalar(x2[:], x2[:], 1.0 / (2 * math.pi), None, ALU.mult), reads=[b2], writes=[b2])
    if not need_coef:
        return {"r": (x1, b1), "f": (x2, b2)}
    S.op("dve", lambda: nc.vector.tensor_copy(ki[:], x2[:]), reads=[b2], writes=[kib])
    S.op("dve", lambda: nc.vector.tensor_tensor(x3[:], x2[:], ki[:], ALU.subtract), reads=[b2, kib], writes=[b3])
    S.op("act", lambda: nc.scalar.activation(out=x4[:], in_=x3[:], func=AF.Sin, scale=TWO_PI), reads=[b3], writes=[b4])
    S.op("dve", lambda: nc.vector.tensor_scalar(ki[:], x2[:], 0.25, None, ALU.add), reads=[b2], writes=[kib])
    S.op("dve", lambda: nc.vector.tensor_tensor(x3[:], x2[:], ki[:], ALU.subtract), reads=[b2, kib], writes=[b3])
    S.op("act", lambda: nc.scalar.activation(out=x5[:], in_=x3[:], func=AF.Sin, scale=TWO_PI, bias=C.hpi_t[:, 0:1]), reads=[b3, C.hpi_b], writes=[b5])
    S.op("dve", lambda: nc.vector.tensor_tensor(x5[:], x1[:], x5[:], ALU.mult), reads=[b1, b5], writes=[b5])
    S.op("dve", lambda: nc.vector.tensor_scalar(x5[:], x5[:], -1.0, None, ALU.add), reads=[b5], writes=[b5])
    S.op("dve", lambda: nc.vector.tensor_tensor(x4[:], x1[:], x4[:], ALU.mult), reads=[b1, b4], writes=[b4])
    S.op("dve", lambda: nc.vector.tensor_tensor(x1[:], are, are, ALU.mult), reads=[tagb], writes=[b1])
    S.op("dve", lambda: nc.vector.tensor_tensor(x3[:], aim, aim, ALU.mult), reads=[tagb], writes=[b3])
    S.op("dve", lambda: nc.vector.tensor_tensor(x1[:], x1[:], x3[:], ALU.add), reads=[b1, b3], writes=[b1])
    S.op("dve", lambda: nc.vector.reciprocal(x1[:], x1[:]), reads=[b1], writes=[b1])
    S.op("dve", lambda: nc.vector.tensor_tensor(x2[:], x5[:], are, ALU.mult), reads=[b5, tagb], writes=[b2])
    S.op("dve", lambda: nc.vector.tensor_tensor(x3[:], x4[:], aim, ALU.mult), reads=[b4, tagb], writes=[b3])
    S.op("dve", lambda: nc.vector.tensor_tensor(x2[:], x2[:], x3[:], ALU.add), reads=[b2, b3], writes=[b2])
    S.op("dve", lambda: nc.vector.tensor_tensor(x2[:], x2[:], x1[:], ALU.mult), reads=[b2, b1], writes=[b2])
    S.op("dve", lambda: nc.vector.tensor_tensor(x6[:], x4[:], are, ALU.mult), reads=[b4, tagb], writes=[b6])
    S.op("dve", lambda: nc.vector.tensor_tensor(x3[:], x5[:], aim, ALU.mult), reads=[b5, tagb], writes=[b3])
    S.op("dve", lambda: nc.vector.tensor_tensor(x6[:], x6[:], x3[:], ALU.subtract), reads=[b6, b3], writes=[b6])
    S.op("dve", lambda: nc.vector.tensor_tensor(x6[:], x6[:], x1[:], ALU.mult), reads=[b6, b1], writes=[b6])
    return {"cre": (x2, b2), "cim": (x6, b6), "t": [(x1, b1), (x3, b3), (x4, b4), (x5, b5)]}


def phase_s5(C, li):
    S, nc, I, Sx = C.S, C.nc, C.I, C.Sx
    with ExitStack() as ph:
        BbR, BbRb = S.sbuf("BbR", [128, 32, 128], BF16, es=ph)
        BbI, BbIb = S.sbuf("BbI", [128, 32, 128], BF16, es=ph)
        CR, CRb = S.sbuf("CR", [128, 32, 128], BF16, es=ph)
        CIn, CInb = S.sbuf("CIn", [128, 32, 128], BF16, es=ph)
        CRn, CRnb = S.sbuf("CRn", [128, 32, 128], BF16, es=ph)
        pp, ppb = S.sbuf("s5pp", [128, 3, 32], F32, es=ph)
        S.dma("sp", pp[:], I["s5_pp"][li], writes=[ppb])
        dpp = _s5_disc(C, ph, pp[:, 0, :], pp[:, 1, :], pp[:, 2, :], 32, False, ppb)
        rpp, rppb = dpp["r"]
        fpp, fppb = dpp["f"]
        with ExitStack() as wp:
            pp2, pp2b = S.sbuf("s5pp2", [128, 3, 32], F32, es=wp)
            S.dma("sp", pp2[:], I["s5_pp"][li], writes=[pp2b])
            dc = _s5_disc(C, wp, pp2[:, 0, :], pp2[:, 1, :], pp2[:, 2, :], 32, True, pp2b)
            cre, creb = dc["cre"]
            cim, cimb = dc["cim"]
            onesf, onesfb = S.sbuf("onesf", [128, 128], F32, es=wp)
            S.op("pool", lambda: nc.gpsimd.memset(onesf[:], 1.0), writes=[onesfb])
            Bre, Breb = S.sbuf("Bre32", [128, 4096], F32, es=wp)
            Bim, Bimb = S.sbuf("Bim32", [128, 4096], F32, es=wp)
            Cre, Creb = S.sbuf("Cre32", [128, 4096], F32, es=wp)
            Cim, Cimb = S.sbuf("Cim32", [128, 4096], F32, es=wp)
            S.dma("sp", Bre[:], I["s5_Bre"][li], writes=[Breb])
            S.dma("sp", Bim[:], I["s5_Bim"][li], writes=[Bimb])
            S.dma("sp", Cre[:], I["s5_Cre"][li], writes=[Creb])
            S.dma("sp", Cim[:], I["s5_Cim"][li], writes=[Cimb])
            S.op("act", lambda: nc.scalar.activation(out=CR[:].rearrange("p a b -> p (a b)"), in_=Cre[:], func=AF.Copy), reads=[Creb], writes=[CRb])
            S.op("act", lambda: nc.scalar.activation(out=CIn[:].rearrange("p a b -> p (a b)"), in_=Cim[:], func=AF.Copy, scale=-1.0), reads=[Cimb], writes=[CInb])
            S.op("act", lambda: nc.scalar.activation(out=CRn[:].rearrange("p a b -> p (a b)"), in_=Cre[:], func=AF.Copy, scale=-1.0), reads=[Creb], writes=[CRnb])
            dgr = Ring(S, wp, "dg", [128, 4, 128], F32, 4)
            rpr = Ring(S, wp, "rp", [128, 512], F32, 4, psum=True)
            t3r = Ring(S, wp, "s5p3", [128, 512], F32, 2)
            t4r = Ring(S, wp, "s5p4", [128, 512], F32, 2)
            for g4 in range(8):
                reps = []
                for (v, vb) in ((cre, creb), (cim, cimb)):
                    dg, dgb = dgr.next()

                    def mkd():
                        ins = None
                        for j in range(4):
                            ins = nc.vector.tensor_scalar(dg[:, j, :], C.ident_f[:, :], v[:, g4 * 4 + j:g4 * 4 + j + 1], None, ALU.mult)
                        return ins
                    S.op("dve", mkd, reads=[vb, C.ident_f_b], writes=[dgb])
                    rp, rpb = rpr.next()

                    def mmr():
                        ins = None
                        for j in range(4):
                            ins = nc.tensor.matmul(rp[:, j * 128:(j + 1) * 128], lhsT=onesf[:, :], rhs=dg[:, j, :], start=True, stop=True)
                        return ins
                    S.op("pe", mmr, reads=[onesfb, dgb], writes=[rpb])
                    reps.append((rp, rpb))
                (rcr, rcrb), (rci, rcib) = reps
                sl = slice(g4 * 512, (g4 + 1) * 512)
                osl = slice(g4 * 4, (g4 + 1) * 4)
                t3, t3b = t3r.next()
                t4, t4b = t4r.next()
                S.op("dve", lambda: nc.vector.tensor_tensor(t3[:], rcr[:, :], Bre[:, sl], ALU.mult), reads=[rcrb, Breb], writes=[t3b])
                S.op("dve", lambda: nc.vector.tensor_tensor(t4[:], rci[:, :], Bim[:, sl], ALU.mult), reads=[rcib, Bimb], writes=[t4b])
                S.op("dve", lambda: nc.vector.tensor_tensor(BbR[:, osl, :].rearrange("p a b -> p (a b)"), t3[:], t4[:], ALU.subtract),
                     reads=[t3b, t4b], writes=[BbRb])
                t3, t3b = t3r.next()
                t4, t4b = t4r.next()
                S.op("dve", lambda: nc.vector.tensor_tensor(t3[:], rcr[:, :], Bim[:, sl], ALU.mult), reads=[rcrb, Bimb], writes=[t3b])
                S.op("dve", lambda: nc.vector.tensor_tensor(t4[:], rci[:, :], Bre[:, sl], ALU.mult), reads=[rcib, Breb], writes=[t4b])
                S.op("dve", lambda: nc.vector.tensor_tensor(BbI[:, osl, :].rearrange("p a b -> p (a b)"), t3[:], t4[:], ALU.add),
                     reads=[t3b, t4b], writes=[BbIb])
            S.barrier()
        TA = 1536
        mst = ExitStack()
        ubr = Ring(S, mst, "ubf", [128, T], BF16, 2)
        tau, taub = S.sbuf("tau", [128, 2, T], F32, es=mst)
        S.dma("sp", tau[:].rearrange("p a b -> p (a b)"), I["s5_tau"][0:1, :].to_broadcast([128, 2 * T]), writes=[taub])

        def two(name, dt):
            t, ba = S.sbuf(name, [128, T], dt, es=mst)
            bb = Buf(name + "B")
            S.bufs.append(bb)
            return t, (ba, bb)
        tabr = [(two("cosT%d" % i, F32), two("sinT%d" % i, F32)) for i in range(2)]
        kis, kisb = S.sbuf("kis", [128, T], I32, es=mst)
        evr, evrb = two("evr", F32)
        evi, evib = two("evi", F32)
        bre, breb = two("bre", F32)
        bim, bimb = two("bim", F32)
        sre, sreb = two("sre", F32)
        sim, simb = two("sim", F32)
        pa, pab = two("pa", BF16)
        pb, pbb = two("pb", BF16)
        pc, pcb = two("pc", BF16)
        pd, pdb = two("pd", BF16)
        dsk, dskb = S.sbuf("dsk", [128, 4], F32, es=mst)
        S.dma("sp", dsk[:], I["s5_d"][li], writes=[dskb])
        yps = [S.psum("yps%d" % i, [128, 512], F32, es=mst) for i in range(5)]
        bur = Ring(S, mst, "bu", [128, 512], F32, 3, psum=True)
        tr = Ring(S, mst, "s5t", [128, 512], F32, 4)
        gor = Ring(S, mst, "s5g", [128, 512], BF16, 3)
        iters = [(ct, d, ns) for ct in range(4) for d in range(2) for ns in range(4)]

        def ew(o, ob, x, xb, y, yb, op):
            S.op("dve", lambda: nc.vector.tensor_tensor(o[:], x[:], y[:], op), reads=list(xb) + list(yb), writes=list(ob))

        def colof(it):
            ct, d, ns = it
            return (d * 4 + ct) * 4 + ns
        tabs = {}

        def tab_pool(i):
            pass

        def tab_rest(i):
            ct, d, ns = iters[i]
            fcol = fpp[:, colof(iters[i]):colof(iters[i]) + 1]
            (cosT, cosb), (sinT, sinb) = tabr[i % 2]
            S.op("dve", lambda: nc.vector.tensor_scalar(kis[:], tau[:, d, :], fcol, None, ALU.mult), reads=[taub, fppb], writes=[kisb])
            S.op("dve", lambda: nc.vector.scalar_tensor_tensor(sinT[:], tau[:, d, :], fcol, kis[:], ALU.mult, ALU.subtract),
                 reads=[taub, fppb, kisb], writes=list(sinb))
            S.op("act", lambda: nc.scalar.activation(out=cosT[:], in_=sinT[:], func=AF.Abs), reads=list(sinb), writes=list(cosb))
            S.op("act", lambda: nc.scalar.activation(out=sinT[:], in_=sinT[:], func=AF.Sin, scale=TWO_PI), reads=list(sinb) + list(cosb), writes=list(sinb))
            S.op("act", lambda: nc.scalar.activation(out=cosT[:], in_=cosT[:], func=AF.Sin, scale=-TWO_PI, bias=C.hpi_t[:, 0:1]),
                 reads=list(cosb) + [C.hpi_b], writes=list(cosb))
            tabs[i] = (cosT, cosb, sinT, sinb)
        tab_pool(0)
        tab_rest(0)
        ub_next = ubr.next()
        S.dma("sp", ub_next[0][:], Sx["s5uT"][0:128, :], writes=[ub_next[1]])
        for idx, (ct, d, ns) in enumerate(iters):
            col = colof((ct, d, ns))
            if d == 0 and ns == 0:
                ubf, ubfb = ub_next
                if ct < 3:
                    ub_next = ubr.next()
                    S.dma("sp", ub_next[0][:], Sx["s5uT"][(ct + 1) * 128:(ct + 2) * 128, :], writes=[ub_next[1]])
            cosT, cosb, sinT, sinb = tabs.pop(idx)
            if idx + 1 < len(iters):
                tab_pool(idx + 1)
            for (t0, n, lc) in TB:
                part = 0 if t0 < TA else 1
                pr_, prb_ = bur.next()
                pi_, pib_ = bur.next()
                S.op("pe", lambda: nc.tensor.matmul(pr_[:, :n], lhsT=BbR[:, col, :], rhs=ubf[:, t0:t0 + n], start=True, stop=True),
                     reads=[BbRb, ubfb], writes=[prb_])
                S.op("pe", lambda: nc.tensor.matmul(pi_[:, :n], lhsT=BbI[:, col, :], rhs=ubf[:, t0:t0 + n], start=True, stop=True),
                     reads=[BbIb, ubfb], writes=[pib_])
                S.op("act", lambda: nc.scalar.activation(out=evr[:, t0:t0 + n], in_=pr_[:, :n], func=AF.Copy), reads=[prb_], writes=[evrb[part]])
                S.op("act", lambda: nc.scalar.activation(out=evi[:, t0:t0 + n], in_=pi_[:, :n], func=AF.Copy), reads=[pib_], writes=[evib[part]])
            ew(bre, breb, evr, evrb, cosT, cosb, ALU.mult)
            ew(sre, sreb, evi, evib, sinT, sinb, ALU.mult)
            ew(bim, bimb, evi, evib, cosT, cosb, ALU.mult)
            ew(sim, simb, evr, evrb, sinT, sinb, ALU.mult)
            ew(bre, breb, bre, breb, sre, sreb, ALU.add)
            ew(bim, bimb, bim, bimb, sim, simb, ALU.subtract)
            if idx + 1 < len(iters):
                tab_rest(idx + 1)
            rdec = rpp[:, col:col + 1]
            sq_ = []
            for (src, dst) in ((bre, sre), (bim, sim)):
                if d == 0:
                    sq_.append(lambda src=src, dst=dst: nc.vector.tensor_tensor_scan(dst[:, NL:T], rdec.to_broadcast([128, NX]), src[:, NL:T], 0.0, ALU.mult, ALU.add))
                else:
                    sq_.append(lambda src=src, dst=dst: nc.vector.tensor_tensor_scan(dst[:, NL:T][:, ::-1], rdec.to_broadcast([128, NX]), src[:, NL:T][:, ::-1], 0.0, ALU.mult, ALU.add))
            for (src, dst) in ((bre, sre), (bim, sim)):
                if d == 0:
                    sq_.append(lambda src=src, dst=dst: nc.vector.tensor_tensor_scan(dst[:, 0:NL], rdec.to_broadcast([128, NL]), src[:, 0:NL], dst[:, T - 1:T], ALU.mult, ALU.add))
                else:
                    sq_.append(lambda src=src, dst=dst: nc.vector.tensor_tensor_scan(dst[:, 0:NL][:, ::-1], rdec.to_broadcast([128, NL]), src[:, 0:NL][:, ::-1],
                                                                                     dst[:, NL:NL + 1], ALU.mult, ALU.add))
            S.seq("dve", sq_, reads=list(breb) + list(bimb) + [rppb], writes=list(sreb) + list(simb))
            ew(pa, pab, sre, sreb, cosT, cosb, ALU.mult)
            ew(pb, pbb, sim, simb, sinT, sinb, ALU.mult)
            ew(pc, pcb, sre, sreb, sinT, sinb, ALU.mult)
            ew(pd, pdb, sim, simb, cosT, cosb, ALU.mult)
            first = (d == 0 and ns == 0)
            last = (d == 1 and ns == 3)
            for bi, (t0, n, lc) in enumerate(TB):
                yp, ypb = yps[bi]

                def rd():
                    nc.tensor.matmul(yp[:, :n], lhsT=CR[:, col, :], rhs=pa[:, t0:t0 + n], start=first, stop=False)
                    nc.tensor.matmul(yp[:, :n], lhsT=CRn[:, col, :], rhs=pb[:, t0:t0 + n], start=False, stop=False)
                    nc.tensor.matmul(yp[:, :n], lhsT=CIn[:, col, :], rhs=pc[:, t0:t0 + n], start=False, stop=False)
                    return nc.tensor.matmul(yp[:, :n], lhsT=CIn[:, col, :], rhs=pd[:, t0:t0 + n], start=False, stop=last)
                S.op("pe", rd, reads=[CRb, CRnb, CInb] + list(pab) + list(pbb) + list(pcb) + list(pdb), writes=[ypb])
            if last:
                for bi, (t0, n, lc) in enumerate(TB):
                    yp, ypb = yps[bi]
                    u32, u32b = tr.next()
                    S.dma("sp", u32[:, :n], Sx["s5u32"][ct * 128:(ct + 1) * 128, t0:t0 + n], writes=[u32b])
                    a1, a1b = tr.next()
                    S.op("dve", lambda: nc.vector.scalar_tensor_tensor(a1[:, :n], u32[:, :n], dsk[:, ct:ct + 1], yp[:, :n], ALU.mult, ALU.add),
                         reads=[u32b, dskb, ypb], writes=[a1b])
                    go, gob = gor.next()
                    S.op("act", lambda: nc.scalar.activation(out=go[:, :n], in_=a1[:, :n], func=AF.Gelu), reads=[a1b], writes=[gob])
                    S.dma("sp", Sx["dbg_g"][ct * 128:(ct + 1) * 128, t0:t0 + n], go[:, :n], reads=[gob])
        S.barrier()
        mst.close()
        gT, gTb = S.sbuf("gT", [128, 4, T], BF16, es=ph)
        S.dma("sp", gT[:], Sx["dbg_g"].rearrange("(c p) t -> p c t", p=128), writes=[gTb])
        wgl, wglb = S.sbuf("wglu", [128, 4, 512], BF16, es=ph)
        S.dma("pool", wgl[:], I["w_glu"][li].rearrange("(c p) f -> p c f", p=128), writes=[wglb])
        bgl, bglb = S.sbuf("bglu", [128, 4], F32, es=ph)
        S.dma("sp", bgl[:], I["b_glu"][li], writes=[bglb])
        obr = Ring(S, ph, "s5o", [128, 512], BF16, 3)
        for fch in range(4):
            for (t0, n, lc) in TB:
                ps, psb = bur.next()

                def mm():
                    ins = None
                    for c in range(4):
                        ins = nc.tensor.matmul(ps[:, :n], lhsT=wgl[:, c, fch * 128:(fch + 1) * 128], rhs=gT[:, c, t0:t0 + n], start=(c == 0), stop=(c == 3))
                    return ins
                S.op("pe", mm, reads=[wglb, gTb], writes=[psb])
                a1, a1b = tr.next()
                S.op("act", lambda: nc.scalar.activation(out=a1[:, :n], in_=ps[:, :n], func=AF.Sigmoid, bias=bgl[:, fch:fch + 1]), reads=[psb, bglb], writes=[a1b])
                ob, obb = obr.next()
                S.op("dve", lambda: nc.vector.tensor_tensor(ob[:, :n], a1[:, :n], gT[:, fch, t0:t0 + n], ALU.mult), reads=[a1b, gTb], writes=[obb])
                S.dma("sp", Sx["catT"][1024 + fch * 128:1024 + (fch + 1) * 128, t0:t0 + n], ob[:, :n], reads=[obb])


def phase_out(C, li, x_in, x_out):
    S, nc, I, Sx = C.S, C.nc, C.I, C.Sx
    m = C.mod[li]
    with ExitStack() as ph:
        cat, catb = S.sbuf("cat", [128, KD, T], BF16, es=ph)
        for k4 in range(4):
            S.dma("sp", cat[:, k4 * 4:(k4 + 1) * 4, :], Sx["catT"][k4 * 512:(k4 + 1) * 512, :].rearrange("(k p) t -> p k t", p=128),
                  writes=[catb] if k4 == 0 else [], awrites=[] if k4 == 0 else [catb])
        wr = Ring(S, ph, "wo", [128, KD, 512], BF16, 2)
        pr = Ring(S, ph, "po", [128, 512], F32, 4, psum=True)
        xr = Ring(S, ph, "xo", [128, 512], F32, 3)
        orr = Ring(S, ph, "oo", [128, 512], F32, 3)
        nxt = wr.next()
        S.dma("pool", nxt[0][:], I["w_out"][li][:, 0:512].rearrange("(k p) n -> p k n", p=128), writes=[nxt[1]])
        for nb in range(4):
            w, wb = nxt
            if nb < 3:
                nxt = wr.next()
                S.dma("pool", nxt[0][:], I["w_out"][li][:, (nb + 1) * 512:(nb + 2) * 512].rearrange("(k p) n -> p k n", p=128), writes=[nxt[1]])
            for dl in range(4):
                dch = nb * 4 + dl
                for (t0, n, lc) in TB:
                    xt, xtb = xr.next()
                    S.dma("sp", xt[:, :n], x_in[dch * 128:(dch + 1) * 128, t0:t0 + n], writes=[xtb])
                    ps, psb = pr.next()

                    def mm():
                        ins = None
                        for k in range(KD):
                            ins = nc.tensor.matmul(ps[:, :n], lhsT=w[:, k, dl * 128:(dl + 1) * 128], rhs=cat[:, k, t0:t0 + n], start=(k == 0), stop=(k == KD - 1))
                        return ins
                    S.op("pe", mm, reads=[wb, catb], writes=[psb])
                    o, ob = orr.next()
                    S.op("dve", lambda: nc.vector.scalar_tensor_tensor(o[:, :n], ps[:, :n], m.t[:, 32 + dch, lc:lc + 1], xt[:, :n], ALU.mult, ALU.add),
                         reads=[psb, m.b, xtb], writes=[ob])
                    S.dma("sp", x_out[dch * 128:(dch + 1) * 128, t0:t0 + n], o[:, :n], reads=[ob])


def phase_moe(C, li, x_in, x_out):
    S, nc, I, Sx = C.S, C.nc, C.I, C.Sx
    m = C.mod[li]
    with ExitStack() as ph:
        posmT, posmTb = S.sbuf("posmT", [16, T], F32, es=ph)
        posmB, posmBb = S.sbuf("posmB", [16, T], BF16, es=ph)
        with ExitStack() as ph8:
            h2tok, h2tokb = S.sbuf("h2tok", [128, 18, D], BF16, es=ph8)
            aff3, aff3b = S.sbuf("aff3", [128, 18, 16, 3], BF16, es=ph8)
            posm_tok, posm_tokb = S.sbuf("posm_tok", [128, 18, 16], F32, es=ph8)
            with ExitStack() as p7:
                wrt, wrtb = S.sbuf("wrt", [128, KD, 16], F32, es=p7)
                S.dma("sp", wrt[:], I["w_router"][li].rearrange("(k p) e -> p k e", p=128), writes=[wrtb])
                aff, affb = S.sbuf("aff", [128, 18, 16], F32, es=p7)
                lg_, lgb = S.psum("lg", [128, 512], F32, es=p7)
                lg = lg_[:, 0:288].rearrange("p (a b) -> p a b", b=16)
                with ExitStack() as p7a:
                    hb, hbb = S.sbuf("hb", [128, KD, 512], BF16, es=p7a)
                    ptr = Ring(S, p7a, "pT", [128, 1024], BF16, 2, psum=True)

                    def cb(bi, t0, n, lc, hf, hfb):
                        S.op("act", lambda: nc.scalar.activation(out=hb[:, :, :n], in_=hf[:, :, :n], func=AF.Copy), reads=[hfb], writes=[hbb])
                        for a in range(n // 128):
                            tt = t0 // 128 + a

                            def mm():
                                ins = None
                                for k in range(KD):
                                    ins = nc.tensor.matmul(lg[:, tt, :], lhsT=hf[:, k, a * 128:(a + 1) * 128], rhs=wrt[:, k, :], start=(k == 0), stop=(k == KD - 1))
                                return ins
                            S.op("pe", mm, reads=[hfb, wrtb], writes=[lgb])
                            for kq in range(4):
                                pT, pTb = ptr.next()

                                def tp():
                                    ins = None
                                    for j in range(4):
                                        ins = nc.tensor.transpose(pT[:, j * 128:(j + 1) * 128], hb[:, kq * 4 + j, a * 128:(a + 1) * 128], C.ident_bf[:, :])
                                    return ins
                                S.op("pe", tp, reads=[hbb, C.ident_bf_b], writes=[pTb])
                                if kq % 2:
                                    S.op("act", lambda: nc.scalar.activation(out=h2tok[:, tt, kq * 512:(kq + 1) * 512], in_=pT[:, 0:512], func=AF.Copy),
                                         reads=[pTb], writes=[h2tokb])
                                else:
                                    S.op("dve", lambda: nc.vector.tensor_copy(h2tok[:, tt, kq * 512:(kq + 1) * 512], pT[:, 0:512]), reads=[pTb], writes=[h2tokb])
                    norm_blocks(C, p7a, x_in, m.A2, m.A2b, modsl(m, 3), m.b, cb)
                    S.barrier()
                mx, mxb = S.sbuf("mx", [128, 18], F32, es=p7)
                S.op("dve", lambda: nc.vector.tensor_reduce(mx[:], lg, AX.X, ALU.max), reads=[lgb], writes=[mxb])
                S.op("dve", lambda: nc.vector.tensor_tensor(aff[:], lg, mx[:].unsqueeze(2).to_broadcast([128, 18, 16]), ALU.subtract),
                     reads=[lgb, mxb], writes=[affb])
                S.op("act", lambda: nc.scalar.activation(out=aff[:], in_=aff[:], func=AF.Exp), reads=[affb], writes=[affb])
                S.op("dve", lambda: nc.vector.tensor_reduce(mx[:], aff[:], AX.X, ALU.add), reads=[affb], writes=[mxb])
                S.op("dve", lambda: nc.vector.reciprocal(mx[:], mx[:]), reads=[mxb], writes=[mxb])
                S.op("dve", lambda: nc.vector.tensor_tensor(aff[:], aff[:], mx[:].unsqueeze(2).to_broadcast([128, 18, 16]), ALU.mult),
                     reads=[affb, mxb], writes=[affb])
                if "dbg_aff" in C.dump:
                    S.dma("sp", Sx["dbg_aff"], aff[:], reads=[affb])
                r1, r1b = S.sbuf("r1", [128, 18, 16], F32, es=p7)
                S.op("dve", lambda: nc.vector.tensor_copy(aff3[:, :, :, 0], aff[:]), reads=[affb], writes=[aff3b])
                S.op("dve", lambda: nc.vector.tensor_tensor(r1[:], aff[:], aff3[:, :, :, 0], ALU.subtract), reads=[affb, aff3b], writes=[r1b])
                S.op("dve", lambda: nc.vector.tensor_copy(aff3[:, :, :, 1], r1[:]), reads=[r1b], writes=[aff3b])
                S.op("dve", lambda: nc.vector.tensor_tensor(r1[:], r1[:], aff3[:, :, :, 1], ALU.subtract), reads=[r1b, aff3b], writes=[r1b])
                S.op("dve", lambda: nc.vector.tensor_copy(aff3[:, :, :, 2], r1[:]), reads=[r1b], writes=[aff3b])
                affT, affTb = S.sbuf("affT", [16, T], F32, es=p7)
                work, workb = S.sbuf("work", [16, T], F32, es=p7)
                pa_, pab = S.psum("pa", [128, 512], F32, es=p7)
                pa = pa_[0:16, :]
                for (t0, n, lc) in TB:
                    def tpa():
                        ins = None
                        for a in range(n // 128):
                            ins = nc.tensor.transpose(pa[:, a * 128:(a + 1) * 128], aff[:, t0 // 128 + a, :], C.ident_f[:, :])
                        return ins
                    S.op("pe", tpa, reads=[affb, C.ident_f_b], writes=[pab])
                    S.op("dve", lambda: nc.vector.tensor_copy(affT[:, t0:t0 + n], pa[:, :n]), reads=[pab], writes=[affTb])
                S.op("dve", lambda: nc.vector.tensor_copy(work[:], affT[:]), reads=[affTb], writes=[workb])
                m8, m8b = S.sbuf("m8", [16, 16], F32, es=p7)

                tk = []
                for (lo, hi, rounds, oc) in ((0, NL, 32, 0), (NL, T, 4, 8)):
                    for r in range(rounds):
                        tk.append(lambda lo=lo, hi=hi, oc=oc: nc.vector.max(m8[:, oc:oc + 8], work[:, lo:hi]))
                        if r < rounds - 1:
                            tk.append(lambda lo=lo, hi=hi, oc=oc: nc.vector.match_replace(work[:, lo:hi], m8[:, oc:oc + 8], work[:, lo:hi], -1.0))
                S.seq("dve", tk, reads=[workb], writes=[workb, m8b])
                mk, mkb = S.sbuf("mk", [16, T], F32, es=p7)

                S.seq("dve", [
                    lambda: nc.vector.tensor_scalar(mk[:, 0:NL], affT[:, 0:NL], m8[:, 7:8], None, ALU.is_ge),
                    lambda: nc.vector.tensor_scalar(mk[:, NL:T], affT[:, NL:T], m8[:, 15:16], None, ALU.is_ge),
                    lambda: nc.vector.tensor_tensor_scan(work[:, 0:NL], C.one_f[:16, 0:1].to_broadcast([16, NL]), mk[:, 0:NL], 0.0, ALU.mult, ALU.add),
                    lambda: nc.vector.tensor_tensor_scan(work[:, NL:T], C.one_f[:16, 0:1].to_broadcast([16, NX]), mk[:, NL:T], 0.0, ALU.mult, ALU.add),
                    lambda: nc.vector.tensor_scalar(work[:, NL:T], work[:, NL:T], 256.0, None, ALU.add),
                    lambda: nc.vector.tensor_tensor(work[:], work[:], mk[:], ALU.mult),
                    lambda: nc.vector.tensor_scalar(posmT[:], work[:], -1.0, None, ALU.add),
                ], reads=[affTb, m8b, C.one_f_b, workb], writes=[mkb, workb, posmTb])
                S.seq("dve", [
                    lambda: nc.vector.tensor_copy(posmB[:, 0:NL], posmT[:, 0:NL]),
                    lambda: nc.vector.tensor_scalar(posmB[:, NL:T], posmT[:, NL:T], -256.0, None, ALU.add),
                ], reads=[posmTb], writes=[posmBb])
                if "dbg_posm" in C.dump:
                    S.dma("sp", Sx["dbg_posm"], posmT[:], reads=[posmTb])
                pp__, ppb_ = S.psum("ppm", [128, 512], F32, es=p7)
                pp_ = pp__[:, 0:288].rearrange("p (a b) -> p a b", b=16)

                def tpp():
                    ins = None
                    for tt in range(18):
                        ins = nc.tensor.transpose(pp_[:, tt, :], posmT[:, tt * 128:(tt + 1) * 128], C.ident_f[:16, :16])
                    return ins
                S.op("pe", tpp, reads=[posmTb, C.ident_f_b], writes=[ppb_])
                S.op("dve", lambda: nc.vector.tensor_copy(posm_tok[:], pp_), reads=[ppb_], writes=[posm_tokb])
                S.barrier()
            with ExitStack() as p8:
                ioj, iojb = S.sbuf("ioj", [128, NJ], F32, es=p8)
                S.dma("sp", ioj[:], I["iota_j"][0:1, :].to_broadcast([128, NJ]), writes=[iojb])
                Se, Seb = S.sbuf("Se", [128, 18, NJ], BF16, es=p8)
                xsr = Ring(S, p8, "xs", [128, KD, NJ], BF16, 2)
                hidr = Ring(S, p8, "hid", [128, 8, NJ], BF16, 2)
                ysb_, ysbb = S.sbuf("ysb", [128, 3, D], BF16, es=p8)
                wring = Ring(S, p8, "wu", [128, 4096], BF16, 6)
                pr = Ring(S, p8, "pe8", [128, 512], F32, 6, psum=True)
                tap_, tapb = S.psum("tap", [128, 512], F32, es=p8)
                tap = tap_[:, 0:9]
                ta, tab = S.sbuf("ta", [128, 3], F32, es=p8)
                sgr = Ring(S, p8, "sg", [128, NJ], F32, 3)

                def units(e):
                    u = []
                    for fq in range(4):
                        u.append(("g", fq, I["w_gate"][li, e][:, fq * 256:(fq + 1) * 256].rearrange("(k p) f -> p k f", p=128)))
                        u.append(("u", fq, I["w_up"][li, e][:, fq * 256:(fq + 1) * 256].rearrange("(k p) f -> p k f", p=128)))
                    for dq in range(4):
                        u.append(("d", dq, I["w_down"][li, e][:, dq * 512:(dq + 1) * 512].rearrange("(c p) d -> p c d", p=128)))
                    return u
                allu = [(e,) + u for e in range(16) for u in units(e)]
                loaded = {}

                def issue(i):
                    if i >= len(allu):
                        return
                    e, kind, idx, src = allu[i]
                    wt, wtb = wring.next()
                    if kind == "d":
                        S.dma("pool", wt[:].rearrange("p (c d) -> p c d", c=8), src, writes=[wtb])
                    else:
                        S.dma("pool", wt[:].rearrange("p (k f) -> p k f", k=KD), src, writes=[wtb])
                    loaded[i] = (wt, wtb)
                PRE = 4
                for i in range(PRE):
                    issue(i)
                ui = 0
                for e in range(16):
                    def mkS():
                        ins = None
                        for tt in range(18):
                            ins = nc.vector.tensor_scalar(Se[:, tt, :], ioj[:, :], posm_tok[:, tt, e:e + 1], None, ALU.is_equal)
                        return ins
                    S.op("dve", mkS, reads=[iojb, posm_tokb], writes=[Seb])

                    def mta():
                        ins = None
                        for jc, (tts, M) in enumerate(((range(16), 128), (range(16), 128), ((16, 17), 32))):
                            tts = list(tts)
                            for ii, tt in enumerate(tts):
                                ins = nc.tensor.matmul(tap[:M, jc * 3:(jc + 1) * 3], lhsT=Se[:, tt, jc * 128:jc * 128 + M], rhs=aff3[:, tt, e, :],
                                                       start=(ii == 0), stop=(ii == len(tts) - 1))
                        return ins
                    S.op("pe", mta, reads=[Seb, aff3b], writes=[tapb])
                    S.op("dve", lambda: nc.vector.tensor_reduce(ta[:], tap_[:, 0:9].rearrange("p (a b) -> p a b", b=3), AX.X, ALU.add), reads=[tapb], writes=[tab])
                    xs, xsb = xsr.next()
                    for k in range(KD):
                        ps, psb = pr.next()

                        def gm():
                            ins = None
                            for tt in range(16):
                                ins = nc.tensor.matmul(ps[:, 0:256], lhsT=h2tok[:, tt, k * 128:(k + 1) * 128], rhs=Se[:, tt, 0:256], start=(tt == 0), stop=(tt == 15))
                            for tt in (16, 17):
                                ins = nc.tensor.matmul(ps[:, 256:NJ], lhsT=h2tok[:, tt, k * 128:(k + 1) * 128], rhs=Se[:, tt, 256:NJ], start=(tt == 16), stop=(tt == 17))
                            return ins
                        S.op("pe", gm, reads=[h2tokb, Seb], writes=[psb])
                        if k % 2:
                            S.op("act", lambda: nc.scalar.activation(out=xs[:, k, :], in_=ps[:, :NJ], func=AF.Copy), reads=[psb], writes=[xsb])
                        else:
                            S.op("dve", lambda: nc.vector.tensor_copy(xs[:, k, :], ps[:, :NJ]), reads=[psb], writes=[xsb])
                    hid, hidb = hidr.next()
                    for fq in range(4):
                        wg, wgb = loaded.pop(ui)
                        wu, wub = loaded.pop(ui + 1)
                        ui += 2
                        wg3 = wg[:].rearrange("p (k f) -> p k f", k=KD)
                        wu3 = wu[:].rearrange("p (k f) -> p k f", k=KD)
                        for fcl in range(2):
                            fc = fq * 2 + fcl
                            pg, pgb = pr.next()
                            pu, pub = pr.next()

                            def mg():
                                ins = None
                                for k in range(KD):
                                    ins = nc.tensor.matmul(pg[:, :NJ], lhsT=wg3[:, k, fcl * 128:(fcl + 1) * 128], rhs=xs[:, k, :], start=(k == 0), stop=(k == KD - 1))
                                return ins

                            def mu():
                                ins = None
                                for k in range(KD):
                                    ins = nc.tensor.matmul(pu[:, :NJ], lhsT=wu3[:, k, fcl * 128:(fcl + 1) * 128], rhs=xs[:, k, :], start=(k == 0), stop=(k == KD - 1))
                                return ins
                            S.op("pe", mg, reads=[wgb, xsb], writes=[pgb])
                            S.op("pe", mu, reads=[wub, xsb], writes=[pub])
                            sg, sgb = sgr.next()
                            S.op("act", lambda: nc.scalar.activation(out=sg[:, :], in_=pg[:, :NJ], func=AF.Silu), reads=[pgb], writes=[sgb])
                            S.op("dve", lambda: nc.vector.tensor_tensor(hid[:, fc, :], sg[:, :], pu[:, :NJ], ALU.mult), reads=[sgb, pub], writes=[hidb])
                        issue(ui - 2 + PRE)
                        issue(ui - 1 + PRE)
                    for dq in range(4):
                        wd, wdb = loaded.pop(ui)
                        ui += 1
                        wd3 = wd[:].rearrange("p (c d) -> p c d", c=8)
                        for jc, M in enumerate((128, 128, 32)):
                            ps, psb = pr.next()

                            def md():
                                ins = None
                                for fc in range(8):
                                    ins = nc.tensor.matmul(ps[:M, :], lhsT=hid[:, fc, jc * 128:jc * 128 + M], rhs=wd3[:, fc, :], start=(fc == 0), stop=(fc == 7))
                                return ins
                            S.op("pe", md, reads=[hidb, wdb], writes=[psb])
                            if jc == 1:
                                S.op("act", lambda: nc.scalar.activation(out=ysb_[:M, jc, dq * 512:(dq + 1) * 512], in_=ps[:M, :], func=AF.Copy, scale=ta[:M, jc:jc + 1]),
                                     reads=[psb, tab], writes=[ysbb])
                            else:
                                S.op("dve", lambda: nc.vector.tensor_scalar(ysb_[:M, jc, dq * 512:(dq + 1) * 512], ps[:M, :], ta[:M, jc:jc + 1], None, ALU.mult),
                                     reads=[psb, tab], writes=[ysbb])
                        issue(ui - 1 + PRE)
                    S.dma("sp", Sx["ys"][e, 0:256, :].rearrange("(c j) d -> j c d", j=128), ysb_[:, 0:2, :], reads=[ysbb])
                    S.dma("sp", Sx["ys"][e, 256:NJ, :], ysb_[:32, 2, :], reads=[ysbb])
                S.barrier()
        with ExitStack() as p9:
            ysh, yshb = S.sbuf("ysh", [128, 16, 3, 1024], BF16, es=p9)
            STr = Ring(S, p9, "ST", [128, 16, 2, 512], BF16, 2)
            selt, seltb = S.sbuf("selt", [16, 16, 128], BF16, es=p9)
            S.dma("pool", selt[:], I["sel"][:, :, :], writes=[seltb])
            ip3, ip3b = S.sbuf("ip3", [128, 3], F32, es=p9)
            S.dma("sp", ip3[:], I["iota_p3"][:, :], writes=[ip3b])
            bcr = Ring(S, p9, "bc", [128, 512], F32, 3, psum=True)
            pr = Ring(S, p9, "p9", [128, 512], F32, 4, psum=True)
            xr = Ring(S, p9, "x9", [128, 512], F32, 3)
            orr = Ring(S, p9, "o9", [128, 512], F32, 3)
            for dh in range(2):
                for jc in range(2):
                    S.dma("sp", ysh[:, :, jc, :], Sx["ys"][:, jc * 128:(jc + 1) * 128, dh * 1024:(dh + 1) * 1024].rearrange("e j d -> j e d"),
                          writes=[yshb] if jc == 0 else [], awrites=[] if jc == 0 else [yshb])
                S.dma("sp", ysh[:32, :, 2, :], Sx["ys"][:, 256:NJ, dh * 1024:(dh + 1) * 1024].rearrange("e j d -> j e d"), awrites=[yshb])
                for (t0, n, lc) in TB:
                    ST, STb = STr.next()
                    for e in range(16):
                        bc, bcb = bcr.next()
                        S.op("pe", lambda: nc.tensor.matmul(bc[:, :n], lhsT=selt[:, e, :], rhs=posmB[:, t0:t0 + n], start=True, stop=True),
                             reads=[seltb, posmBb], writes=[bcb])
                        if lc == 0:
                            S.op("dve", lambda: nc.vector.tensor_scalar(ST[:, e, 0, :n], bc[:, :n], ip3[:, 0:1], None, ALU.is_equal), reads=[bcb, ip3b], writes=[STb])
                            S.op("pool" if False else "dve", lambda: nc.vector.tensor_scalar(ST[:, e, 1, :n], bc[:, :n], ip3[:, 1:2], None, ALU.is_equal),
                                 reads=[bcb, ip3b], writes=[STb])
                        else:
                            S.op("dve", lambda: nc.vector.tensor_scalar(ST[:32, e, 0, :n], bc[:32, :n], ip3[:32, 0:1], None, ALU.is_equal), reads=[bcb, ip3b], writes=[STb])
                    for dl in range(8):
                        dch = dh * 8 + dl
                        xt, xtb = xr.next()
                        S.dma("sp", xt[:, :n], x_in[dch * 128:(dch + 1) * 128, t0:t0 + n], writes=[xtb])
                        ps, psb = pr.next()

                        def msc():
                            ins = None
                            if lc == 0:
                                for e in range(16):
                                    for jc in range(2):
                                        ins = nc.tensor.matmul(ps[:, :n], lhsT=ysh[:, e, jc, dl * 128:(dl + 1) * 128], rhs=ST[:, e, jc, :n],
                                                               start=(e == 0 and jc == 0), stop=(e == 15 and jc == 1))
                            else:
                                for e in range(16):
                                    ins = nc.tensor.matmul(ps[:, :n], lhsT=ysh[:32, e, 2, dl * 128:(dl + 1) * 128], rhs=ST[:32, e, 0, :n],
                                                           start=(e == 0), stop=(e == 15))
                            return ins
                        S.op("pe", msc, reads=[yshb, STb], writes=[psb])
                        o, ob = orr.next()
                        S.op("dve", lambda: nc.vector.scalar_tensor_tensor(o[:, :n], ps[:, :n], m.t[:, 80 + dch, lc:lc + 1], xt[:, :n], ALU.mult, ALU.add),
                             reads=[psb, m.b, xtb], writes=[ob])
                        S.dma("sp", x_out[dch * 128:(dch + 1) * 128, t0:t0 + n], o[:, :n], reads=[ob])


def phase_final(C, x_in):
    S, nc, I = C.S, C.nc, C.I
    with ExitStack() as ph:
        gf, gfb = S.sbuf("gf", [128, KD, 2], F32, es=ph)
        S.dma("sp", gf[:], I["gfT"][:, :, :], writes=[gfb])
        zs, zsb = S.sbuf("zs", [128, KD, 2], F32, es=ph)
        S.op("pool", lambda: nc.gpsimd.memset(zs[:], 0.0), writes=[zsb])

        def cb(bi, t0, n, lc, hf, hfb):
            S.dma("sp", C.outT[:, t0:t0 + n].rearrange("(k p) t -> p k t", p=128), hf[:, :, :n], reads=[hfb])
        norm_blocks(C, ph, x_in, gf, gfb, zs, zsb, cb, blocks=TB[:4])


def _prep_shared(inp):
    f = np.float32
    L = DEPTH
    sh = {}
    sh["w_ada"] = np.ascontiguousarray(inp["w_ada"], dtype=f)
    sh["b_adaT"] = np.ascontiguousarray(np.repeat(inp["b_ada"].reshape(L, 96, 128).transpose(0, 2, 1)[..., None], 2, axis=-1), dtype=f)
    for nm, src in (("g1T", "norm1_g"), ("g2T", "norm2_g")):
        sh[nm] = np.ascontiguousarray(np.repeat(inp[src].reshape(L, KD, 128).transpose(0, 2, 1)[..., None], 2, axis=-1), dtype=f)
    sh["gfT"] = np.ascontiguousarray(np.repeat(inp["final_norm_g"].reshape(KD, 128).T[..., None], 2, axis=-1), dtype=f)
    sh["w_in"] = np.ascontiguousarray(inp["w_in"], dtype=f)
    perm = np.array([(r // 32) * 32 + ((r % 32) + 16) % 32 for r in range(64)])
    sh["w_in_sw"] = np.ascontiguousarray(inp["w_in"][:, :, 1664:1728][:, :, perm], dtype=f)
    sh["w_out"] = np.ascontiguousarray(inp["w_out"], dtype=f)
    sh["sgu_g"] = np.ascontiguousarray(inp["sgu_norm_g"].reshape(L, 1, 512), dtype=f)
    sh["sgu_wT"] = np.ascontiguousarray(inp["sgu_w"].transpose(0, 3, 1, 2), dtype=f)
    sh["sgu_b4"] = np.ascontiguousarray(np.repeat(inp["sgu_b"][:, :, None, :], 4, axis=2).reshape(L, 1, 2048), dtype=f)
    sh["qg"] = np.ascontiguousarray(inp["mla_q_norm_g"].reshape(L, 3, 128).transpose(0, 2, 1), dtype=f)
    sh["kvg"] = np.ascontiguousarray(inp["mla_kv_norm_g"].reshape(L, 2, 128).transpose(0, 2, 1), dtype=f)
    sh["w_uq"] = np.ascontiguousarray(inp["mla_w_uq"], dtype=f)
    sh["w_uq_sw"] = np.ascontiguousarray(np.concatenate([inp["mla_w_uq"][:, :, h * 192 + 128 + perm] for h in range(4)], axis=-1), dtype=f)
    sh["w_ukv"] = np.ascontiguousarray(inp["mla_w_ukv"], dtype=f)
    t = np.arange(NL)
    row_id = (t // 64).astype(f)
    col_id = (t % 64).astype(f)
    inv_freq = (f(10000.0) ** (-np.arange(16, dtype=f) / f(16))).astype(f)
    cosT = np.ones((64, T), f)
    sinT = np.zeros((64, T), f)
    for r in range(64):
        pos = row_id if r < 32 else col_id
        ang = (pos * inv_freq[r % 16]).astype(f)
        cosT[r, :NL] = np.cos(ang)
        sinT[r, :NL] = np.sin(ang) * (-1.0 if (r % 32) < 16 else 1.0)
    sh["rope_cos"] = cosT
    sh["rope_sin"] = sinT

    def pp(a):
        return a.reshape(L, 2, 4, 8, 4, 16).transpose(0, 3, 5, 1, 2, 4).reshape(L, 128, 32)

    def rowl(a):
        return a.reshape(L, 2, 4, 8, 4, 16).transpose(0, 1, 2, 4, 3, 5).reshape(L, 4096)
    ldt_full = np.repeat(inp["s5_log_dt"][..., None], 64, axis=-1)
    sh["s5_pp"] = np.ascontiguousarray(np.stack([pp(inp["s5_a_re"]), pp(inp["s5_a_im"]), pp(ldt_full)], axis=2), dtype=f)
    sh["s5_row"] = np.ascontiguousarray(np.stack([rowl(inp["s5_a_re"]), rowl(inp["s5_a_im"]), rowl(ldt_full)], axis=1), dtype=f)

    def bblk(b):
        o = np.zeros((L, 8, 16, 2, 4, 4, 8, 16), f)
        bb = b.reshape(L, 2, 4, 8, 4, 16, 16)
        for g in range(8):
            o[:, g, :, :, :, :, g, :] = bb[:, :, :, g].transpose(0, 4, 1, 2, 3, 5)[:, :, :, :, :, :] if False else \
                np.transpose(bb[:, :, :, g], (0, 5, 1, 2, 3, 4))
        return o.reshape(L, 128, 4096)

    def cblk(c):
        o = np.zeros((L, 8, 16, 2, 4, 4, 8, 16), f)
        cc = c.reshape(L, 2, 4, 8, 16, 4, 16)
        for g in range(8):
            o[:, g, :, :, :, :, g, :] = np.transpose(cc[:, :, :, g], (0, 5, 1, 2, 4, 3))
        return o.reshape(L, 128, 4096)
    sh["s5_Bre"] = bblk(inp["s5_b_re"])
    sh["s5_Bim"] = bblk(inp["s5_b_im"])
    sh["s5_Cre"] = cblk(inp["s5_c_re"])
    sh["s5_Cim"] = cblk(inp["s5_c_im"])
    sh["s5_d"] = np.ascontiguousarray(inp["s5_d"].reshape(L, 4, 128).transpose(0, 2, 1), dtype=f)
    tau = np.zeros((2, T), f)
    tau[0, NL:] = np.arange(NX)
    tau[0, :NL] = NX + np.arange(NL)
    tau[1, NL:] = NX - 1 - np.arange(NX)
    tau[1, :NL] = NX + (NL - 1 - np.arange(NL))
    sh["s5_tau"] = tau.reshape(1, 2 * T)
    sh["w_glu"] = np.ascontiguousarray(inp["s5_w_glu"], dtype=f)
    sh["b_glu"] = np.ascontiguousarray(inp["s5_b_glu"].reshape(L, 4, 128).transpose(0, 2, 1), dtype=f)
    sh["conv_wT"] = np.ascontiguousarray(inp["conv_w"].reshape(L, 3, 4, 128).transpose(0, 3, 2, 1), dtype=f)
    sh["w_router"] = np.ascontiguousarray(inp["moe_w_router"], dtype=f)
    sh["w_gate"] = np.ascontiguousarray(inp["moe_w_gate"], dtype=f)
    sh["w_up"] = np.ascontiguousarray(inp["moe_w_up"], dtype=f)
    sh["w_down"] = np.ascontiguousarray(inp["moe_w_down"], dtype=f)
    sh["ident"] = np.eye(128, dtype=f)
    sh["iota_j"] = np.arange(NJ, dtype=f).reshape(1, NJ)
    sh["iota_p3"] = (np.arange(128, dtype=f)[:, None] + np.array([0, 128, 256], f)[None, :]).astype(f)
    sel = np.zeros((16, 16, 128), f)
    for e in range(16):
        sel[e, e, :] = 1.0
    sh["sel"] = sel
    return sh


def _prep_core(inp, b):
    f = np.float32
    d = {}
    d["xT0"] = np.ascontiguousarray(np.concatenate([inp["x"][b].T, inp["ctx"][b].T], axis=1), dtype=f)
    c2 = np.stack([inp["c"][b], inp["c_ctx"]], axis=0)
    d["cTp"] = np.ascontiguousarray(c2.reshape(2, KD, 128).transpose(2, 1, 0), dtype=f)
    return d


_NC_CACHE = {}


def used_inputs(nc_I, m):
    return {k: v for k, v in m.items() if k in nc_I}


def kernel(**inputs):
    inp = {k: np.asarray(v) for k, v in inputs.items()}
    B = inp["x"].shape[0]
    if "full" not in _NC_CACHE:
        _NC_CACHE["full"] = build_program()
    nc = _NC_CACHE["full"]
    sh = _prep_shared(inp)
    in_maps = []
    for b in range(B):
        m = dict(sh)
        m.update(_prep_core(inp, b))
        in_maps.append({k: v for k, v in m.items() if k in nc._used_inputs})
    res = run_bass_kernel_spmd(nc, in_maps, core_ids=list(range(B)))
    out = np.stack([np.asarray(res.results[b]["outT"]).T for b in range(B)], axis=0)
    return np.ascontiguousarray(out, dtype=np.float32)
```

```python
import math
import numpy as np
from contextlib import ExitStack
import concourse.bass as bass
import concourse.mybir as mybir
from concourse.bass_utils import run_bass_kernel_spmd

F32 = mybir.dt.float32
BF16 = mybir.dt.bfloat16
I32 = mybir.dt.int32
AF = mybir.ActivationFunctionType
ALU = mybir.AluOpType
AX = mybir.AxisListType

D = 2048
T = 2304
NL = 2048
NX = 256
KD = 16
DEPTH = 2
TB = [(0, 512, 0), (512, 512, 0), (1024, 512, 0), (1536, 512, 0), (2048, 256, 1)]
NJ = 288
EPS = 1e-6
N_DMA_SEMS = 24
TWO_PI = 6.283185


class Buf:
    __slots__ = ("name", "w", "r")

    def __init__(self, name=""):
        self.name = name
        self.w = {}
        self.r = {}


class Sched:
    def __init__(self, nc, es):
        self.nc = nc
        self.es = es
        self.eng = {"pe": nc.tensor, "dve": nc.vector, "act": nc.scalar, "pool": nc.gpsimd, "sp": nc.sync}
        self.sems = {}
        self.cnt = {}
        for k in ["pe", "dve", "act", "pool"]:
            self.sems[k] = es.enter_context(nc.semaphore("s_" + k))
            self.cnt[k] = 0
        for i in range(N_DMA_SEMS):
            k = "d%d" % i
            self.sems[k] = es.enter_context(nc.semaphore("s_" + k))
            self.cnt[k] = 0
        self.dma_rr = 0
        self.waited = {e: {} for e in self.eng}
        self.bufs = []
        self.uid = 0

    def sbuf(self, name, shape, dt, es=None):
        self.uid += 1
        t = (es or self.es).enter_context(self.nc.sbuf_tensor("%s_%d" % (name, self.uid), list(shape), dt))
        b = Buf(name)
        self.bufs.append(b)
        return t, b

    def psum(self, name, shape, dt, es=None):
        self.uid += 1
        t = (es or self.es).enter_context(self.nc.psum_tensor("%s_%d" % (name, self.uid), list(shape), dt))
        b = Buf(name)
        self.bufs.append(b)
        return t, b

    def _wait(self, e, dep):
        sk, val, deng = dep
        if deng == e and e == "pe":
            return
        if self.waited[e].get(sk, 0) >= val:
            return
        self.eng[e].wait_ge(self.sems[sk], val)
        self.waited[e][sk] = val

    def _deps(self, e, reads, writes):
        for b in reads:
            for d in b.w.values():
                self._wait(e, d)
        for b in writes:
            for d in b.w.values():
                self._wait(e, d)
            for d in b.r.values():
                self._wait(e, d)

    def _commit(self, tag, reads, writes):
        for b in reads:
            b.r[tag[0]] = tag
        for b in writes:
            b.w = {tag[0]: tag}
            b.r = {}

    def op(self, e, fn, reads=(), writes=()):
        self._deps(e, reads, writes)
        ins = fn()
        self.cnt[e] += 1
        ins.then_inc(self.sems[e], 1)
        self._commit((e, self.cnt[e], e), reads, writes)
        return ins

    def seq(self, e, fns, reads=(), writes=()):
        self._deps(e, reads, writes)
        ins = None
        for i, fn in enumerate(fns):
            if i > 0:
                self.eng[e].wait_ge(self.sems[e], self.cnt[e])
                self.waited[e][e] = self.cnt[e]
            ins = fn()
            self.cnt[e] += 1
            ins.then_inc(self.sems[e], 1)
        self._commit((e, self.cnt[e], e), reads, writes)
        return ins

    def dma(self, q, out, in_, reads=(), writes=(), awrites=()):
        self._deps(q, reads, writes)
        sk = "d%d" % self.dma_rr
        self.dma_rr = (self.dma_rr + 1) % N_DMA_SEMS
        if self.cnt[sk] > 0:
            self._wait(q, (sk, self.cnt[sk], "dma"))
        ins = self.eng[q].dma_start(out=out, in_=in_)
        self.cnt[sk] += 16
        ins.then_inc(self.sems[sk], 16)
        self._commit((sk, self.cnt[sk], "dma"), reads, writes)
        for b in awrites:
            b.w[sk] = (sk, self.cnt[sk], "dma")
        return ins

    def barrier(self):
        for e in self.eng:
            for sk, c in self.cnt.items():
                if c > 0:
                    self._wait(e, (sk, c, "x"))
        for b in self.bufs:
            b.w = {}
            b.r = {}


class Ring:
    def __init__(self, S, es, name, shape, dt, n, psum=False):
        mk = S.psum if psum else S.sbuf
        self.items = [mk("%s%d" % (name, i), shape, dt, es=es) for i in range(n)]
        self.i = 0

    def next(self):
        it = self.items[self.i % len(self.items)]
        self.i += 1
        return it


class Ctx:
    pass


def build_program(stop_after=None, dump=()):
    nc = bass.Bass("TRN2", target_bir_lowering=False)
    C = Ctx()
    C.nc = nc
    C.dump = set(dump)
    C.stop_after = stop_after

    def din(name, shape, dt=F32):
        return nc.dram_tensor(name, list(shape), dt, kind="ExternalInput").ap()

    def dscr(name, shape, dt):
        kind = "ExternalOutput" if name in C.dump else "Internal"
        return nc.dram_tensor(name, list(shape), dt, kind=kind).ap()

    SPEC = {
        "xT0": [D, T],
        "cTp": [128, KD, 2],
        "w_ada": [DEPTH, D, 6 * D],
        "b_adaT": [DEPTH, 128, 96, 2],
        "g1T": [DEPTH, 128, KD, 2],
        "g2T": [DEPTH, 128, KD, 2],
        "gfT": [128, KD, 2],
        "w_in": [DEPTH, D, 3776],
        "w_in_sw": [DEPTH, D, 64],
        "w_out": [DEPTH, D, D],
        "sgu_g": [DEPTH, 1, 512],
        "sgu_wT": [DEPTH, 128, 4, 128],
        "sgu_b4": [DEPTH, 1, 2048],
        "qg": [DEPTH, 128, 3],
        "kvg": [DEPTH, 128, 2],
        "w_uq": [DEPTH, 384, 768],
        "w_uq_sw": [DEPTH, 384, 256],
        "w_ukv": [DEPTH, 256, 1024],
        "rope_cos": [64, T],
        "rope_sin": [64, T],
        "s5_pp": [DEPTH, 128, 3, 32],
        "s5_row": [DEPTH, 3, 4096],
        "s5_Bre": [DEPTH, 128, 4096],
        "s5_Bim": [DEPTH, 128, 4096],
        "s5_Cre": [DEPTH, 128, 4096],
        "s5_Cim": [DEPTH, 128, 4096],
        "s5_d": [DEPTH, 128, 4],
        "s5_tau": [1, 2 * T],
        "w_glu": [DEPTH, 512, 512],
        "b_glu": [DEPTH, 128, 4],
        "conv_wT": [DEPTH, 128, 4, 3],
        "w_router": [DEPTH, D, 16],
        "w_gate": [DEPTH, 16, D, 1024],
        "w_up": [DEPTH, 16, D, 1024],
        "w_down": [DEPTH, 16, 1024, D],
        "ident": [128, 128],
        "iota_j": [1, NJ],
        "iota_p3": [128, 3],
        "sel": [16, 16, 128],
    }

    class LazyIn(dict):
        def __missing__(self, k):
            v = din(k, SPEC[k])
            self[k] = v
            return v
    I = LazyIn()
    C.I = I
    C.outT = nc.dram_tensor("outT", [D, NL], F32, kind="ExternalOutput").ap()

    Sx = {}
    Sx["xA"] = dscr("xA", [D, T], F32)
    Sx["xB"] = dscr("xB", [D, T], F32)
    Sx["uTg"] = dscr("uTg", [512, T], BF16)
    Sx["v_tok"] = dscr("v_tok", [T, 512], BF16)
    Sx["cqT"] = dscr("cqT", [384, T], BF16)
    Sx["ckvT"] = dscr("ckvT", [256, T], BF16)
    Sx["krT"] = dscr("krT", [64, T], BF16)
    Sx["s5uT"] = dscr("s5uT", [512, T], BF16)
    Sx["s5u32"] = dscr("s5u32", [512, T], F32)
    Sx["catT"] = dscr("catT", [D, T], BF16)
    Sx["ys"] = dscr("ys", [16, NJ, D], BF16)
    Sx["dbg_mod"] = dscr("dbg_mod", [DEPTH, 128, 96, 2], F32)
    Sx["dbg_hT"] = dscr("dbg_hT", [D, T], BF16)
    Sx["dbg_aff"] = dscr("dbg_aff", [128, 18, 16], F32)
    Sx["dbg_posm"] = dscr("dbg_posm", [16, T], F32)
    Sx["dbg_g"] = dscr("dbg_g", [512, T], BF16)
    C.Sx = Sx

    with ExitStack() as es:
        S = Sched(nc, es)
        C.S = S
        _consts(C)
        phase_mod(C)
        S.barrier()
        x_in = I["xT0"]
        done = (stop_after == "mod")
        for li in range(DEPTH):
            if done:
                break
            x1 = Sx["xA"]
            x2 = Sx["xB"]
            for ph in (phase_in, phase_sgu, phase_mla, phase_s5, phase_out, phase_moe):
                if ph is phase_in:
                    ph(C, li, x_in)
                elif ph is phase_out:
                    ph(C, li, x_in, x1)
                elif ph is phase_moe:
                    ph(C, li, x1, x2)
                else:
                    ph(C, li)
                S.barrier()
                if stop_after == (li, ph.__name__) or (isinstance(stop_after, tuple) and len(stop_after) == 3 and stop_after[0] == li and ph is phase_in):
                    done = True
                    break
            if done:
                break
            x_in = x2
        if not done:
            phase_final(C, x_in)
        S.barrier()
    nc._used_inputs = set(I.keys())
    return nc


def _consts(C):
    S, nc, I = C.S, C.nc, C.I
    C.ones_bf, C.ones_bf_b = S.sbuf("ones_bf", [128, 128], BF16)
    S.op("pool", lambda: nc.gpsimd.memset(C.ones_bf[:], 1.0), writes=[C.ones_bf_b])
    C.eps_t, C.eps_b = S.sbuf("eps", [128, 1], F32)
    S.op("pool", lambda: nc.gpsimd.memset(C.eps_t[:], EPS), writes=[C.eps_b])
    C.one_f, C.one_f_b = S.sbuf("one_f", [128, 1], F32)
    S.op("pool", lambda: nc.gpsimd.memset(C.one_f[:], 1.0), writes=[C.one_f_b])
    C.hpi_t, C.hpi_b = S.sbuf("hpi", [128, 1], F32)
    S.op("pool", lambda: nc.gpsimd.memset(C.hpi_t[:], math.pi / 2), writes=[C.hpi_b])
    C.ident_f, C.ident_f_b = S.sbuf("ident_f", [128, 128], F32)
    S.dma("sp", C.ident_f[:], I["ident"][:, :], writes=[C.ident_f_b])
    C.ident_bf, C.ident_bf_b = S.sbuf("ident_bf", [128, 128], BF16)
    S.dma("pool", C.ident_bf[:], I["ident"][:, :], writes=[C.ident_bf_b])
    C.mod = []
    C.modalloc = []
    for li in range(DEPTH):
        C.modalloc.append((S.sbuf("mod%d" % li, [128, 96, 2], F32), S.sbuf("A1_%d" % li, [128, KD, 2], F32), S.sbuf("A2_%d" % li, [128, KD, 2], F32)))


def _dbg(C, name, src_ap, reads):
    if name in C.dump:
        C.S.dma("sp", C.Sx[name], src_ap, reads=reads)


def phase_mod(C):
    S, nc, I = C.S, C.nc, C.I
    with ExitStack() as ph:
        sc, scb = S.sbuf("sc", [128, KD, 2], F32, es=ph)
        S.dma("sp", sc[:], I["cTp"][:, :, :], writes=[scb])
        S.op("act", lambda: nc.scalar.activation(out=sc[:], in_=sc[:], func=AF.Silu), reads=[scb], writes=[scb])
        war = Ring(S, ph, "wa", [128, KD, 512], F32, 2)
        mps_, mpsb = S.psum("mps", [128, 512], F32, es=ph)
        mps = mps_[:, 0:192]
        rowr = Ring(S, ph, "mrow", [128, 512], F32, 2, psum=True)
        mrow, mrowb = S.sbuf("mrow_sb", [2, 6 * D], F32, es=ph)
        for li in range(DEPTH):
            nxt = war.next()
            S.dma("sp", nxt[0][:], I["w_ada"][li, :, 0:512].rearrange("(k p) n -> p k n", p=128), writes=[nxt[1]])
            for nb in range(24):
                wa, wab = nxt
                if nb + 1 < 24:
                    nxt = war.next()
                    S.dma("sp", nxt[0][:], I["w_ada"][li, :, (nb + 1) * 512:(nb + 2) * 512].rearrange("(k p) n -> p k n", p=128),
                          writes=[nxt[1]])
                pr_, prb_ = rowr.next()

                def mm():
                    ins = None
                    for k in range(KD):
                        ins = nc.tensor.matmul(pr_[0:2, :], lhsT=sc[:, k, :], rhs=wa[:, k, :], start=(k == 0), stop=(k == KD - 1))
                    return ins
                S.op("pe", mm, reads=[wab, scb], writes=[prb_])
                S.op("act", lambda: nc.scalar.activation(out=mrow[0:2, nb * 512:(nb + 1) * 512], in_=pr_[0:2, :], func=AF.Copy), reads=[prb_], writes=[mrowb])

            def tps():
                ins = None
                for fc in range(96):
                    ins = nc.tensor.transpose(mps_[:, fc * 2:fc * 2 + 2], mrow[0:2, fc * 128:(fc + 1) * 128], C.ident_f[0:2, 0:2])
                return ins
            S.op("pe", tps, reads=[mrowb, C.ident_f_b], writes=[mpsb])
            m = Ctx()
            modt, modb = C.modalloc[li][0]
            bt, btb = S.sbuf("badat", [128, 96, 2], F32, es=ph)
            S.dma("sp", bt[:], I["b_adaT"][li], writes=[btb])
            S.op("dve", lambda: nc.vector.tensor_tensor(modt[:].rearrange("p a b -> p (a b)"), mps_[:, 0:192], bt[:].rearrange("p a b -> p (a b)"), ALU.add),
                 reads=[mpsb, btb], writes=[modb])
            m.t, m.b = modt, modb
            for nm, gname, j in (("A1", "g1T", 1), ("A2", "g2T", 4)):
                gt, gtb = S.sbuf("g" + nm, [128, KD, 2], F32, es=ph)
                S.dma("sp", gt[:], I[gname][li], writes=[gtb])
                at, atb = C.modalloc[li][1 if nm == "A1" else 2]
                S.op("dve", lambda: nc.vector.scalar_tensor_tensor(at[:], modt[:, j * 16:(j + 1) * 16, :], 1.0, gt[:], ALU.add, ALU.mult),
                     reads=[modb, gtb], writes=[atb])
                setattr(m, nm, at)
                setattr(m, nm + "b", atb)
            C.mod.append(m)
            if "dbg_mod" in C.dump:
                S.dma("sp", C.Sx["dbg_mod"][li], modt[:], reads=[modb])


def modsl(m, j):
    return m.t[:, j * 16:(j + 1) * 16, :]


def norm_blocks(C, ph, x_dram, A_ap, A_b, B_ap, B_b, cb, blocks=TB):
    S, nc = C.S, C.nc
    xr = Ring(S, ph, "xblk", [128, KD, 512], F32, 2)
    sqt, sqb = S.sbuf("nsq", [128, KD, 512], BF16, es=ph)
    ssr = Ring(S, ph, "nss", [128, 512], F32, 2, psum=True)
    rs, rsb = S.sbuf("nrstd", [128, 512], F32, es=ph)
    for bi, (t0, n, lc) in enumerate(blocks):
        xb, xbb = xr.next()
        S.dma("sp", xb[:, :, :n], x_dram[:, t0:t0 + n].rearrange("(k p) t -> p k t", p=128), writes=[xbb])
        S.op("act", lambda: nc.scalar.activation(out=sqt[:, :, :n], in_=xb[:, :, :n], func=AF.Square), reads=[xbb], writes=[sqb])
        ss, ssb = ssr.next()

        def mm():
            ins = None
            for k in range(KD):
                ins = nc.tensor.matmul(ss[:, :n], lhsT=C.ones_bf[:, :], rhs=sqt[:, k, :n], start=(k == 0), stop=(k == KD - 1))
            return ins
        S.op("pe", mm, reads=[sqb, C.ones_bf_b], writes=[ssb])
        S.op("act", lambda: nc.scalar.activation(out=rs[:, :n], in_=ss[:, :n], func=AF.Sqrt, bias=C.eps_t[:, 0:1], scale=1.0 / D),
             reads=[ssb, C.eps_b], writes=[rsb])
        S.op("dve", lambda: nc.vector.reciprocal(rs[:, :n], rs[:, :n]), reads=[rsb], writes=[rsb])

        def nrm():
            ins = None
            for k in range(KD):
                ins = nc.vector.tensor_tensor(xb[:, k, :n], xb[:, k, :n], rs[:, :n], ALU.mult)
            return ins
        S.op("dve", nrm, reads=[xbb, rsb], writes=[xbb])

        def aff_act():
            ins = None
            for k in range(0, KD, 2):
                ins = nc.scalar.activation(out=xb[:, k, :n], in_=xb[:, k, :n], func=AF.Identity,
                                           bias=B_ap[:, k, lc:lc + 1], scale=A_ap[:, k, lc:lc + 1])
            return ins

        def aff_pool():
            ins = None
            for k in range(1, KD, 2):
                ins = nc.vector.tensor_scalar(xb[:, k, :n], xb[:, k, :n], A_ap[:, k, lc:lc + 1], B_ap[:, k, lc:lc + 1], ALU.mult, ALU.add)
            return ins
        S.op("act", aff_act, reads=[xbb, A_b, B_b], writes=[xbb])
        S.op("dve", aff_pool, reads=[xbb, A_b, B_b], writes=[xbb])
        cb(bi, t0, n, lc, xb, xbb)


def phase_in(C, li, x_dram):
    S, nc, I, Sx = C.S, C.nc, C.I, C.Sx
    m = C.mod[li]
    with ExitStack() as ph:
        hT, hTb = S.sbuf("hT", [128, KD, T], BF16, es=ph)
        with ExitStack() as ph1:
            def cb(bi, t0, n, lc, xb, xbb):
                S.op("dve", lambda: nc.vector.tensor_copy(hT[:, :, t0:t0 + n], xb[:, :, :n]), reads=[xbb], writes=[hTb])
            norm_blocks(C, ph1, x_dram, m.A1, m.A1b, modsl(m, 0), m.b, cb)
        if "dbg_hT" in C.dump:
            S.dma("sp", Sx["dbg_hT"].rearrange("(k p) t -> p k t", p=128), hT[:], reads=[hTb])
        if C.stop_after == (li, "in", "norm"):
            return
        S.barrier()
        wr = Ring(S, ph, "wt", [128, KD, 512], BF16, 2)
        pr = Ring(S, ph, "pin", [128, 512], F32, 4, psum=True)
        win = I["w_in"][li]

        def wload(segs):
            wt, wtb = wr.next()
            for si, (src, c0, ncol, off) in enumerate(segs):
                S.dma("pool", wt[:, :, off:off + ncol], src[:, c0:c0 + ncol].rearrange("(k p) n -> p k n", p=128),
                      writes=[wtb] if si == 0 else [], awrites=[] if si == 0 else [wtb])
            return wt, wtb

        def proj_fm(wt, wtb, woff, M, t0, n):
            ps, psb = pr.next()

            def mm():
                ins = None
                for k in range(KD):
                    ins = nc.tensor.matmul(ps[:M, :n], lhsT=wt[:, k, woff:woff + M], rhs=hT[:, k, t0:t0 + n], start=(k == 0), stop=(k == KD - 1))
                return ins
            S.op("pe", mm, reads=[wtb, hTb], writes=[psb])
            return ps, psb

        obr = Ring(S, ph, "ob", [128, 512], BF16, 4)
        ofr = Ring(S, ph, "of", [128, 512], F32, 3)

        wt, wtb = wload([(win, 0, 512, 0)])
        for (t0, n, lc) in TB:
            for c in range(4):
                ps, psb = proj_fm(wt, wtb, c * 128, 128, t0, n)
                ob, obb = obr.next()
                S.op("act", lambda: nc.scalar.activation(out=ob[:, :n], in_=ps[:, :n], func=AF.Gelu), reads=[psb], writes=[obb])
                S.dma("sp", Sx["uTg"][c * 128:(c + 1) * 128, t0:t0 + n], ob[:, :n], reads=[obb])

        if C.stop_after == (li, "in", "A"):
            return
        wt, wtb = wload([(win, 512, 512, 0)])
        gs, gsb = S.sbuf("gs", [128, 512], F32, es=ph)
        S.dma("sp", gs[:], I["sgu_g"][li].to_broadcast([128, 512]), writes=[gsb])
        junk, junkb = S.sbuf("junk", [128, 512], BF16, es=ph)
        st, stb = S.sbuf("vst", [128, 2], F32, es=ph)
        for tt in range(18):
            ps, psb = pr.next()

            def mm():
                ins = None
                for k in range(KD):
                    ins = nc.tensor.matmul(ps[:, :], lhsT=hT[:, k, tt * 128:(tt + 1) * 128], rhs=wt[:, k, :], start=(k == 0), stop=(k == KD - 1))
                return ins
            S.op("pe", mm, reads=[wtb, hTb], writes=[psb])
            gv, gvb = ofr.next()
            S.op("act", lambda: nc.scalar.activation(out=gv[:, :], in_=ps[:, :], func=AF.Gelu), reads=[psb], writes=[gvb])
            S.op("act", lambda: nc.scalar.activation(out=junk[:, :], in_=gv[:, :], func=AF.Square, accum_out=st[:, 0:1]),
                 reads=[gvb], writes=[junkb, stb])
            S.op("act", lambda: nc.scalar.activation(out=st[:, 1:2], in_=st[:, 0:1], func=AF.Sqrt, bias=C.eps_t[:, 0:1], scale=1.0 / 512),
                 reads=[stb, C.eps_b], writes=[stb])
            S.op("dve", lambda: nc.vector.reciprocal(st[:, 1:2], st[:, 1:2]), reads=[stb], writes=[stb])
            ob, obb = obr.next()
            S.op("dve", lambda: nc.vector.scalar_tensor_tensor(ob[:, :], gv[:, :], st[:, 1:2], gs[:, :], ALU.mult, ALU.mult),
                 reads=[gvb, stb, gsb], writes=[obb])
            S.dma("sp", Sx["v_tok"][tt * 128:(tt + 1) * 128, :], ob[:, :], reads=[obb])

        if C.stop_after == (li, "in", "v"):
            return
        wt, wtb = wload([(win, 1024, 512, 0)])
        for (t0, n, lc) in TB:
            for c in range(4):
                ps, psb = proj_fm(wt, wtb, c * 128, 128, t0, n)
                ob, obb = obr.next()
                S.op("dve" if c % 2 else "act", (lambda: nc.vector.tensor_copy(ob[:, :n], ps[:, :n])) if c % 2 else
                     (lambda: nc.scalar.activation(out=ob[:, :n], in_=ps[:, :n], func=AF.Copy)), reads=[psb], writes=[obb])
                dst = Sx["cqT"][c * 128:(c + 1) * 128, t0:t0 + n] if c < 3 else Sx["ckvT"][0:128, t0:t0 + n]
                S.dma("sp", dst, ob[:, :n], reads=[obb])

        if C.stop_after == (li, "in", "B"):
            return
        wt, wtb = wload([(win, 1536, 192, 0), (I["w_in_sw"][li], 0, 64, 192)])
        rc, rcb = S.sbuf("ropec", [64, T], F32, es=ph)
        rsn, rsnb = S.sbuf("ropes", [64, T], F32, es=ph)
        S.dma("sp", rc[:], I["rope_cos"][:, :], writes=[rcb])
        S.dma("sp", rsn[:], I["rope_sin"][:, :], writes=[rsnb])
        for (t0, n, lc) in TB:
            ps, psb = proj_fm(wt, wtb, 0, 128, t0, n)
            ob, obb = obr.next()
            S.op("act", lambda: nc.scalar.activation(out=ob[:, :n], in_=ps[:, :n], func=AF.Copy), reads=[psb], writes=[obb])
            S.dma("sp", Sx["ckvT"][128:256, t0:t0 + n], ob[:, :n], reads=[obb])
            ps1, ps1b = proj_fm(wt, wtb, 128, 64, t0, n)
            ps2, ps2b = proj_fm(wt, wtb, 192, 64, t0, n)
            f1, f1b = ofr.next()
            f2, f2b = ofr.next()
            S.op("dve", lambda: nc.vector.tensor_tensor(f1[:64, :n], ps1[:64, :n], rc[:, t0:t0 + n], ALU.mult), reads=[ps1b, rcb], writes=[f1b])
            S.op("dve", lambda: nc.vector.tensor_tensor(f2[:64, :n], ps2[:64, :n], rsn[:, t0:t0 + n], ALU.mult), reads=[ps2b, rsnb], writes=[f2b])
            ob, obb = obr.next()
            S.op("dve", lambda: nc.vector.tensor_tensor(ob[:64, :n], f1[:64, :n], f2[:64, :n], ALU.add), reads=[f1b, f2b], writes=[obb])
            S.dma("sp", Sx["krT"][:, t0:t0 + n], ob[:64, :n], reads=[obb])

        if C.stop_after == (li, "in", "C"):
            return
        wt, wtb = wload([(win, 1728, 512, 0)])
        for (t0, n, lc) in TB:
            for c in range(4):
                ps, psb = proj_fm(wt, wtb, c * 128, 128, t0, n)
                of, ofb = ofr.next()
                ob, obb = obr.next()
                S.op("dve", lambda: nc.vector.tensor_copy(of[:, :n], ps[:, :n]), reads=[psb], writes=[ofb])
                S.op("act", lambda: nc.scalar.activation(out=ob[:, :n], in_=of[:, :n], func=AF.Copy), reads=[ofb], writes=[obb])
                S.dma("sp", Sx["s5uT"][c * 128:(c + 1) * 128, t0:t0 + n], ob[:, :n], reads=[obb])
                S.dma("sp", Sx["s5u32"][c * 128:(c + 1) * 128, t0:t0 + n], of[:, :n], reads=[ofb])

        if C.stop_after == (li, "in", "D"):
            return
        cw, cwb = S.sbuf("convw", [128, 4, 3], F32, es=ph)
        S.dma("sp", cw[:], I["conv_wT"][li], writes=[cwb])
        ZW = 2307
        zr = Ring(S, ph, "zbuf", [128, ZW], F32, 2)
        yr = Ring(S, ph, "ybuf", [128, ZW], F32, 2)
        bgr = Ring(S, ph, "bgbuf", [128, T], BF16, 2)
        cor = Ring(S, ph, "cobuf", [128, T], BF16, 2)

        def zoff(t0):
            return t0 + 1 if t0 < NL else t0 + 2
        for c in range(4):
            wt, wtb = wload([(win, 2240 + c * 128, 128, 0), (win, 2752 + c * 128, 128, 128), (win, 3264 + c * 128, 128, 256)])
            zb, zbb = zr.next()
            yb, ybb = yr.next()
            bg, bgb = bgr.next()
            co, cob = cor.next()

            def zz():
                nc.gpsimd.memset(zb[:, 0:1], 0.0)
                nc.gpsimd.memset(zb[:, NL + 1:NL + 2], 0.0)
                return nc.gpsimd.memset(zb[:, ZW - 1:ZW], 0.0)
            S.op("pool", zz, writes=[zbb])
            for (t0, n, lc) in TB:
                psB, psBb = proj_fm(wt, wtb, 0, 128, t0, n)
                psC, psCb = proj_fm(wt, wtb, 128, 128, t0, n)
                psH, psHb = proj_fm(wt, wtb, 256, 128, t0, n)
                S.op("act", lambda: nc.scalar.activation(out=bg[:, t0:t0 + n], in_=psB[:, :n], func=AF.Copy), reads=[psBb], writes=[bgb])
                of, ofb = ofr.next()
                S.op("act", lambda: nc.scalar.activation(out=of[:, :n], in_=psC[:, :n], func=AF.Copy), reads=[psCb], writes=[ofb])
                zo = zoff(t0)
                S.op("dve", lambda: nc.vector.tensor_tensor(zb[:, zo:zo + n], psH[:, :n], of[:, :n], ALU.mult), reads=[psHb, ofb], writes=[zbb])

            S.seq("dve", [
                lambda: nc.vector.tensor_scalar(yb[:, 1:ZW - 1], zb[:, 1:ZW - 1], cw[:, c, 1:2], None, ALU.mult),
                lambda: nc.vector.scalar_tensor_tensor(yb[:, 1:ZW - 1], zb[:, 0:ZW - 2], cw[:, c, 0:1], yb[:, 1:ZW - 1], ALU.mult, ALU.add),
                lambda: nc.vector.scalar_tensor_tensor(yb[:, 1:ZW - 1], zb[:, 2:ZW], cw[:, c, 2:3], yb[:, 1:ZW - 1], ALU.mult, ALU.add),
            ], reads=[zbb, cwb], writes=[ybb])

            def gate():
                nc.vector.tensor_tensor(co[:, 0:NL], yb[:, 1:NL + 1], bg[:, 0:NL], ALU.mult)
                return nc.vector.tensor_tensor(co[:, NL:T], yb[:, NL + 2:ZW - 1], bg[:, NL:T], ALU.mult)
            S.op("dve", gate, reads=[ybb, bgb], writes=[cob])
            S.dma("sp", Sx["catT"][1536 + c * 128:1536 + (c + 1) * 128, :], co[:, :], reads=[cob])


def phase_sgu(C, li):
    S, nc, I, Sx = C.S, C.nc, C.I, C.Sx
    with ExitStack() as ph:
        ws, wsb = S.sbuf("wsT", [128, 4, 128], BF16, es=ph)
        S.dma("pool", ws[:], I["sgu_wT"][li], writes=[wsb])
        bsr, bsrb = S.sbuf("bsr", [128, 4, 512], F32, es=ph)
        S.dma("sp", bsr[:].rearrange("p a b -> p (a b)"), I["sgu_b4"][li].to_broadcast([128, 2048]), writes=[bsrb])
        vr = Ring(S, ph, "vt", [128, 4, 512], BF16, 2)
        ur = Ring(S, ph, "ut", [128, 4, 512], BF16, 2)
        pr = Ring(S, ph, "psg", [128, 512], F32, 4, psum=True)
        tr = Ring(S, ph, "tmp", [128, 512], F32, 3)
        orr = Ring(S, ph, "osg", [128, 512], BF16, 3)
        for (t0, n, lc) in TB:
            na = n // 128
            vt, vtb = vr.next()
            ut, utb = ur.next()
            S.dma("sp", vt[:, :na, :], Sx["v_tok"][t0:t0 + n, :].rearrange("(a q) c -> q a c", q=128), writes=[vtb])
            S.dma("sp", ut[:, :, :n], Sx["uTg"][:, t0:t0 + n].rearrange("(h c) t -> c h t", c=128), writes=[utb])
            for h in range(4):
                ps, psb = pr.next()

                def mm():
                    ins = None
                    for a in range(na):
                        ins = nc.tensor.matmul(ps[:, a * 128:(a + 1) * 128], lhsT=vt[:, a, h * 128:(h + 1) * 128], rhs=ws[:, h, :], start=True, stop=True)
                    return ins
                S.op("pe", mm, reads=[vtb, wsb], writes=[psb])
                tm, tmb = tr.next()
                S.op("dve", lambda: nc.vector.tensor_tensor(tm[:, :n], ps[:, :n], bsr[:, h, :n], ALU.add), reads=[psb, bsrb], writes=[tmb])
                ob, obb = orr.next()
                S.op("dve", lambda: nc.vector.tensor_tensor(ob[:, :n], tm[:, :n], ut[:, h, :n], ALU.mult), reads=[tmb, utb], writes=[obb])
                S.dma("sp", Sx["catT"][h * 128:(h + 1) * 128, t0:t0 + n], ob[:, :n], reads=[obb])


def phase_mla(C, li):
    S, nc, I, Sx = C.S, C.nc, C.I, C.Sx
    SC = 192.0 ** -0.5
    with ExitStack() as ph:
        cq, cqb = S.sbuf("cq", [128, 3, T], BF16, es=ph)
        ckv, ckvb = S.sbuf("ckv", [128, 2, T], BF16, es=ph)
        kr, krb = S.sbuf("kr", [64, T], BF16, es=ph)
        S.dma("sp", cq[:], Sx["cqT"].rearrange("(c p) t -> p c t", p=128), writes=[cqb])
        S.dma("sp", ckv[:], Sx["ckvT"].rearrange("(c p) t -> p c t", p=128), writes=[ckvb])
        S.dma("sp", kr[:], Sx["krT"][:, :], writes=[krb])
        wuq, wuqb = S.sbuf("wuq", [128, 3, 768], BF16, es=ph)
        wuqs, wuqsb = S.sbuf("wuqs", [128, 3, 256], BF16, es=ph)
        wukv, wukvb = S.sbuf("wukv", [128, 2, 1024], BF16, es=ph)
        wv, wvb = S.sbuf("wv", [128, 2, 512], BF16, es=ph)
        rq, rqb = S.sbuf("rq", [128, T], F32, es=ph)
        rk, rkb = S.sbuf("rk", [128, T], F32, es=ph)
        rkt, rktb = S.sbuf("rkt", [128, 18], F32, es=ph)
        pr = Ring(S, ph, "pj", [128, 512], F32, 3, psum=True)
        pst_, pstb = S.psum("pst", [128, 512], F32, es=ph)
        pst = pst_[:, 0:18]
        with ExitStack() as wp:
            w32, w32b = S.sbuf("w32", [128, 3, 768], F32, es=wp)
            ws32, ws32b = S.sbuf("ws32", [128, 3, 256], F32, es=wp)
            wk32, wk32b = S.sbuf("wk32", [128, 2, 1024], F32, es=wp)
            qg, qgb = S.sbuf("qg", [128, 3], F32, es=wp)
            kg, kgb = S.sbuf("kg", [128, 2], F32, es=wp)
            S.dma("sp", w32[:], I["w_uq"][li].rearrange("(c p) n -> p c n", p=128), writes=[w32b])
            S.dma("sp", ws32[:], I["w_uq_sw"][li].rearrange("(c p) n -> p c n", p=128), writes=[ws32b])
            S.dma("sp", wk32[:], I["w_ukv"][li].rearrange("(c p) n -> p c n", p=128), writes=[wk32b])
            S.dma("sp", qg[:], I["qg"][li], writes=[qgb])
            S.dma("sp", kg[:], I["kvg"][li], writes=[kgb])

            def sc1():
                ins = None
                for c in range(3):
                    nc.vector.tensor_scalar(wuq[:, c, :], w32[:, c, :], qg[:, c:c + 1], None, ALU.mult)
                    ins = nc.vector.tensor_scalar(wuqs[:, c, :], ws32[:, c, :], qg[:, c:c + 1], None, ALU.mult)
                return ins
            S.op("dve", sc1, reads=[w32b, ws32b, qgb], writes=[wuqb, wuqsb])

            def sc2():
                ins = None
                for c in range(2):
                    ins = nc.vector.tensor_scalar(wukv[:, c, :], wk32[:, c, :], kg[:, c:c + 1], None, ALU.mult)
                return ins
            S.op("dve", sc2, reads=[wk32b, kgb], writes=[wukvb])
            S.op("dve", lambda: nc.vector.tensor_copy(wv[:].rearrange("p c (h x) -> p c h x", x=128),
                                                       wukv[:].rearrange("p c (h x) -> p c h x", x=256)[:, :, :, 128:256]),
                 reads=[wukvb], writes=[wvb])
            sqq, sqqb = S.sbuf("sqq", [128, 3, T], BF16, es=wp)
            sqk, sqkb = S.sbuf("sqk", [128, 2, T], BF16, es=wp)
            S.op("act", lambda: nc.scalar.activation(out=sqq[:], in_=cq[:], func=AF.Square), reads=[cqb], writes=[sqqb])
            S.op("act", lambda: nc.scalar.activation(out=sqk[:], in_=ckv[:], func=AF.Square), reads=[ckvb], writes=[sqkb])
            for (sq, sqb_, nch, rt, rtb) in ((sqq, sqqb, 3, rq, rqb), (sqk, sqkb, 2, rk, rkb)):
                for (t0, n, lc) in TB:
                    ps, psb = pr.next()

                    def mm():
                        ins = None
                        for c in range(nch):
                            ins = nc.tensor.matmul(ps[:, :n], lhsT=C.ones_bf[:, :], rhs=sq[:, c, t0:t0 + n], start=(c == 0), stop=(c == nch - 1))
                        return ins
                    S.op("pe", mm, reads=[sqb_, C.ones_bf_b], writes=[psb])
                    S.op("act", lambda: nc.scalar.activation(out=rt[:, t0:t0 + n], in_=ps[:, :n], func=AF.Sqrt, bias=C.eps_t[:, 0:1],
                                                             scale=1.0 / (128 * nch)), reads=[psb, C.eps_b], writes=[rtb])
            S.op("dve", lambda: nc.vector.reciprocal(rq[:], rq[:]), reads=[rqb], writes=[rqb])
            S.op("dve", lambda: nc.vector.reciprocal(rk[:], rk[:]), reads=[rkb], writes=[rkb])

            def mmt():
                ins = None
                for tt in range(18):
                    for c in range(2):
                        ins = nc.tensor.matmul(pst[:, tt:tt + 1], lhsT=sqk[:, c, tt * 128:(tt + 1) * 128], rhs=C.ones_bf[:, 0:1],
                                               start=(c == 0), stop=(c == 1))
                return ins
            S.op("pe", mmt, reads=[sqkb, C.ones_bf_b], writes=[pstb])
            S.op("act", lambda: nc.scalar.activation(out=rkt[:], in_=pst_[:, 0:18], func=AF.Sqrt, bias=C.eps_t[:, 0:1], scale=1.0 / 256),
                 reads=[pstb, C.eps_b], writes=[rktb])
            S.op("dve", lambda: nc.vector.reciprocal(rkt[:], rkt[:]), reads=[rktb], writes=[rktb])
            S.barrier()
        rc, rcb = S.sbuf("ropec", [64, T], F32, es=ph)
        rsn, rsnb = S.sbuf("ropes", [64, T], F32, es=ph)
        S.dma("sp", rc[:], I["rope_cos"][:, :], writes=[rcb])
        S.dma("sp", rsn[:], I["rope_sin"][:, :], writes=[rsnb])
        qn, qnb = S.sbuf("qn", [128, 4, T], BF16, es=ph)
        qr, qrb = S.sbuf("qr", [64, 4, T], BF16, es=ph)
        kn, knb = S.sbuf("kn", [128, 4, T], BF16, es=ph)
        vtok, vtokb = S.sbuf("vtok", [128, 18, 512], BF16, es=ph)
        fr = Ring(S, ph, "mf", [128, 512], F32, 4)

        def proj(wt, wtb, nch, c0, M, src, srcb, t0, n):
            ps, psb = pr.next()

            def mm():
                ins = None
                for c in range(nch):
                    ins = nc.tensor.matmul(ps[:M, :n], lhsT=wt[:, c, c0:c0 + M], rhs=src[:, c, t0:t0 + n], start=(c == 0), stop=(c == nch - 1))
                return ins
            S.op("pe", mm, reads=[wtb, srcb], writes=[psb])
            return ps, psb
        for h in range(4):
            for (t0, n, lc) in TB:
                ps, psb = proj(wuq, wuqb, 3, h * 192, 128, cq, cqb, t0, n)
                S.op("dve", lambda: nc.vector.scalar_tensor_tensor(qn[:, h, t0:t0 + n], ps[:, :n], SC, rq[:, t0:t0 + n], ALU.mult, ALU.mult),
                     reads=[psb, rqb], writes=[qnb])
                ps1, ps1b = proj(wuq, wuqb, 3, h * 192 + 128, 64, cq, cqb, t0, n)
                ps2, ps2b = proj(wuqs, wuqsb, 3, h * 64, 64, cq, cqb, t0, n)
                f1, f1b = fr.next()
                f2, f2b = fr.next()
                S.op("dve", lambda: nc.vector.tensor_tensor(f1[:64, :n], ps1[:64, :n], rc[:, t0:t0 + n], ALU.mult), reads=[ps1b, rcb], writes=[f1b])
                S.op("dve", lambda: nc.vector.tensor_tensor(f2[:64, :n], ps2[:64, :n], rsn[:, t0:t0 + n], ALU.mult), reads=[ps2b, rsnb], writes=[f2b])
                S.op("dve", lambda: nc.vector.tensor_tensor(f1[:64, :n], f1[:64, :n], f2[:64, :n], ALU.add), reads=[f1b, f2b], writes=[f1b])
                S.op("dve", lambda: nc.vector.scalar_tensor_tensor(qr[:, h, t0:t0 + n], f1[:64, :n], SC, rq[:64, t0:t0 + n], ALU.mult, ALU.mult),
                     reads=[f1b, rqb], writes=[qrb])
                ps, psb = proj(wukv, wukvb, 2, h * 256, 128, ckv, ckvb, t0, n)
                S.op("dve", lambda: nc.vector.tensor_tensor(kn[:, h, t0:t0 + n], ps[:, :n], rk[:, t0:t0 + n], ALU.mult), reads=[psb, rkb], writes=[knb])
        for tt in range(18):
            ps, psb = pr.next()

            def mm():
                ins = None
                for c in range(2):
                    ins = nc.tensor.matmul(ps[:, :], lhsT=ckv[:, c, tt * 128:(tt + 1) * 128], rhs=wv[:, c, :], start=(c == 0), stop=(c == 1))
                return ins
            S.op("pe", mm, reads=[ckvb, wvb], writes=[psb])
            S.op("dve", lambda: nc.vector.tensor_scalar(vtok[:, tt, :], ps[:, :], rkt[:, tt:tt + 1], None, ALU.mult), reads=[psb, rktb], writes=[vtokb])
        opr = Ring(S, ph, "ops", [128, 512], F32, 2, psum=True)
        spr = Ring(S, ph, "sps", [128, 512], F32, 2, psum=True)
        ptr = Ring(S, ph, "pt", [128, 512], BF16, 3)
        rsr = Ring(S, ph, "rsm", [128, 512], F32, 2)
        atr = Ring(S, ph, "att", [128, 512], BF16, 2)
        for h in range(4):
            for (t0, n, lc) in TB:
                kts = list(range(18)) if lc == 0 else [16, 17]
                ops, opsb = opr.next()
                sps, spsb = spr.next()
                pend = None
                nk = len(kts)

                def pv(pd):
                    kt_, pt_, ptb_, i_ = pd

                    def mmo():
                        nc.tensor.matmul(ops[:, :n], lhsT=vtok[:, kt_, h * 128:(h + 1) * 128], rhs=pt_[:, :n], start=(i_ == 0), stop=(i_ == nk - 1))
                        return nc.tensor.matmul(sps[:, :n], lhsT=C.ones_bf[:, :], rhs=pt_[:, :n], start=(i_ == 0), stop=(i_ == nk - 1))
                    S.op("pe", mmo, reads=[vtokb, ptb_, C.ones_bf_b], writes=[opsb, spsb])
                for i, kt in enumerate(kts):
                    st, stb = pr.next()

                    def mms():
                        nc.tensor.matmul(st[:, :n], lhsT=kn[:, h, kt * 128:(kt + 1) * 128], rhs=qn[:, h, t0:t0 + n], start=True, stop=False)
                        return nc.tensor.matmul(st[:, :n], lhsT=kr[:, kt * 128:(kt + 1) * 128], rhs=qr[:, h, t0:t0 + n], start=False, stop=True)
                    S.op("pe", mms, reads=[knb, qnb, krb, qrb], writes=[stb])
                    pt, ptb = ptr.next()
                    S.op("act", lambda: nc.scalar.activation(out=pt[:, :n], in_=st[:, :n], func=AF.Exp), reads=[stb], writes=[ptb])
                    if pend is not None:
                        pv(pend)
                    pend = (kt, pt, ptb, i)
                pv(pend)
                rs_, rsb_ = rsr.next()
                S.op("dve", lambda: nc.vector.reciprocal(rs_[:, :n], sps[:, :n]), reads=[spsb], writes=[rsb_])
                at, atb = atr.next()
                S.op("dve", lambda: nc.vector.tensor_tensor(at[:, :n], ops[:, :n], rs_[:, :n], ALU.mult), reads=[opsb, rsb_], writes=[atb])
                S.dma("sp", Sx["catT"][512 + h * 128:512 + (h + 1) * 128, t0:t0 + n], at[:, :n], reads=[atb])


def _s5_disc(C, es, are, aim, ldt, n, need_coef, tagb):
    S, nc = C.S, C.nc
    X = [S.sbuf("s5x%d" % i, [128, n], F32, es=es) for i in range(6)]
    KI = S.sbuf("s5ki", [128, n], I32, es=es)
    (x1, b1), (x2, b2), (x3, b3), (x4, b4), (x5, b5), (x6, b6) = X
    ki, kib = KI
    S.op("dve", lambda: nc.vector.tensor_scalar(are, are, -1e-4, None, ALU.min), reads=[tagb], writes=[tagb])
    ea = [
        lambda: nc.vector.tensor_scalar(ki[:], ldt, 1.0 / math.log(2.0), None, ALU.mult),
        lambda: nc.vector.tensor_copy(x3[:], ki[:]),
        lambda: nc.vector.scalar_tensor_tensor(x4[:], x3[:], -0.693145751953125, ldt, ALU.mult, ALU.add),
        lambda: nc.vector.scalar_tensor_tensor(x4[:], x3[:], -1.42860682030941723212e-6, x4[:], ALU.mult, ALU.add),
        lambda: nc.vector.tensor_scalar(x5[:], x4[:], 1.0 / 9.0, 1.0, ALU.mult, ALU.add),
    ]
    for j in range(8, 0, -1):
        ea.append(lambda: nc.vector.tensor_tensor(x5[:], x5[:], x4[:], ALU.mult))
        ea.append(lambda j=j: nc.vector.tensor_scalar(x5[:], x5[:], 1.0 / j, 1.0, ALU.mult, ALU.add))
    ea.append(lambda: nc.vector.tensor_scalar(ki[:], x3[:], 127.0, 8388608.0, ALU.add, ALU.mult))
    ea.append(lambda: nc.vector.tensor_tensor(ldt, x5[:], ki[:].bitcast(F32), ALU.mult))
    S.seq("dve", ea, reads=[tagb], writes=[tagb, kib, b3, b4, b5])
    S.op("dve", lambda: nc.vector.tensor_tensor(x1[:], are, ldt, ALU.mult), reads=[tagb], writes=[b1])
    S.op("act", lambda: nc.scalar.activation(out=x1[:], in_=x1[:], func=AF.Exp), reads=[b1], writes=[b1])
    S.op("dve", lambda: nc.vector.tensor_tensor(x2[:], aim, ldt, ALU.mult), reads=[tagb], writes=[b2])
    S.op("dve", lambda: nc.vector.tensor_scalar(x2[:], x2[:], 1.0 / (2 * math.pi), None, ALU.mult), reads=[b2], writes=[b2])
    if not need_coef:
        return {"r": (x1, b1), "f": (x2, b2)}
    S.op("dve", lambda: nc.vector.tensor_copy(ki[:], x2[:]), reads=[b2], writes=[kib])
    S.op("dve", lambda: nc.vector.tensor_tensor(x3[:], x2[:], ki[:], ALU.subtract), reads=[b2, kib], writes=[b3])
    S.op("act", lambda: nc.scalar.activation(out=x4[:], in_=x3[:], func=AF.Sin, scale=TWO_PI), reads=[b3], writes=[b4])
    S.op("dve", lambda: nc.vector.tensor_scalar(ki[:], x2[:], 0.25, None, ALU.add), reads=[b2], writes=[kib])
    S.op("dve", lambda: nc.vector.tensor_tensor(x3[:], x2[:], ki[:], ALU.subtract), reads=[b2, kib], writes=[b3])
    S.op("act", lambda: nc.scalar.activation(out=x5[:], in_=x3[:], func=AF.Sin, scale=TWO_PI, bias=C.hpi_t[:, 0:1]), reads=[b3, C.hpi_b], writes=[b5])
    S.op("dve", lambda: nc.vector.tensor_tensor(x5[:], x1[:], x5[:], ALU.mult), reads=[b1, b5], writes=[b5])
    S.op("dve", lambda: nc.vector.tensor_scalar(x5[:], x5[:], -1.0, None, ALU.add), reads=[b5], writes=[b5])
    S.op("dve", lambda: nc.vector.tensor_tensor(x4[:], x1[:], x4[:], ALU.mult), reads=[b1, b4], writes=[b4])
    S.op("dve", lambda: nc.vector.tensor_tensor(x1[:], are, are, ALU.mult), reads=[tagb], writes=[b1])
    S.op("dve", lambda: nc.vector.tensor_tensor(x3[:], aim, aim, ALU.mult), reads=[tagb], writes=[b3])
    S.op("dve", lambda: nc.vector.tensor_tensor(x1[:], x1[:], x3[:], ALU.add), reads=[b1, b3], writes=[b1])
    S.op("dve", lambda: nc.vector.reciprocal(x1[:], x1[:]), reads=[b1], writes=[b1])
    S.op("dve", lambda: nc.vector.tensor_tensor(x2[:], x5[:], are, ALU.mult), reads=[b5, tagb], writes=[b2])
    S.op("dve", lambda: nc.vector.tensor_tensor(x3[:], x4[:], aim, ALU.mult), reads=[b4, tagb], writes=[b3])
    S.op("dve", lambda: nc.vector.tensor_tensor(x2[:], x2[:], x3[:], ALU.add), reads=[b2, b3], writes=[b2])
    S.op("dve", lambda: nc.vector.tensor_tensor(x2[:], x2[:], x1[:], ALU.mult), reads=[b2, b1], writes=[b2])
    S.op("dve", lambda: nc.vector.tensor_tensor(x6[:], x4[:], are, ALU.mult), reads=[b4, tagb], writes=[b6])
    S.op("dve", lambda: nc.vector.tensor_tensor(x3[:], x5[:], aim, ALU.mult), reads=[b5, tagb], writes=[b3])
    S.op("dve", lambda: nc.vector.tensor_tensor(x6[:], x6[:], x3[:], ALU.subtract), reads=[b6, b3], writes=[b6])
    S.op("dve", lambda: nc.vector.tensor_tensor(x6[:], x6[:], x1[:], ALU.mult), reads=[b6, b1], writes=[b6])
    return {"cre": (x2, b2), "cim": (x6, b6), "t": [(x1, b1), (x3, b3), (x4, b4), (x5, b5)]}


def phase_s5(C, li):
    S, nc, I, Sx = C.S, C.nc, C.I, C.Sx
    with ExitStack() as ph:
        BbR, BbRb = S.sbuf("BbR", [128, 32, 128], BF16, es=ph)
        BbI, BbIb = S.sbuf("BbI", [128, 32, 128], BF16, es=ph)
        CR, CRb = S.sbuf("CR", [128, 32, 128], BF16, es=ph)
        CIn, CInb = S.sbuf("CIn", [128, 32, 128], BF16, es=ph)
        CRn, CRnb = S.sbuf("CRn", [128, 32, 128], BF16, es=ph)
        pp, ppb = S.sbuf("s5pp", [128, 3, 32], F32, es=ph)
        S.dma("sp", pp[:], I["s5_pp"][li], writes=[ppb])
        dpp = _s5_disc(C, ph, pp[:, 0, :], pp[:, 1, :], pp[:, 2, :], 32, False, ppb)
        rpp, rppb = dpp["r"]
        fpp, fppb = dpp["f"]
        with ExitStack() as wp:
            pp2, pp2b = S.sbuf("s5pp2", [128, 3, 32], F32, es=wp)
            S.dma("sp", pp2[:], I["s5_pp"][li], writes=[pp2b])
            dc = _s5_disc(C, wp, pp2[:, 0, :], pp2[:, 1, :], pp2[:, 2, :], 32, True, pp2b)
            cre, creb = dc["cre"]
            cim, cimb = dc["cim"]
            onesf, onesfb = S.sbuf("onesf", [128, 128], F32, es=wp)
            S.op("pool", lambda: nc.gpsimd.memset(onesf[:], 1.0), writes=[onesfb])
            Bre, Breb = S.sbuf("Bre32", [128, 4096], F32, es=wp)
            Bim, Bimb = S.sbuf("Bim32", [128, 4096], F32, es=wp)
            Cre, Creb = S.sbuf("Cre32", [128, 4096], F32, es=wp)
            Cim, Cimb = S.sbuf("Cim32", [128, 4096], F32, es=wp)
            S.dma("sp", Bre[:], I["s5_Bre"][li], writes=[Breb])
            S.dma("sp", Bim[:], I["s5_Bim"][li], writes=[Bimb])
            S.dma("sp", Cre[:], I["s5_Cre"][li], writes=[Creb])
            S.dma("sp", Cim[:], I["s5_Cim"][li], writes=[Cimb])
            S.op("act", lambda: nc.scalar.activation(out=CR[:].rearrange("p a b -> p (a b)"), in_=Cre[:], func=AF.Copy), reads=[Creb], writes=[CRb])
            S.op("act", lambda: nc.scalar.activation(out=CIn[:].rearrange("p a b -> p (a b)"), in_=Cim[:], func=AF.Copy, scale=-1.0), reads=[Cimb], writes=[CInb])
            S.op("act", lambda: nc.scalar.activation(out=CRn[:].rearrange("p a b -> p (a b)"), in_=Cre[:], func=AF.Copy, scale=-1.0), reads=[Creb], writes=[CRnb])
            dgr = Ring(S, wp, "dg", [128, 4, 128], F32, 4)
            rpr = Ring(S, wp, "rp", [128, 512], F32, 4, psum=True)
            t3r = Ring(S, wp, "s5p3", [128, 512], F32, 2)
            t4r = Ring(S, wp, "s5p4", [128, 512], F32, 2)
            for g4 in range(8):
                reps = []
                for (v, vb) in ((cre, creb), (cim, cimb)):
                    dg, dgb = dgr.next()

                    def mkd():
                        ins = None
                        for j in range(4):
                            ins = nc.vector.tensor_scalar(dg[:, j, :], C.ident_f[:, :], v[:, g4 * 4 + j:g4 * 4 + j + 1], None, ALU.mult)
                        return ins
                    S.op("dve", mkd, reads=[vb, C.ident_f_b], writes=[dgb])
                    rp, rpb = rpr.next()

                    def mmr():
                        ins = None
                        for j in range(4):
                            ins = nc.tensor.matmul(rp[:, j * 128:(j + 1) * 128], lhsT=onesf[:, :], rhs=dg[:, j, :], start=True, stop=True)
                        return ins
                    S.op("pe", mmr, reads=[onesfb, dgb], writes=[rpb])
                    reps.append((rp, rpb))
                (rcr, rcrb), (rci, rcib) = reps
                sl = slice(g4 * 512, (g4 + 1) * 512)
                osl = slice(g4 * 4, (g4 + 1) * 4)
                t3, t3b = t3r.next()
                t4, t4b = t4r.next()
                S.op("dve", lambda: nc.vector.tensor_tensor(t3[:], rcr[:, :], Bre[:, sl], ALU.mult), reads=[rcrb, Breb], writes=[t3b])
                S.op("dve", lambda: nc.vector.tensor_tensor(t4[:], rci[:, :], Bim[:, sl], ALU.mult), reads=[rcib, Bimb], writes=[t4b])
                S.op("dve", lambda: nc.vector.tensor_tensor(BbR[:, osl, :].rearrange("p a b -> p (a b)"), t3[:], t4[:], ALU.subtract),
                     reads=[t3b, t4b], writes=[BbRb])
                t3, t3b = t3r.next()
                t4, t4b = t4r.next()
                S.op("dve", lambda: nc.vector.tensor_tensor(t3[:], rcr[:, :], Bim[:, sl], ALU.mult), reads=[rcrb, Bimb], writes=[t3b])
                S.op("dve", lambda: nc.vector.tensor_tensor(t4[:], rci[:, :], Bre[:, sl], ALU.mult), reads=[rcib, Breb], writes=[t4b])
                S.op("dve", lambda: nc.vector.tensor_tensor(BbI[:, osl, :].rearrange("p a b -> p (a b)"), t3[:], t4[:], ALU.add),
                     reads=[t3b, t4b], writes=[BbIb])
            S.barrier()
        TA = 1536
        mst = ExitStack()
        ubr = Ring(S, mst, "ubf", [128, T], BF16, 2)
        tau, taub = S.sbuf("tau", [128, 2, T], F32, es=mst)
        S.dma("sp", tau[:].rearrange("p a b -> p (a b)"), I["s5_tau"][0:1, :].to_broadcast([128, 2 * T]), writes=[taub])

        def two(name, dt):
            t, ba = S.sbuf(name, [128, T], dt, es=mst)
            bb = Buf(name + "B")
            S.bufs.append(bb)
            return t, (ba, bb)
        tabr = [(two("cosT%d" % i, F32), two("sinT%d" % i, F32)) for i in range(2)]
        kis, kisb = S.sbuf("kis", [128, T], I32, es=mst)
        evr, evrb = two("evr", F32)
        evi, evib = two("evi", F32)
        bre, breb = two("bre", F32)
        bim, bimb = two("bim", F32)
        sre, sreb = two("sre", F32)
        sim, simb = two("sim", F32)
        pa, pab = two("pa", BF16)
        pb, pbb = two("pb", BF16)
        pc, pcb = two("pc", BF16)
        pd, pdb = two("pd", BF16)
        dsk, dskb = S.sbuf("dsk", [128, 4], F32, es=mst)
        S.dma("sp", dsk[:], I["s5_d"][li], writes=[dskb])
        yps = [S.psum("yps%d" % i, [128, 512], F32, es=mst) for i in range(5)]
        bur = Ring(S, mst, "bu", [128, 512], F32, 3, psum=True)
        tr = Ring(S, mst, "s5t", [128, 512], F32, 4)
        gor = Ring(S, mst, "s5g", [128, 512], BF16, 3)
        iters = [(ct, d, ns) for ct in range(4) for d in range(2) for ns in range(4)]

        def ew(o, ob, x, xb, y, yb, op):
            S.op("dve", lambda: nc.vector.tensor_tensor(o[:], x[:], y[:], op), reads=list(xb) + list(yb), writes=list(ob))

        def colof(it):
            ct, d, ns = it
            return (d * 4 + ct) * 4 + ns
        tabs = {}

        def tab_pool(i):
            pass

        def tab_rest(i):
            ct, d, ns = iters[i]
            fcol = fpp[:, colof(iters[i]):colof(iters[i]) + 1]
            (cosT, cosb), (sinT, sinb) = tabr[i % 2]
            S.op("dve", lambda: nc.vector.tensor_scalar(kis[:], tau[:, d, :], fcol, None, ALU.mult), reads=[taub, fppb], writes=[kisb])
            S.op("dve", lambda: nc.vector.scalar_tensor_tensor(sinT[:], tau[:, d, :], fcol, kis[:], ALU.mult, ALU.subtract),
                 reads=[taub, fppb, kisb], writes=list(sinb))
            S.op("act", lambda: nc.scalar.activation(out=cosT[:], in_=sinT[:], func=AF.Abs), reads=list(sinb), writes=list(cosb))
            S.op("act", lambda: nc.scalar.activation(out=sinT[:], in_=sinT[:], func=AF.Sin, scale=TWO_PI), reads=list(sinb) + list(cosb), writes=list(sinb))
            S.op("act", lambda: nc.scalar.activation(out=cosT[:], in_=cosT[:], func=AF.Sin, scale=-TWO_PI, bias=C.hpi_t[:, 0:1]),
                 reads=list(cosb) + [C.hpi_b], writes=list(cosb))
            tabs[i] = (cosT, cosb, sinT, sinb)
        tab_pool(0)
        tab_rest(0)
        ub_next = ubr.next()
        S.dma("sp", ub_next[0][:], Sx["s5uT"][0:128, :], writes=[ub_next[1]])
        for idx, (ct, d, ns) in enumerate(iters):
            col = colof((ct, d, ns))
            if d == 0 and ns == 0:
                ubf, ubfb = ub_next
                if ct < 3:
                    ub_next = ubr.next()
                    S.dma("sp", ub_next[0][:], Sx["s5uT"][(ct + 1) * 128:(ct + 2) * 128, :], writes=[ub_next[1]])
            cosT, cosb, sinT, sinb = tabs.pop(idx)
            if idx + 1 < len(iters):
                tab_pool(idx + 1)
            for (t0, n, lc) in TB:
                part = 0 if t0 < TA else 1
                pr_, prb_ = bur.next()
                pi_, pib_ = bur.next()
                S.op("pe", lambda: nc.tensor.matmul(pr_[:, :n], lhsT=BbR[:, col, :], rhs=ubf[:, t0:t0 + n], start=True, stop=True),
                     reads=[BbRb, ubfb], writes=[prb_])
                S.op("pe", lambda: nc.tensor.matmul(pi_[:, :n], lhsT=BbI[:, col, :], rhs=ubf[:, t0:t0 + n], start=True, stop=True),
                     reads=[BbIb, ubfb], writes=[pib_])
                S.op("act", lambda: nc.scalar.activation(out=evr[:, t0:t0 + n], in_=pr_[:, :n], func=AF.Copy), reads=[prb_], writes=[evrb[part]])
                S.op("act", lambda: nc.scalar.activation(out=evi[:, t0:t0 + n], in_=pi_[:, :n], func=AF.Copy), reads=[pib_], writes=[evib[part]])
            ew(bre, breb, evr, evrb, cosT, cosb, ALU.mult)
            ew(sre, sreb, evi, evib, sinT, sinb, ALU.mult)
            ew(bim, bimb, evi, evib, cosT, cosb, ALU.mult)
            ew(sim, simb, evr, evrb, sinT, sinb, ALU.mult)
            ew(bre, breb, bre, breb, sre, sreb, ALU.add)
            ew(bim, bimb, bim, bimb, sim, simb, ALU.subtract)
            if idx + 1 < len(iters):
                tab_rest(idx + 1)
            rdec = rpp[:, col:col + 1]
            sq_ = []
            for (src, dst) in ((bre, sre), (bim, sim)):
                if d == 0:
                    sq_.append(lambda src=src, dst=dst: nc.vector.tensor_tensor_scan(dst[:, NL:T], rdec.to_broadcast([128, NX]), src[:, NL:T], 0.0, ALU.mult, ALU.add))
                else:
                    sq_.append(lambda src=src, dst=dst: nc.vector.tensor_tensor_scan(dst[:, NL:T][:, ::-1], rdec.to_broadcast([128, NX]), src[:, NL:T][:, ::-1], 0.0, ALU.mult, ALU.add))
            for (src, dst) in ((bre, sre), (bim, sim)):
                if d == 0:
                    sq_.append(lambda src=src, dst=dst: nc.vector.tensor_tensor_scan(dst[:, 0:NL], rdec.to_broadcast([128, NL]), src[:, 0:NL], dst[:, T - 1:T], ALU.mult, ALU.add))
                else:
                    sq_.append(lambda src=src, dst=dst: nc.vector.tensor_tensor_scan(dst[:, 0:NL][:, ::-1], rdec.to_broadcast([128, NL]), src[:, 0:NL][:, ::-1],
                                                                                     dst[:, NL:NL + 1], ALU.mult, ALU.add))
            S.seq("dve", sq_, reads=list(breb) + list(bimb) + [rppb], writes=list(sreb) + list(simb))
            ew(pa, pab, sre, sreb, cosT, cosb, ALU.mult)
            ew(pb, pbb, sim, simb, sinT, sinb, ALU.mult)
            ew(pc, pcb, sre, sreb, sinT, sinb, ALU.mult)
            ew(pd, pdb, sim, simb, cosT, cosb, ALU.mult)
            first = (d == 0 and ns == 0)
            last = (d == 1 and ns == 3)
            for bi, (t0, n, lc) in enumerate(TB):
                yp, ypb = yps[bi]

                def rd():
                    nc.tensor.matmul(yp[:, :n], lhsT=CR[:, col, :], rhs=pa[:, t0:t0 + n], start=first, stop=False)
                    nc.tensor.matmul(yp[:, :n], lhsT=CRn[:, col, :], rhs=pb[:, t0:t0 + n], start=False, stop=False)
                    nc.tensor.matmul(yp[:, :n], lhsT=CIn[:, col, :], rhs=pc[:, t0:t0 + n], start=False, stop=False)
                    return nc.tensor.matmul(yp[:, :n], lhsT=CIn[:, col, :], rhs=pd[:, t0:t0 + n], start=False, stop=last)
                S.op("pe", rd, reads=[CRb, CRnb, CInb] + list(pab) + list(pbb) + list(pcb) + list(pdb), writes=[ypb])
            if last:
                for bi, (t0, n, lc) in enumerate(TB):
                    yp, ypb = yps[bi]
                    u32, u32b = tr.next()
                    S.dma("sp", u32[:, :n], Sx["s5u32"][ct * 128:(ct + 1) * 128, t0:t0 + n], writes=[u32b])
                    a1, a1b = tr.next()
                    S.op("dve", lambda: nc.vector.scalar_tensor_tensor(a1[:, :n], u32[:, :n], dsk[:, ct:ct + 1], yp[:, :n], ALU.mult, ALU.add),
                         reads=[u32b, dskb, ypb], writes=[a1b])
                    go, gob = gor.next()
                    S.op("act", lambda: nc.scalar.activation(out=go[:, :n], in_=a1[:, :n], func=AF.Gelu), reads=[a1b], writes=[gob])
                    S.dma("sp", Sx["dbg_g"][ct * 128:(ct + 1) * 128, t0:t0 + n], go[:, :n], reads=[gob])
        S.barrier()
        mst.close()
        gT, gTb = S.sbuf("gT", [128, 4, T], BF16, es=ph)
        S.dma("sp", gT[:], Sx["dbg_g"].rearrange("(c p) t -> p c t", p=128), writes=[gTb])
        wgl, wglb = S.sbuf("wglu", [128, 4, 512], BF16, es=ph)
        S.dma("pool", wgl[:], I["w_glu"][li].rearrange("(c p) f -> p c f", p=128), writes=[wglb])
        bgl, bglb = S.sbuf("bglu", [128, 4], F32, es=ph)
        S.dma("sp", bgl[:], I["b_glu"][li], writes=[bglb])
        obr = Ring(S, ph, "s5o", [128, 512], BF16, 3)
        for fch in range(4):
            for (t0, n, lc) in TB:
                ps, psb = bur.next()

                def mm():
                    ins = None
                    for c in range(4):
                        ins = nc.tensor.matmul(ps[:, :n], lhsT=wgl[:, c, fch * 128:(fch + 1) * 128], rhs=gT[:, c, t0:t0 + n], start=(c == 0), stop=(c == 3))
                    return ins
                S.op("pe", mm, reads=[wglb, gTb], writes=[psb])
                a1, a1b = tr.next()
                S.op("act", lambda: nc.scalar.activation(out=a1[:, :n], in_=ps[:, :n], func=AF.Sigmoid, bias=bgl[:, fch:fch + 1]), reads=[psb, bglb], writes=[a1b])
                ob, obb = obr.next()
                S.op("dve", lambda: nc.vector.tensor_tensor(ob[:, :n], a1[:, :n], gT[:, fch, t0:t0 + n], ALU.mult), reads=[a1b, gTb], writes=[obb])
                S.dma("sp", Sx["catT"][1024 + fch * 128:1024 + (fch + 1) * 128, t0:t0 + n], ob[:, :n], reads=[obb])


def phase_out(C, li, x_in, x_out):
    S, nc, I, Sx = C.S, C.nc, C.I, C.Sx
    m = C.mod[li]
    with ExitStack() as ph:
        cat, catb = S.sbuf("cat", [128, KD, T], BF16, es=ph)
        for k4 in range(4):
            S.dma("sp", cat[:, k4 * 4:(k4 + 1) * 4, :], Sx["catT"][k4 * 512:(k4 + 1) * 512, :].rearrange("(k p) t -> p k t", p=128),
                  writes=[catb] if k4 == 0 else [], awrites=[] if k4 == 0 else [catb])
        wr = Ring(S, ph, "wo", [128, KD, 512], BF16, 2)
        pr = Ring(S, ph, "po", [128, 512], F32, 4, psum=True)
        xr = Ring(S, ph, "xo", [128, 512], F32, 3)
        orr = Ring(S, ph, "oo", [128, 512], F32, 3)
        nxt = wr.next()
        S.dma("pool", nxt[0][:], I["w_out"][li][:, 0:512].rearrange("(k p) n -> p k n", p=128), writes=[nxt[1]])
        for nb in range(4):
            w, wb = nxt
            if nb < 3:
                nxt = wr.next()
                S.dma("pool", nxt[0][:], I["w_out"][li][:, (nb + 1) * 512:(nb + 2) * 512].rearrange("(k p) n -> p k n", p=128), writes=[nxt[1]])
            for dl in range(4):
                dch = nb * 4 + dl
                for (t0, n, lc) in TB:
                    xt, xtb = xr.next()
                    S.dma("sp", xt[:, :n], x_in[dch * 128:(dch + 1) * 128, t0:t0 + n], writes=[xtb])
                    ps, psb = pr.next()

                    def mm():
                        ins = None
                        for k in range(KD):
                            ins = nc.tensor.matmul(ps[:, :n], lhsT=w[:, k, dl * 128:(dl + 1) * 128], rhs=cat[:, k, t0:t0 + n], start=(k == 0), stop=(k == KD - 1))
                        return ins
                    S.op("pe", mm, reads=[wb, catb], writes=[psb])
                    o, ob = orr.next()
                    S.op("dve", lambda: nc.vector.scalar_tensor_tensor(o[:, :n], ps[:, :n], m.t[:, 32 + dch, lc:lc + 1], xt[:, :n], ALU.mult, ALU.add),
                         reads=[psb, m.b, xtb], writes=[ob])
                    S.dma("sp", x_out[dch * 128:(dch + 1) * 128, t0:t0 + n], o[:, :n], reads=[ob])


def phase_moe(C, li, x_in, x_out):
    S, nc, I, Sx = C.S, C.nc, C.I, C.Sx
    m = C.mod[li]
    with ExitStack() as ph:
        posmT, posmTb = S.sbuf("posmT", [16, T], F32, es=ph)
        posmB, posmBb = S.sbuf("posmB", [16, T], BF16, es=ph)
        with ExitStack() as ph8:
            h2tok, h2tokb = S.sbuf("h2tok", [128, 18, D], BF16, es=ph8)
            aff3, aff3b = S.sbuf("aff3", [128, 18, 16, 3], BF16, es=ph8)
            posm_tok, posm_tokb = S.sbuf("posm_tok", [128, 18, 16], F32, es=ph8)
            with ExitStack() as p7:
                wrt, wrtb = S.sbuf("wrt", [128, KD, 16], F32, es=p7)
                S.dma("sp", wrt[:], I["w_router"][li].rearrange("(k p) e -> p k e", p=128), writes=[wrtb])
                aff, affb = S.sbuf("aff", [128, 18, 16], F32, es=p7)
                lg_, lgb = S.psum("lg", [128, 512], F32, es=p7)
                lg = lg_[:, 0:288].rearrange("p (a b) -> p a b", b=16)
                with ExitStack() as p7a:
                    hb, hbb = S.sbuf("hb", [128, KD, 512], BF16, es=p7a)
                    ptr = Ring(S, p7a, "pT", [128, 1024], BF16, 2, psum=True)

                    def cb(bi, t0, n, lc, hf, hfb):
                        S.op("act", lambda: nc.scalar.activation(out=hb[:, :, :n], in_=hf[:, :, :n], func=AF.Copy), reads=[hfb], writes=[hbb])
                        for a in range(n // 128):
                            tt = t0 // 128 + a

                            def mm():
                                ins = None
                                for k in range(KD):
                                    ins = nc.tensor.matmul(lg[:, tt, :], lhsT=hf[:, k, a * 128:(a + 1) * 128], rhs=wrt[:, k, :], start=(k == 0), stop=(k == KD - 1))
                                return ins
                            S.op("pe", mm, reads=[hfb, wrtb], writes=[lgb])
                            for kq in range(4):
                                pT, pTb = ptr.next()

                                def tp():
                                    ins = None
                                    for j in range(4):
                                        ins = nc.tensor.transpose(pT[:, j * 128:(j + 1) * 128], hb[:, kq * 4 + j, a * 128:(a + 1) * 128], C.ident_bf[:, :])
                                    return ins
                                S.op("pe", tp, reads=[hbb, C.ident_bf_b], writes=[pTb])
                                if kq % 2:
                                    S.op("act", lambda: nc.scalar.activation(out=h2tok[:, tt, kq * 512:(kq + 1) * 512], in_=pT[:, 0:512], func=AF.Copy),
                                         reads=[pTb], writes=[h2tokb])
                                else:
                                    S.op("dve", lambda: nc.vector.tensor_copy(h2tok[:, tt, kq * 512:(kq + 1) * 512], pT[:, 0:512]), reads=[pTb], writes=[h2tokb])
                    norm_blocks(C, p7a, x_in, m.A2, m.A2b, modsl(m, 3), m.b, cb)
                    S.barrier()
                mx, mxb = S.sbuf("mx", [128, 18], F32, es=p7)
                S.op("dve", lambda: nc.vector.tensor_reduce(mx[:], lg, AX.X, ALU.max), reads=[lgb], writes=[mxb])
                S.op("dve", lambda: nc.vector.tensor_tensor(aff[:], lg, mx[:].unsqueeze(2).to_broadcast([128, 18, 16]), ALU.subtract),
                     reads=[lgb, mxb], writes=[affb])
                S.op("act", lambda: nc.scalar.activation(out=aff[:], in_=aff[:], func=AF.Exp), reads=[affb], writes=[affb])
                S.op("dve", lambda: nc.vector.tensor_reduce(mx[:], aff[:], AX.X, ALU.add), reads=[affb], writes=[mxb])
                S.op("dve", lambda: nc.vector.reciprocal(mx[:], mx[:]), reads=[mxb], writes=[mxb])
                S.op("dve", lambda: nc.vector.tensor_tensor(aff[:], aff[:], mx[:].unsqueeze(2).to_broadcast([128, 18, 16]), ALU.mult),
                     reads=[affb, mxb], writes=[affb])
                if "dbg_aff" in C.dump:
                    S.dma("sp", Sx["dbg_aff"], aff[:], reads=[affb])
                r1, r1b = S.sbuf("r1", [128, 18, 16], F32, es=p7)
                S.op("dve", lambda: nc.vector.tensor_copy(aff3[:, :, :, 0], aff[:]), reads=[affb], writes=[aff3b])
                S.op("dve", lambda: nc.vector.tensor_tensor(r1[:], aff[:], aff3[:, :, :, 0], ALU.subtract), reads=[affb, aff3b], writes=[r1b])
                S.op("dve", lambda: nc.vector.tensor_copy(aff3[:, :, :, 1], r1[:]), reads=[r1b], writes=[aff3b])
                S.op("dve", lambda: nc.vector.tensor_tensor(r1[:], r1[:], aff3[:, :, :, 1], ALU.subtract), reads=[r1b, aff3b], writes=[r1b])
                S.op("dve", lambda: nc.vector.tensor_copy(aff3[:, :, :, 2], r1[:]), reads=[r1b], writes=[aff3b])
                affT, affTb = S.sbuf("affT", [16, T], F32, es=p7)
                work, workb = S.sbuf("work", [16, T], F32, es=p7)
                pa_, pab = S.psum("pa", [128, 512], F32, es=p7)
                pa = pa_[0:16, :]
                for (t0, n, lc) in TB:
                    def tpa():
                        ins = None
                        for a in range(n // 128):
                            ins = nc.tensor.transpose(pa[:, a * 128:(a + 1) * 128], aff[:, t0 // 128 + a, :], C.ident_f[:, :])
                        return ins
                    S.op("pe", tpa, reads=[affb, C.ident_f_b], writes=[pab])
                    S.op("dve", lambda: nc.vector.tensor_copy(affT[:, t0:t0 + n], pa[:, :n]), reads=[pab], writes=[affTb])
                S.op("dve", lambda: nc.vector.tensor_copy(work[:], affT[:]), reads=[affTb], writes=[workb])
                m8, m8b = S.sbuf("m8", [16, 16], F32, es=p7)

                tk = []
                for (lo, hi, rounds, oc) in ((0, NL, 32, 0), (NL, T, 4, 8)):
                    for r in range(rounds):
                        tk.append(lambda lo=lo, hi=hi, oc=oc: nc.vector.max(m8[:, oc:oc + 8], work[:, lo:hi]))
                        if r < rounds - 1:
                            tk.append(lambda lo=lo, hi=hi, oc=oc: nc.vector.match_replace(work[:, lo:hi], m8[:, oc:oc + 8], work[:, lo:hi], -1.0))
                S.seq("dve", tk, reads=[workb], writes=[workb, m8b])
                mk, mkb = S.sbuf("mk", [16, T], F32, es=p7)

                S.seq("dve", [
                    lambda: nc.vector.tensor_scalar(mk[:, 0:NL], affT[:, 0:NL], m8[:, 7:8], None, ALU.is_ge),
                    lambda: nc.vector.tensor_scalar(mk[:, NL:T], affT[:, NL:T], m8[:, 15:16], None, ALU.is_ge),
                    lambda: nc.vector.tensor_tensor_scan(work[:, 0:NL], C.one_f[:16, 0:1].to_broadcast([16, NL]), mk[:, 0:NL], 0.0, ALU.mult, ALU.add),
                    lambda: nc.vector.tensor_tensor_scan(work[:, NL:T], C.one_f[:16, 0:1].to_broadcast([16, NX]), mk[:, NL:T], 0.0, ALU.mult, ALU.add),
                    lambda: nc.vector.tensor_scalar(work[:, NL:T], work[:, NL:T], 256.0, None, ALU.add),
                    lambda: nc.vector.tensor_tensor(work[:], work[:], mk[:], ALU.mult),
                    lambda: nc.vector.tensor_scalar(posmT[:], work[:], -1.0, None, ALU.add),
                ], reads=[affTb, m8b, C.one_f_b, workb], writes=[mkb, workb, posmTb])
                S.seq("dve", [
                    lambda: nc.vector.tensor_copy(posmB[:, 0:NL], posmT[:, 0:NL]),
                    lambda: nc.vector.tensor_scalar(posmB[:, NL:T], posmT[:, NL:T], -256.0, None, ALU.add),
                ], reads=[posmTb], writes=[posmBb])
                if "dbg_posm" in C.dump:
                    S.dma("sp", Sx["dbg_posm"], posmT[:], reads=[posmTb])
                pp__, ppb_ = S.psum("ppm", [128, 512], F32, es=p7)
                pp_ = pp__[:, 0:288].rearrange("p (a b) -> p a b", b=16)

                def tpp():
                    ins = None
                    for tt in range(18):
                        ins = nc.tensor.transpose(pp_[:, tt, :], posmT[:, tt * 128:(tt + 1) * 128], C.ident_f[:16, :16])
                    return ins
                S.op("pe", tpp, reads=[posmTb, C.ident_f_b], writes=[ppb_])
                S.op("dve", lambda: nc.vector.tensor_copy(posm_tok[:], pp_), reads=[ppb_], writes=[posm_tokb])
                S.barrier()
            with ExitStack() as p8:
                ioj, iojb = S.sbuf("ioj", [128, NJ], F32, es=p8)
                S.dma("sp", ioj[:], I["iota_j"][0:1, :].to_broadcast([128, NJ]), writes=[iojb])
                Se, Seb = S.sbuf("Se", [128, 18, NJ], BF16, es=p8)
                xsr = Ring(S, p8, "xs", [128, KD, NJ], BF16, 2)
                hidr = Ring(S, p8, "hid", [128, 8, NJ], BF16, 2)
                ysb_, ysbb = S.sbuf("ysb", [128, 3, D], BF16, es=p8)
                wring = Ring(S, p8, "wu", [128, 4096], BF16, 6)
                pr = Ring(S, p8, "pe8", [128, 512], F32, 6, psum=True)
                tap_, tapb = S.psum("tap", [128, 512], F32, es=p8)
                tap = tap_[:, 0:9]
                ta, tab = S.sbuf("ta", [128, 3], F32, es=p8)
                sgr = Ring(S, p8, "sg", [128, NJ], F32, 3)

                def units(e):
                    u = []
                    for fq in range(4):
                        u.append(("g", fq, I["w_gate"][li, e][:, fq * 256:(fq + 1) * 256].rearrange("(k p) f -> p k f", p=128)))
                        u.append(("u", fq, I["w_up"][li, e][:, fq * 256:(fq + 1) * 256].rearrange("(k p) f -> p k f", p=128)))
                    for dq in range(4):
                        u.append(("d", dq, I["w_down"][li, e][:, dq * 512:(dq + 1) * 512].rearrange("(c p) d -> p c d", p=128)))
                    return u
                allu = [(e,) + u for e in range(16) for u in units(e)]
                loaded = {}

                def issue(i):
                    if i >= len(allu):
                        return
                    e, kind, idx, src = allu[i]
                    wt, wtb = wring.next()
                    if kind == "d":
                        S.dma("pool", wt[:].rearrange("p (c d) -> p c d", c=8), src, writes=[wtb])
                    else:
                        S.dma("pool", wt[:].rearrange("p (k f) -> p k f", k=KD), src, writes=[wtb])
                    loaded[i] = (wt, wtb)
                PRE = 4
                for i in range(PRE):
                    issue(i)
                ui = 0
                for e in range(16):
                    def mkS():
                        ins = None
                        for tt in range(18):
                            ins = nc.vector.tensor_scalar(Se[:, tt, :], ioj[:, :], posm_tok[:, tt, e:e + 1], None, ALU.is_equal)
                        return ins
                    S.op("dve", mkS, reads=[iojb, posm_tokb], writes=[Seb])

                    def mta():
                        ins = None
                        for jc, (tts, M) in enumerate(((range(16), 128), (range(16), 128), ((16, 17), 32))):
                            tts = list(tts)
                            for ii, tt in enumerate(tts):
                                ins = nc.tensor.matmul(tap[:M, jc * 3:(jc + 1) * 3], lhsT=Se[:, tt, jc * 128:jc * 128 + M], rhs=aff3[:, tt, e, :],
                                                       start=(ii == 0), stop=(ii == len(tts) - 1))
                        return ins
                    S.op("pe", mta, reads=[Seb, aff3b], writes=[tapb])
                    S.op("dve", lambda: nc.vector.tensor_reduce(ta[:], tap_[:, 0:9].rearrange("p (a b) -> p a b", b=3), AX.X, ALU.add), reads=[tapb], writes=[tab])
                    xs, xsb = xsr.next()
                    for k in range(KD):
                        ps, psb = pr.next()

                        def gm():
                            ins = None
                            for tt in range(16):
                                ins = nc.tensor.matmul(ps[:, 0:256], lhsT=h2tok[:, tt, k * 128:(k + 1) * 128], rhs=Se[:, tt, 0:256], start=(tt == 0), stop=(tt == 15))
                            for tt in (16, 17):
                                ins = nc.tensor.matmul(ps[:, 256:NJ], lhsT=h2tok[:, tt, k * 128:(k + 1) * 128], rhs=Se[:, tt, 256:NJ], start=(tt == 16), stop=(tt == 17))
                            return ins
                        S.op("pe", gm, reads=[h2tokb, Seb], writes=[psb])
                        if k % 2:
                            S.op("act", lambda: nc.scalar.activation(out=xs[:, k, :], in_=ps[:, :NJ], func=AF.Copy), reads=[psb], writes=[xsb])
                        else:
                            S.op("dve", lambda: nc.vector.tensor_copy(xs[:, k, :], ps[:, :NJ]), reads=[psb], writes=[xsb])
                    hid, hidb = hidr.next()
                    for fq in range(4):
                        wg, wgb = loaded.pop(ui)
                        wu, wub = loaded.pop(ui + 1)
                        ui += 2
                        wg3 = wg[:].rearrange("p (k f) -> p k f", k=KD)
                        wu3 = wu[:].rearrange("p (k f) -> p k f", k=KD)
                        for fcl in range(2):
                            fc = fq * 2 + fcl
                            pg, pgb = pr.next()
                            pu, pub = pr.next()

                            def mg():
                                ins = None
                                for k in range(KD):
                                    ins = nc.tensor.matmul(pg[:, :NJ], lhsT=wg3[:, k, fcl * 128:(fcl + 1) * 128], rhs=xs[:, k, :], start=(k == 0), stop=(k == KD - 1))
                                return ins

                            def mu():
                                ins = None
                                for k in range(KD):
                                    ins = nc.tensor.matmul(pu[:, :NJ], lhsT=wu3[:, k, fcl * 128:(fcl + 1) * 128], rhs=xs[:, k, :], start=(k == 0), stop=(k == KD - 1))
                                return ins
                            S.op("pe", mg, reads=[wgb, xsb], writes=[pgb])
                            S.op("pe", mu, reads=[wub, xsb], writes=[pub])
                            sg, sgb = sgr.next()
                            S.op("act", lambda: nc.scalar.activation(out=sg[:, :], in_=pg[:, :NJ], func=AF.Silu), reads=[pgb], writes=[sgb])
                            S.op("dve", lambda: nc.vector.tensor_tensor(hid[:, fc, :], sg[:, :], pu[:, :NJ], ALU.mult), reads=[sgb, pub], writes=[hidb])
                        issue(ui - 2 + PRE)
                        issue(ui - 1 + PRE)
                    for dq in range(4):
                        wd, wdb = loaded.pop(ui)
                        ui += 1
                        wd3 = wd[:].rearrange("p (c d) -> p c d", c=8)
                        for jc, M in enumerate((128, 128, 32)):
                            ps, psb = pr.next()

                            def md():
                                ins = None
                                for fc in range(8):
                                    ins = nc.tensor.matmul(ps[:M, :], lhsT=hid[:, fc, jc * 128:jc * 128 + M], rhs=wd3[:, fc, :], start=(fc == 0), stop=(fc == 7))
                                return ins
                            S.op("pe", md, reads=[hidb, wdb], writes=[psb])
                            if jc == 1:
                                S.op("act", lambda: nc.scalar.activation(out=ysb_[:M, jc, dq * 512:(dq + 1) * 512], in_=ps[:M, :], func=AF.Copy, scale=ta[:M, jc:jc + 1]),
                                     reads=[psb, tab], writes=[ysbb])
                            else:
                                S.op("dve", lambda: nc.vector.tensor_scalar(ysb_[:M, jc, dq * 512:(dq + 1) * 512], ps[:M, :], ta[:M, jc:jc + 1], None, ALU.mult),
                                     reads=[psb, tab], writes=[ysbb])
                        issue(ui - 1 + PRE)
                    S.dma("sp", Sx["ys"][e, 0:256, :].rearrange("(c j) d -> j c d", j=128), ysb_[:, 0:2, :], reads=[ysbb])
                    S.dma("sp", Sx["ys"][e, 256:NJ, :], ysb_[:32, 2, :], reads=[ysbb])
                S.barrier()
        with ExitStack() as p9:
            ysh, yshb = S.sbuf("ysh", [128, 16, 3, 1024], BF16, es=p9)
            STr = Ring(S, p9, "ST", [128, 16, 2, 512], BF16, 2)
            selt, seltb = S.sbuf("selt", [16, 16, 128], BF16, es=p9)
            S.dma("pool", selt[:], I["sel"][:, :, :], writes=[seltb])
            ip3, ip3b = S.sbuf("ip3", [128, 3], F32, es=p9)
            S.dma("sp", ip3[:], I["iota_p3"][:, :], writes=[ip3b])
            bcr = Ring(S, p9, "bc", [128, 512], F32, 3, psum=True)
            pr = Ring(S, p9, "p9", [128, 512], F32, 4, psum=True)
            xr = Ring(S, p9, "x9", [128, 512], F32, 3)
            orr = Ring(S, p9, "o9", [128, 512], F32, 3)
            for dh in range(2):
                for jc in range(2):
                    S.dma("sp", ysh[:, :, jc, :], Sx["ys"][:, jc * 128:(jc + 1) * 128, dh * 1024:(dh + 1) * 1024].rearrange("e j d -> j e d"),
                          writes=[yshb] if jc == 0 else [], awrites=[] if jc == 0 else [yshb])
                S.dma("sp", ysh[:32, :, 2, :], Sx["ys"][:, 256:NJ, dh * 1024:(dh + 1) * 1024].rearrange("e j d -> j e d"), awrites=[yshb])
                for (t0, n, lc) in TB:
                    ST, STb = STr.next()
                    for e in range(16):
                        bc, bcb = bcr.next()
                        S.op("pe", lambda: nc.tensor.matmul(bc[:, :n], lhsT=selt[:, e, :], rhs=posmB[:, t0:t0 + n], start=True, stop=True),
                             reads=[seltb, posmBb], writes=[bcb])
                        if lc == 0:
                            S.op("dve", lambda: nc.vector.tensor_scalar(ST[:, e, 0, :n], bc[:, :n], ip3[:, 0:1], None, ALU.is_equal), reads=[bcb, ip3b], writes=[STb])
                            S.op("pool" if False else "dve", lambda: nc.vector.tensor_scalar(ST[:, e, 1, :n], bc[:, :n], ip3[:, 1:2], None, ALU.is_equal),
                                 reads=[bcb, ip3b], writes=[STb])
                        else:
                            S.op("dve", lambda: nc.vector.tensor_scalar(ST[:32, e, 0, :n], bc[:32, :n], ip3[:32, 0:1], None, ALU.is_equal), reads=[bcb, ip3b], writes=[STb])
                    for dl in range(8):
                        dch = dh * 8 + dl
                        xt, xtb = xr.next()
                        S.dma("sp", xt[:, :n], x_in[dch * 128:(dch + 1) * 128, t0:t0 + n], writes=[xtb])
                        ps, psb = pr.next()

                        def msc():
                            ins = None
                            if lc == 0:
                                for e in range(16):
                                    for jc in range(2):
                                        ins = nc.tensor.matmul(ps[:, :n], lhsT=ysh[:, e, jc, dl * 128:(dl + 1) * 128], rhs=ST[:, e, jc, :n],
                                                               start=(e == 0 and jc == 0), stop=(e == 15 and jc == 1))
                            else:
                                for e in range(16):
                                    ins = nc.tensor.matmul(ps[:, :n], lhsT=ysh[:32, e, 2, dl * 128:(dl + 1) * 128], rhs=ST[:32, e, 0, :n],
                                                           start=(e == 0), stop=(e == 15))
                            return ins
                        S.op("pe", msc, reads=[yshb, STb], writes=[psb])
                        o, ob = orr.next()
                        S.op("dve", lambda: nc.vector.scalar_tensor_tensor(o[:, :n], ps[:, :n], m.t[:, 80 + dch, lc:lc + 1], xt[:, :n], ALU.mult, ALU.add),
                             reads=[psb, m.b, xtb], writes=[ob])
                        S.dma("sp", x_out[dch * 128:(dch + 1) * 128, t0:t0 + n], o[:, :n], reads=[ob])


def phase_final(C, x_in):
    S, nc, I = C.S, C.nc, C.I
    with ExitStack() as ph:
        gf, gfb = S.sbuf("gf", [128, KD, 2], F32, es=ph)
        S.dma("sp", gf[:], I["gfT"][:, :, :], writes=[gfb])
        zs, zsb = S.sbuf("zs", [128, KD, 2], F32, es=ph)
        S.op("pool", lambda: nc.gpsimd.memset(zs[:], 0.0), writes=[zsb])

        def cb(bi, t0, n, lc, hf, hfb):
            S.dma("sp", C.outT[:, t0:t0 + n].rearrange("(k p) t -> p k t", p=128), hf[:, :, :n], reads=[hfb])
        norm_blocks(C, ph, x_in, gf, gfb, zs, zsb, cb, blocks=TB[:4])


def _prep_shared(inp):
    f = np.float32
    L = DEPTH
    sh = {}
    sh["w_ada"] = np.ascontiguousarray(inp["w_ada"], dtype=f)
    sh["b_adaT"] = np.ascontiguousarray(np.repeat(inp["b_ada"].reshape(L, 96, 128).transpose(0, 2, 1)[..., None], 2, axis=-1), dtype=f)
    for nm, src in (("g1T", "norm1_g"), ("g2T", "norm2_g")):
        sh[nm] = np.ascontiguousarray(np.repeat(inp[src].reshape(L, KD, 128).transpose(0, 2, 1)[..., None], 2, axis=-1), dtype=f)
    sh["gfT"] = np.ascontiguousarray(np.repeat(inp["final_norm_g"].reshape(KD, 128).T[..., None], 2, axis=-1), dtype=f)
    sh["w_in"] = np.ascontiguousarray(inp["w_in"], dtype=f)
    perm = np.array([(r // 32) * 32 + ((r % 32) + 16) % 32 for r in range(64)])
    sh["w_in_sw"] = np.ascontiguousarray(inp["w_in"][:, :, 1664:1728][:, :, perm], dtype=f)
    sh["w_out"] = np.ascontiguousarray(inp["w_out"], dtype=f)
    sh["sgu_g"] = np.ascontiguousarray(inp["sgu_norm_g"].reshape(L, 1, 512), dtype=f)
    sh["sgu_wT"] = np.ascontiguousarray(inp["sgu_w"].transpose(0, 3, 1, 2), dtype=f)
    sh["sgu_b4"] = np.ascontiguousarray(np.repeat(inp["sgu_b"][:, :, None, :], 4, axis=2).reshape(L, 1, 2048), dtype=f)
    sh["qg"] = np.ascontiguousarray(inp["mla_q_norm_g"].reshape(L, 3, 128).transpose(0, 2, 1), dtype=f)
    sh["kvg"] = np.ascontiguousarray(inp["mla_kv_norm_g"].reshape(L, 2, 128).transpose(0, 2, 1), dtype=f)
    sh["w_uq"] = np.ascontiguousarray(inp["mla_w_uq"], dtype=f)
    sh["w_uq_sw"] = np.ascontiguousarray(np.concatenate([inp["mla_w_uq"][:, :, h * 192 + 128 + perm] for h in range(4)], axis=-1), dtype=f)
    sh["w_ukv"] = np.ascontiguousarray(inp["mla_w_ukv"], dtype=f)
    t = np.arange(NL)
    row_id = (t // 64).astype(f)
    col_id = (t % 64).astype(f)
    inv_freq = (f(10000.0) ** (-np.arange(16, dtype=f) / f(16))).astype(f)
    cosT = np.ones((64, T), f)
    sinT = np.zeros((64, T), f)
    for r in range(64):
        pos = row_id if r < 32 else col_id
        ang = (pos * inv_freq[r % 16]).astype(f)
        cosT[r, :NL] = np.cos(ang)
        sinT[r, :NL] = np.sin(ang) * (-1.0 if (r % 32) < 16 else 1.0)
    sh["rope_cos"] = cosT
    sh["rope_sin"] = sinT

    def pp(a):
        return a.reshape(L, 2, 4, 8, 4, 16).transpose(0, 3, 5, 1, 2, 4).reshape(L, 128, 32)

    def rowl(a):
        return a.reshape(L, 2, 4, 8, 4, 16).transpose(0, 1, 2, 4, 3, 5).reshape(L, 4096)
    ldt_full = np.repeat(inp["s5_log_dt"][..., None], 64, axis=-1)
    sh["s5_pp"] = np.ascontiguousarray(np.stack([pp(inp["s5_a_re"]), pp(inp["s5_a_im"]), pp(ldt_full)], axis=2), dtype=f)
    sh["s5_row"] = np.ascontiguousarray(np.stack([rowl(inp["s5_a_re"]), rowl(inp["s5_a_im"]), rowl(ldt_full)], axis=1), dtype=f)

    def bblk(b):
        o = np.zeros((L, 8, 16, 2, 4, 4, 8, 16), f)
        bb = b.reshape(L, 2, 4, 8, 4, 16, 16)
        for g in range(8):
            o[:, g, :, :, :, :, g, :] = bb[:, :, :, g].transpose(0, 4, 1, 2, 3, 5)[:, :, :, :, :, :] if False else \
                np.transpose(bb[:, :, :, g], (0, 5, 1, 2, 3, 4))
        return o.reshape(L, 128, 4096)

    def cblk(c):
        o = np.zeros((L, 8, 16, 2, 4, 4, 8, 16), f)
        cc = c.reshape(L, 2, 4, 8, 16, 4, 16)
        for g in range(8):
            o[:, g, :, :, :, :, g, :] = np.transpose(cc[:, :, :, g], (0, 5, 1, 2, 4, 3))
        return o.reshape(L, 128, 4096)
    sh["s5_Bre"] = bblk(inp["s5_b_re"])
    sh["s5_Bim"] = bblk(inp["s5_b_im"])
    sh["s5_Cre"] = cblk(inp["s5_c_re"])
    sh["s5_Cim"] = cblk(inp["s5_c_im"])
    sh["s5_d"] = np.ascontiguousarray(inp["s5_d"].reshape(L, 4, 128).transpose(0, 2, 1), dtype=f)
    tau = np.zeros((2, T), f)
    tau[0, NL:] = np.arange(NX)
    tau[0, :NL] = NX + np.arange(NL)
    tau[1, NL:] = NX - 1 - np.arange(NX)
    tau[1, :NL] = NX + (NL - 1 - np.arange(NL))
    sh["s5_tau"] = tau.reshape(1, 2 * T)
    sh["w_glu"] = np.ascontiguousarray(inp["s5_w_glu"], dtype=f)
    sh["b_glu"] = np.ascontiguousarray(inp["s5_b_glu"].reshape(L, 4, 128).transpose(0, 2, 1), dtype=f)
    sh["conv_wT"] = np.ascontiguousarray(inp["conv_w"].reshape(L, 3, 4, 128).transpose(0, 3, 2, 1), dtype=f)
    sh["w_router"] = np.ascontiguousarray(inp["moe_w_router"], dtype=f)
    sh["w_gate"] = np.ascontiguousarray(inp["moe_w_gate"], dtype=f)
    sh["w_up"] = np.ascontiguousarray(inp["moe_w_up"], dtype=f)
    sh["w_down"] = np.ascontiguousarray(inp["moe_w_down"], dtype=f)
    sh["ident"] = np.eye(128, dtype=f)
    sh["iota_j"] = np.arange(NJ, dtype=f).reshape(1, NJ)
    sh["iota_p3"] = (np.arange(128, dtype=f)[:, None] + np.array([0, 128, 256], f)[None, :]).astype(f)
    sel = np.zeros((16, 16, 128), f)
    for e in range(16):
        sel[e, e, :] = 1.0
    sh["sel"] = sel
    return sh


def _prep_core(inp, b):
    f = np.float32
    d = {}
    d["xT0"] = np.ascontiguousarray(np.concatenate([inp["x"][b].T, inp["ctx"][b].T], axis=1), dtype=f)
    c2 = np.stack([inp["c"][b], inp["c_ctx"]], axis=0)
    d["cTp"] = np.ascontiguousarray(c2.reshape(2, KD, 128).transpose(2, 1, 0), dtype=f)
    return d


_NC_CACHE = {}


def used_inputs(nc_I, m):
    return {k: v for k, v in m.items() if k in nc_I}


def kernel(**inputs):
    inp = {k: np.asarray(v) for k, v in inputs.items()}
    B = inp["x"].shape[0]
    if "full" not in _NC_CACHE:
        _NC_CACHE["full"] = build_program()
    nc = _NC_CACHE["full"]
    sh = _prep_shared(inp)
    in_maps = []
    for b in range(B):
        m = dict(sh)
        m.update(_prep_core(inp, b))
        in_maps.append({k: v for k, v in m.items() if k in nc._used_inputs})
    res = run_bass_kernel_spmd(nc, in_maps, core_ids=list(range(B)))
    out = np.stack([np.asarray(res.results[b]["outT"]).T for b in range(B)], axis=0)
    return np.ascontiguousarray(out, dtype=np.float32)
```
